# Optimizing a Trainium2 kernel written in Bass

```python
import math
import numpy as np
import jax, jax.numpy as jnp
from jax import lax

D_MODEL = 1024
BATCH = 8
SEQ = 2048
DEPTH = 1
DEC_BATCH = 128
DEC_SEQ = 4
PAST_LEN = 8192
PAGE_SIZE = 128

N_HEADS = 8
N_KV = 2
HEAD_DIM = 64
Q_PER_KV = N_HEADS // N_KV
ATT_WIDTH = N_HEADS * HEAD_DIM
KV_WIDTH = N_KV * HEAD_DIM
CMP_BLOCK = 32
SEL_BLOCK = 64
CMP_PER_SEL = SEL_BLOCK // CMP_BLOCK
N_SEL = 16
WINDOW = 512
SCALE = HEAD_DIM ** -0.5
SSM_HEADS = 8
SSM_HEAD_DIM = 64
SSM_WIDTH = SSM_HEADS * SSM_HEAD_DIM
SSM_GROUPS = 2
SSM_STATE = 64
CONV_W = 4
SSD_CHUNK = 128
CONV_DIM = SSM_WIDTH + 2 * SSM_GROUPS * SSM_STATE
MIX_WIDTH = ATT_WIDTH + SSM_WIDTH
IN_SPLITS = (ATT_WIDTH, KV_WIDTH, KV_WIDTH, KV_WIDTH, KV_WIDTH, KV_WIDTH, KV_WIDTH, 3 * N_HEADS, SSM_WIDTH, CONV_DIM, SSM_HEADS)
IN_WIDTH = sum(IN_SPLITS)
N_EGROUPS = 4
E_PER_GROUP = 4
N_EXPERTS = N_EGROUPS * E_PER_GROUP
TOP_IN_GROUP = 2
EXPERT_FF = 256
EPS = 1e-6
NEG = -1e30
BIG = 1e9
TINY = 1e-30

kernel_name = 'hybrid_nsa_ssd_hmoe_adaln_step'


def rmsnorm(x, g):
    xf = x.astype(jnp.float32)
    y = xf * lax.rsqrt(jnp.mean(xf * xf, axis=-1, keepdims=True) + EPS)
    return (y * g.astype(jnp.float32)).astype(x.dtype)


def masked_softmax(s, mask):
    s = jnp.where(mask, s, NEG)
    e = jnp.where(mask, jnp.exp(s - jnp.max(s, axis=-1, keepdims=True)), 0.0)
    return e / jnp.maximum(jnp.sum(e, axis=-1, keepdims=True), TINY)


def dense_attend(q, k, v, mask):
    s = jnp.einsum('btgqd,bmgd->btgqm', q, k).astype(jnp.float32) * SCALE
    p = masked_softmax(s, mask[None, :, None, None, :])
    o = jnp.einsum('btgqm,bmgd->btgqd', p.astype(v.dtype), v)
    return o, p


def sel_attend(q, kv_g, g_valid, kv_cur, cur_mask):
    b, t, g, k, sb = kv_g.shape[:5]
    c = kv_cur.shape[1]
    s_g = jnp.einsum('btgqd,btgksd->btgqks', q, kv_g[..., 0, :]).reshape(b, t, g, Q_PER_KV, k * sb)
    s_c = jnp.einsum('btgqd,bcgd->btgqc', q, kv_cur[:, :, 0])
    s = jnp.concatenate([s_g, s_c], axis=-1).astype(jnp.float32) * SCALE
    m_g = jnp.broadcast_to(g_valid[:, :, :, None, :, None], (b, t, g, Q_PER_KV, k, sb)).reshape(b, t, g, Q_PER_KV, k * sb)
    m_c = jnp.broadcast_to(cur_mask[None, :, None, None, :], (b, t, g, Q_PER_KV, c))
    p = masked_softmax(s, jnp.concatenate([m_g, m_c], axis=-1)).astype(q.dtype)
    o_g = jnp.einsum('btgqks,btgksd->btgqd', p[..., :k * sb].reshape(b, t, g, Q_PER_KV, k, sb), kv_g[..., 1, :])
    o_c = jnp.einsum('btgqc,bcgd->btgqd', p[..., k * sb:], kv_cur[:, :, 1])
    return o_g + o_c


def compress(rows, w_pos):
    b, l, g, d = rows.shape
    return jnp.einsum('bcjgd,j->bcgd', rows.reshape(b, l // CMP_BLOCK, CMP_BLOCK, g, d), w_pos)


def select_blocks(p_cmp, n_blocks, cur):
    b, t, g = p_cmp.shape[:3]
    imp = p_cmp[..., :n_blocks * CMP_PER_SEL].sum(axis=3).reshape(b, t, g, n_blocks, CMP_PER_SEL).sum(axis=-1)
    j = jnp.arange(n_blocks)[None, :]
    allowed = (j < cur[:, None])[None, :, None, :]
    forced = ((j == 0) | (j == cur[:, None] - 1))[None, :, None, :]
    score = jnp.where(allowed, jnp.where(forced, BIG, imp), NEG)
    vals, idx = lax.top_k(score, min(N_SEL - 1, n_blocks))
    return idx, vals > 0.5 * NEG


def in_proj(h, w_in, g_q, g_k_sel, g_k_win):
    b, l, _ = h.shape
    u = h @ w_in
    offs = np.cumsum(IN_SPLITS)[:-1].tolist()
    q, kc, vc, ks, vs, kw, vw, gt, z, xbc, dt = jnp.split(u, offs, axis=-1)
    heads = lambda a: a.reshape(b, l, N_KV, HEAD_DIM)
    q = rmsnorm(q.reshape(b, l, N_KV, Q_PER_KV, HEAD_DIM), g_q)
    kv_sel = jnp.stack([rmsnorm(heads(ks), g_k_sel), heads(vs)], axis=2)
    kv_win = jnp.stack([rmsnorm(heads(kw), g_k_win), heads(vw)], axis=2)
    gates = jax.nn.sigmoid(gt.astype(jnp.float32)).reshape(b, l, 3, N_KV, Q_PER_KV)
    return q, heads(kc), heads(vc), kv_sel, kv_win, gates, z, xbc, dt


def nsa_prompt(q, kc_raw, vc, kv_sel, kv_win, w_pos_k, w_pos_v, g_k_cmp):
    b, s = q.shape[:2]
    qpos = jnp.arange(s)
    kcmp = rmsnorm(compress(kc_raw, w_pos_k), g_k_cmp)
    vcmp = compress(vc, w_pos_v)
    ends = jnp.arange(s // CMP_BLOCK) * CMP_BLOCK + CMP_BLOCK - 1
    o_cmp, p_cmp = dense_attend(q, kcmp, vcmp, ends[None, :] <= qpos[:, None])
    nb = s // SEL_BLOCK
    idx, g_valid = select_blocks(p_cmp, nb, qpos // SEL_BLOCK)
    kv_blocks = kv_sel.reshape(b, nb, SEL_BLOCK, 2, N_KV, HEAD_DIM)
    kv_win_pad = jnp.pad(kv_win, ((0, 0), (WINDOW, 0), (0, 0), (0, 0), (0, 0)))
    bi = jnp.arange(b)[:, None, None, None]
    gi = jnp.arange(N_KV)[None, None, :, None]
    causal = jnp.tril(jnp.ones((SEL_BLOCK, SEL_BLOCK), bool))
    rel = jnp.arange(SEL_BLOCK)[:, None] - (jnp.arange(WINDOW + SEL_BLOCK)[None, :] - WINDOW)
    to_blocks = lambda a: jnp.moveaxis(a.reshape(b, nb, SEL_BLOCK, *a.shape[2:]), 1, 0)

    def body(inp):
        n, qb, idxb, vb = inp
        kv_g = kv_blocks[bi, idxb, :, :, gi]
        kv_cur = lax.dynamic_index_in_dim(kv_blocks, n, axis=1, keepdims=False)
        o_s = sel_attend(qb, kv_g, vb, kv_cur, causal)
        start = n * SEL_BLOCK
        slab = lax.dynamic_slice_in_dim(kv_win_pad, start, WINDOW + SEL_BLOCK, axis=1)
        pos = start - WINDOW + jnp.arange(WINDOW + SEL_BLOCK)
        wmask = (rel >= 0) & (rel < WINDOW) & (pos[None, :] >= 0)
        o_w, _ = dense_attend(qb, slab[:, :, 0], slab[:, :, 1], wmask)
        return o_s, o_w

    o_sel, o_win = lax.map(body, (jnp.arange(nb), to_blocks(q), to_blocks(idx), to_blocks(g_valid)))
    back = lambda a: jnp.moveaxis(a, 0, 1).reshape(b, s, *a.shape[3:])
    return o_cmp, back(o_sel), back(o_win)


def nsa_sample(q, kv_sel, kv_win, cache_cmp, cache_sel, cache_win, page_table, layer, w_pos_k, w_pos_v, g_k_cmp):
    bd, t = q.shape[:2]
    qpos = PAST_LEN + jnp.arange(t)
    rows = cache_cmp[layer, page_table].reshape(bd, PAST_LEN, 2, N_KV, HEAD_DIM)
    kcmp = rmsnorm(compress(rows[:, :, 0], w_pos_k), g_k_cmp)
    vcmp = compress(rows[:, :, 1], w_pos_v)
    ends = jnp.arange(PAST_LEN // CMP_BLOCK) * CMP_BLOCK + CMP_BLOCK - 1
    o_cmp, p_cmp = dense_attend(q, kcmp, vcmp, ends[None, :] <= qpos[:, None])
    nb = PAST_LEN // SEL_BLOCK
    idx, g_valid = select_blocks(p_cmp, nb, qpos // SEL_BLOCK)
    sub = PAGE_SIZE // SEL_BLOCK
    pool = cache_sel.reshape(cache_sel.shape[0], -1, SEL_BLOCK, 2, N_KV, HEAD_DIM)
    bi = jnp.arange(bd)[:, None, None, None]
    gi = jnp.arange(N_KV)[None, None, :, None]
    phys = page_table[bi, idx // sub] * sub + idx % sub
    kv_g = pool[layer, phys, :, :, gi]
    o_sel = sel_attend(q, kv_g, g_valid, kv_sel, jnp.tril(jnp.ones((t, t), bool)))
    w_buf = cache_win.shape[2]
    kv_w = jnp.concatenate([cache_win[layer].astype(kv_win.dtype), kv_win], axis=1)
    pos = PAST_LEN - w_buf + jnp.arange(w_buf + t)
    rel = qpos[:, None] - pos[None, :]
    o_win, _ = dense_attend(q, kv_w[:, :, 0], kv_w[:, :, 1], (rel >= 0) & (rel < WINDOW))
    return o_cmp, o_sel, o_win, kv_w[:, -min(WINDOW, w_buf + t):]


def combine_branches(gates, o_cmp, o_sel, o_win):
    b, t = o_cmp.shape[:2]
    g = gates.astype(o_cmp.dtype)[..., None]
    o = g[:, :, 0] * o_cmp + g[:, :, 1] * o_sel + g[:, :, 2] * o_win
    return o.reshape(b, t, ATT_WIDTH)


def ssd_scan(x, dt, a, bm, cm, h0):
    b, l = x.shape[:2]
    cl = min(SSD_CHUNK, l)
    nc = l // cl
    rep = SSM_HEADS // SSM_GROUPS
    bh = jnp.repeat(bm, rep, axis=2)
    ch = jnp.repeat(cm, rep, axis=2)
    chunk = lambda v: jnp.moveaxis(v.reshape(b, nc, cl, *v.shape[2:]), 1, 0)
    causal = jnp.tril(jnp.ones((cl, cl), bool))[None, :, :, None]

    def step(h, inp):
        xc, dtc, bc, cc = inp
        acs = jnp.cumsum(dtc * a, axis=1)
        seg = acs[:, :, None, :] - acs[:, None, :, :]
        decay = jnp.where(causal, jnp.exp(jnp.where(causal, seg, 0.0)), 0.0)
        cb = jnp.einsum('blhn,bshn->blsh', cc, bc)
        y_diag = jnp.einsum('blsh,bsh,bshp->blhp', cb * decay, dtc, xc)
        y_off = jnp.einsum('blhn,bhpn,blh->blhp', cc, h, jnp.exp(acs))
        w_end = jnp.exp(acs[:, -1:, :] - acs) * dtc
        h_new = jnp.exp(acs[:, -1])[:, :, None, None] * h + jnp.einsum('bsh,bshn,bshp->bhpn', w_end, bc, xc)
        return h_new, y_diag + y_off

    h_t, ys = lax.scan(step, h0, (chunk(x), chunk(dt), chunk(bh), chunk(ch)))
    return jnp.moveaxis(ys, 0, 1).reshape(b, l, SSM_HEADS, SSM_HEAD_DIM), h_t


def ssd_mixer(z, xbc, dt_raw, conv_prev, h0, conv_w, conv_b, dt_bias, a_log, d_skip, g_out):
    b, l, _ = xbc.shape
    xpad = jnp.concatenate([conv_prev.astype(xbc.dtype), xbc], axis=1)
    new_conv = xpad[:, -(CONV_W - 1):]
    xc = lax.conv_general_dilated(xpad, conv_w[:, None, :].astype(xbc.dtype), (1,), 'VALID',
                                  dimension_numbers=('NWC', 'WIO', 'NWC'), feature_group_count=CONV_DIM)
    xc = jax.nn.silu(xc + conv_b)
    gn = SSM_GROUPS * SSM_STATE
    xs = xc[..., :SSM_WIDTH].reshape(b, l, SSM_HEADS, SSM_HEAD_DIM).astype(jnp.float32)
    bm = xc[..., SSM_WIDTH:SSM_WIDTH + gn].reshape(b, l, SSM_GROUPS, SSM_STATE).astype(jnp.float32)
    cm = xc[..., SSM_WIDTH + gn:].reshape(b, l, SSM_GROUPS, SSM_STATE).astype(jnp.float32)
    dt = jax.nn.softplus((dt_raw + dt_bias).astype(jnp.float32))
    a = -jnp.exp(a_log.astype(jnp.float32))
    y, h_t = ssd_scan(xs, dt, a, bm, cm, h0)
    y = (y + d_skip.astype(jnp.float32)[:, None] * xs).reshape(b, l, SSM_WIDTH).astype(z.dtype)
    return rmsnorm(y * jax.nn.silu(z), g_out), new_conv, h_t


def hier_moe(h, w_rg, b_rg, w_re, b_re, w_gate, w_up, w_down):
    b, l, _ = h.shape
    lg = (h @ w_rg + b_rg).astype(jnp.float32)
    _, gsel = lax.top_k(lg, 1)
    p_top = jnp.take_along_axis(jax.nn.softmax(lg, axis=-1), gsel, axis=-1)
    le = (jnp.einsum('bld,gde->blge', h, w_re) + b_re).astype(jnp.float32)
    le = jnp.take_along_axis(le, jnp.broadcast_to(gsel[..., None], (b, l, 1, E_PER_GROUP)), axis=2)[:, :, 0]
    v2, i2 = lax.top_k(le, TOP_IN_GROUP)
    weight = p_top * jax.nn.softmax(v2, axis=-1)
    eid = gsel * E_PER_GROUP + i2
    combine = jnp.sum(jax.nn.one_hot(eid, N_EXPERTS, dtype=jnp.float32) * weight[..., None], axis=-2).astype(h.dtype)
    y = jnp.zeros_like(h)
    for e in range(N_EXPERTS):
        he = jax.nn.silu(h @ w_gate[e]) * (h @ w_up[e])
        y = y + combine[..., e:e + 1] * (he @ w_down[e])
    return y


def adaln_mods(c, w_ada, b_ada):
    m = (jax.nn.silu(c) @ w_ada + b_ada)[:, None, :]
    return jnp.split(m, 6, axis=-1)


def finish(x, att, y_ssm, mods, g_att_out, w_out, g_norm2, w_rg, b_rg, w_re, b_re, w_gate, w_up, w_down):
    mixed = jnp.concatenate([rmsnorm(att, g_att_out), y_ssm], axis=-1) @ w_out
    x = x + mods[2] * mixed
    h = rmsnorm(x, g_norm2) * (1 + mods[4]) + mods[3]
    return x + mods[5] * hier_moe(h, w_rg, b_rg, w_re, b_re, w_gate, w_up, w_down)


def setup_inputs(seed: int = 0) -> dict:
    key = jax.random.key(seed)
    k = jax.random.split(key, 40)
    nrm = lambda kk, shape, s=1.0: jax.random.normal(kk, shape, jnp.float32) * s
    n_pages = PAST_LEN // PAGE_SIZE
    n_pool = (5 * DEC_BATCH * n_pages + 3) // 4
    w_buf = min(WINDOW, PAST_LEN)
    page_table = jax.random.permutation(k[0], n_pool)[:DEC_BATCH * n_pages].reshape(DEC_BATCH, n_pages).astype(jnp.int32)
    dt0 = jnp.exp(jax.random.uniform(k[1], (DEPTH, SSM_HEADS), jnp.float32, math.log(1e-3), math.log(1e-1)))
    dt_bias = dt0 + jnp.log(-jnp.expm1(-dt0))
    a_log = jnp.log(jax.random.uniform(k[2], (DEPTH, SSM_HEADS), jnp.float32, 1.0, 16.0))
    gain = lambda kk, n: 1.0 + nrm(kk, (DEPTH, n), 0.02)
    return {
        'x_prompt': nrm(k[3], (BATCH, SEQ, D_MODEL)),
        'x_sample': nrm(k[4], (DEC_BATCH, DEC_SEQ, D_MODEL)),
        'cache_cmp': nrm(k[5], (DEPTH, n_pool, PAGE_SIZE, 2, N_KV, HEAD_DIM)),
        'cache_sel': nrm(k[6], (DEPTH, n_pool, PAGE_SIZE, 2, N_KV, HEAD_DIM)),
        'cache_win': nrm(k[7], (DEPTH, DEC_BATCH, w_buf, 2, N_KV, HEAD_DIM)),
        'state_ssm': nrm(k[8], (DEPTH, DEC_BATCH, SSM_HEADS, SSM_HEAD_DIM, SSM_STATE), 0.5),
        'state_conv': nrm(k[9], (DEPTH, DEC_BATCH, CONV_W - 1, CONV_DIM)),
        'page_table': page_table,
        'c_prompt': nrm(k[10], (BATCH, D_MODEL)),
        'c_sample': nrm(k[11], (DEC_BATCH, D_MODEL)),
        'g_norm1': gain(k[12], D_MODEL),
        'g_norm2': gain(k[13], D_MODEL),
        'w_ada': nrm(k[14], (DEPTH, D_MODEL, 6 * D_MODEL), 0.3 * D_MODEL ** -0.5),
        'b_ada': nrm(k[15], (DEPTH, 6 * D_MODEL), 0.02),
        'w_in': nrm(k[16], (DEPTH, D_MODEL, IN_WIDTH), D_MODEL ** -0.5),
        'g_q': gain(k[17], HEAD_DIM),
        'g_k_cmp': gain(k[18], HEAD_DIM),
        'g_k_sel': gain(k[19], HEAD_DIM),
        'g_k_win': gain(k[20], HEAD_DIM),
        'w_pos_k': (1.0 + nrm(k[21], (DEPTH, CMP_BLOCK), 0.1)) / CMP_BLOCK,
        'w_pos_v': (1.0 + nrm(k[22], (DEPTH, CMP_BLOCK), 0.1)) / CMP_BLOCK,
        'conv_w': nrm(k[23], (DEPTH, CONV_W, CONV_DIM), CONV_W ** -0.5),
        'conv_b': nrm(k[24], (DEPTH, CONV_DIM), 0.02),
        'dt_bias': dt_bias,
        'a_log': a_log,
        'd_skip': 1.0 + nrm(k[25], (DEPTH, SSM_HEADS), 0.1),
        'g_att_out': gain(k[26], ATT_WIDTH),
        'g_ssm_out': gain(k[27], SSM_WIDTH),
        'w_out': nrm(k[28], (DEPTH, MIX_WIDTH, D_MODEL), MIX_WIDTH ** -0.5),
        'w_rg': nrm(k[29], (DEPTH, D_MODEL, N_EGROUPS), D_MODEL ** -0.5),
        'b_rg': nrm(k[30], (DEPTH, N_EGROUPS), 0.01),
        'w_re': nrm(k[31], (DEPTH, N_EGROUPS, D_MODEL, E_PER_GROUP), D_MODEL ** -0.5),
        'b_re': nrm(k[32], (DEPTH, N_EGROUPS, E_PER_GROUP), 0.01),
        'w_gate': nrm(k[33], (DEPTH, N_EXPERTS, D_MODEL, EXPERT_FF), D_MODEL ** -0.5),
        'w_up': nrm(k[34], (DEPTH, N_EXPERTS, D_MODEL, EXPERT_FF), D_MODEL ** -0.5),
        'w_down': nrm(k[35], (DEPTH, N_EXPERTS, EXPERT_FF, D_MODEL), EXPERT_FF ** -0.5),
    }


def reference(x_prompt, x_sample, cache_cmp, cache_sel, cache_win, state_ssm, state_conv, page_table, c_prompt, c_sample,
              g_norm1, g_norm2, w_ada, b_ada, w_in, g_q, g_k_cmp, g_k_sel, g_k_win, w_pos_k, w_pos_v, conv_w, conv_b,
              dt_bias, a_log, d_skip, g_att_out, g_ssm_out, w_out, w_rg, b_rg, w_re, b_re, w_gate, w_up, w_down):
    xp, xs = x_prompt, x_sample
    cmp_p, cmp_s, sel_p, sel_s, win_p, win_s, ssm_p, ssm_s, conv_p, conv_s = [[] for _ in range(10)]
    for l in range(DEPTH):
        mods = adaln_mods(c_prompt, w_ada[l], b_ada[l])
        h = rmsnorm(xp, g_norm1[l]) * (1 + mods[1]) + mods[0]
        q, kc, vc, kv_s, kv_w, gates, z, xbc, dt = in_proj(h, w_in[l], g_q[l], g_k_sel[l], g_k_win[l])
        o_c, o_s, o_w = nsa_prompt(q, kc, vc, kv_s, kv_w, w_pos_k[l], w_pos_v[l], g_k_cmp[l])
        att = combine_branches(gates, o_c, o_s, o_w)
        b = xp.shape[0]
        y_ssm, cv, hs = ssd_mixer(z, xbc, dt, jnp.zeros((b, CONV_W - 1, CONV_DIM), xbc.dtype),
                                  jnp.zeros((b, SSM_HEADS, SSM_HEAD_DIM, SSM_STATE), jnp.float32),
                                  conv_w[l], conv_b[l], dt_bias[l], a_log[l], d_skip[l], g_ssm_out[l])
        xp = finish(xp, att, y_ssm, mods, g_att_out[l], w_out[l], g_norm2[l], w_rg[l], b_rg[l], w_re[l], b_re[l],
                    w_gate[l], w_up[l], w_down[l])
        cmp_p.append(jnp.stack([kc, vc], axis=2))
        sel_p.append(kv_s)
        win_p.append(kv_w[:, -min(WINDOW, kv_w.shape[1]):])
        ssm_p.append(hs)
        conv_p.append(cv)
        mods = adaln_mods(c_sample, w_ada[l], b_ada[l])
        h = rmsnorm(xs, g_norm1[l]) * (1 + mods[1]) + mods[0]
        q, kc, vc, kv_s, kv_w, gates, z, xbc, dt = in_proj(h, w_in[l], g_q[l], g_k_sel[l], g_k_win[l])
        o_c, o_s, o_w, wbuf = nsa_sample(q, kv_s, kv_w, cache_cmp, cache_sel, cache_win, page_table, l,
                                         w_pos_k[l], w_pos_v[l], g_k_cmp[l])
        att = combine_branches(gates, o_c, o_s, o_w)
        y_ssm, cv, hs = ssd_mixer(z, xbc, dt, state_conv[l], state_ssm[l].astype(jnp.float32),
                                  conv_w[l], conv_b[l], dt_bias[l], a_log[l], d_skip[l], g_ssm_out[l])
        xs = finish(xs, att, y_ssm, mods, g_att_out[l], w_out[l], g_norm2[l], w_rg[l], b_rg[l], w_re[l], b_re[l],
                    w_gate[l], w_up[l], w_down[l])
        cmp_s.append(jnp.stack([kc, vc], axis=2))
        sel_s.append(kv_s)
        win_s.append(wbuf)
        ssm_s.append(hs.astype(state_ssm.dtype))
        conv_s.append(cv)
    return (xp, xs, jnp.stack(cmp_p), jnp.stack(cmp_s), jnp.stack(sel_p), jnp.stack(sel_s), jnp.stack(win_p),
            jnp.stack(win_s), jnp.stack(ssm_p), jnp.stack(ssm_s), jnp.stack(conv_p), jnp.stack(conv_s))
```

```python
import numpy as np
import ml_dtypes
from contextlib import ExitStack
import concourse.bass as bass
import concourse.mybir as mybir
from concourse.bass_utils import run_bass_kernel_spmd

F32 = mybir.dt.float32; BF16 = mybir.dt.bfloat16; I32 = mybir.dt.int32
AF = mybir.ActivationFunctionType; ALU = mybir.AluOpType; AX = mybir.AxisListType
NCORES = 8
SEQ = 2048; D = 1024; NT = 16; TS = 64; NSEQ = 16
INW = 2592
SCALE = 0.125
EPS = 1e-6
NEGB = -30000.0


class Tok:
    __slots__ = ("name", "w", "rs")

    def __init__(self, name="t"):
        self.name = name; self.w = None; self.rs = []


class KB:
    NSLOT = 8

    def __init__(self, nc):
        self.nc = nc; self.es = ExitStack(); self.stack = [self.es]
        self.eng = {"pe": nc.tensor, "act": nc.scalar, "dve": nc.vector, "pool": nc.gpsimd, "sp": nc.sync}
        self.sem = {k: self.es.enter_context(nc.semaphore("s_" + k)) for k in self.eng}
        self.cnt = {k: 0 for k in self.eng}
        self.seen = {k: {} for k in self.eng}
        self.dsem = {}; self.dcnt = {}; self.dnext = {}
        self.nslot = {"sp": 8, "act": 2, "pool": 16}
        for q in ("sp", "act", "pool"):
            self.dsem[q] = [self.es.enter_context(nc.semaphore(f"d_{q}{i}")) for i in range(self.nslot[q])]
            self.dcnt[q] = [0] * self.nslot[q]; self.dnext[q] = 0
        self.nins = 0

    def sb(self, name, shape, dt):
        return self.stack[-1].enter_context(self.nc.sbuf_tensor(name, list(shape), dt))

    def ps(self, name, shape, dt):
        return self.es.enter_context(self.nc.psum_tensor(name, list(shape), dt))

    def _wait(self, e, ev):
        if ev is None:
            return
        key, val = ev
        if self.seen[e].get(key, 0) >= val:
            return
        self.seen[e][key] = val
        sem = self.sem[key[1]] if key[0] == "c" else self.dsem[key[1]][key[2]]
        self.eng[e].wait_ge(sem, val)

    def _deps(self, e, reads, writes):
        for t in reads:
            self._wait(e, t.w)
        for t in writes:
            self._wait(e, t.w)
            for r in t.rs:
                self._wait(e, r)

    def _commit(self, ev, reads, writes):
        for t in reads:
            t.rs = [r for r in t.rs if r[0] != ev[0]] + [ev]
        for t in writes:
            t.w = ev; t.rs = []

    def op(self, e, fn, reads=(), writes=()):
        self._deps(e, reads, writes)
        ins = fn(self.eng[e])
        self.cnt[e] += 1
        ins.then_inc(self.sem[e], 1)
        ev = (("c", e), self.cnt[e])
        self._commit(ev, reads, writes); self.nins += 1
        return ins

    def dma(self, q, fn, reads=(), writes=()):
        s = self.dnext[q]; self.dnext[q] = (s + 1) % self.nslot[q]
        key = ("d", q, s)
        if self.dcnt[q][s] > 0:
            self._wait(q, (key, self.dcnt[q][s]))
        self._deps(q, reads, writes)
        ins = fn(self.eng[q])
        self.dcnt[q][s] += 16
        ins.then_inc(self.dsem[q][s], 16)
        ev = (key, self.dcnt[q][s])
        self._commit(ev, reads, writes); self.nins += 1
        return ins

    def push(self):
        self.stack.append(ExitStack())

    def barrier(self):
        for e in self.eng:
            for e2 in self.eng:
                if e2 != e and self.cnt[e2] > 0:
                    self._wait(e, (("c", e2), self.cnt[e2]))
            for q in self.dsem:
                for s in range(self.nslot[q]):
                    if self.dcnt[q][s] > 0:
                        self._wait(e, (("d", q, s), self.dcnt[q][s]))

    def pop(self):
        self.barrier()
        self.stack.pop().close()

    def finish(self, toks):
        for t in toks:
            self._wait("sp", t.w)
            for r in t.rs:
                self._wait("sp", r)


def _bf(a):
    return np.ascontiguousarray(a.astype(np.float32)).astype(ml_dtypes.bfloat16)


def make_consts():
    c = {}
    c["ident_bf"] = _bf(np.eye(128))
    c["ident_f"] = np.eye(128, dtype=np.float32)
    tk = np.arange(128)[:, None]; tq = np.arange(128)[None, :]
    cb = np.where(tk <= tq, 0.0, NEGB)
    c["causalb"] = _bf(np.tile(cb, (1, 4)))
    ab = np.where(tk > tq, 0.0, NEGB)
    c["antib"] = _bf(np.tile(ab, (1, 4)))
    E = np.zeros((32, 16, 128), np.float32)
    for cc in range(16):
        for t in range(128):
            E[2 * cc + t // 64, cc, t] = 1.0
    c["Eall"] = _bf(E.reshape(32, 16 * 128))
    Dsel = np.zeros((4, 124), np.float32)
    for jj in range(4):
        Dsel[jj, 60 + jj] = 1.0
    c["Dsel"] = _bf(Dsel)
    p = np.arange(128)[None, :]; jj = np.arange(4)[:, None]
    cmpB = np.where(32 * jj + 31 <= p, 0.0, NEGB)
    c["cmpB"] = _bf(np.tile(cmpB, (1, 4)))
    pc = np.arange(128)[:, None]; cc = np.arange(124)[None, :]
    c["cmpM0"] = ((cc - 60) <= np.floor((pc - 31) / 32.0)).astype(np.float32)
    selA = np.zeros((128, 16, 32), np.float32); selB = np.zeros((128, 16, 32), np.float32)
    allowed = np.zeros((128, 16, 32), np.float32)
    for i in range(16):
        for pp in range(128):
            cur = 2 * i + (1 if pp >= 64 else 0)
            for n in range(32):
                if n < cur:
                    allowed[pp, i, n] = 1.0
                    if n == cur - 1:
                        selB[pp, i, n] = 2e9
                    elif n == 0:
                        selB[pp, i, n] = 1e9
                    else:
                        selA[pp, i, n] = 1.0
                else:
                    selB[pp, i, n] = -1e30
    c["selA"] = selA.reshape(128, 512); c["selB"] = selB.reshape(128, 512); c["allowed"] = allowed.reshape(128, 512)
    s = np.arange(128)[:, None]; l = np.arange(128)[None, :]
    c["U"] = (s <= l).astype(np.float32)
    c["NB"] = np.where(l >= s, 0.0, -1e30).astype(np.float32)
    c["ones_f"] = np.ones((128, 128), np.float32)
    pp = np.arange(128)
    c["PM4"] = (pp % 4).astype(np.float32).reshape(128, 1)
    c["R64"] = (pp % 64).astype(np.float32).reshape(128, 1)
    H = np.zeros((32, 8), np.float32)
    for g in range(2):
        for t in range(4):
            for q in range(4):
                H[g * 16 + t * 4 + q, g * 4 + t] = 1.0
    c["HSEL"] = H
    B = np.zeros((128, 128), np.float32); B[:, 0] = 1e9; B[:, 127] = 2e9
    HB_ = np.zeros((32, 16, 128), np.float32)
    for b_ in range(16):
        for g in range(2):
            for t in range(4):
                for q in range(4):
                    HB_[g * 16 + t * 4 + q, b_, b_ * 8 + g * 4 + t] = 1.0
    c["HSELB"] = HB_.reshape(32, 2048)
    c["BIGS"] = B
    c["I8"] = np.eye(8, dtype=np.float32)
    E16 = np.zeros((16, 128), np.float32)
    for p_ in range(128):
        E16[p_ // 8, p_] = 1.0
    c["EXP16"] = E16
    c["PM8"] = (pp % 8).astype(np.float32).reshape(128, 1)
    c["PIDX"] = pp.astype(np.float32).reshape(128, 1)
    c["M120"] = (pp < 120).astype(np.float32).reshape(128, 1)
    m15 = np.ones((128, 8), np.float32); m15[64:, 7] = 0.0
    c["MASK15"] = m15
    c["MASKW"] = (pp[:, None] > np.arange(4)[None, :]).astype(np.float32)
    c["CM4"] = (np.arange(4)[:, None] <= np.arange(4)[None, :]).astype(np.float32)
    return c


def build(nc, cshapes, with_moe=True, debug=False, pool_rows=1310720):
    k = KB(nc)
    k.es.enter_context(nc.allow_non_contiguous_dma(reason="small strided loads"))

    def din(name, shape, dt=F32):
        return nc.dram_tensor(name, list(shape), dt, kind="ExternalInput").ap()

    def dout(name, shape, dt=F32):
        return nc.dram_tensor(name, list(shape), dt, kind="ExternalOutput").ap()

    def dint(name, shape, dt=F32):
        kind = "ExternalOutput" if (debug and name in ("att_d", "x1_d", "comb_d")) else "Internal"
        return nc.dram_tensor(name, list(shape), dt, kind=kind).ap()

    xp = din("xp", [SEQ, D]); xs = din("xs", [TS, D]); cpr = din("cp", [1, D]); csm = din("cs", [NSEQ, D])
    sssm = din("sssm", [128, 4096]); sconv = din("sconv", [NSEQ, 3, 768]); cwin = din("cwin", [NSEQ, 512, 256])
    gn1 = din("g_norm1", [D]); gn2 = din("g_norm2", [D])
    w_ada = din("w_ada", [D, 6 * D]); b_ada = din("b_ada", [6 * D])
    w_in = din("w_in", [D, INW])
    gq8 = din("gq8", [512]); gks2 = din("gks2", [128]); gkw2 = din("gkw2", [128]); gkc = din("gkc", [64, 1])
    convw = din("convw", [4 * 768]); convb = din("convb", [768])
    dtb = din("dtb", [8]); alog = din("alog", [8]); dsk = din("dsk", [512])
    abh = din("abh", [128, 1]); dbh = din("dbh", [128, 1])
    gatt = din("gatt", [512]); gssm = din("gssm", [512])
    w_out = din("w_out", [D, D]); w_r = din("w_r", [D, 20]); b_r = din("b_r", [20])
    w_gate = din("w_gate", [16, D, 256]); w_up = din("w_up", [16, D, 256]); w_down = din("w_down", [16, 256, D])
    wkblk = din("wkblk", [128, 4]); wvsel = din("wvsel", [128, 16 * 64])
    ccmp = din("ccmp", [pool_rows, 256]); csel = din("csel", [pool_rows, 256]); ptab = din("ptab", [NSEQ, 64], I32)
    wk32 = din("wk32", [128, 4096]); wv32 = din("wv32", [128, 4096]); gkc2 = din("gkc2", [128])
    cd = {}
    for n, (shp, isbf) in cshapes.items():
        cd[n] = din("c_" + n, shp, BF16 if isbf else F32)

    yp = dout("yp", [SEQ, D]); ys = dout("ys", [TS, D])
    cmpp = dout("cmpp", [SEQ, 256]); cmps = dout("cmps", [TS, 256])
    selp = dout("selp", [SEQ, 256]); sels = dout("sels", [TS, 256])
    winp = dout("winp", [512, 256]); wins = dout("wins", [NSEQ, 512, 256])
    ssmp = dout("ssmp", [8, 64, 64]); ssms = dout("ssms", [128, 4096])
    convp = dout("convp", [3, 768]); convs = dout("convs", [NSEQ, 3, 768])

    NTOK = SEQ + TS
    mods_d = dint("mods_d", [65, 6 * D]); t_mods_d = Tok()
    xbc_d = dint("xbc_d", [SEQ + 3, 768]); t_xbc_d = Tok()
    xbcs_d = dint("xbcs_d", [NSEQ, 7, 768]); t_xbcs_d = Tok()
    zdt_d = dint("zdt_d", [NTOK, 520]); t_zdt_d = Tok()
    att_d = dint("att_d", [NTOK, 512]); t_att_d = Tok()
    x1_d = dint("x1_d", [NTOK, D]); t_x1_d = Tok()
    h2T_d = dint("h2T_d", [8, 128, NTOK], BF16); t_h2T_d = Tok()
    comb_d = dint("comb_d", [NTOK, 16]); t_comb_d = Tok()
    ssc_d = dint("ssc_d", [TS, 776]); t_ssc_d = Tok()
    ysd_d = dint("ysd_d", [NSEQ, 4, 512]); t_ysd_d = Tok()
    qs_d = dint("qs_d", [TS, 512], BF16); t_qs_d = Tok()
    kvs_d = dint("kvs_d", [TS, 512]); t_kvs_d = Tok()
    gts_d = dint("gts_d", [TS, 24]); t_gts_d = Tok()
    atts_d = dint("atts_d", [3, NSEQ, 4, 2, 4, 64]); t_atts_d = Tok()
    out_toks = []

    def otok():
        t = Tok(); out_toks.append(t); return t

    C = {}; tC = Tok("consts")
    for n, (shp, isbf) in cshapes.items():
        C[n] = k.sb("C_" + n, shp, BF16 if isbf else F32)
        k.dma("sp", lambda e, n=n: e.dma_start(out=C[n][:], in_=cd[n]), writes=[tC])
    EPSC = k.sb("EPSC", [128, 1], F32)
    k.op("dve", lambda e: e.memset(EPSC[:], EPS), writes=[tC])
    SM = k.sb("SM", [128, 64], F32); tSM = Tok()
    PB = [k.es.enter_context(nc.psum_tensor(f"pb{i}", [128, 512], F32)) for i in range(6)]
    tPB = [Tok(f"pb{i}") for i in range(6)]
    PT = [k.es.enter_context(nc.psum_tensor(f"pt{i}", [128, 1024], BF16)) for i in range(2)]
    tPT = [Tok(f"pt{i}") for i in range(2)]

    def bc_load(name, src1d, width, parts=128, tok=None):
        t = k.sb(name, [parts, width], F32)
        k.dma("sp", lambda e: e.dma_start(out=t[:], in_=src1d.partition_broadcast(parts)), writes=[tok or tC])
        return t

    def rstd_of(ap, T, scale):
        k.op("act", lambda e: e.activation(out=ap, in_=ap, func=AF.Sqrt, bias=EPSC[:T, :], scale=scale), reads=[tSM, tC], writes=[tSM])
        k.op("dve", lambda e: e.reciprocal(out=ap, in_=ap), reads=[tSM], writes=[tSM])

    k.push()
    GN1 = bc_load("GN1", gn1, D, 65); GN2 = bc_load("GN2", gn2, D, 65)
    stg = [k.sb(f"stg{i}", [128, 512], F32) for i in range(2)]; tstg = [Tok(), Tok()]
    cT = k.sb("cT", [128, 8, 65], F32); tcT = Tok()
    k.dma("sp", lambda e: e.dma_start(out=cT[:, :, 0:1], in_=cpr.rearrange("b (k p) -> p k b", p=128)), writes=[tcT])
    for kk in range(8):
        k.dma("sp", lambda e, kk=kk: e.dma_start(out=cT[:, kk, 1:17], in_=csm[:, kk * 128:(kk + 1) * 128].rearrange("b p -> p b")), writes=[tcT])
    scT = k.sb("scT", [128, 8, 17], F32)
    k.op("act", lambda e: e.activation(out=scT[:], in_=cT[:, :, 0:17], func=AF.Silu), reads=[tcT], writes=[tcT])
    L = k.sb("L", [128, 8, 65], BF16)
    k.op("dve", lambda e: e.tensor_copy(out=L[:, :, 0:1], in_=scT[:, :, 0:1]), reads=[tcT], writes=[tcT])
    k.op("dve", lambda e: e.tensor_copy(out=L[:, :, 1:65].rearrange("p k (b t) -> p k b t", t=4),
                                        in_=scT[:, :, 1:17].unsqueeze(3).to_broadcast([128, 8, NSEQ, 4])), reads=[tcT], writes=[tcT])
    wab = [k.sb(f"wab{i}", [128, 8, 512], BF16) for i in range(2)]; twab = [Tok(), Tok()]
    bab = [k.sb(f"bab{i}", [65, 512], F32) for i in range(2)]; tbab = [Tok(), Tok()]
    M65 = k.sb("M65", [65, 6 * D], F32); tM65 = Tok()
    for cg in range(12):
        s = cg % 2
        for kk in range(8):
            ss = kk % 2
            k.dma("sp", lambda e, kk=kk, ss=ss, cg=cg: e.dma_start(out=stg[ss][:, :], in_=w_ada[kk * 128:(kk + 1) * 128, cg * 512:(cg + 1) * 512]), writes=[tstg[ss]])
            k.op("pool", lambda e, kk=kk, ss=ss, s=s: e.tensor_copy(out=wab[s][:, kk, :], in_=stg[ss][:, :]), reads=[tstg[ss]], writes=[twab[s]])
        k.dma("sp", lambda e, s=s, cg=cg: e.dma_start(out=bab[s][:], in_=b_ada[cg * 512:(cg + 1) * 512].partition_broadcast(65)), writes=[tbab[s]])
        for kk in range(8):
            k.op("pe", lambda e, kk=kk, s=s: e.matmul(PB[s][:65, :], lhsT=L[:, kk, :], rhs=wab[s][:, kk, :], start=(kk == 0), stop=(kk == 7)),
                 reads=[tcT, twab[s]], writes=[tPB[s]])
        k.op("dve", lambda e, s=s, cg=cg: e.tensor_tensor(out=M65[:, cg * 512:(cg + 1) * 512], in0=PB[s][:65, :], in1=bab[s][:, :], op=ALU.add),
             reads=[tPB[s], tbab[s]], writes=[tM65])
    for (sl, G) in ((1, GN1), (4, GN2)):
        k.op("dve", lambda e, sl=sl, G=G: e.scalar_tensor_tensor(out=M65[:, sl * D:(sl + 1) * D], in0=M65[:, sl * D:(sl + 1) * D], scalar=1.0, in1=G[:, :], op0=ALU.add, op1=ALU.mult),
             reads=[tM65, tC], writes=[tM65])
    k.dma("sp", lambda e: e.dma_start(out=mods_d[:, :], in_=M65[:, :]), reads=[tM65], writes=[t_mods_d])
    k.pop()

    def load_mod(tile, T, slot, tok):
        if T == 128:
            k.dma("sp", lambda e: e.dma_start(out=tile[:, :], in_=mods_d[0, slot * D:(slot + 1) * D].partition_broadcast(128)), reads=[t_mods_d], writes=[tok])
        else:
            k.dma("sp", lambda e: e.dma_start(out=tile[:TS, :], in_=mods_d[1:65, slot * D:(slot + 1) * D]), reads=[t_mods_d], writes=[tok])

    k.push()
    tG = Tok("gains1")
    GQ = bc_load("GQ", gq8, 512, tok=tG); GKS = bc_load("GKS", gks2, 128, tok=tG); GKW = bc_load("GKW", gkw2, 128, tok=tG)
    GKC = k.sb("GKC", [64, 1], F32)
    k.dma("sp", lambda e: e.dma_start(out=GKC[:], in_=gkc), writes=[tG])
    WKB = k.sb("WKB", [128, 4], F32); WVS = k.sb("WVS", [128, 16 * 64], F32)
    k.dma("sp", lambda e: e.dma_start(out=WKB[:], in_=wkblk), writes=[tG])
    k.dma("sp", lambda e: e.dma_start(out=WVS[:], in_=wvsel), writes=[tG])
    stgw = [k.sb(f"stgw{i}", [128, INW], F32) for i in range(2)]; tstgw = [Tok(), Tok()]
    WIN = k.sb("WIN", [128, 8, INW], BF16); tWIN = Tok()
    for kk in range(8):
        s = kk % 2
        k.dma("sp", lambda e, kk=kk, s=s: e.dma_start(out=stgw[s][:], in_=w_in[kk * 128:(kk + 1) * 128, :]), writes=[tstgw[s]])
        k.op("pool", lambda e, kk=kk, s=s: e.tensor_copy(out=WIN[:, kk, :], in_=stgw[s][:]), reads=[tstgw[s]], writes=[tWIN])
    A1 = k.sb("A1", [128, D], F32); B1 = k.sb("B1", [128, D], F32); tM1 = Tok()
    KST = k.sb("KST", [64, 2, SEQ], BF16); KWT = k.sb("KWT", [64, 2, SEQ], BF16)
    VS = k.sb("VS", [128, NT, 2, 65], BF16); VW = k.sb("VW", [128, NT, 2, 65], BF16)
    KCT = k.sb("KCT", [64, 2, 64], BF16); VCA = k.sb("VCA", [64, 128], F32); VC = k.sb("VC", [64, 2, 65], BF16)
    tKS = [Tok() for _ in range(NT)]; tKW = [Tok() for _ in range(NT)]; tVS = [Tok() for _ in range(NT)]; tVW = [Tok() for _ in range(NT)]
    tKC = Tok(); tVC = Tok()
    k.op("pool", lambda e: e.memset(VS[:], 1.0), writes=tVS)
    k.op("pool", lambda e: e.memset(VW[:], 1.0), writes=tVW)
    k.op("pool", lambda e: e.memset(VC[:], 1.0), writes=[tVC])
    k.op("pool", lambda e: e.memset(KCT[:], 0.0), writes=[tKC])
    k.op("pool", lambda e: e.memset(VCA[:], 0.0), writes=[tVC])
    ZR = k.sb("ZR", [3, 768], F32); tZR = Tok()
    k.op("pool", lambda e: e.memset(ZR[:], 0.0), writes=[tZR])
    k.dma("sp", lambda e: e.dma_start(out=xbc_d[0:3, :], in_=ZR[:]), reads=[tZR], writes=[t_xbc_d])

    XT = k.sb("XT", [128, D], F32); tXT = Tok()
    TMP = k.sb("TMP", [128, D], F32); tTMP = Tok()
    HB = k.sb("HB", [128, D], BF16); tHB = Tok()
    HTt = k.sb("HTt", [128, 8, 128], BF16); tHTt = Tok()
    Ut = k.sb("Ut", [128, INW], F32); tU = Tok()
    QN = k.sb("QN", [128, 512], BF16); tQN = Tok()
    QT = k.sb("QT", [64, 8, 128], BF16); tQT = Tok()
    SELO = k.sb("SELO", [128, 256], F32); tSELO = Tok()
    WINO = k.sb("WINO", [128, 256], F32); tWINO = Tok()
    KNB = k.sb("KNB", [128, 256], BF16); tKNB = Tok()
    ATT = k.sb("ATT", [128, 512], F32); tATT = Tok()
    PTt = [k.sb(f"PTt{i}", [128, 512], BF16) for i in range(3)]; tPTt = [Tok(), Tok(), Tok()]
    GT = k.sb("GT", [128, 24], F32); tGT = Tok()
    SE = k.sb("SE", [128, 256], F32); tSE = Tok()
    S2 = k.sb("S2", [128, 64], F32); IMP = k.sb("IMP", [128, 32], F32); SC = k.sb("SC", [128, 32], F32); SC2 = k.sb("SC2", [128, 32], F32)
    M1 = k.sb("M1", [128, 8], F32); M2 = k.sb("M2", [128, 8], F32); NMB = k.sb("NMB", [128, 32], BF16); tSEL = Tok()
    NMT = k.sb("NMT", [32, 4, 128], BF16); tNMT = Tok()
    KR = k.sb("KR", [64, 16], F32); tKR = Tok()
    COEF = k.sb("COEF", [128, 8], F32); tCOEF = Tok()
    OT = k.sb("OT", [128, 256], F32); tOT = Tok()

    def headnorm(T, src, nh, gain, out32, outbf, rd, wr, col):
        w = nh * 64
        k.op("dve", lambda e: e.tensor_tensor(out=TMP[:T, :w], in0=src, in1=src, op=ALU.mult), reads=rd, writes=[tTMP])
        k.op("dve", lambda e: e.tensor_reduce(out=SM[:T, col:col + nh], in_=TMP[:T, :w].rearrange("p (h d) -> p h d", d=64), axis=AX.X, op=ALU.add), reads=[tTMP], writes=[tSM])
        rstd_of(SM[:T, col:col + nh], T, 1.0 / 64)
        k.op("dve", lambda e: e.tensor_tensor(out=TMP[:T, :w].rearrange("p (h d) -> p h d", d=64), in0=src.rearrange("p (h d) -> p h d", d=64),
                                              in1=SM[:T, col:col + nh].unsqueeze(2).to_broadcast([T, nh, 64]), op=ALU.mult), reads=rd + [tSM], writes=[tTMP])
        if out32 is not None:
            k.op("dve", lambda e: e.tensor_tensor(out=out32, in0=TMP[:T, :w], in1=gain, op=ALU.mult), reads=[tTMP, tG], writes=wr)
            k.op("act", lambda e: e.copy(out=outbf, in_=out32), reads=wr, writes=[tKNB])
        else:
            k.op("dve", lambda e: e.tensor_tensor(out=outbf, in0=TMP[:T, :w], in1=gain, op=ALU.mult), reads=[tTMP, tG], writes=wr)

    def front(T, xsrc, cmp_o, sel_o, xbc_dst, zrow0):
        k.dma("sp", lambda e: e.dma_start(out=XT[:T, :], in_=xsrc), writes=[tXT])
        k.op("act", lambda e: e.activation(out=HB[:T, :], in_=XT[:T, :], func=AF.Square, accum_out=SM[:T, 0:1]), reads=[tXT], writes=[tHB, tSM])
        rstd_of(SM[:T, 0:1], T, 1.0 / D)
        k.op("dve", lambda e: e.scalar_tensor_tensor(out=TMP[:T, :], in0=XT[:T, :], scalar=SM[:T, 0:1], in1=A1[:T, :], op0=ALU.mult, op1=ALU.mult),
             reads=[tXT, tSM, tM1], writes=[tTMP])
        k.op("dve", lambda e: e.tensor_tensor(out=HB[:T, :], in0=TMP[:T, :], in1=B1[:T, :], op=ALU.add), reads=[tTMP, tM1], writes=[tHB])
        for kk in range(8):
            k.op("pe", lambda e, kk=kk: e.transpose(PT[0][:, kk * 128:kk * 128 + T], HB[:T, kk * 128:(kk + 1) * 128], C["ident_bf"][:T, :T]), reads=[tHB, tC], writes=[tPT[0]])
        k.op("act", lambda e: e.copy(out=HTt[:, :, :T], in_=PT[0][:, :].rearrange("p (k t) -> p k t", k=8)[:, :, :T]), reads=[tPT[0]], writes=[tHTt])
        groups = [(0, 512), (512, 512), (1024, 288), (1312, 512), (1824, 512), (2336, 256)]
        for gi, (c0, w) in enumerate(groups):
            pb = gi % 2
            for kk in range(8):
                k.op("pe", lambda e, kk=kk, pb=pb, c0=c0, w=w: e.matmul(PB[pb][:T, :w], lhsT=HTt[:, kk, :T], rhs=WIN[:, kk, c0:c0 + w], start=(kk == 0), stop=(kk == 7)),
                     reads=[tHTt, tWIN], writes=[tPB[pb]])
            if gi % 2 == 0:
                k.op("act", lambda e, pb=pb, c0=c0, w=w: e.copy(out=Ut[:T, c0:c0 + w], in_=PB[pb][:T, :w]), reads=[tPB[pb]], writes=[tU])
            else:
                k.op("dve", lambda e, pb=pb, c0=c0, w=w: e.tensor_copy(out=Ut[:T, c0:c0 + w], in_=PB[pb][:T, :w]), reads=[tPB[pb]], writes=[tU])
        k.dma("sp", lambda e: e.dma_start(out=cmp_o, in_=Ut[:T, 512:768]), reads=[tU], writes=[otok()])
        if T == 128:
            k.dma("sp", lambda e: e.dma_start(out=xbc_dst, in_=Ut[:T, 1824:2592]), reads=[tU], writes=[t_xbc_d])
        else:
            for b in range(NSEQ):
                k.dma("sp", lambda e, b=b: e.dma_start(out=xbc_dst[b, :, :], in_=Ut[4 * b:4 * b + 4, 1824:2592]), reads=[tU], writes=[t_xbcs_d])
        k.dma("sp", lambda e: e.dma_start(out=zdt_d[zrow0:zrow0 + T, 0:512], in_=Ut[:T, 1312:1824]), reads=[tU], writes=[t_zdt_d])
        k.dma("sp", lambda e: e.dma_start(out=zdt_d[zrow0:zrow0 + T, 512:520], in_=Ut[:T, 1304:1312]), reads=[tU], writes=[t_zdt_d])
        headnorm(T, Ut[:T, 0:512], 8, GQ[:T, :], None, QN[:T, :], [tU], [tQN], 8)
        for h in range(8):
            k.op("pe", lambda e, h=h: e.transpose(PT[1][:64, h * 128:h * 128 + T], QN[:T, h * 64:(h + 1) * 64], C["ident_bf"][:T, :T]), reads=[tQN, tC], writes=[tPT[1]])
        k.op("act", lambda e: e.copy(out=QT[:, :, :T], in_=PT[1][:64, :].rearrange("p (h t) -> p h t", h=8)[:, :, :T]), reads=[tPT[1]], writes=[tQT])
        headnorm(T, Ut[:T, 768:896], 2, GKS[:T, :], SELO[:T, 0:128], KNB[:T, 0:128], [tU], [tSELO], 16)
        k.op("act", lambda e: e.copy(out=SELO[:T, 128:256], in_=Ut[:T, 896:1024]), reads=[tU], writes=[tSELO])
        headnorm(T, Ut[:T, 1024:1152], 2, GKW[:T, :], WINO[:T, 0:128], KNB[:T, 128:256], [tU], [tWINO], 18)
        k.op("act", lambda e: e.copy(out=WINO[:T, 128:256], in_=Ut[:T, 1152:1280]), reads=[tU], writes=[tWINO])
        k.dma("sp", lambda e: e.dma_start(out=sel_o, in_=SELO[:T, :]), reads=[tSELO], writes=[otok()])
        k.op("act", lambda e: e.activation(out=GT[:T, :], in_=Ut[:T, 1280:1304], func=AF.Sigmoid), reads=[tU], writes=[tGT])

    pvn = [0]

    pend = []; pvbank = [5]

    def flush():
        while pend:
            pend.pop(0)()

    def combine(br, g):
        flush()
        bk = pvbank[0]; pvbank[0] = 9 - bk
        PBk = PB[bk]; tPBk = tPB[bk]
        den = PBk[:, 0:260].rearrange("p (h c) -> p h c", c=65)[:, :, 64]
        k.op("dve", lambda e: e.tensor_scalar_max(out=COEF[:, 0:4], in0=den, scalar1=1e-30), reads=[tPBk], writes=[tCOEF])
        k.op("dve", lambda e: e.reciprocal(out=COEF[:, 0:4], in_=COEF[:, 0:4]), reads=[tCOEF], writes=[tCOEF])
        k.op("dve", lambda e: e.tensor_tensor(out=COEF[:, 4:8], in0=COEF[:, 0:4], in1=GT[:, br * 8 + g * 4:br * 8 + g * 4 + 4], op=ALU.mult), reads=[tCOEF, tGT], writes=[tCOEF])
        ov = PBk[:, 0:260].rearrange("p (h c) -> p h c", c=65)[:, :, 0:64]
        cf = COEF[:, 4:8].unsqueeze(2).to_broadcast([128, 4, 64])
        av = ATT[:, g * 256:(g + 1) * 256].rearrange("p (h d) -> p h d", d=64)
        if br == 0:
            k.op("dve", lambda e: e.tensor_tensor(out=av, in0=ov, in1=cf, op=ALU.mult), reads=[tPBk, tCOEF], writes=[tATT])
        else:
            k.op("dve", lambda e: e.tensor_tensor(out=OT[:, :].rearrange("p (h d) -> p h d", d=64), in0=ov, in1=cf, op=ALU.mult), reads=[tPBk, tCOEF], writes=[tOT])
            k.op("pool", lambda e: e.tensor_tensor(out=ATT[:, g * 256:(g + 1) * 256], in0=ATT[:, g * 256:(g + 1) * 256], in1=OT[:, :], op=ALU.add), reads=[tOT, tATT], writes=[tATT])

    def chunk(g, kT_ap, ktoks, bias, v_ap, vtoks, nk, first, last):
        n = pvn[0]; pvn[0] += 1
        pb = n % 2; s = n % 3
        k.op("pe", lambda e: e.matmul(PB[pb][:nk, :], lhsT=kT_ap, rhs=QT[:, 4 * g:4 * g + 4, :], start=True, stop=(bias is None)),
             reads=[tQT] + ktoks, writes=[tPB[pb]])
        if bias is not None:
            k.op("pe", lambda e: e.matmul(PB[pb][:nk, :], lhsT=bias[0], rhs=bias[1], start=False, stop=True), reads=bias[2], writes=[tPB[pb]])
        k.op("act", lambda e: e.activation(out=PTt[s][:nk, :], in_=PB[pb][:nk, :], func=AF.Exp, scale=SCALE), reads=[tPB[pb]], writes=[tPTt[s]])
        bk = pvbank[0]

        def pv():
            for h in range(4):
                k.op("pe", lambda e, h=h: e.matmul(PB[bk][:, h * 65:(h + 1) * 65], lhsT=PTt[s][:nk, h * 128:(h + 1) * 128], rhs=v_ap, start=(first and h == 0), stop=last, skip_group_check=True),
                     reads=[tPTt[s]] + vtoks, writes=[tPB[bk]])
        pend.append(pv)
        if len(pend) > 2:
            pend.pop(0)()

    load_mod(A1, 128, 1, tM1); load_mod(B1, 128, 0, tM1)
    for i in range(NT):
        r0 = i * 128
        front(128, xp[r0:r0 + 128, :], cmpp[r0:r0 + 128, :], selp[r0:r0 + 128, :], xbc_d[3 + r0:3 + r0 + 128, :], r0)
        if i >= 12:
            k.dma("sp", lambda e, i=i: e.dma_start(out=winp[(i - 12) * 128:(i - 11) * 128, :], in_=WINO[:, :]), reads=[tWINO], writes=[otok()])
        if i == NT - 1:
            k.dma("sp", lambda e: e.dma_start(out=convp[:, :], in_=xbc_d[SEQ:SEQ + 3, :]), reads=[t_xbc_d], writes=[otok()])
        for j in range(4):
            k.op("pe", lambda e, j=j: e.transpose(PT[1][:64, j * 128:(j + 1) * 128], KNB[:, j * 64:(j + 1) * 64], C["ident_bf"][:, :]), reads=[tKNB, tC], writes=[tPT[1]])
        k.op("act", lambda e: e.copy(out=KST[:, :, r0:r0 + 128], in_=PT[1][:64, 0:256].rearrange("p (g t) -> p g t", g=2)), reads=[tPT[1]], writes=[tKS[i]])
        k.op("act", lambda e: e.copy(out=KWT[:, :, r0:r0 + 128], in_=PT[1][:64, 256:512].rearrange("p (g t) -> p g t", g=2)), reads=[tPT[1]], writes=[tKW[i]])
        k.op("pool", lambda e: e.tensor_copy(out=VS[:, i, :, 0:64], in_=Ut[:, 896:1024].rearrange("p (g d) -> p g d", g=2)), reads=[tU], writes=[tVS[i]])
        k.op("pool", lambda e: e.tensor_copy(out=VW[:, i, :, 0:64], in_=Ut[:, 1152:1280].rearrange("p (g d) -> p g d", g=2)), reads=[tU], writes=[tVW[i]])
        for g in range(2):
            k.op("pe", lambda e, g=g: e.matmul(PB[2][:64, g * 4:(g + 1) * 4], lhsT=Ut[:, 512 + g * 64:576 + g * 64], rhs=WKB[:, :], start=True, stop=True), reads=[tU, tG], writes=[tPB[2]])
        k.op("act", lambda e: e.activation(out=KR[:, 0:8], in_=PB[2][:64, 0:8], func=AF.Square), reads=[tPB[2]], writes=[tKR])
        k.op("pe", lambda e: e.matmul(PB[3][:64, 0:8], lhsT=C["ones_f"][:64, :64], rhs=KR[:, 0:8], start=True, stop=True), reads=[tKR, tC], writes=[tPB[3]])
        k.op("act", lambda e: e.activation(out=KR[:, 8:16], in_=PB[3][:64, 0:8], func=AF.Sqrt, bias=EPSC[:64, :], scale=1.0 / 64), reads=[tPB[3], tC], writes=[tKR])
        k.op("dve", lambda e: e.reciprocal(out=KR[:, 8:16], in_=KR[:, 8:16]), reads=[tKR], writes=[tKR])
        k.op("dve", lambda e: e.scalar_tensor_tensor(out=KCT[:, :, 4 * i:4 * i + 4], in0=PB[2][:64, 0:8].rearrange("p (g j) -> p g j", g=2), scalar=GKC[:, 0:1],
                                                     in1=KR[:, 8:16].rearrange("p (g j) -> p g j", g=2), op0=ALU.mult, op1=ALU.mult), reads=[tPB[2], tKR, tG], writes=[tKC])
        k.op("pe", lambda e: e.matmul(PB[3][:64, 128:256], lhsT=WVS[:, i * 64:(i + 1) * 64], rhs=Ut[:, 640:768], start=True, stop=True), reads=[tU, tG], writes=[tPB[3]])
        k.op("dve", lambda e: e.tensor_tensor(out=VCA[:, :], in0=VCA[:, :], in1=PB[3][:64, 128:256], op=ALU.add), reads=[tPB[3], tVC], writes=[tVC])
        k.op("dve", lambda e: e.tensor_copy(out=VC[:, :, 0:64], in_=VCA[:, :].rearrange("p (g d) -> p g d", g=2)), reads=[tVC], writes=[tVC])
        nk = 4 * (i + 1)
        for g in range(2):
            chunk(g, KCT[:, g, 0:nk], [tKC], (C["Dsel"][:, 60 - 4 * i:60 - 4 * i + nk], C["cmpB"][:, :], [tC]), VC[:nk, g, :], [tVC], nk, True, True)
            combine(0, g)
            for h in range(4):
                k.op("pe", lambda e, h=h: e.matmul(PB[2][:, h * 64:(h + 1) * 64], lhsT=QT[:, 4 * g + h, :], rhs=KCT[:, g, :], start=True, stop=True), reads=[tQT, tKC], writes=[tPB[2]])
            k.op("act", lambda e: e.activation(out=SE[:, :], in_=PB[2][:, 0:256], func=AF.Exp, scale=SCALE), reads=[tPB[2]], writes=[tSE])
            k.op("dve", lambda e: e.tensor_tensor(out=SE[:, :].rearrange("p (h j) -> p h j", h=4), in0=SE[:, :].rearrange("p (h j) -> p h j", h=4),
                                                  in1=C["cmpM0"][:, 60 - 4 * i:124 - 4 * i].unsqueeze(1).to_broadcast([128, 4, 64]), op=ALU.mult), reads=[tSE, tC], writes=[tSE])
            k.op("dve", lambda e: e.tensor_reduce(out=SM[:, 24:28], in_=SE[:, :].rearrange("p (h j) -> p h j", h=4), axis=AX.X, op=ALU.add), reads=[tSE], writes=[tSM])
            k.op("dve", lambda e: e.tensor_scalar_max(out=SM[:, 24:28], in0=SM[:, 24:28], scalar1=1e-30), reads=[tSM], writes=[tSM])
            k.op("dve", lambda e: e.reciprocal(out=SM[:, 24:28], in_=SM[:, 24:28]), reads=[tSM], writes=[tSM])
            k.op("dve", lambda e: e.tensor_tensor(out=SE[:, :].rearrange("p (h j) -> p h j", h=4), in0=SE[:, :].rearrange("p (h j) -> p h j", h=4),
                                                  in1=SM[:, 24:28].unsqueeze(2).to_broadcast([128, 4, 64]), op=ALU.mult), reads=[tSE, tSM], writes=[tSE])
            k.op("dve", lambda e: e.tensor_reduce(out=S2[:, :], in_=SE[:, :].rearrange("p (h j) -> p j h", h=4), axis=AX.X, op=ALU.add), reads=[tSE], writes=[tSEL])
            s2v = S2[:, :].rearrange("p (n two) -> p n two", two=2)
            k.op("dve", lambda e: e.tensor_tensor(out=IMP[:, :], in0=s2v[:, :, 0], in1=s2v[:, :, 1], op=ALU.add), reads=[tSEL], writes=[tSEL])
            k.op("dve", lambda e: e.tensor_tensor(out=SC[:, :], in0=IMP[:, :], in1=C["selA"][:, i * 32:(i + 1) * 32], op=ALU.mult), reads=[tSEL, tC], writes=[tSEL])
            k.op("dve", lambda e: e.tensor_tensor(out=SC[:, :], in0=SC[:, :], in1=C["selB"][:, i * 32:(i + 1) * 32], op=ALU.add), reads=[tSEL, tC], writes=[tSEL])
            k.op("dve", lambda e: e.max(out=M1[:, :], in_=SC[:, :]), reads=[tSEL], writes=[tSEL])
            k.op("dve", lambda e: e.match_replace(out=SC2[:, :], in_to_replace=M1[:, :], in_values=SC[:, :], imm_value=-3e38), reads=[tSEL], writes=[tSEL])
            k.op("dve", lambda e: e.max(out=M2[:, :], in_=SC2[:, :]), reads=[tSEL], writes=[tSEL])
            k.op("dve", lambda e: e.tensor_scalar(out=SC2[:, :], in0=SC[:, :], scalar1=M2[:, 6:7], scalar2=None, op0=ALU.is_ge), reads=[tSEL], writes=[tSEL])
            k.op("dve", lambda e: e.tensor_tensor(out=SC2[:, :], in0=SC2[:, :], in1=C["allowed"][:, i * 32:(i + 1) * 32], op=ALU.mult), reads=[tSEL, tC], writes=[tSEL])
            k.op("dve", lambda e: e.tensor_scalar(out=NMB[:, :], in0=SC2[:, :], scalar1=-1.0, scalar2=-NEGB, op0=ALU.add, op1=ALU.mult), reads=[tSEL], writes=[tSEL])
            k.op("pe", lambda e: e.transpose(PT[1][:32, 0:128], NMB[:, :], C["ident_bf"][:, :]), reads=[tSEL, tC], writes=[tPT[1]])
            k.op("act", lambda e: e.copy(out=NMT[:, :, :], in_=PT[1][:32, 0:128].unsqueeze(1).to_broadcast([32, 4, 128])), reads=[tPT[1]], writes=[tNMT])
            for c in range(i + 1):
                if c < i:
                    bias = (C["Eall"][:, c * 128:(c + 1) * 128], NMT[:, :, :], [tC, tNMT])
                else:
                    bias = (C["ident_bf"][:, :], C["causalb"][:, :], [tC])
                chunk(g, KST[:, g, c * 128:(c + 1) * 128], [tKS[c]], bias, VS[:, c, g, :], [tVS[c]], 128, c == 0, c == i)
            combine(1, g)
            c0 = max(0, i - 4)
            for c in range(c0, i + 1):
                if c == i:
                    bias = (C["ident_bf"][:, :], C["causalb"][:, :], [tC])
                elif c == i - 4:
                    bias = (C["ident_bf"][:, :], C["antib"][:, :], [tC])
                else:
                    bias = None
                chunk(g, KWT[:, g, c * 128:(c + 1) * 128], [tKW[c]], bias, VW[:, c, g, :], [tVW[c]], 128, c == c0, c == i)
            combine(2, g)
        k.dma("sp", lambda e, r0=r0: e.dma_start(out=att_d[r0:r0 + 128, :], in_=ATT[:, :]), reads=[tATT], writes=[t_att_d])

    load_mod(A1, TS, 1, tM1); load_mod(B1, TS, 0, tM1)
    k.dma("sp", lambda e: e.dma_start(out=xbcs_d[:, 0:3, :], in_=sconv), writes=[t_xbcs_d])
    front(TS, xs[:, :], cmps[:, :], sels[:, :], xbcs_d[:, 3:7, :], SEQ)
    k.dma("sp", lambda e: e.dma_start(out=convs[:, :, :], in_=xbcs_d[:, 4:7, :]), reads=[t_xbcs_d], writes=[otok()])
    k.dma("sp", lambda e: e.dma_start(out=wins[:, 0:508, :], in_=cwin[:, 4:512, :]), writes=[otok()])
    for b in range(NSEQ):
        k.dma("sp", lambda e, b=b: e.dma_start(out=wins[b, 508:512, :], in_=WINO[4 * b:4 * b + 4, :]), reads=[tWINO], writes=[otok()])
    k.dma("sp", lambda e: e.dma_start(out=qs_d[:, :], in_=QN[:TS, :]), reads=[tQN], writes=[t_qs_d])
    k.dma("sp", lambda e: e.dma_start(out=kvs_d[:, 0:256], in_=SELO[:TS, :]), reads=[tSELO], writes=[t_kvs_d])
    k.dma("sp", lambda e: e.dma_start(out=kvs_d[:, 256:512], in_=WINO[:TS, :]), reads=[tWINO], writes=[t_kvs_d])
    GTP = k.sb("GTP", [TS, 24], F32); tGTP = Tok()
    k.op("dve", lambda e: e.tensor_copy(out=GTP[:, :].rearrange("p (q m) -> p q m", m=6), in_=GT[:TS, :].rearrange("p (m q) -> p q m", m=6)), reads=[tGT], writes=[tGTP])
    k.dma("sp", lambda e: e.dma_start(out=gts_d[:, :], in_=GTP[:, :]), reads=[tGTP], writes=[t_gts_d])
    k.pop()

    k.push()
    tCS = Tok("sconst")
    GKC2 = bc_load("GKC2", gkc2, 128, tok=tCS)
    KVA = k.sb("KVA", [128, NSEQ, 2, 257], F32); tKVA = [Tok() for _ in range(NSEQ)]
    k.op("pool", lambda e: e.memset(KVA[:], 1.0), writes=tKVA)
    k.push()
    WK32 = k.sb("WK32", [128, 32 * 128], F32); WV32 = k.sb("WV32", [128, 32 * 128], F32); tW32 = Tok()
    k.dma("sp", lambda e: e.dma_start(out=WK32[:, :], in_=wk32), writes=[tW32])
    k.dma("sp", lambda e: e.dma_start(out=WV32[:, :], in_=wv32), writes=[tW32])
    PG = [k.sb(f"PG{i}", [128, 32, 256], F32) for i in range(2)]; tPG = [Tok(), Tok()]
    PTI = k.sb("PTI", [128, NSEQ * 64], I32); PTF = k.sb("PTF", [128, NSEQ * 64], F32); IDXP = k.sb("IDXP", [128, NSEQ * 64], I32); tIDXC = Tok()
    k.dma("sp", lambda e: e.dma_start(out=PTI[:, :], in_=ptab.rearrange("b j -> (b j)").partition_broadcast(128)), writes=[tIDXC])
    k.op("dve", lambda e: e.tensor_copy(out=PTF[:, :], in_=PTI[:, :]), reads=[tIDXC], writes=[tIDXC])
    k.op("dve", lambda e: e.tensor_scalar(out=PTF[:, :], in0=PTF[:, :], scalar1=128.0, scalar2=C["PIDX"][:, 0:1], op0=ALU.mult, op1=ALU.add), reads=[tIDXC, tC], writes=[tIDXC])
    k.op("dve", lambda e: e.tensor_copy(out=IDXP[:, :], in_=PTF[:, :]), reads=[tIDXC], writes=[tIDXC])
    for b in range(NSEQ):
        for c in range(2):
            s_ = (2 * b + c) % 2
            for j in range(32):
                col = b * 64 + c * 32 + j
                k.dma("pool", lambda e, j=j, col=col, s_=s_: e.indirect_dma_start(out=PG[s_][:, j, :], out_offset=None, in_=ccmp[:, :],
                                                                                 in_offset=bass.IndirectOffsetOnAxis(ap=IDXP[:, col:col + 1], axis=0)), reads=[tIDXC], writes=[tPG[s_]])
            pk = 2 * (c % 2)
            for j in range(32):
                k.op("pe", lambda e, j=j, s_=s_, pk=pk: e.matmul(PB[pk][:, 0:128], lhsT=WK32[:, j * 128:(j + 1) * 128], rhs=PG[s_][:, j, 0:128], start=(j == 0), stop=(j == 31)), reads=[tW32, tPG[s_]], writes=[tPB[pk]])
                k.op("pe", lambda e, j=j, s_=s_, pk=pk: e.matmul(PB[pk + 1][:, 0:128], lhsT=WV32[:, j * 128:(j + 1) * 128], rhs=PG[s_][:, j, 128:256], start=(j == 0), stop=(j == 31)), reads=[tW32, tPG[s_]], writes=[tPB[pk + 1]])
            k.op("act", lambda e, b=b, c=c, pk=pk: e.copy(out=KVA[:, b, c, 0:128], in_=PB[pk][:, 0:128]), reads=[tPB[pk]], writes=[tKVA[b]])
            k.op("dve", lambda e, b=b, c=c, pk=pk: e.tensor_copy(out=KVA[:, b, c, 128:256], in_=PB[pk + 1][:, 0:128]), reads=[tPB[pk + 1]], writes=[tKVA[b]])
    k.pop()
    QB = [k.sb(f"QB{i}", [128, 4, 512], BF16) for i in range(2)]; tQB = [Tok(), Tok()]
    PROD = k.sb("PROD", [128, 2048], F32); tPROD = Tok()
    STc = k.sb("STc", [128, NSEQ * 64], F32); PTc = k.sb("PTc", [128, NSEQ * 64], F32); tSTc = [Tok() for _ in range(NSEQ)]
    PCM = k.sb("PCM", [32, NSEQ, 256], F32); tPCM = Tok()
    GT16 = [k.sb(f"GT16{i}", [16, 6], F32) for i in range(2)]; GT4 = [k.sb(f"GT4{i}", [4, 24], F32) for i in range(2)]; tGTs = [Tok(), Tok()]
    OB = k.sb("OB", [16, 64], F32); tOB = Tok()
    CF = k.sb("CF", [16, 8], F32); tCF = Tok()
    KNS = [k.sb(f"KNS{i}", [4, 257], F32) for i in range(2)]; KNW = [k.sb(f"KNW{i}", [4, 257], F32) for i in range(2)]; tKN = [Tok(), Tok()]
    for i_ in range(2):
        k.op("pool", lambda e, i_=i_: e.memset(KNS[i_][:], 1.0), writes=[tKN[i_]])
        k.op("pool", lambda e, i_=i_: e.memset(KNW[i_][:], 1.0), writes=[tKN[i_]])
    STn = k.sb("STn", [4, 64], F32); PTn = k.sb("PTn", [4, 64], F32); tSTn = Tok()

    def dots(K_ap, kshape_b, Q_ap, out_ap, rd, wr):
        P, a, b_ = kshape_b
        pv = PROD[:P, :a * b_ * 64].rearrange("p (a b d) -> p a b d", a=a, b=b_)
        k.op("dve", lambda e: e.tensor_tensor(out=pv, in0=K_ap, in1=Q_ap, op=ALU.mult), reads=rd, writes=[tPROD])
        k.op("dve", lambda e: e.tensor_reduce(out=out_ap, in_=pv, axis=AX.X, op=ALU.add), reads=[tPROD], writes=wr)

    def norm_gate_store(ps_ap, nq, g, gate_ap, gate_tok, dst_ap, eng_tok):
        k.op("dve", lambda e: e.tensor_scalar_max(out=CF[:nq, 0:1], in0=ps_ap[:, 128:129], scalar1=1e-30), reads=eng_tok, writes=[tCF])
        k.op("dve", lambda e: e.reciprocal(out=CF[:nq, 0:1], in_=CF[:nq, 0:1]), reads=[tCF], writes=[tCF])
        k.op("dve", lambda e: e.tensor_tensor(out=CF[:nq, 1:2], in0=CF[:nq, 0:1], in1=gate_ap, op=ALU.mult), reads=[tCF, gate_tok], writes=[tCF])
        k.op("dve", lambda e: e.tensor_scalar(out=OB[:nq, 0:64], in0=ps_ap[:, g * 64:(g + 1) * 64], scalar1=CF[:nq, 1:2], scalar2=None, op0=ALU.mult), reads=eng_tok + [tCF], writes=[tOB])
        k.dma("sp", lambda e: e.dma_start(out=dst_ap, in_=OB[:nq, 0:64]), reads=[tOB], writes=[t_atts_d])

    def load_seq(b, s, what):
        k.dma("sp", lambda e: e.dma_start(out=QB[s][:, :, :], in_=qs_d[4 * b:4 * b + 4, :].partition_broadcast(128)), reads=[t_qs_d], writes=[tQB[s]])
        k.dma("sp", lambda e: e.dma_start(out=GT16[s][:, :], in_=gts_d[4 * b:4 * b + 4, :].rearrange("t (q m) -> (t q) m", m=6)), reads=[t_gts_d], writes=[tGTs[s]])
        k.dma("sp", lambda e: e.dma_start(out=GT4[s][:, :].rearrange("q (t m) -> q t m", m=6), in_=gts_d[4 * b:4 * b + 4, :].rearrange("t (q m) -> q t m", m=6)), reads=[t_gts_d], writes=[tGTs[s]])
        if what >= 1:
            k.dma("sp", lambda e: e.dma_start(out=KNS[s][:, 0:256], in_=kvs_d[4 * b:4 * b + 4, 0:256]), reads=[t_kvs_d], writes=[tKN[s]])
            k.dma("sp", lambda e: e.dma_start(out=KNW[s][:, 0:256], in_=kvs_d[4 * b:4 * b + 4, 256:512]), reads=[t_kvs_d], writes=[tKN[s]])

    RS = k.sb("RS", [128, 64], F32)
    for b in range(NSEQ):
        k.op("dve", lambda e, b=b: e.tensor_tensor(out=PROD[:, 0:256].rearrange("p (c f) -> p c f", c=2), in0=KVA[:, b, :, 0:128], in1=KVA[:, b, :, 0:128], op=ALU.mult), reads=[tKVA[b]], writes=[tPROD])
        k.op("dve", lambda e, b=b: e.tensor_reduce(out=RS[:, 4 * b:4 * b + 4], in_=PROD[:, 0:256].rearrange("p (a d) -> p a d", d=64), axis=AX.X, op=ALU.add), reads=[tPROD], writes=[tSM])
    k.op("act", lambda e: e.activation(out=RS[:, :], in_=RS[:, :], func=AF.Sqrt, bias=EPSC[:, :], scale=1.0 / 64), reads=[tSM, tC], writes=[tSM])
    k.op("dve", lambda e: e.reciprocal(out=RS[:, :], in_=RS[:, :]), reads=[tSM], writes=[tSM])
    for b in range(NSEQ):
        kb_ = KVA[:, b, :, 0:128].rearrange("p c (g d) -> p c g d", g=2)
        k.op("dve", lambda e, b=b, kb_=kb_: e.tensor_tensor(out=kb_, in0=kb_, in1=RS[:, 4 * b:4 * b + 4].rearrange("p (c g) -> p c g", c=2).unsqueeze(3).to_broadcast([128, 2, 2, 64]), op=ALU.mult), reads=[tKVA[b], tSM], writes=[tKVA[b]])
        k.op("pool", lambda e, b=b: e.tensor_tensor(out=KVA[:, b, :, 0:128], in0=KVA[:, b, :, 0:128], in1=GKC2[:, :].unsqueeze(1).to_broadcast([128, 2, 128]), op=ALU.mult), reads=[tKVA[b], tCS], writes=[tKVA[b]])
    for b in range(NSEQ):
        s = b % 2
        load_seq(b, s, 0)
        qv = QB[s][:, :, :].rearrange("p t (h d) -> p t h d", d=64)
        for c in range(2):
            for g in range(2):
                dots(KVA[:, b, c, g * 64:(g + 1) * 64].unsqueeze(1).unsqueeze(1).to_broadcast([128, 4, 4, 64]), (128, 4, 4), qv[:, :, 4 * g:4 * g + 4, :],
                     STc[:, b * 64 + (c * 2 + g) * 16:b * 64 + (c * 2 + g + 1) * 16].rearrange("p (t q) -> p t q", t=4), [tKVA[b], tQB[s]], [tSTc[b]])
        k.op("act", lambda e, b=b: e.activation(out=PTc[:, b * 64:(b + 1) * 64], in_=STc[:, b * 64:(b + 1) * 64], func=AF.Exp, scale=SCALE), reads=[tSTc[b]], writes=[tSTc[b]])
        for g in range(2):
            for c in range(2):
                k.op("pe", lambda e, b=b, g=g, c=c: e.matmul(PB[2][:16, 0:129], lhsT=PTc[:, b * 64 + (c * 2 + g) * 16:b * 64 + (c * 2 + g + 1) * 16], rhs=KVA[:, b, c, 128:257], start=(c == 0), stop=(c == 1)), reads=[tSTc[b], tKVA[b]], writes=[tPB[2]])
            norm_gate_store(PB[2][:16, 0:129], 16, g, GT16[s][:, g:g + 1], tGTs[s], atts_d[0, b, :, g, :, :], [tPB[2]])
        for c in range(2):
            k.op("pe", lambda e, b=b, c=c: e.transpose(PB[3][:32, c * 128:(c + 1) * 128], PTc[:, b * 64 + c * 32:b * 64 + (c + 1) * 32], C["ident_f"][:, :]), reads=[tSTc[b], tC], writes=[tPB[3]])
        k.op("act", lambda e, b=b: e.copy(out=PCM[:, b, :], in_=PB[3][:32, 0:256]), reads=[tPB[3]], writes=[tPCM])
    RSM = k.sb("RSM", [32, NSEQ], F32)
    k.op("dve", lambda e: e.tensor_reduce(out=RSM[:, :], in_=PCM[:, :, :], axis=AX.X, op=ALU.add), reads=[tPCM], writes=[tSM])
    k.op("dve", lambda e: e.reciprocal(out=RSM[:, :], in_=RSM[:, :]), reads=[tSM], writes=[tSM])
    k.op("dve", lambda e: e.tensor_tensor(out=PCM[:, :, :], in0=PCM[:, :, :], in1=RSM[:, :].unsqueeze(2).to_broadcast([32, NSEQ, 256]), op=ALU.mult), reads=[tPCM, tSM], writes=[tPCM])
    for b in range(NSEQ):
        k.op("pe", lambda e, b=b: e.matmul(PB[4][:, 0:256], lhsT=C["HSELB"][:, b * 128:(b + 1) * 128], rhs=PCM[:, b, :], start=(b == 0), stop=(b == NSEQ - 1)), reads=[tPCM, tC], writes=[tPB[4]])
    IMPs = k.sb("IMPs", [128, 128], F32); SCs = k.sb("SCs", [128, 128], F32); SC2s = k.sb("SC2s", [128, 128], F32); tSELs = Tok()
    M1s = k.sb("M1s", [128, 16], F32); RBT = k.sb("RBT", [16, 128], F32)
    PHY = k.sb("PHY", [128, 64, 2], F32); PHI = k.sb("PHI", [128, 64], I32); PHF = k.sb("PHF", [128, 64], F32); tPHY = Tok()
    IDXF = k.sb("IDXF", [128, 128], F32); IDXS = k.sb("IDXS", [128, 128], I32); tIDXS = Tok()
    for b in range(NSEQ):
        k.dma("sp", lambda e, b=b: e.dma_start(out=PHI[8 * b:8 * b + 8, :], in_=ptab[b, :].partition_broadcast(8)), writes=[tPHY])
    k.op("dve", lambda e: e.tensor_copy(out=PHF[:, :], in_=PHI[:, :]), reads=[tPHY], writes=[tPHY])
    k.op("dve", lambda e: e.tensor_scalar(out=PHY[:, :, 0], in0=PHF[:, :], scalar1=2.0, scalar2=1.0, op0=ALU.mult, op1=ALU.add), reads=[tPHY], writes=[tPHY])
    k.op("dve", lambda e: e.tensor_scalar(out=PHY[:, :, 1], in0=PHF[:, :], scalar1=2.0, scalar2=2.0, op0=ALU.mult, op1=ALU.add), reads=[tPHY], writes=[tPHY])
    pv2 = PB[4][:, 0:256].rearrange("p (n two) -> p n two", two=2)
    k.op("act", lambda e: e.copy(out=SC2s[:, :], in_=pv2[:, :, 0]), reads=[tPB[4]], writes=[tSELs])
    k.op("dve", lambda e: e.tensor_tensor(out=IMPs[:, :], in0=pv2[:, :, 1], in1=SC2s[:, :], op=ALU.add), reads=[tPB[4], tSELs], writes=[tSELs])
    k.op("dve", lambda e: e.tensor_tensor(out=SCs[:, :], in0=IMPs[:, :], in1=C["BIGS"][:, :], op=ALU.add), reads=[tSELs, tC], writes=[tSELs])
    k.op("dve", lambda e: e.max(out=M1s[:, 0:8], in_=SCs[:, :]), reads=[tSELs], writes=[tSELs])
    k.op("dve", lambda e: e.match_replace(out=SC2s[:, :], in_to_replace=M1s[:, 0:8], in_values=SCs[:, :], imm_value=-3e38), reads=[tSELs], writes=[tSELs])
    k.op("dve", lambda e: e.max(out=M1s[:, 8:16], in_=SC2s[:, :]), reads=[tSELs], writes=[tSELs])
    k.op("dve", lambda e: e.tensor_scalar(out=SC2s[:, :], in0=SCs[:, :], scalar1=M1s[:, 14:15], scalar2=None, op0=ALU.is_ge), reads=[tSELs], writes=[tSELs])
    k.op("dve", lambda e: e.tensor_tensor(out=SCs[:, :], in0=SC2s[:, :], in1=PHY[:, :, :].rearrange("p j two -> p (j two)"), op=ALU.mult), reads=[tSELs, tPHY], writes=[tSELs])
    k.op("dve", lambda e: e.max(out=M1s[:, 0:8], in_=SCs[:, :]), reads=[tSELs], writes=[tSELs])
    k.op("dve", lambda e: e.match_replace(out=SC2s[:, :], in_to_replace=M1s[:, 0:8], in_values=SCs[:, :], imm_value=0.0), reads=[tSELs], writes=[tSELs])
    k.op("dve", lambda e: e.max(out=M1s[:, 8:16], in_=SC2s[:, :]), reads=[tSELs], writes=[tSELs])
    k.op("dve", lambda e: e.tensor_scalar(out=M1s[:, :], in0=M1s[:, :], scalar1=-1.0, scalar2=0.0, op0=ALU.add, op1=ALU.max), reads=[tSELs], writes=[tSELs])
    k.op("dve", lambda e: e.tensor_scalar(out=M1s[:, :], in0=M1s[:, :], scalar1=64.0, scalar2=None, op0=ALU.mult), reads=[tSELs], writes=[tSELs])
    k.op("pe", lambda e: e.transpose(PB[3][:16, 0:128], M1s[:, :], C["ident_f"][:, :]), reads=[tSELs, tC], writes=[tPB[3]])
    k.op("act", lambda e: e.copy(out=RBT[:, :], in_=PB[3][:16, 0:128]), reads=[tPB[3]], writes=[tSELs])
    k.op("pe", lambda e: e.matmul(PB[3][:, 128:256], lhsT=C["EXP16"][:, :], rhs=RBT[:, :], start=True, stop=True), reads=[tSELs, tC], writes=[tPB[3]])
    k.op("dve", lambda e: e.tensor_scalar(out=IDXF[:, :], in0=PB[3][:, 128:256], scalar1=C["PM8"][:, 0:1], scalar2=None, op0=ALU.add), reads=[tPB[3], tC], writes=[tIDXS])
    k.op("dve", lambda e: e.tensor_copy(out=IDXS[:, :], in_=IDXF[:, :]), reads=[tIDXS], writes=[tIDXS])
    NKS = 3
    KSEL = [k.sb(f"KSEL{i}", [128, 8, 257], F32) for i in range(NKS)]; tKSEL = [Tok() for _ in range(NKS)]
    for i_ in range(NKS):
        k.op("pool", lambda e, i_=i_: e.memset(KSEL[i_][:], 1.0), writes=[tKSEL[i_]])
    STs = k.sb("STs", [128, 32], F32); PTs = k.sb("PTs", [128, 32], F32); tSTs = Tok()
    KWB = [k.sb(f"KWB{i}", [128, 4, 257], F32) for i in range(2)]; tKWB = [Tok(), Tok()]
    for i_ in range(2):
        k.op("pool", lambda e, i_=i_: e.memset(KWB[i_][:], 1.0), writes=[tKWB[i_]])
    STw = k.sb("STw", [128, 128], F32); PTw = k.sb("PTw", [128, 128], F32); tSTw = Tok()
    un = 0
    for b in range(NSEQ):
        sq = b % 2
        load_seq(b, sq, 1)
        qv = QB[sq][:, :, :].rearrange("p t (h d) -> p t h d", d=64)
        for g in range(2):
            for t in range(4):
                gt = g * 4 + t; s = un % NKS; un += 1
                for ch in range(8):
                    k.dma("pool", lambda e, b=b, gt=gt, ch=ch, s=s: e.indirect_dma_start(out=KSEL[s][:, ch, 0:256], out_offset=None, in_=csel[:, :],
                                                                                           in_offset=bass.IndirectOffsetOnAxis(ap=IDXS[:, b * 8 + gt:b * 8 + gt + 1], axis=0), element_offset=8 * ch * 256), reads=[tIDXS], writes=[tKSEL[s]])
                dots(KSEL[s][:, :, g * 64:(g + 1) * 64].unsqueeze(2).to_broadcast([128, 8, 4, 64]), (128, 8, 4),
                     qv[:, t, 4 * g:4 * g + 4, :].unsqueeze(1).to_broadcast([128, 8, 4, 64]), STs[:, :].rearrange("p (c q) -> p c q", c=8), [tKSEL[s], tQB[sq]], [tSTs])
                k.op("act", lambda e: e.activation(out=PTs[:, :], in_=STs[:, :], func=AF.Exp, scale=SCALE), reads=[tSTs], writes=[tSTs])
                k.op("dve", lambda e: e.tensor_scalar(out=PTs[:, :], in0=PTs[:, :], scalar1=C["M120"][:, 0:1], scalar2=None, op0=ALU.mult), reads=[tSTs, tC], writes=[tSTs])
                dots(KNS[sq][:, g * 64:(g + 1) * 64].unsqueeze(1).unsqueeze(1).to_broadcast([4, 1, 4, 64]), (4, 1, 4), qv[:4, t:t + 1, 4 * g:4 * g + 4, :],
                     STn[:, 0:4].rearrange("p (a q) -> p a q", a=1), [tKN[sq], tQB[sq]], [tSTn])
                k.op("act", lambda e: e.activation(out=PTn[:, 0:4], in_=STn[:, 0:4], func=AF.Exp, scale=SCALE), reads=[tSTn], writes=[tSTn])
                k.op("dve", lambda e, t=t: e.tensor_scalar(out=PTn[:, 0:4], in0=PTn[:, 0:4], scalar1=C["CM4"][:, t:t + 1], scalar2=None, op0=ALU.mult), reads=[tSTn, tC], writes=[tSTn])
                for ch in range(8):
                    k.op("pe", lambda e, ch=ch, s=s: e.matmul(PB[5][:4, 0:129], lhsT=PTs[:, ch * 4:(ch + 1) * 4], rhs=KSEL[s][:, ch, 128:257], start=(ch == 0), stop=False), reads=[tSTs, tKSEL[s]], writes=[tPB[5]])
                k.op("pe", lambda e, sq=sq: e.matmul(PB[5][:4, 0:129], lhsT=PTn[:, 0:4], rhs=KNS[sq][:, 128:257], start=False, stop=True), reads=[tSTn, tKN[sq]], writes=[tPB[5]])
                norm_gate_store(PB[5][:4, 0:129], 4, g, GT4[sq][:, t * 6 + 2 + g:t * 6 + 3 + g], tGTs[sq], atts_d[1, b, t, g, :, :], [tPB[5]])
        KWt = KWB[b % 2]; tKWt = tKWB[b % 2]
        k.dma("sp", lambda e, b=b, KWt=KWt: e.dma_start(out=KWt[:, :, 0:256], in_=cwin[b].rearrange("(c p) f -> p c f", p=128)), writes=[tKWt])
        for g in range(2):
            for ch in range(4):
                dots(KWt[:, ch, g * 64:(g + 1) * 64].unsqueeze(1).unsqueeze(1).to_broadcast([128, 4, 4, 64]), (128, 4, 4), qv[:, :, 4 * g:4 * g + 4, :],
                     STw[:, (g * 4 + ch) * 16:(g * 4 + ch + 1) * 16].rearrange("p (t q) -> p t q", t=4), [tKWt, tQB[sq]], [tSTw])
            dots(KNW[sq][:, g * 64:(g + 1) * 64].unsqueeze(1).unsqueeze(1).to_broadcast([4, 4, 4, 64]), (4, 4, 4), qv[:4, :, 4 * g:4 * g + 4, :],
                 STn[:, 16 + g * 16:32 + g * 16].rearrange("p (t q) -> p t q", t=4), [tKN[sq], tQB[sq]], [tSTn])
        k.op("act", lambda e: e.activation(out=PTw[:, :], in_=STw[:, :], func=AF.Exp, scale=SCALE), reads=[tSTw], writes=[tSTw])
        k.op("dve", lambda e: e.tensor_tensor(out=PTw[:, :].rearrange("p (g c t q) -> p g c t q", g=2, c=4, t=4)[:, :, 0, :, :], in0=PTw[:, :].rearrange("p (g c t q) -> p g c t q", g=2, c=4, t=4)[:, :, 0, :, :],
                                              in1=C["MASKW"][:, :].unsqueeze(1).unsqueeze(3).to_broadcast([128, 2, 4, 4]), op=ALU.mult), reads=[tSTw, tC], writes=[tSTw])
        k.op("act", lambda e: e.activation(out=PTn[:, 16:48], in_=STn[:, 16:48], func=AF.Exp, scale=SCALE), reads=[tSTn], writes=[tSTn])
        k.op("dve", lambda e: e.tensor_tensor(out=PTn[:, 16:48].rearrange("p (g t q) -> p g t q", g=2, t=4), in0=PTn[:, 16:48].rearrange("p (g t q) -> p g t q", g=2, t=4),
                                              in1=C["CM4"][:, :].unsqueeze(1).unsqueeze(3).to_broadcast([4, 2, 4, 4]), op=ALU.mult), reads=[tSTn, tC], writes=[tSTn])
        for g in range(2):
            for ch in range(4):
                k.op("pe", lambda e, g=g, ch=ch, KWt=KWt: e.matmul(PB[2][:16, 0:129], lhsT=PTw[:, (g * 4 + ch) * 16:(g * 4 + ch + 1) * 16], rhs=KWt[:, ch, 128:257], start=(ch == 0), stop=False), reads=[tSTw, tKWt], writes=[tPB[2]])
            k.op("pe", lambda e, g=g, sq=sq: e.matmul(PB[2][:16, 0:129], lhsT=PTn[:, 16 + g * 16:32 + g * 16], rhs=KNW[sq][:, 128:257], start=False, stop=True), reads=[tSTn, tKN[sq]], writes=[tPB[2]])
            norm_gate_store(PB[2][:16, 0:129], 16, g, GT16[sq][:, 4 + g:5 + g], tGTs[sq], atts_d[2, b, :, g, :, :], [tPB[2]])
    AS = [k.sb(f"AS{i}", [TS, 512], F32) for i in range(3)]; tAS = Tok()
    for r in range(3):
        k.dma("sp", lambda e, r=r: e.dma_start(out=AS[r][:, :], in_=atts_d[r].rearrange("b t g q d -> (b t) (g q d)")), reads=[t_atts_d], writes=[tAS])
    k.op("dve", lambda e: e.tensor_tensor(out=AS[0][:, :], in0=AS[0][:, :], in1=AS[1][:, :], op=ALU.add), reads=[tAS], writes=[tAS])
    k.op("dve", lambda e: e.tensor_tensor(out=AS[0][:, :], in0=AS[0][:, :], in1=AS[2][:, :], op=ALU.add), reads=[tAS], writes=[tAS])
    k.dma("sp", lambda e: e.dma_start(out=att_d[SEQ:SEQ + TS, :], in_=AS[0][:, :]), reads=[tAS], writes=[t_att_d])
    k.pop()

    k.push()
    tG2 = Tok("gains2")
    CW = bc_load("CW", convw, 4 * 768, tok=tG2); CBt = bc_load("CBt", convb, 768, tok=tG2)
    DTB = bc_load("DTB", dtb, 8, tok=tG2); ALG = bc_load("ALG", alog, 8, tok=tG2); DSK = bc_load("DSK", dsk, 512, tok=tG2)
    GATT = bc_load("GATT", gatt, 512, tok=tG2); GSSM = bc_load("GSSM", gssm, 512, tok=tG2); BR = bc_load("BR", b_r, 20, tok=tG2)
    AN = k.sb("AN", [128, 8], F32)
    k.op("act", lambda e: e.activation(out=AN[:], in_=ALG[:], func=AF.Exp), reads=[tG2], writes=[tG2])
    k.op("dve", lambda e: e.tensor_scalar_mul(out=AN[:], in0=AN[:], scalar1=-1.0), reads=[tG2], writes=[tG2])
    ABH = k.sb("ABH", [128, 1], F32); DBH = k.sb("DBH", [128, 1], F32)
    k.dma("sp", lambda e: e.dma_start(out=ABH[:], in_=abh), writes=[tG2])
    k.dma("sp", lambda e: e.dma_start(out=DBH[:], in_=dbh), writes=[tG2])
    k.op("act", lambda e: e.activation(out=ABH[:], in_=ABH[:], func=AF.Exp), reads=[tG2], writes=[tG2])
    k.op("dve", lambda e: e.tensor_scalar_mul(out=ABH[:], in0=ABH[:], scalar1=-1.0), reads=[tG2], writes=[tG2])
    stg2 = [k.sb(f"stg2{i}", [128, D], F32) for i in range(2)]; tstg2 = [Tok(), Tok()]
    WOUT = k.sb("WOUT", [128, 8, D], BF16); tWOUT = Tok()
    for kk in range(8):
        s = kk % 2
        k.dma("sp", lambda e, kk=kk, s=s: e.dma_start(out=stg2[s][:, :], in_=w_out[kk * 128:(kk + 1) * 128, :]), writes=[tstg2[s]])
        k.op("pool", lambda e, kk=kk, s=s: e.tensor_copy(out=WOUT[:, kk, :], in_=stg2[s][:, :]), reads=[tstg2[s]], writes=[tWOUT])
    WR = k.sb("WR", [128, 8, 20], BF16); tWR = Tok()
    for kk in range(8):
        k.dma("sp", lambda e, kk=kk: e.dma_start(out=stg2[0][:, kk * 20:(kk + 1) * 20], in_=w_r[kk * 128:(kk + 1) * 128, :]), writes=[tstg2[0]])
    k.op("pool", lambda e: e.tensor_copy(out=WR[:], in_=stg2[0][:, :160].rearrange("p (k c) -> p k c", k=8)), reads=[tstg2[0]], writes=[tWR])
    G1 = k.sb("G1", [128, D], F32); A2 = k.sb("A2", [128, D], F32); B2 = k.sb("B2", [128, D], F32); tM2 = Tok()
    HT = k.sb("HT", [64, 8, 64], F32); HTB = k.sb("HTB", [64, 8, 64], BF16); tHT = Tok()
    k.op("pool", lambda e: e.memset(HT[:], 0.0), writes=[tHT])
    k.op("pool", lambda e: e.memset(HTB[:], 0.0), writes=[tHT])
    XT = k.sb("XT2", [128, D], F32); tXT = Tok()
    ATT = k.sb("ATT2", [128, 512], F32); tATT = Tok()
    ZD = k.sb("ZD", [128, 520], F32); tZD = Tok()
    XC = [k.sb(f"XC{i}", [128, 768], F32) for i in range(4)]; tXC = [Tok() for _ in range(4)]
    ACC = k.sb("ACC", [128, 768], F32); tACC = Tok()
    TMPc = k.sb("TMPc", [128, D], F32); tTMPc = Tok()
    XS = k.sb("XS", [128, 768], F32); XSB = k.sb("XSB", [128, 768], BF16); tXS = Tok()
    DT = k.sb("DT", [128, 24], F32); tDT = Tok()
    RU = k.sb("RU", [128, 8, 128], F32); tRU = Tok()
    SG = k.sb("SG", [128, 4, 128], F32); tSG = Tok()
    DEC = k.sb("DEC", [128, 8, 128], F32); tDEC = Tok()
    EX = k.sb("EX", [64, 8, 128], F32); tEX = Tok()
    BCT = k.sb("BCT", [64, 4, 128], BF16); tBCT = Tok()
    CBs = k.sb("CBs", [128, 2, 128], F32); tCBs = Tok()
    MTt = [k.sb(f"MTt{i}", [128, 128], BF16) for i in range(2)]; tMT = [Tok(), Tok()]
    CEt = [k.sb(f"CEt{i}", [64, 128], BF16) for i in range(2)]; tCE = [Tok(), Tok()]
    BWt = [k.sb(f"BWt{i}", [128, 64], BF16) for i in range(2)]; tBW = [Tok(), Tok()]
    YS = k.sb("YS", [128, 512], F32); tYS = Tok()
    MIX = k.sb("MIX", [128, D], BF16); tMIX = Tok()
    MIXT = k.sb("MIXT", [128, 8, 128], BF16); tMIXT = Tok()
    X1 = k.sb("X1", [128, D], F32); tX1 = Tok()
    H2 = k.sb("H2", [128, D], BF16); tH2 = Tok()
    H2T = k.sb("H2T", [128, 8, 128], BF16); tH2T = Tok()
    LG = k.sb("LG", [128, 20], F32); RT = k.sb("RT", [128, 64], F32); COMB = k.sb("COMB", [128, 16], F32); tRT = Tok()

    def conv_silu(T, taps):
        for w in range(4):
            if T == 128:
                k.dma("sp", lambda e, w=w: e.dma_start(out=XC[w][:T, :], in_=taps[w]), reads=[t_xbc_d, t_xbcs_d], writes=[tXC[w]])
            else:
                for b in range(NSEQ):
                    k.dma("sp", lambda e, w=w, b=b: e.dma_start(out=XC[w][4 * b:4 * b + 4, :], in_=taps[w][b, :, :]), reads=[t_xbc_d, t_xbcs_d], writes=[tXC[w]])
        k.op("dve", lambda e: e.tensor_tensor(out=ACC[:T, :], in0=XC[0][:T, :], in1=CW[:T, 0:768], op=ALU.mult), reads=[tXC[0], tG2], writes=[tACC])
        for w in range(1, 4):
            k.op("pool", lambda e, w=w: e.tensor_tensor(out=TMPc[:T, :768], in0=XC[w][:T, :], in1=CW[:T, w * 768:(w + 1) * 768], op=ALU.mult), reads=[tXC[w], tG2], writes=[tTMPc])
            k.op("dve", lambda e: e.tensor_tensor(out=ACC[:T, :], in0=ACC[:T, :], in1=TMPc[:T, :768], op=ALU.add), reads=[tACC, tTMPc], writes=[tACC])
        k.op("dve", lambda e: e.tensor_tensor(out=ACC[:T, :], in0=ACC[:T, :], in1=CBt[:T, :], op=ALU.add), reads=[tACC, tG2], writes=[tACC])
        k.op("act", lambda e: e.activation(out=XS[:T, :], in_=ACC[:T, :], func=AF.Silu), reads=[tACC], writes=[tXS])
        k.op("pool", lambda e: e.tensor_copy(out=XSB[:T, :], in_=XS[:T, :]), reads=[tXS], writes=[tXS])

    def softplus_dt(T):
        k.op("dve", lambda e: e.tensor_tensor(out=DT[:T, 0:8], in0=ZD[:T, 512:520], in1=DTB[:T, :], op=ALU.add), reads=[tZD, tG2], writes=[tDT])
        k.op("dve", lambda e: e.tensor_scalar_min(out=DT[:T, 0:8], in0=DT[:T, 0:8], scalar1=30.0), reads=[tDT], writes=[tDT])
        k.op("act", lambda e: e.activation(out=DT[:T, 0:8], in_=DT[:T, 0:8], func=AF.Exp), reads=[tDT], writes=[tDT])
        k.op("act", lambda e: e.activation(out=DT[:T, 0:8], in_=DT[:T, 0:8], func=AF.Ln, bias=1.0), reads=[tDT], writes=[tDT])

    def finish(T, row0, yout):
        k.op("act", lambda e: e.activation(out=TMPc[:T, :512], in_=ZD[:T, 0:512], func=AF.Silu), reads=[tZD], writes=[tTMPc])
        k.op("dve", lambda e: e.tensor_tensor(out=YS[:T, :], in0=YS[:T, :], in1=TMPc[:T, :512], op=ALU.mult), reads=[tYS, tTMPc], writes=[tYS])
        k.op("act", lambda e: e.activation(out=TMPc[:T, :512], in_=YS[:T, :], func=AF.Square, accum_out=SM[:T, 0:1]), reads=[tYS], writes=[tTMPc, tSM])
        rstd_of(SM[:T, 0:1], T, 1.0 / 512)
        k.op("dve", lambda e: e.scalar_tensor_tensor(out=MIX[:T, 512:1024], in0=YS[:T, :], scalar=SM[:T, 0:1], in1=GSSM[:T, :], op0=ALU.mult, op1=ALU.mult), reads=[tYS, tSM, tG2], writes=[tMIX])
        k.op("act", lambda e: e.activation(out=TMPc[:T, :512], in_=ATT[:T, :], func=AF.Square, accum_out=SM[:T, 1:2]), reads=[tATT], writes=[tTMPc, tSM])
        rstd_of(SM[:T, 1:2], T, 1.0 / 512)
        k.op("dve", lambda e: e.scalar_tensor_tensor(out=MIX[:T, 0:512], in0=ATT[:T, :], scalar=SM[:T, 1:2], in1=GATT[:T, :], op0=ALU.mult, op1=ALU.mult), reads=[tATT, tSM, tG2], writes=[tMIX])
        for kk in range(8):
            k.op("pe", lambda e, kk=kk: e.transpose(PT[0][:, kk * 128:kk * 128 + T], MIX[:T, kk * 128:(kk + 1) * 128], C["ident_bf"][:T, :T]), reads=[tMIX, tC], writes=[tPT[0]])
        k.op("act", lambda e: e.copy(out=MIXT[:, :, :T], in_=PT[0][:, :].rearrange("p (k t) -> p k t", k=8)[:, :, :T]), reads=[tPT[0]], writes=[tMIXT])
        for hf in range(2):
            for kk in range(8):
                k.op("pe", lambda e, kk=kk, hf=hf: e.matmul(PB[hf][:T, :], lhsT=MIXT[:, kk, :T], rhs=WOUT[:, kk, hf * 512:(hf + 1) * 512], start=(kk == 0), stop=(kk == 7)),
                     reads=[tMIXT, tWOUT], writes=[tPB[hf]])
            k.op("dve", lambda e, hf=hf: e.tensor_tensor(out=TMPc[:T, hf * 512:(hf + 1) * 512], in0=PB[hf][:T, :], in1=G1[:T, hf * 512:(hf + 1) * 512], op=ALU.mult), reads=[tPB[hf], tM2], writes=[tTMPc])
        k.op("dve", lambda e: e.tensor_tensor(out=X1[:T, :], in0=TMPc[:T, :], in1=XT[:T, :], op=ALU.add), reads=[tTMPc, tXT], writes=[tX1])
        k.dma("sp", lambda e: e.dma_start(out=x1_d[row0:row0 + T, :], in_=X1[:T, :]), reads=[tX1], writes=[t_x1_d])
        k.op("act", lambda e: e.activation(out=H2[:T, :], in_=X1[:T, :], func=AF.Square, accum_out=SM[:T, 2:3]), reads=[tX1], writes=[tH2, tSM])
        rstd_of(SM[:T, 2:3], T, 1.0 / D)
        k.op("dve", lambda e: e.scalar_tensor_tensor(out=TMPc[:T, :], in0=X1[:T, :], scalar=SM[:T, 2:3], in1=A2[:T, :], op0=ALU.mult, op1=ALU.mult), reads=[tX1, tSM, tM2], writes=[tTMPc])
        k.op("dve", lambda e: e.tensor_tensor(out=H2[:T, :], in0=TMPc[:T, :], in1=B2[:T, :], op=ALU.add), reads=[tTMPc, tM2], writes=[tH2])
        for kk in range(8):
            k.op("pe", lambda e, kk=kk: e.transpose(PT[0][:, kk * 128:kk * 128 + T], H2[:T, kk * 128:(kk + 1) * 128], C["ident_bf"][:T, :T]), reads=[tH2, tC], writes=[tPT[0]])
        k.op("act", lambda e: e.copy(out=H2T[:, :, :T], in_=PT[0][:, :].rearrange("p (k t) -> p k t", k=8)[:, :, :T]), reads=[tPT[0]], writes=[tH2T])
        k.dma("sp", lambda e: e.dma_start(out=h2T_d[:, :, row0:row0 + T].rearrange("k p t -> p k t"), in_=H2T[:, :, :T]), reads=[tH2T], writes=[t_h2T_d])
        for kk in range(8):
            k.op("pe", lambda e, kk=kk: e.matmul(PB[2][:T, 0:20], lhsT=H2T[:, kk, :T], rhs=WR[:, kk, :], start=(kk == 0), stop=(kk == 7)), reads=[tH2T, tWR], writes=[tPB[2]])
        k.op("dve", lambda e: e.tensor_tensor(out=LG[:T, :], in0=PB[2][:T, 0:20], in1=BR[:T, :], op=ALU.add), reads=[tPB[2], tG2], writes=[tRT])
        R = lambda a, b: RT[:T, a:b]

        def dv(fn):
            k.op("dve", fn, reads=[tRT], writes=[tRT])
        dv(lambda e: e.tensor_reduce(out=R(0, 1), in_=LG[:T, 0:4], axis=AX.X, op=ALU.max))
        dv(lambda e: e.tensor_scalar(out=R(4, 8), in0=LG[:T, 0:4], scalar1=R(0, 1), scalar2=None, op0=ALU.is_equal))
        dv(lambda e: e.tensor_scalar(out=R(8, 12), in0=LG[:T, 0:4], scalar1=R(0, 1), scalar2=None, op0=ALU.subtract))
        k.op("act", lambda e: e.activation(out=R(8, 12), in_=R(8, 12), func=AF.Exp), reads=[tRT], writes=[tRT])
        dv(lambda e: e.tensor_reduce(out=R(1, 2), in_=R(8, 12), axis=AX.X, op=ALU.add))
        dv(lambda e: e.reciprocal(out=R(1, 2), in_=R(1, 2)))
        dv(lambda e: e.tensor_tensor(out=RT[:T, 16:32].rearrange("p (g j) -> p g j", g=4), in0=LG[:T, 4:20].rearrange("p (g j) -> p g j", g=4),
                                     in1=R(4, 8).unsqueeze(2).to_broadcast([T, 4, 4]), op=ALU.mult))
        dv(lambda e: e.tensor_reduce(out=R(12, 16), in_=RT[:T, 16:32].rearrange("p (g j) -> p j g", g=4), axis=AX.X, op=ALU.add))
        dv(lambda e: e.tensor_reduce(out=R(2, 3), in_=R(12, 16), axis=AX.X, op=ALU.max))
        dv(lambda e: e.tensor_scalar(out=R(32, 36), in0=R(12, 16), scalar1=R(2, 3), scalar2=None, op0=ALU.is_equal))
        dv(lambda e: e.scalar_tensor_tensor(out=R(36, 40), in0=R(32, 36), scalar=-1e9, in1=R(12, 16), op0=ALU.mult, op1=ALU.add))
        dv(lambda e: e.tensor_reduce(out=R(3, 4), in_=R(36, 40), axis=AX.X, op=ALU.max))
        dv(lambda e: e.tensor_scalar(out=R(40, 44), in0=R(36, 40), scalar1=R(3, 4), scalar2=None, op0=ALU.is_equal))
        dv(lambda e: e.tensor_tensor(out=R(44, 45), in0=R(3, 4), in1=R(2, 3), op=ALU.subtract))
        k.op("act", lambda e: e.activation(out=R(44, 45), in_=R(44, 45), func=AF.Exp), reads=[tRT], writes=[tRT])
        dv(lambda e: e.tensor_scalar_add(out=R(45, 46), in0=R(44, 45), scalar1=1.0))
        dv(lambda e: e.reciprocal(out=R(45, 46), in_=R(45, 46)))
        dv(lambda e: e.tensor_tensor(out=R(46, 47), in0=R(45, 46), in1=R(44, 45), op=ALU.mult))
        dv(lambda e: e.tensor_tensor(out=R(45, 47), in0=R(45, 47), in1=R(1, 2).to_broadcast([T, 2]), op=ALU.mult))
        dv(lambda e: e.tensor_scalar(out=R(48, 52), in0=R(32, 36), scalar1=R(45, 46), scalar2=None, op0=ALU.mult))
        dv(lambda e: e.scalar_tensor_tensor(out=R(48, 52), in0=R(40, 44), scalar=R(46, 47), in1=R(48, 52), op0=ALU.mult, op1=ALU.add))
        dv(lambda e: e.tensor_tensor(out=COMB[:T, :].rearrange("p (g j) -> p g j", g=4), in0=R(4, 8).unsqueeze(2).to_broadcast([T, 4, 4]),
                                     in1=R(48, 52).unsqueeze(1).to_broadcast([T, 4, 4]), op=ALU.mult))
        k.dma("sp", lambda e: e.dma_start(out=comb_d[row0:row0 + T, :], in_=COMB[:T, :]), reads=[tRT], writes=[t_comb_d])

    load_mod(G1, 128, 2, tM2); load_mod(B2, 128, 3, tM2); load_mod(A2, 128, 4, tM2)
    for i in range(NT):
        r0 = i * 128
        k.dma("sp", lambda e, r0=r0: e.dma_start(out=XT[:, :], in_=xp[r0:r0 + 128, :]), writes=[tXT])
        k.dma("sp", lambda e, r0=r0: e.dma_start(out=ATT[:, :], in_=att_d[r0:r0 + 128, :]), reads=[t_att_d], writes=[tATT])
        k.dma("sp", lambda e, r0=r0: e.dma_start(out=ZD[:, :], in_=zdt_d[r0:r0 + 128, :]), reads=[t_zdt_d], writes=[tZD])
        conv_silu(128, [xbc_d[r0 + w:r0 + w + 128, :] for w in range(4)])
        softplus_dt(128)
        k.op("dve", lambda e: e.tensor_tensor(out=DT[:, 8:16], in0=DT[:, 0:8], in1=AN[:, :], op=ALU.mult), reads=[tDT, tG2], writes=[tDT])
        k.op("dve", lambda e: e.tensor_tensor(out=RU[:, :, :], in0=C["U"][:, :].unsqueeze(1).to_broadcast([128, 8, 128]),
                                              in1=DT[:, 8:16].unsqueeze(2).to_broadcast([128, 8, 128]), op=ALU.mult), reads=[tDT, tC], writes=[tRU])
        k.op("pe", lambda e: e.matmul(PB[4][:, 0:8], lhsT=C["U"][:, :], rhs=DT[:, 8:16], start=True, stop=True), reads=[tDT, tC], writes=[tPB[4]])
        k.op("dve", lambda e: e.tensor_scalar_mul(out=DT[:, 16:24], in0=PB[4][:, 0:8], scalar1=-1.0), reads=[tPB[4]], writes=[tDT])
        for hf in range(2):
            k.op("pe", lambda e, hf=hf: e.matmul(PB[2 + hf][:, :], lhsT=C["ones_f"][:, :], rhs=RU[:, 4 * hf:4 * hf + 4, :], start=True, stop=True), reads=[tRU, tC], writes=[tPB[2 + hf]])
            k.op("dve", lambda e, hf=hf: e.tensor_tensor(out=SG[:, :, :], in0=PB[2 + hf][:, :].rearrange("p (h l) -> p h l", h=4),
                                                         in1=C["NB"][:, :].unsqueeze(1).to_broadcast([128, 4, 128]), op=ALU.add), reads=[tPB[2 + hf], tC], writes=[tSG])
            for hh in range(4):
                h = 4 * hf + hh
                k.op("act", lambda e, h=h, hh=hh: e.activation(out=DEC[:, h, :], in_=SG[:, hh, :], func=AF.Exp, bias=DT[:, 16 + h:17 + h]), reads=[tSG, tDT], writes=[tDEC])
            k.op("act", lambda e, hf=hf: e.activation(out=EX[:, 4 * hf:4 * hf + 4, :], in_=PB[2 + hf][:64, :].rearrange("p (h l) -> p h l", h=4), func=AF.Exp), reads=[tPB[2 + hf]], writes=[tEX])
        for j in range(4):
            k.op("pe", lambda e, j=j: e.transpose(PT[1][:64, j * 128:(j + 1) * 128], XSB[:, 512 + j * 64:576 + j * 64], C["ident_bf"][:, :]), reads=[tXS, tC], writes=[tPT[1]])
        k.op("act", lambda e: e.copy(out=BCT[:, :, :], in_=PT[1][:64, 0:512].rearrange("p (j t) -> p j t", j=4)), reads=[tPT[1]], writes=[tBCT])
        for g in range(2):
            k.op("pe", lambda e, g=g: e.matmul(PB[5][:, g * 128:(g + 1) * 128], lhsT=BCT[:, g, :], rhs=BCT[:, 2 + g, :], start=True, stop=True), reads=[tBCT], writes=[tPB[5]])
        k.op("act", lambda e: e.copy(out=CBs[:, :, :], in_=PB[5][:, 0:256].rearrange("p (g l) -> p g l", g=2)), reads=[tPB[5]], writes=[tCBs])
        k.op("dve", lambda e: e.tensor_tensor(out=DT[:, 0:8], in0=DT[:, 0:8], in1=DT[:, 0:8], op=ALU.max), reads=[tDT], writes=[tDT])
        k.op("dve", lambda e: e.tensor_tensor(out=SM[:, 40:48], in0=DEC[:, :, 127], in1=DT[:, 0:8], op=ALU.mult), reads=[tDEC, tDT], writes=[tSM])
        for h in range(8):
            g = h // 4; s = h % 2
            k.op("dve", lambda e, h=h, g=g, s=s: e.scalar_tensor_tensor(out=MTt[s][:, :], in0=DEC[:, h, :], scalar=DT[:, h:h + 1], in1=CBs[:, g, :], op0=ALU.mult, op1=ALU.mult),
                 reads=[tDEC, tDT, tCBs], writes=[tMT[s]])
            k.op("pool", lambda e, h=h, g=g, s=s: e.tensor_tensor(out=CEt[s][:, :], in0=BCT[:, 2 + g, :], in1=EX[:, h, :], op=ALU.mult), reads=[tBCT, tEX], writes=[tCE[s]])
            k.op("pe", lambda e, h=h, s=s: e.matmul(PB[0][:, h * 64:(h + 1) * 64], lhsT=MTt[s][:, :], rhs=XSB[:, h * 64:(h + 1) * 64], start=True, stop=False), reads=[tMT[s], tXS], writes=[tPB[0]])
            k.op("pe", lambda e, h=h, s=s: e.matmul(PB[0][:, h * 64:(h + 1) * 64], lhsT=CEt[s][:, :], rhs=HTB[:, h, :], start=False, stop=True), reads=[tCE[s], tHT], writes=[tPB[0]])
            k.op("dve", lambda e, h=h, g=g, s=s: e.tensor_scalar(out=BWt[s][:, :], in0=XS[:, 512 + g * 64:576 + g * 64], scalar1=SM[:, 40 + h:41 + h], scalar2=None, op0=ALU.mult), reads=[tXS, tSM], writes=[tBW[s]])
            k.op("pe", lambda e, h=h, s=s: e.matmul(PB[1][:64, h * 64:(h + 1) * 64], lhsT=BWt[s][:, :], rhs=XSB[:, h * 64:(h + 1) * 64], start=True, stop=True), reads=[tBW[s], tXS], writes=[tPB[1]])
        k.op("dve", lambda e: e.tensor_tensor(out=HT[:, :, :], in0=HT[:, :, :], in1=EX[:, :, 127:128].to_broadcast([64, 8, 64]), op=ALU.mult), reads=[tHT, tEX], writes=[tHT])
        k.op("dve", lambda e: e.tensor_tensor(out=HT[:, :, :], in0=HT[:, :, :], in1=PB[1][:64, :].rearrange("p (h q) -> p h q", h=8), op=ALU.add), reads=[tHT, tPB[1]], writes=[tHT])
        k.op("pool", lambda e: e.tensor_copy(out=HTB[:, :, :], in_=HT[:, :, :]), reads=[tHT], writes=[tHT])
        k.op("dve", lambda e: e.tensor_tensor(out=TMPc[:, :512], in0=XS[:, 0:512], in1=DSK[:, :], op=ALU.mult), reads=[tXS, tG2], writes=[tTMPc])
        k.op("dve", lambda e: e.tensor_tensor(out=YS[:, :], in0=TMPc[:, :512], in1=PB[0][:, :], op=ALU.add), reads=[tTMPc, tPB[0]], writes=[tYS])
        finish(128, r0, None)
    for h in range(8):
        k.op("pe", lambda e, h=h: e.transpose(PB[2][:64, h * 64:(h + 1) * 64], HT[:, h, :], C["ident_f"][:64, :64]), reads=[tHT, tC], writes=[tPB[2]])
    k.op("act", lambda e: e.copy(out=TMPc[:64, :512], in_=PB[2][:64, :]), reads=[tPB[2]], writes=[tTMPc])
    k.dma("sp", lambda e: e.dma_start(out=ssmp.rearrange("h p n -> p h n"), in_=TMPc[:64, :512].rearrange("p (h n) -> p h n", h=8)), reads=[tTMPc], writes=[otok()])

    T = TS
    load_mod(G1, TS, 2, tM2); load_mod(B2, TS, 3, tM2); load_mod(A2, TS, 4, tM2)
    k.dma("sp", lambda e: e.dma_start(out=XT[:T, :], in_=xs[:, :]), writes=[tXT])
    k.dma("sp", lambda e: e.dma_start(out=ATT[:T, :], in_=att_d[SEQ:SEQ + T, :]), reads=[t_att_d], writes=[tATT])
    k.dma("sp", lambda e: e.dma_start(out=ZD[:T, :], in_=zdt_d[SEQ:SEQ + T, :]), reads=[t_zdt_d], writes=[tZD])
    conv_silu(T, [xbcs_d[:, w:w + 4, :] for w in range(4)])
    softplus_dt(T)
    k.dma("sp", lambda e: e.dma_start(out=ssc_d[:, 0:768], in_=XS[:T, :]), reads=[tXS], writes=[t_ssc_d])
    k.dma("sp", lambda e: e.dma_start(out=ssc_d[:, 768:776], in_=DT[:T, 0:8]), reads=[tDT], writes=[t_ssc_d])
    Hs = k.sb("Hs", [128, 4096], F32); tHs = Tok()
    k.dma("sp", lambda e: e.dma_start(out=Hs[:, :], in_=sssm[:, :]), writes=[tHs])
    Xbh = k.sb("Xbh", [128, 4, 64], F32); Bbh = k.sb("Bbh", [128, 4, 64], F32); Cbh = k.sb("Cbh", [128, 4, 64], F32); Dbh = k.sb("Dbh", [128, 4], F32); tBH = Tok()
    for b in range(NSEQ):
        k.dma("sp", lambda e, b=b: e.dma_start(out=Xbh[8 * b:8 * b + 8, :, :], in_=ssc_d[4 * b:4 * b + 4, 0:512].rearrange("t (h p) -> h t p", p=64)), reads=[t_ssc_d], writes=[tBH])
        k.dma("sp", lambda e, b=b: e.dma_start(out=Dbh[8 * b:8 * b + 8, :], in_=ssc_d[4 * b:4 * b + 4, 768:776].rearrange("t h -> h t")), reads=[t_ssc_d], writes=[tBH])
        for g in range(2):
            k.dma("sp", lambda e, b=b, g=g: e.dma_start(out=Bbh[8 * b + 4 * g:8 * b + 4 * g + 4, :, :], in_=ssc_d[4 * b:4 * b + 4, 512 + 64 * g:576 + 64 * g].partition_broadcast(4)), reads=[t_ssc_d], writes=[tBH])
            k.dma("sp", lambda e, b=b, g=g: e.dma_start(out=Cbh[8 * b + 4 * g:8 * b + 4 * g + 4, :, :], in_=ssc_d[4 * b:4 * b + 4, 640 + 64 * g:704 + 64 * g].partition_broadcast(4)), reads=[t_ssc_d], writes=[tBH])
    OUTER = k.sb("OUTER", [128, 4096], F32); tOUT = Tok()
    Ybh = k.sb("Ybh", [128, 4, 64], F32); tY = Tok()
    SS = k.sb("SS", [128, 8], F32); XDT = k.sb("XDT", [128, 64], F32); tSS = Tok()
    for t in range(4):
        k.op("act", lambda e, t=t: e.activation(out=SS[:, 0:1], in_=Dbh[:, t:t + 1], func=AF.Exp, scale=ABH[:, 0:1]), reads=[tBH, tG2], writes=[tSS])
        k.op("dve", lambda e, t=t: e.tensor_scalar(out=XDT[:, :], in0=Xbh[:, t, :], scalar1=Dbh[:, t:t + 1], scalar2=None, op0=ALU.mult), reads=[tBH], writes=[tSS])
        k.op("dve", lambda e, t=t: e.tensor_tensor(out=OUTER[:, :].rearrange("p (a n) -> p a n", n=64), in0=XDT[:, :].unsqueeze(2).to_broadcast([128, 64, 64]),
                                                   in1=Bbh[:, t, :].unsqueeze(1).to_broadcast([128, 64, 64]), op=ALU.mult), reads=[tSS, tBH], writes=[tOUT])
        k.op("dve", lambda e: e.scalar_tensor_tensor(out=Hs[:, :], in0=Hs[:, :], scalar=SS[:, 0:1], in1=OUTER[:, :], op0=ALU.mult, op1=ALU.add), reads=[tHs, tSS, tOUT], writes=[tHs])
        k.op("dve", lambda e, t=t: e.tensor_tensor(out=OUTER[:, :].rearrange("p (a n) -> p a n", n=64), in0=Hs[:, :].rearrange("p (a n) -> p a n", n=64),
                                                   in1=Cbh[:, t, :].unsqueeze(1).to_broadcast([128, 64, 64]), op=ALU.mult), reads=[tHs, tBH], writes=[tOUT])
        k.op("dve", lambda e, t=t: e.tensor_reduce(out=Ybh[:, t, :], in_=OUTER[:, :].rearrange("p (a n) -> p a n", n=64), axis=AX.X, op=ALU.add), reads=[tOUT], writes=[tY])
        k.op("dve", lambda e, t=t: e.scalar_tensor_tensor(out=Ybh[:, t, :], in0=Xbh[:, t, :], scalar=DBH[:, 0:1], in1=Ybh[:, t, :], op0=ALU.mult, op1=ALU.add), reads=[tBH, tY, tG2], writes=[tY])
    k.dma("sp", lambda e: e.dma_start(out=ssms[:, :], in_=Hs[:, :]), reads=[tHs], writes=[otok()])
    for b in range(NSEQ):
        k.dma("sp", lambda e, b=b: e.dma_start(out=ysd_d[b, :, :].rearrange("t (h p) -> h t p", p=64), in_=Ybh[8 * b:8 * b + 8, :, :]), reads=[tY], writes=[t_ysd_d])
    k.dma("sp", lambda e: e.dma_start(out=YS[:T, :], in_=ysd_d.rearrange("b t c -> (b t) c")), reads=[t_ysd_d], writes=[tYS])
    finish(T, SEQ, None)
    k.pop()

    k.push()
    NG = [(g * 512, 512) for g in range(4)] + [(SEQ, TS)]
    H2A = k.sb("H2A", [128, 8, NTOK], BF16); tH2A = Tok()
    for kk in range(8):
        k.dma("sp", lambda e, kk=kk: e.dma_start(out=H2A[:, kk, :], in_=h2T_d[kk, :, :]), reads=[t_h2T_d], writes=[tH2A])
    NTL = 17
    MACC = k.sb("MACC", [128, NTL, D], F32); tMACC = [Tok() for _ in range(NTL)]
    CMB = k.sb("CMB", [128, NTL, 16], F32); tCMB = Tok()
    for j in range(NTL):
        T = 128 if j < 16 else TS
        k.dma("sp", lambda e, j=j, T=T: e.dma_start(out=CMB[:T, j, :], in_=comb_d[j * 128:j * 128 + T, :]), reads=[t_comb_d], writes=[tCMB])
    k.op("pool", lambda e: e.memset(MACC[:, :, :], 0.0), writes=tMACC)
    stm = [k.sb(f"stm{i}", [128, 8, 256], F32) for i in range(2)]; tstm = [Tok(), Tok()]
    WG = [k.sb(f"WG{i}", [128, 8, 256], BF16) for i in range(2)]; WU = [k.sb(f"WU{i}", [128, 8, 256], BF16) for i in range(2)]
    WD = [k.sb(f"WD{i}", [128, 2, D], BF16) for i in range(2)]; tW = [Tok(), Tok()]
    HE = k.sb("HE", [128, 2, 512], BF16); tHE = Tok()
    SG_ = k.sb("SGm", [128, 512], F32); tSGm = Tok()
    n_exp = 16 if with_moe else 0
    sidx = 0
    for ex in range(n_exp):
        s = ex % 2
        for (W_, src) in ((WG[s], w_gate), (WU[s], w_up)):
            ss = sidx % 2; sidx += 1
            k.dma("sp", lambda e, src=src, ss=ss, ex=ex: e.dma_start(out=stm[ss][:, :, :], in_=src[ex].rearrange("(k p) f -> p k f", p=128)), writes=[tstm[ss]])
            k.op("pool", lambda e, W_=W_, ss=ss: e.tensor_copy(out=W_[:, :, :], in_=stm[ss][:, :, :]), reads=[tstm[ss]], writes=[tW[s]])
        ss = sidx % 2; sidx += 1
        k.dma("sp", lambda e, ss=ss, ex=ex: e.dma_start(out=stm[ss][:, :, :].rearrange("p (c a) f -> p c (a f)", c=2), in_=w_down[ex].rearrange("(c p) n -> p c n", p=128)), writes=[tstm[ss]])
        k.op("pool", lambda e, s=s, ss=ss: e.tensor_copy(out=WD[s][:, :, :], in_=stm[ss][:, :, :].rearrange("p (c a) f -> p c (a f)", c=2)), reads=[tstm[ss]], writes=[tW[s]])
        for (t0, nt) in NG:
            for c in range(2):
                for (W_, pb) in ((WG[s], 0), (WU[s], 1)):
                    for kk in range(8):
                        k.op("pe", lambda e, W_=W_, pb=pb, kk=kk, c=c, t0=t0, nt=nt: e.matmul(PB[pb][:, :nt], lhsT=W_[:, kk, c * 128:(c + 1) * 128], rhs=H2A[:, kk, t0:t0 + nt], start=(kk == 0), stop=(kk == 7)),
                             reads=[tW[s], tH2A], writes=[tPB[pb]])
                k.op("act", lambda e, nt=nt: e.activation(out=SG_[:, :nt], in_=PB[0][:, :nt], func=AF.Silu), reads=[tPB[0]], writes=[tSGm])
                k.op("dve", lambda e, c=c, nt=nt: e.tensor_tensor(out=HE[:, c, :nt], in0=SG_[:, :nt], in1=PB[1][:, :nt], op=ALU.mult), reads=[tSGm, tPB[1]], writes=[tHE])
            for tt in range((nt + 127) // 128):
                T = min(128, nt - tt * 128); j = (t0 + tt * 128) // 128
                for hf in range(2):
                    pb = 2 + hf
                    for c in range(2):
                        k.op("pe", lambda e, pb=pb, c=c, tt=tt, T=T, hf=hf: e.matmul(PB[pb][:T, :], lhsT=HE[:, c, tt * 128:tt * 128 + T], rhs=WD[s][:, c, hf * 512:(hf + 1) * 512], start=(c == 0), stop=(c == 1)),
                             reads=[tHE, tW[s]], writes=[tPB[pb]])
                    k.op("dve", lambda e, pb=pb, T=T, j=j, hf=hf, ex=ex: e.scalar_tensor_tensor(out=MACC[:T, j, hf * 512:(hf + 1) * 512], in0=PB[pb][:T, :], scalar=CMB[:T, j, ex:ex + 1],
                                                                                         in1=MACC[:T, j, hf * 512:(hf + 1) * 512], op0=ALU.mult, op1=ALU.add), reads=[tPB[pb], tCMB, tMACC[j]], writes=[tMACC[j]])
    G2t = k.sb("G2t", [128, D], F32); tG2t = Tok()
    X1b = [k.sb(f"X1b{i}", [128, D], F32) for i in range(2)]; tX1b = [Tok(), Tok()]
    load_mod(G2t, 128, 5, tG2t)
    for j in range(NTL):
        T = 128 if j < 16 else TS
        if j == 16:
            load_mod(G2t, TS, 5, tG2t)
        s = j % 2
        k.dma("sp", lambda e, j=j, T=T, s=s: e.dma_start(out=X1b[s][:T, :], in_=x1_d[j * 128:j * 128 + T, :]), reads=[t_x1_d], writes=[tX1b[s]])
        k.op("dve", lambda e, j=j, T=T: e.tensor_tensor(out=MACC[:T, j, :], in0=MACC[:T, j, :], in1=G2t[:T, :], op=ALU.mult), reads=[tMACC[j], tG2t], writes=[tMACC[j]])
        k.op("pool", lambda e, j=j, T=T, s=s: e.tensor_tensor(out=X1b[s][:T, :], in0=X1b[s][:T, :], in1=MACC[:T, j, :], op=ALU.add), reads=[tMACC[j], tX1b[s]], writes=[tX1b[s]])
        dst = yp[j * 128:j * 128 + T, :] if j < 16 else ys[:, :]
        k.dma("sp", lambda e, T=T, s=s, dst=dst: e.dma_start(out=dst, in_=X1b[s][:T, :]), reads=[tX1b[s]], writes=[otok()])
    k.finish(out_toks)
    k.pop()
    k.es.close()
    return k


def _run(inp, debug=False, compact=False):
    f32 = lambda a: np.ascontiguousarray(np.asarray(a, dtype=np.float32))
    consts = make_consts()
    cshapes = {n: (list(v.shape), v.dtype != np.float32) for n, v in consts.items()}
    nc = bass.Bass("TRN2", target_bir_lowering=False)
    kb = build(nc, cshapes, debug=debug, pool_rows=(1024 * 128 if compact else 1310720))

    xp = f32(inp["x_prompt"]); xs = f32(inp["x_sample"])
    w_in = f32(inp["w_in"])[0]
    perm = np.concatenate([np.arange(0, 1304), np.arange(2584, 2592), np.arange(1304, 2584)])
    w_in_p = np.ascontiguousarray(w_in[:, perm])
    w_rg = f32(inp["w_rg"])[0]; w_re = f32(inp["w_re"])[0]
    w_r = np.ascontiguousarray(np.concatenate([w_rg] + [w_re[g] for g in range(4)], axis=1))
    b_r = np.ascontiguousarray(np.concatenate([f32(inp["b_rg"])[0], f32(inp["b_re"])[0].reshape(-1)]))
    wpk = f32(inp["w_pos_k"])[0]; wpv = f32(inp["w_pos_v"])[0]
    wkblk = np.zeros((128, 4), np.float32); wvsel = np.zeros((128, 16, 64), np.float32)
    for r in range(128):
        wkblk[r, r // 32] = wpk[r % 32]
        for i in range(16):
            wvsel[r, i, 4 * i + r // 32] = wpv[r % 32]
    wk32 = np.zeros((128, 32, 128), np.float32); wv32 = np.zeros((128, 32, 128), np.float32)
    for r in range(128):
        for j in range(32):
            wk32[r, j, 4 * j + r // 32] = wpk[r % 32]
            wv32[r, j, 4 * j + r // 32] = wpv[r % 32]
    shared = {
        "g_norm1": f32(inp["g_norm1"])[0], "g_norm2": f32(inp["g_norm2"])[0],
        "w_ada": f32(inp["w_ada"])[0], "b_ada": f32(inp["b_ada"])[0], "w_in": w_in_p,
        "gq8": np.tile(f32(inp["g_q"])[0], 8), "gks2": np.tile(f32(inp["g_k_sel"])[0], 2), "gkw2": np.tile(f32(inp["g_k_win"])[0], 2),
        "gkc": f32(inp["g_k_cmp"])[0].reshape(64, 1),
        "convw": f32(inp["conv_w"])[0].reshape(-1), "convb": f32(inp["conv_b"])[0],
        "dtb": f32(inp["dt_bias"])[0], "alog": f32(inp["a_log"])[0], "abh": np.tile(f32(inp["a_log"])[0], 16).reshape(128, 1), "dbh": np.tile(f32(inp["d_skip"])[0], 16).reshape(128, 1), "dsk": np.repeat(f32(inp["d_skip"])[0], 64),
        "gatt": f32(inp["g_att_out"])[0], "gssm": f32(inp["g_ssm_out"])[0],
        "w_out": f32(inp["w_out"])[0], "w_r": w_r, "b_r": b_r,
        "w_gate": f32(inp["w_gate"])[0], "w_up": f32(inp["w_up"])[0], "w_down": f32(inp["w_down"])[0],
        "wkblk": wkblk, "wvsel": wvsel.reshape(128, 1024),
        "wk32": wk32.reshape(128, 4096), "wv32": wv32.reshape(128, 4096), "gkc2": np.tile(f32(inp["g_k_cmp"])[0], 2),
    }
    for n, v in consts.items():
        shared["c_" + n] = v
    cwin = f32(inp["cache_win"])[0].reshape(128, 512, 256)
    sssm = f32(inp["state_ssm"])[0].reshape(128 * 8, 4096)
    sconv = f32(inp["state_conv"])[0]
    ccmp = np.asarray(inp["cache_cmp"], dtype=np.float32).reshape(10240 * 128, 256)
    csel = np.asarray(inp["cache_sel"], dtype=np.float32).reshape(10240 * 128, 256)
    ptab = np.ascontiguousarray(np.asarray(inp["page_table"], dtype=np.int32))
    in_maps = []
    for c in range(NCORES):
        m = dict(shared)
        if compact:
            pg = ptab[16 * c:16 * c + 16].reshape(-1)
            m["ccmp"] = ccmp.reshape(10240, 128 * 256)[pg].reshape(-1, 256); m["csel"] = csel.reshape(10240, 128 * 256)[pg].reshape(-1, 256)
            m["ptab"] = np.arange(1024, dtype=np.int32).reshape(16, 64)
        else:
            m["ccmp"] = ccmp; m["csel"] = csel; m["ptab"] = ptab[16 * c:16 * c + 16]
        m["xp"] = xp[c]; m["xs"] = xs[16 * c:16 * c + 16].reshape(TS, D)
        m["cp"] = f32(inp["c_prompt"])[c:c + 1]; m["cs"] = f32(inp["c_sample"])[16 * c:16 * c + 16]
        m["sssm"] = sssm[128 * c:128 * c + 128]; m["sconv"] = sconv[16 * c:16 * c + 16]; m["cwin"] = cwin[16 * c:16 * c + 16]
        in_maps.append(m)
    res = run_bass_kernel_spmd(nc, in_maps, core_ids=list(range(NCORES)))
    R = res.results
    cat = lambda n: np.stack([R[c][n] for c in range(NCORES)])
    y_p = cat("yp"); y_s = cat("ys").reshape(128, 4, D)
    cmp_p = cat("cmpp").reshape(1, 8, SEQ, 2, 2, 64); cmp_s = cat("cmps").reshape(1, 128, 4, 2, 2, 64)
    sel_p = cat("selp").reshape(1, 8, SEQ, 2, 2, 64); sel_s = cat("sels").reshape(1, 128, 4, 2, 2, 64)
    win_p = cat("winp").reshape(1, 8, 512, 2, 2, 64); win_s = cat("wins").reshape(1, 128, 512, 2, 2, 64)
    ssm_p = cat("ssmp").reshape(1, 8, 8, 64, 64); ssm_s = cat("ssms").reshape(1, 128, 8, 64, 64)
    conv_p = cat("convp").reshape(1, 8, 3, 768); conv_s = cat("convs").reshape(1, 128, 3, 768)
    outs = (y_p, y_s, cmp_p, cmp_s, sel_p, sel_s, win_p, win_s, ssm_p, ssm_s, conv_p, conv_s)
    if debug:
        return outs, {n: cat(n) for n in ("att_d", "x1_d", "comb_d")}
    return outs


def kernel(**inp):
    return _run(inp, False)
```

```python
import numpy as np
import ml_dtypes
from contextlib import ExitStack
import concourse.bass as bass
import concourse.mybir as mybir
from concourse.bass_utils import run_bass_kernel_spmd

F32 = mybir.dt.float32; BF16 = mybir.dt.bfloat16; I32 = mybir.dt.int32
AF = mybir.ActivationFunctionType; ALU = mybir.AluOpType; AX = mybir.AxisListType
NCORES = 8
SEQ = 2048; D = 1024; NT = 16; TS = 64; NSEQ = 16
INW = 2592
SCALE = 0.125
EPS = 1e-6
NEGB = -30000.0


class Tok:
    __slots__ = ("name", "w", "rs")

    def __init__(self, name="t"):
        self.name = name; self.w = None; self.rs = []


class KB:
    NSLOT = 8

    def __init__(self, nc):
        self.nc = nc; self.es = ExitStack(); self.stack = [self.es]
        self.eng = {"pe": nc.tensor, "act": nc.scalar, "dve": nc.vector, "pool": nc.gpsimd, "sp": nc.sync}
        self.sem = {k: self.es.enter_context(nc.semaphore("s_" + k)) for k in self.eng}
        self.cnt = {k: 0 for k in self.eng}
        self.seen = {k: {} for k in self.eng}
        self.dsem = {}; self.dcnt = {}; self.dnext = {}
        self.nslot = {"sp": 8, "act": 2, "pool": 16}
        for q in ("sp", "act", "pool"):
            self.dsem[q] = [self.es.enter_context(nc.semaphore(f"d_{q}{i}")) for i in range(self.nslot[q])]
            self.dcnt[q] = [0] * self.nslot[q]; self.dnext[q] = 0
        self.nins = 0

    def sb(self, name, shape, dt):
        return self.stack[-1].enter_context(self.nc.sbuf_tensor(name, list(shape), dt))

    def ps(self, name, shape, dt):
        return self.es.enter_context(self.nc.psum_tensor(name, list(shape), dt))

    def _wait(self, e, ev):
        if ev is None:
            return
        key, val = ev
        if self.seen[e].get(key, 0) >= val:
            return
        self.seen[e][key] = val
        sem = self.sem[key[1]] if key[0] == "c" else self.dsem[key[1]][key[2]]
        self.eng[e].wait_ge(sem, val)

    def _deps(self, e, reads, writes):
        for t in reads:
            self._wait(e, t.w)
        for t in writes:
            self._wait(e, t.w)
            for r in t.rs:
                self._wait(e, r)

    def _commit(self, ev, reads, writes):
        for t in reads:
            t.rs = [r for r in t.rs if r[0] != ev[0]] + [ev]
        for t in writes:
            t.w = ev; t.rs = []

    def op(self, e, fn, reads=(), writes=()):
        self._deps(e, reads, writes)
        ins = fn(self.eng[e])
        self.cnt[e] += 1
        ins.then_inc(self.sem[e], 1)
        ev = (("c", e), self.cnt[e])
        self._commit(ev, reads, writes); self.nins += 1
        return ins

    def dma(self, q, fn, reads=(), writes=()):
        s = self.dnext[q]; self.dnext[q] = (s + 1) % self.nslot[q]
        key = ("d", q, s)
        if self.dcnt[q][s] > 0:
            self._wait(q, (key, self.dcnt[q][s]))
        self._deps(q, reads, writes)
        ins = fn(self.eng[q])
        self.dcnt[q][s] += 16
        ins.then_inc(self.dsem[q][s], 16)
        ev = (key, self.dcnt[q][s])
        self._commit(ev, reads, writes); self.nins += 1
        return ins

    def push(self):
        self.stack.append(ExitStack())

    def barrier(self):
        for e in self.eng:
            for e2 in self.eng:
                if e2 != e and self.cnt[e2] > 0:
                    self._wait(e, (("c", e2), self.cnt[e2]))
            for q in self.dsem:
                for s in range(self.nslot[q]):
                    if self.dcnt[q][s] > 0:
                        self._wait(e, (("d", q, s), self.dcnt[q][s]))

    def pop(self):
        self.barrier()
        self.stack.pop().close()

    def finish(self, toks):
        for t in toks:
            self._wait("sp", t.w)
            for r in t.rs:
                self._wait("sp", r)


def _bf(a):
    return np.ascontiguousarray(a.astype(np.float32)).astype(ml_dtypes.bfloat16)


def make_consts():
    c = {}
    c["ident_bf"] = _bf(np.eye(128))
    c["ident_f"] = np.eye(128, dtype=np.float32)
    tk = np.arange(128)[:, None]; tq = np.arange(128)[None, :]
    cb = np.where(tk <= tq, 0.0, NEGB)
    c["causalb"] = _bf(np.tile(cb, (1, 4)))
    ab = np.where(tk > tq, 0.0, NEGB)
    c["antib"] = _bf(np.tile(ab, (1, 4)))
    E = np.zeros((32, 16, 128), np.float32)
    for cc in range(16):
        for t in range(128):
            E[2 * cc + t // 64, cc, t] = 1.0
    c["Eall"] = _bf(E.reshape(32, 16 * 128))
    Dsel = np.zeros((4, 124), np.float32)
    for jj in range(4):
        Dsel[jj, 60 + jj] = 1.0
    c["Dsel"] = _bf(Dsel)
    p = np.arange(128)[None, :]; jj = np.arange(4)[:, None]
    cmpB = np.where(32 * jj + 31 <= p, 0.0, NEGB)
    c["cmpB"] = _bf(np.tile(cmpB, (1, 4)))
    pc = np.arange(128)[:, None]; cc = np.arange(124)[None, :]
    c["cmpM0"] = ((cc - 60) <= np.floor((pc - 31) / 32.0)).astype(np.float32)
    selA = np.zeros((128, 16, 32), np.float32); selB = np.zeros((128, 16, 32), np.float32)
    allowed = np.zeros((128, 16, 32), np.float32)
    for i in range(16):
        for pp in range(128):
            cur = 2 * i + (1 if pp >= 64 else 0)
            for n in range(32):
                if n < cur:
                    allowed[pp, i, n] = 1.0
                    if n == cur - 1:
                        selB[pp, i, n] = 2e9
                    elif n == 0:
                        selB[pp, i, n] = 1e9
                    else:
                        selA[pp, i, n] = 1.0
                else:
                    selB[pp, i, n] = -1e30
    c["selA"] = selA.reshape(128, 512); c["selB"] = selB.reshape(128, 512); c["allowed"] = allowed.reshape(128, 512)
    s = np.arange(128)[:, None]; l = np.arange(128)[None, :]
    c["U"] = (s <= l).astype(np.float32)
    c["NB"] = np.where(l >= s, 0.0, -1e30).astype(np.float32)
    c["ones_f"] = np.ones((128, 128), np.float32)
    pp = np.arange(128)
    c["PM4"] = (pp % 4).astype(np.float32).reshape(128, 1)
    c["R64"] = (pp % 64).astype(np.float32).reshape(128, 1)
    H = np.zeros((32, 8), np.float32)
    for g in range(2):
        for t in range(4):
            for q in range(4):
                H[g * 16 + t * 4 + q, g * 4 + t] = 1.0
    c["HSEL"] = H
    B = np.zeros((128, 128), np.float32); B[:, 0] = 1e9; B[:, 127] = 2e9
    HB_ = np.zeros((32, 16, 128), np.float32)
    for b_ in range(16):
        for g in range(2):
            for t in range(4):
                for q in range(4):
                    HB_[g * 16 + t * 4 + q, b_, b_ * 8 + g * 4 + t] = 1.0
    c["HSELB"] = HB_.reshape(32, 2048)
    c["BIGS"] = B
    c["I8"] = np.eye(8, dtype=np.float32)
    E16 = np.zeros((16, 128), np.float32)
    for p_ in range(128):
        E16[p_ // 8, p_] = 1.0
    c["EXP16"] = E16
    c["PM8"] = (pp % 8).astype(np.float32).reshape(128, 1)
    c["PIDX"] = pp.astype(np.float32).reshape(128, 1)
    c["PM32"] = (pp % 32).astype(np.float32).reshape(128, 1)
    c["OH4"] = (pp[:, None] // 32 == np.arange(4)[None, :]).astype(np.float32)
    c["M120"] = (pp < 120).astype(np.float32).reshape(128, 1)
    m15 = np.ones((128, 8), np.float32); m15[64:, 7] = 0.0
    c["MASK15"] = m15
    c["MASKW"] = (pp[:, None] > np.arange(4)[None, :]).astype(np.float32)
    c["CM4"] = (np.arange(4)[:, None] <= np.arange(4)[None, :]).astype(np.float32)
    return c


def build(nc, cshapes, with_moe=True, debug=False, pool_rows=1310720):
    k = KB(nc)
    k.es.enter_context(nc.allow_non_contiguous_dma(reason="small strided loads"))

    def din(name, shape, dt=F32):
        return nc.dram_tensor(name, list(shape), dt, kind="ExternalInput").ap()

    def dout(name, shape, dt=F32):
        return nc.dram_tensor(name, list(shape), dt, kind="ExternalOutput").ap()

    def dint(name, shape, dt=F32):
        kind = "ExternalOutput" if (debug and name in ("att_d", "x1_d", "comb_d")) else "Internal"
        return nc.dram_tensor(name, list(shape), dt, kind=kind).ap()

    xp = din("xp", [SEQ, D]); xs = din("xs", [TS, D]); cpr = din("cp", [1, D]); csm = din("cs", [NSEQ, D])
    sssm = din("sssm", [128, 4096]); sconv = din("sconv", [NSEQ, 3, 768]); cwin = din("cwin", [NSEQ, 512, 256])
    gn1 = din("g_norm1", [D]); gn2 = din("g_norm2", [D])
    w_ada = din("w_ada", [D, 6 * D]); b_ada = din("b_ada", [6 * D])
    w_in = din("w_in", [D, INW])
    gq8 = din("gq8", [512]); gks2 = din("gks2", [128]); gkw2 = din("gkw2", [128]); gkc = din("gkc", [64, 1])
    convw = din("convw", [4 * 768]); convb = din("convb", [768])
    dtb = din("dtb", [8]); alog = din("alog", [8]); dsk = din("dsk", [512])
    abh = din("abh", [128, 1]); dbh = din("dbh", [128, 1])
    gatt = din("gatt", [512]); gssm = din("gssm", [512])
    w_out = din("w_out", [D, D]); w_r = din("w_r", [D, 20]); b_r = din("b_r", [20])
    w_gate = din("w_gate", [16, D, 256]); w_up = din("w_up", [16, D, 256]); w_down = din("w_down", [16, 256, D])
    wkblk = din("wkblk", [128, 4]); wvsel = din("wvsel", [128, 16 * 64])
    ccmp = din("ccmp", [pool_rows, 256]); csel = din("csel", [pool_rows, 256]); ptab = din("ptab", [NSEQ, 64], I32)
    wk32 = din("wk32", [128, 4096]); wv32 = din("wv32", [128, 4096]); gkc2 = din("gkc2", [128])
    cd = {}
    for n, (shp, isbf) in cshapes.items():
        cd[n] = din("c_" + n, shp, BF16 if isbf else F32)

    yp = dout("yp", [SEQ, D]); ys = dout("ys", [TS, D])
    cmpp = dout("cmpp", [SEQ, 256]); cmps = dout("cmps", [TS, 256])
    selp = dout("selp", [SEQ, 256]); sels = dout("sels", [TS, 256])
    winp = dout("winp", [512, 256]); wins = dout("wins", [NSEQ, 512, 256])
    ssmp = dout("ssmp", [8, 64, 64]); ssms = dout("ssms", [128, 4096])
    convp = dout("convp", [3, 768]); convs = dout("convs", [NSEQ, 3, 768])

    NTOK = SEQ + TS
    mods_d = dint("mods_d", [65, 6 * D]); t_mods_d = Tok()
    xbc_d = dint("xbc_d", [SEQ + 3, 768]); t_xbc_d = Tok()
    xbcs_d = dint("xbcs_d", [NSEQ, 7, 768]); t_xbcs_d = Tok()
    zdt_d = dint("zdt_d", [NTOK, 520]); t_zdt_d = Tok()
    att_d = dint("att_d", [NTOK, 512]); t_att_d = Tok()
    x1_d = dint("x1_d", [NTOK, D]); t_x1_d = Tok()
    h2T_d = dint("h2T_d", [8, 128, NTOK], BF16); t_h2T_d = Tok()
    comb_d = dint("comb_d", [NTOK, 16]); t_comb_d = Tok()
    ssc_d = dint("ssc_d", [TS, 776]); t_ssc_d = Tok()
    ysd_d = dint("ysd_d", [NSEQ, 4, 512]); t_ysd_d = Tok()
    qs_d = dint("qs_d", [TS, 512], BF16); t_qs_d = Tok()
    kvs_d = dint("kvs_d", [TS, 512]); t_kvs_d = Tok()
    gts_d = dint("gts_d", [TS, 24]); t_gts_d = Tok()
    atts_d = dint("atts_d", [3, NSEQ, 4, 2, 4, 64]); t_atts_d = Tok()
    out_toks = []

    def otok():
        t = Tok(); out_toks.append(t); return t

    C = {}; tC = Tok("consts")
    for n, (shp, isbf) in cshapes.items():
        C[n] = k.sb("C_" + n, shp, BF16 if isbf else F32)
        k.dma("sp", lambda e, n=n: e.dma_start(out=C[n][:], in_=cd[n]), writes=[tC])
    EPSC = k.sb("EPSC", [128, 1], F32)
    k.op("dve", lambda e: e.memset(EPSC[:], EPS), writes=[tC])
    SM = k.sb("SM", [128, 64], F32); tSM = Tok()
    PB = [k.es.enter_context(nc.psum_tensor(f"pb{i}", [128, 512], F32)) for i in range(6)]
    tPB = [Tok(f"pb{i}") for i in range(6)]
    PT = [k.es.enter_context(nc.psum_tensor(f"pt{i}", [128, 1024], BF16)) for i in range(2)]
    tPT = [Tok(f"pt{i}") for i in range(2)]

    def bc_load(name, src1d, width, parts=128, tok=None):
        t = k.sb(name, [parts, width], F32)
        k.dma("sp", lambda e: e.dma_start(out=t[:], in_=src1d.partition_broadcast(parts)), writes=[tok or tC])
        return t

    def rstd_of(ap, T, scale):
        k.op("act", lambda e: e.activation(out=ap, in_=ap, func=AF.Sqrt, bias=EPSC[:T, :], scale=scale), reads=[tSM, tC], writes=[tSM])
        k.op("dve", lambda e: e.reciprocal(out=ap, in_=ap), reads=[tSM], writes=[tSM])

    k.push()
    GN1 = bc_load("GN1", gn1, D, 65); GN2 = bc_load("GN2", gn2, D, 65)
    stg = [k.sb(f"stg{i}", [128, 512], F32) for i in range(2)]; tstg = [Tok(), Tok()]
    cT = k.sb("cT", [128, 8, 65], F32); tcT = Tok()
    k.dma("sp", lambda e: e.dma_start(out=cT[:, :, 0:1], in_=cpr.rearrange("b (k p) -> p k b", p=128)), writes=[tcT])
    for kk in range(8):
        k.dma("sp", lambda e, kk=kk: e.dma_start(out=cT[:, kk, 1:17], in_=csm[:, kk * 128:(kk + 1) * 128].rearrange("b p -> p b")), writes=[tcT])
    scT = k.sb("scT", [128, 8, 17], F32)
    k.op("act", lambda e: e.activation(out=scT[:], in_=cT[:, :, 0:17], func=AF.Silu), reads=[tcT], writes=[tcT])
    L = k.sb("L", [128, 8, 65], BF16)
    k.op("dve", lambda e: e.tensor_copy(out=L[:, :, 0:1], in_=scT[:, :, 0:1]), reads=[tcT], writes=[tcT])
    k.op("dve", lambda e: e.tensor_copy(out=L[:, :, 1:65].rearrange("p k (b t) -> p k b t", t=4),
                                        in_=scT[:, :, 1:17].unsqueeze(3).to_broadcast([128, 8, NSEQ, 4])), reads=[tcT], writes=[tcT])
    wab = [k.sb(f"wab{i}", [128, 8, 512], BF16) for i in range(2)]; twab = [Tok(), Tok()]
    bab = [k.sb(f"bab{i}", [65, 512], F32) for i in range(2)]; tbab = [Tok(), Tok()]
    M65 = k.sb("M65", [65, 6 * D], F32); tM65 = Tok()
    for cg in range(12):
        s = cg % 2
        for kk in range(8):
            ss = kk % 2
            k.dma("sp", lambda e, kk=kk, ss=ss, cg=cg: e.dma_start(out=stg[ss][:, :], in_=w_ada[kk * 128:(kk + 1) * 128, cg * 512:(cg + 1) * 512]), writes=[tstg[ss]])
            k.op("pool", lambda e, kk=kk, ss=ss, s=s: e.tensor_copy(out=wab[s][:, kk, :], in_=stg[ss][:, :]), reads=[tstg[ss]], writes=[twab[s]])
        k.dma("sp", lambda e, s=s, cg=cg: e.dma_start(out=bab[s][:], in_=b_ada[cg * 512:(cg + 1) * 512].partition_broadcast(65)), writes=[tbab[s]])
        for kk in range(8):
            k.op("pe", lambda e, kk=kk, s=s: e.matmul(PB[s][:65, :], lhsT=L[:, kk, :], rhs=wab[s][:, kk, :], start=(kk == 0), stop=(kk == 7)),
                 reads=[tcT, twab[s]], writes=[tPB[s]])
        k.op("dve", lambda e, s=s, cg=cg: e.tensor_tensor(out=M65[:, cg * 512:(cg + 1) * 512], in0=PB[s][:65, :], in1=bab[s][:, :], op=ALU.add),
             reads=[tPB[s], tbab[s]], writes=[tM65])
    for (sl, G) in ((1, GN1), (4, GN2)):
        k.op("dve", lambda e, sl=sl, G=G: e.scalar_tensor_tensor(out=M65[:, sl * D:(sl + 1) * D], in0=M65[:, sl * D:(sl + 1) * D], scalar=1.0, in1=G[:, :], op0=ALU.add, op1=ALU.mult),
             reads=[tM65, tC], writes=[tM65])
    k.dma("sp", lambda e: e.dma_start(out=mods_d[:, :], in_=M65[:, :]), reads=[tM65], writes=[t_mods_d])
    k.pop()

    def load_mod(tile, T, slot, tok):
        if T == 128:
            k.dma("sp", lambda e: e.dma_start(out=tile[:, :], in_=mods_d[0, slot * D:(slot + 1) * D].partition_broadcast(128)), reads=[t_mods_d], writes=[tok])
        else:
            k.dma("sp", lambda e: e.dma_start(out=tile[:TS, :], in_=mods_d[1:65, slot * D:(slot + 1) * D]), reads=[t_mods_d], writes=[tok])

    k.push()
    tG = Tok("gains1")
    GQ = bc_load("GQ", gq8, 512, tok=tG); GKS = bc_load("GKS", gks2, 128, tok=tG); GKW = bc_load("GKW", gkw2, 128, tok=tG)
    GKC = k.sb("GKC", [64, 1], F32)
    k.dma("sp", lambda e: e.dma_start(out=GKC[:], in_=gkc), writes=[tG])
    WKB = k.sb("WKB", [128, 4], F32); WVS = k.sb("WVS", [128, 16 * 64], F32)
    k.dma("sp", lambda e: e.dma_start(out=WKB[:], in_=wkblk), writes=[tG])
    k.dma("sp", lambda e: e.dma_start(out=WVS[:], in_=wvsel), writes=[tG])
    stgw = [k.sb(f"stgw{i}", [128, INW], F32) for i in range(2)]; tstgw = [Tok(), Tok()]
    WIN = k.sb("WIN", [128, 8, INW], BF16); tWIN = Tok()
    for kk in range(8):
        s = kk % 2
        k.dma("sp", lambda e, kk=kk, s=s: e.dma_start(out=stgw[s][:], in_=w_in[kk * 128:(kk + 1) * 128, :]), writes=[tstgw[s]])
        k.op("pool", lambda e, kk=kk, s=s: e.tensor_copy(out=WIN[:, kk, :], in_=stgw[s][:]), reads=[tstgw[s]], writes=[tWIN])
    A1 = k.sb("A1", [128, D], F32); B1 = k.sb("B1", [128, D], F32); tM1 = Tok()
    KST = k.sb("KST", [64, 2, SEQ], BF16); KWT = k.sb("KWT", [64, 2, SEQ], BF16)
    VS = k.sb("VS", [128, NT, 2, 65], BF16); VW = k.sb("VW", [128, NT, 2, 65], BF16)
    KCT = k.sb("KCT", [64, 2, 64], BF16); VCA = k.sb("VCA", [64, 128], F32); VC = k.sb("VC", [64, 2, 65], BF16)
    tKS = [Tok() for _ in range(NT)]; tKW = [Tok() for _ in range(NT)]; tVS = [Tok() for _ in range(NT)]; tVW = [Tok() for _ in range(NT)]
    tKC = Tok(); tVC = Tok()
    k.op("pool", lambda e: e.memset(VS[:], 1.0), writes=tVS)
    k.op("pool", lambda e: e.memset(VW[:], 1.0), writes=tVW)
    k.op("pool", lambda e: e.memset(VC[:], 1.0), writes=[tVC])
    k.op("pool", lambda e: e.memset(KCT[:], 0.0), writes=[tKC])
    k.op("pool", lambda e: e.memset(VCA[:], 0.0), writes=[tVC])
    ZR = k.sb("ZR", [3, 768], F32); tZR = Tok()
    k.op("pool", lambda e: e.memset(ZR[:], 0.0), writes=[tZR])
    k.dma("sp", lambda e: e.dma_start(out=xbc_d[0:3, :], in_=ZR[:]), reads=[tZR], writes=[t_xbc_d])

    XT = k.sb("XT", [128, D], F32); tXT = Tok()
    TMP = k.sb("TMP", [128, D], F32); tTMP = Tok()
    HB = k.sb("HB", [128, D], BF16); tHB = Tok()
    HTt = k.sb("HTt", [128, 8, 128], BF16); tHTt = Tok()
    Ut = k.sb("Ut", [128, INW], F32); tU = Tok()
    QN = k.sb("QN", [128, 512], BF16); tQN = Tok()
    QT = k.sb("QT", [64, 8, 128], BF16); tQT = Tok()
    SELO = k.sb("SELO", [128, 256], F32); tSELO = Tok()
    WINO = k.sb("WINO", [128, 256], F32); tWINO = Tok()
    KNB = k.sb("KNB", [128, 256], BF16); tKNB = Tok()
    ATT = k.sb("ATT", [128, 512], F32); tATT = Tok()
    PTt = [k.sb(f"PTt{i}", [128, 512], BF16) for i in range(3)]; tPTt = [Tok(), Tok(), Tok()]
    GT = k.sb("GT", [128, 24], F32); tGT = Tok()
    SE = k.sb("SE", [128, 256], F32); tSE = Tok()
    S2 = k.sb("S2", [128, 64], F32); IMP = k.sb("IMP", [128, 32], F32); SC = k.sb("SC", [128, 32], F32); SC2 = k.sb("SC2", [128, 32], F32)
    M1 = k.sb("M1", [128, 8], F32); M2 = k.sb("M2", [128, 8], F32); NMB = k.sb("NMB", [128, 32], BF16); tSEL = Tok()
    NMT = k.sb("NMT", [32, 4, 128], BF16); tNMT = Tok()
    KR = k.sb("KR", [64, 16], F32); tKR = Tok()
    COEF = k.sb("COEF", [128, 8], F32); tCOEF = Tok()
    OT = k.sb("OT", [128, 256], F32); tOT = Tok()

    def headnorm(T, src, nh, gain, out32, outbf, rd, wr, col):
        w = nh * 64
        k.op("dve", lambda e: e.tensor_tensor(out=TMP[:T, :w], in0=src, in1=src, op=ALU.mult), reads=rd, writes=[tTMP])
        k.op("dve", lambda e: e.tensor_reduce(out=SM[:T, col:col + nh], in_=TMP[:T, :w].rearrange("p (h d) -> p h d", d=64), axis=AX.X, op=ALU.add), reads=[tTMP], writes=[tSM])
        rstd_of(SM[:T, col:col + nh], T, 1.0 / 64)
        k.op("dve", lambda e: e.tensor_tensor(out=TMP[:T, :w].rearrange("p (h d) -> p h d", d=64), in0=src.rearrange("p (h d) -> p h d", d=64),
                                              in1=SM[:T, col:col + nh].unsqueeze(2).to_broadcast([T, nh, 64]), op=ALU.mult), reads=rd + [tSM], writes=[tTMP])
        if out32 is not None:
            k.op("dve", lambda e: e.tensor_tensor(out=out32, in0=TMP[:T, :w], in1=gain, op=ALU.mult), reads=[tTMP, tG], writes=wr)
            k.op("act", lambda e: e.copy(out=outbf, in_=out32), reads=wr, writes=[tKNB])
        else:
            k.op("dve", lambda e: e.tensor_tensor(out=outbf, in0=TMP[:T, :w], in1=gain, op=ALU.mult), reads=[tTMP, tG], writes=wr)

    def front(T, xsrc, cmp_o, sel_o, xbc_dst, zrow0):
        k.dma("sp", lambda e: e.dma_start(out=XT[:T, :], in_=xsrc), writes=[tXT])
        k.op("act", lambda e: e.activation(out=HB[:T, :], in_=XT[:T, :], func=AF.Square, accum_out=SM[:T, 0:1]), reads=[tXT], writes=[tHB, tSM])
        rstd_of(SM[:T, 0:1], T, 1.0 / D)
        k.op("dve", lambda e: e.scalar_tensor_tensor(out=TMP[:T, :], in0=XT[:T, :], scalar=SM[:T, 0:1], in1=A1[:T, :], op0=ALU.mult, op1=ALU.mult),
             reads=[tXT, tSM, tM1], writes=[tTMP])
        k.op("dve", lambda e: e.tensor_tensor(out=HB[:T, :], in0=TMP[:T, :], in1=B1[:T, :], op=ALU.add), reads=[tTMP, tM1], writes=[tHB])
        for kk in range(8):
            k.op("pe", lambda e, kk=kk: e.transpose(PT[0][:, kk * 128:kk * 128 + T], HB[:T, kk * 128:(kk + 1) * 128], C["ident_bf"][:T, :T]), reads=[tHB, tC], writes=[tPT[0]])
        k.op("act", lambda e: e.copy(out=HTt[:, :, :T], in_=PT[0][:, :].rearrange("p (k t) -> p k t", k=8)[:, :, :T]), reads=[tPT[0]], writes=[tHTt])
        groups = [(0, 512), (512, 512), (1024, 288), (1312, 512), (1824, 512), (2336, 256)]
        for gi, (c0, w) in enumerate(groups):
            pb = gi % 2
            for kk in range(8):
                k.op("pe", lambda e, kk=kk, pb=pb, c0=c0, w=w: e.matmul(PB[pb][:T, :w], lhsT=HTt[:, kk, :T], rhs=WIN[:, kk, c0:c0 + w], start=(kk == 0), stop=(kk == 7)),
                     reads=[tHTt, tWIN], writes=[tPB[pb]])
            if gi % 2 == 0:
                k.op("act", lambda e, pb=pb, c0=c0, w=w: e.copy(out=Ut[:T, c0:c0 + w], in_=PB[pb][:T, :w]), reads=[tPB[pb]], writes=[tU])
            else:
                k.op("dve", lambda e, pb=pb, c0=c0, w=w: e.tensor_copy(out=Ut[:T, c0:c0 + w], in_=PB[pb][:T, :w]), reads=[tPB[pb]], writes=[tU])
        k.dma("sp", lambda e: e.dma_start(out=cmp_o, in_=Ut[:T, 512:768]), reads=[tU], writes=[otok()])
        if T == 128:
            k.dma("sp", lambda e: e.dma_start(out=xbc_dst, in_=Ut[:T, 1824:2592]), reads=[tU], writes=[t_xbc_d])
        else:
            for b in range(NSEQ):
                k.dma("sp", lambda e, b=b: e.dma_start(out=xbc_dst[b, :, :], in_=Ut[4 * b:4 * b + 4, 1824:2592]), reads=[tU], writes=[t_xbcs_d])
        k.dma("sp", lambda e: e.dma_start(out=zdt_d[zrow0:zrow0 + T, 0:512], in_=Ut[:T, 1312:1824]), reads=[tU], writes=[t_zdt_d])
        k.dma("sp", lambda e: e.dma_start(out=zdt_d[zrow0:zrow0 + T, 512:520], in_=Ut[:T, 1304:1312]), reads=[tU], writes=[t_zdt_d])
        headnorm(T, Ut[:T, 0:512], 8, GQ[:T, :], None, QN[:T, :], [tU], [tQN], 8)
        for h in range(8):
            k.op("pe", lambda e, h=h: e.transpose(PT[1][:64, h * 128:h * 128 + T], QN[:T, h * 64:(h + 1) * 64], C["ident_bf"][:T, :T]), reads=[tQN, tC], writes=[tPT[1]])
        k.op("act", lambda e: e.copy(out=QT[:, :, :T], in_=PT[1][:64, :].rearrange("p (h t) -> p h t", h=8)[:, :, :T]), reads=[tPT[1]], writes=[tQT])
        headnorm(T, Ut[:T, 768:896], 2, GKS[:T, :], SELO[:T, 0:128], KNB[:T, 0:128], [tU], [tSELO], 16)
        k.op("act", lambda e: e.copy(out=SELO[:T, 128:256], in_=Ut[:T, 896:1024]), reads=[tU], writes=[tSELO])
        headnorm(T, Ut[:T, 1024:1152], 2, GKW[:T, :], WINO[:T, 0:128], KNB[:T, 128:256], [tU], [tWINO], 18)
        k.op("act", lambda e: e.copy(out=WINO[:T, 128:256], in_=Ut[:T, 1152:1280]), reads=[tU], writes=[tWINO])
        k.dma("sp", lambda e: e.dma_start(out=sel_o, in_=SELO[:T, :]), reads=[tSELO], writes=[otok()])
        k.op("act", lambda e: e.activation(out=GT[:T, :], in_=Ut[:T, 1280:1304], func=AF.Sigmoid), reads=[tU], writes=[tGT])

    pvn = [0]

    pend = []; pvbank = [5]

    def flush():
        while pend:
            pend.pop(0)()

    def combine(br, g):
        flush()
        bk = pvbank[0]; pvbank[0] = 9 - bk
        PBk = PB[bk]; tPBk = tPB[bk]
        den = PBk[:, 0:260].rearrange("p (h c) -> p h c", c=65)[:, :, 64]
        k.op("dve", lambda e: e.tensor_scalar_max(out=COEF[:, 0:4], in0=den, scalar1=1e-30), reads=[tPBk], writes=[tCOEF])
        k.op("dve", lambda e: e.reciprocal(out=COEF[:, 0:4], in_=COEF[:, 0:4]), reads=[tCOEF], writes=[tCOEF])
        k.op("dve", lambda e: e.tensor_tensor(out=COEF[:, 4:8], in0=COEF[:, 0:4], in1=GT[:, br * 8 + g * 4:br * 8 + g * 4 + 4], op=ALU.mult), reads=[tCOEF, tGT], writes=[tCOEF])
        ov = PBk[:, 0:260].rearrange("p (h c) -> p h c", c=65)[:, :, 0:64]
        cf = COEF[:, 4:8].unsqueeze(2).to_broadcast([128, 4, 64])
        av = ATT[:, g * 256:(g + 1) * 256].rearrange("p (h d) -> p h d", d=64)
        if br == 0:
            k.op("dve", lambda e: e.tensor_tensor(out=av, in0=ov, in1=cf, op=ALU.mult), reads=[tPBk, tCOEF], writes=[tATT])
        else:
            k.op("dve", lambda e: e.tensor_tensor(out=OT[:, :].rearrange("p (h d) -> p h d", d=64), in0=ov, in1=cf, op=ALU.mult), reads=[tPBk, tCOEF], writes=[tOT])
            k.op("pool", lambda e: e.tensor_tensor(out=ATT[:, g * 256:(g + 1) * 256], in0=ATT[:, g * 256:(g + 1) * 256], in1=OT[:, :], op=ALU.add), reads=[tOT, tATT], writes=[tATT])

    def chunk(g, kT_ap, ktoks, bias, v_ap, vtoks, nk, first, last):
        n = pvn[0]; pvn[0] += 1
        pb = n % 2; s = n % 3
        k.op("pe", lambda e: e.matmul(PB[pb][:nk, :], lhsT=kT_ap, rhs=QT[:, 4 * g:4 * g + 4, :], start=True, stop=(bias is None)),
             reads=[tQT] + ktoks, writes=[tPB[pb]])
        if bias is not None:
            k.op("pe", lambda e: e.matmul(PB[pb][:nk, :], lhsT=bias[0], rhs=bias[1], start=False, stop=True), reads=bias[2], writes=[tPB[pb]])
        k.op("act", lambda e: e.activation(out=PTt[s][:nk, :], in_=PB[pb][:nk, :], func=AF.Exp, scale=SCALE), reads=[tPB[pb]], writes=[tPTt[s]])
        bk = pvbank[0]

        def pv():
            for h in range(4):
                k.op("pe", lambda e, h=h: e.matmul(PB[bk][:, h * 65:(h + 1) * 65], lhsT=PTt[s][:nk, h * 128:(h + 1) * 128], rhs=v_ap, start=(first and h == 0), stop=last, skip_group_check=True),
                     reads=[tPTt[s]] + vtoks, writes=[tPB[bk]])
        pend.append(pv)
        if len(pend) > 2:
            pend.pop(0)()

    load_mod(A1, 128, 1, tM1); load_mod(B1, 128, 0, tM1)
    for i in range(NT):
        r0 = i * 128
        front(128, xp[r0:r0 + 128, :], cmpp[r0:r0 + 128, :], selp[r0:r0 + 128, :], xbc_d[3 + r0:3 + r0 + 128, :], r0)
        if i >= 12:
            k.dma("sp", lambda e, i=i: e.dma_start(out=winp[(i - 12) * 128:(i - 11) * 128, :], in_=WINO[:, :]), reads=[tWINO], writes=[otok()])
        if i == NT - 1:
            k.dma("sp", lambda e: e.dma_start(out=convp[:, :], in_=xbc_d[SEQ:SEQ + 3, :]), reads=[t_xbc_d], writes=[otok()])
        for j in range(4):
            k.op("pe", lambda e, j=j: e.transpose(PT[1][:64, j * 128:(j + 1) * 128], KNB[:, j * 64:(j + 1) * 64], C["ident_bf"][:, :]), reads=[tKNB, tC], writes=[tPT[1]])
        k.op("act", lambda e: e.copy(out=KST[:, :, r0:r0 + 128], in_=PT[1][:64, 0:256].rearrange("p (g t) -> p g t", g=2)), reads=[tPT[1]], writes=[tKS[i]])
        k.op("act", lambda e: e.copy(out=KWT[:, :, r0:r0 + 128], in_=PT[1][:64, 256:512].rearrange("p (g t) -> p g t", g=2)), reads=[tPT[1]], writes=[tKW[i]])
        k.op("pool", lambda e: e.tensor_copy(out=VS[:, i, :, 0:64], in_=Ut[:, 896:1024].rearrange("p (g d) -> p g d", g=2)), reads=[tU], writes=[tVS[i]])
        k.op("pool", lambda e: e.tensor_copy(out=VW[:, i, :, 0:64], in_=Ut[:, 1152:1280].rearrange("p (g d) -> p g d", g=2)), reads=[tU], writes=[tVW[i]])
        for g in range(2):
            k.op("pe", lambda e, g=g: e.matmul(PB[2][:64, g * 4:(g + 1) * 4], lhsT=Ut[:, 512 + g * 64:576 + g * 64], rhs=WKB[:, :], start=True, stop=True), reads=[tU, tG], writes=[tPB[2]])
        k.op("act", lambda e: e.activation(out=KR[:, 0:8], in_=PB[2][:64, 0:8], func=AF.Square), reads=[tPB[2]], writes=[tKR])
        k.op("pe", lambda e: e.matmul(PB[3][:64, 0:8], lhsT=C["ones_f"][:64, :64], rhs=KR[:, 0:8], start=True, stop=True), reads=[tKR, tC], writes=[tPB[3]])
        k.op("act", lambda e: e.activation(out=KR[:, 8:16], in_=PB[3][:64, 0:8], func=AF.Sqrt, bias=EPSC[:64, :], scale=1.0 / 64), reads=[tPB[3], tC], writes=[tKR])
        k.op("dve", lambda e: e.reciprocal(out=KR[:, 8:16], in_=KR[:, 8:16]), reads=[tKR], writes=[tKR])
        k.op("dve", lambda e: e.scalar_tensor_tensor(out=KCT[:, :, 4 * i:4 * i + 4], in0=PB[2][:64, 0:8].rearrange("p (g j) -> p g j", g=2), scalar=GKC[:, 0:1],
                                                     in1=KR[:, 8:16].rearrange("p (g j) -> p g j", g=2), op0=ALU.mult, op1=ALU.mult), reads=[tPB[2], tKR, tG], writes=[tKC])
        k.op("pe", lambda e: e.matmul(PB[3][:64, 128:256], lhsT=WVS[:, i * 64:(i + 1) * 64], rhs=Ut[:, 640:768], start=True, stop=True), reads=[tU, tG], writes=[tPB[3]])
        k.op("dve", lambda e: e.tensor_tensor(out=VCA[:, :], in0=VCA[:, :], in1=PB[3][:64, 128:256], op=ALU.add), reads=[tPB[3], tVC], writes=[tVC])
        k.op("dve", lambda e: e.tensor_copy(out=VC[:, :, 0:64], in_=VCA[:, :].rearrange("p (g d) -> p g d", g=2)), reads=[tVC], writes=[tVC])
        nk = 4 * (i + 1)
        for g in range(2):
            chunk(g, KCT[:, g, 0:nk], [tKC], (C["Dsel"][:, 60 - 4 * i:60 - 4 * i + nk], C["cmpB"][:, :], [tC]), VC[:nk, g, :], [tVC], nk, True, True)
            combine(0, g)
            for h in range(4):
                k.op("pe", lambda e, h=h: e.matmul(PB[2][:, h * 64:(h + 1) * 64], lhsT=QT[:, 4 * g + h, :], rhs=KCT[:, g, :], start=True, stop=True), reads=[tQT, tKC], writes=[tPB[2]])
            k.op("act", lambda e: e.activation(out=SE[:, :], in_=PB[2][:, 0:256], func=AF.Exp, scale=SCALE), reads=[tPB[2]], writes=[tSE])
            k.op("dve", lambda e: e.tensor_tensor(out=SE[:, :].rearrange("p (h j) -> p h j", h=4), in0=SE[:, :].rearrange("p (h j) -> p h j", h=4),
                                                  in1=C["cmpM0"][:, 60 - 4 * i:124 - 4 * i].unsqueeze(1).to_broadcast([128, 4, 64]), op=ALU.mult), reads=[tSE, tC], writes=[tSE])
            k.op("dve", lambda e: e.tensor_reduce(out=SM[:, 24:28], in_=SE[:, :].rearrange("p (h j) -> p h j", h=4), axis=AX.X, op=ALU.add), reads=[tSE], writes=[tSM])
            k.op("dve", lambda e: e.tensor_scalar_max(out=SM[:, 24:28], in0=SM[:, 24:28], scalar1=1e-30), reads=[tSM], writes=[tSM])
            k.op("dve", lambda e: e.reciprocal(out=SM[:, 24:28], in_=SM[:, 24:28]), reads=[tSM], writes=[tSM])
            k.op("dve", lambda e: e.tensor_tensor(out=SE[:, :].rearrange("p (h j) -> p h j", h=4), in0=SE[:, :].rearrange("p (h j) -> p h j", h=4),
                                                  in1=SM[:, 24:28].unsqueeze(2).to_broadcast([128, 4, 64]), op=ALU.mult), reads=[tSE, tSM], writes=[tSE])
            k.op("dve", lambda e: e.tensor_reduce(out=S2[:, :], in_=SE[:, :].rearrange("p (h j) -> p j h", h=4), axis=AX.X, op=ALU.add), reads=[tSE], writes=[tSEL])
            s2v = S2[:, :].rearrange("p (n two) -> p n two", two=2)
            k.op("dve", lambda e: e.tensor_tensor(out=IMP[:, :], in0=s2v[:, :, 0], in1=s2v[:, :, 1], op=ALU.add), reads=[tSEL], writes=[tSEL])
            k.op("dve", lambda e: e.tensor_tensor(out=SC[:, :], in0=IMP[:, :], in1=C["selA"][:, i * 32:(i + 1) * 32], op=ALU.mult), reads=[tSEL, tC], writes=[tSEL])
            k.op("dve", lambda e: e.tensor_tensor(out=SC[:, :], in0=SC[:, :], in1=C["selB"][:, i * 32:(i + 1) * 32], op=ALU.add), reads=[tSEL, tC], writes=[tSEL])
            k.op("dve", lambda e: e.max(out=M1[:, :], in_=SC[:, :]), reads=[tSEL], writes=[tSEL])
            k.op("dve", lambda e: e.match_replace(out=SC2[:, :], in_to_replace=M1[:, :], in_values=SC[:, :], imm_value=-3e38), reads=[tSEL], writes=[tSEL])
            k.op("dve", lambda e: e.max(out=M2[:, :], in_=SC2[:, :]), reads=[tSEL], writes=[tSEL])
            k.op("dve", lambda e: e.tensor_scalar(out=SC2[:, :], in0=SC[:, :], scalar1=M2[:, 6:7], scalar2=None, op0=ALU.is_ge), reads=[tSEL], writes=[tSEL])
            k.op("dve", lambda e: e.tensor_tensor(out=SC2[:, :], in0=SC2[:, :], in1=C["allowed"][:, i * 32:(i + 1) * 32], op=ALU.mult), reads=[tSEL, tC], writes=[tSEL])
            k.op("dve", lambda e: e.tensor_scalar(out=NMB[:, :], in0=SC2[:, :], scalar1=-1.0, scalar2=-NEGB, op0=ALU.add, op1=ALU.mult), reads=[tSEL], writes=[tSEL])
            k.op("pe", lambda e: e.transpose(PT[1][:32, 0:128], NMB[:, :], C["ident_bf"][:, :]), reads=[tSEL, tC], writes=[tPT[1]])
            k.op("act", lambda e: e.copy(out=NMT[:, :, :], in_=PT[1][:32, 0:128].unsqueeze(1).to_broadcast([32, 4, 128])), reads=[tPT[1]], writes=[tNMT])
            for c in range(i + 1):
                if c < i:
                    bias = (C["Eall"][:, c * 128:(c + 1) * 128], NMT[:, :, :], [tC, tNMT])
                else:
                    bias = (C["ident_bf"][:, :], C["causalb"][:, :], [tC])
                chunk(g, KST[:, g, c * 128:(c + 1) * 128], [tKS[c]], bias, VS[:, c, g, :], [tVS[c]], 128, c == 0, c == i)
            combine(1, g)
            c0 = max(0, i - 4)
            for c in range(c0, i + 1):
                if c == i:
                    bias = (C["ident_bf"][:, :], C["causalb"][:, :], [tC])
                elif c == i - 4:
                    bias = (C["ident_bf"][:, :], C["antib"][:, :], [tC])
                else:
                    bias = None
                chunk(g, KWT[:, g, c * 128:(c + 1) * 128], [tKW[c]], bias, VW[:, c, g, :], [tVW[c]], 128, c == c0, c == i)
            combine(2, g)
        k.dma("sp", lambda e, r0=r0: e.dma_start(out=att_d[r0:r0 + 128, :], in_=ATT[:, :]), reads=[tATT], writes=[t_att_d])

    load_mod(A1, TS, 1, tM1); load_mod(B1, TS, 0, tM1)
    k.dma("sp", lambda e: e.dma_start(out=xbcs_d[:, 0:3, :], in_=sconv), writes=[t_xbcs_d])
    front(TS, xs[:, :], cmps[:, :], sels[:, :], xbcs_d[:, 3:7, :], SEQ)
    k.dma("sp", lambda e: e.dma_start(out=convs[:, :, :], in_=xbcs_d[:, 4:7, :]), reads=[t_xbcs_d], writes=[otok()])
    k.dma("sp", lambda e: e.dma_start(out=wins[:, 0:508, :], in_=cwin[:, 4:512, :]), writes=[otok()])
    for b in range(NSEQ):
        k.dma("sp", lambda e, b=b: e.dma_start(out=wins[b, 508:512, :], in_=WINO[4 * b:4 * b + 4, :]), reads=[tWINO], writes=[otok()])
    k.dma("sp", lambda e: e.dma_start(out=qs_d[:, :], in_=QN[:TS, :]), reads=[tQN], writes=[t_qs_d])
    k.dma("sp", lambda e: e.dma_start(out=kvs_d[:, 0:256], in_=SELO[:TS, :]), reads=[tSELO], writes=[t_kvs_d])
    k.dma("sp", lambda e: e.dma_start(out=kvs_d[:, 256:512], in_=WINO[:TS, :]), reads=[tWINO], writes=[t_kvs_d])
    GTP = k.sb("GTP", [TS, 24], F32); tGTP = Tok()
    k.op("dve", lambda e: e.tensor_copy(out=GTP[:, :].rearrange("p (q m) -> p q m", m=6), in_=GT[:TS, :].rearrange("p (m q) -> p q m", m=6)), reads=[tGT], writes=[tGTP])
    k.dma("sp", lambda e: e.dma_start(out=gts_d[:, :], in_=GTP[:, :]), reads=[tGTP], writes=[t_gts_d])
    k.pop()

    k.push()
    tCS = Tok("sconst")
    GKC2 = bc_load("GKC2", gkc2, 128, tok=tCS)
    KVA = k.sb("KVA", [128, NSEQ, 2, 257], F32); tKVA = [Tok() for _ in range(NSEQ)]
    k.op("pool", lambda e: e.memset(KVA[:], 1.0), writes=tKVA)
    k.push()
    WK32 = k.sb("WK32", [128, 32 * 128], F32); WV32 = k.sb("WV32", [128, 32 * 128], F32); tW32 = Tok()
    k.dma("sp", lambda e: e.dma_start(out=WK32[:, :], in_=wk32), writes=[tW32])
    k.dma("sp", lambda e: e.dma_start(out=WV32[:, :], in_=wv32), writes=[tW32])
    PG = [k.sb(f"PG{i}", [128, 8, 1024], F32) for i in range(2)]; tPG = [Tok(), Tok()]
    PTI = k.sb("PTI", [128, NSEQ * 64], I32); PTF = k.sb("PTF", [128, NSEQ * 64], F32); tIDXC = Tok()
    PT4 = k.sb("PT4", [128, NSEQ * 16], F32); IDX4 = k.sb("IDX4", [128, NSEQ * 16], I32)
    k.dma("sp", lambda e: e.dma_start(out=PTI[:, :], in_=ptab.rearrange("b j -> (b j)").partition_broadcast(128)), writes=[tIDXC])
    k.op("dve", lambda e: e.tensor_copy(out=PTF[:, :], in_=PTI[:, :]), reads=[tIDXC], writes=[tIDXC])
    k.op("dve", lambda e: e.tensor_tensor(out=PTF[:, :].rearrange("p (a f) -> p a f", f=4), in0=PTF[:, :].rearrange("p (a f) -> p a f", f=4),
                                          in1=C["OH4"][:, :].unsqueeze(1).to_broadcast([128, NSEQ * 16, 4]), op=ALU.mult), reads=[tIDXC, tC], writes=[tIDXC])
    k.op("dve", lambda e: e.tensor_reduce(out=PT4[:, :], in_=PTF[:, :].rearrange("p (a f) -> p a f", f=4), axis=AX.X, op=ALU.add), reads=[tIDXC], writes=[tIDXC])
    k.op("dve", lambda e: e.tensor_scalar(out=PT4[:, :], in0=PT4[:, :], scalar1=32.0, scalar2=C["PM32"][:, 0:1], op0=ALU.mult, op1=ALU.add), reads=[tIDXC, tC], writes=[tIDXC])
    k.op("dve", lambda e: e.tensor_copy(out=IDX4[:, :], in_=PT4[:, :]), reads=[tIDXC], writes=[tIDXC])
    ccmp4 = ccmp.rearrange("(g i) c -> g (i c)", i=4)
    for b in range(NSEQ):
        for c in range(2):
            s_ = (2 * b + c) % 2
            for d in range(8):
                col = b * 16 + c * 8 + d
                k.dma("pool", lambda e, d=d, col=col, s_=s_: e.indirect_dma_start(out=PG[s_][:, d, :], out_offset=None, in_=ccmp4,
                                                                                 in_offset=bass.IndirectOffsetOnAxis(ap=IDX4[:, col:col + 1], axis=0)), reads=[tIDXC], writes=[tPG[s_]])
            pk = 2 * (c % 2)
            for j in range(32):
                d, i_ = j // 4, j % 4
                k.op("pe", lambda e, j=j, d=d, i_=i_, s_=s_, pk=pk: e.matmul(PB[pk][:, 0:128], lhsT=WK32[:, j * 128:(j + 1) * 128], rhs=PG[s_][:, d, i_ * 256:i_ * 256 + 128], start=(j == 0), stop=(j == 31)), reads=[tW32, tPG[s_]], writes=[tPB[pk]])
                k.op("pe", lambda e, j=j, d=d, i_=i_, s_=s_, pk=pk: e.matmul(PB[pk + 1][:, 0:128], lhsT=WV32[:, j * 128:(j + 1) * 128], rhs=PG[s_][:, d, i_ * 256 + 128:i_ * 256 + 256], start=(j == 0), stop=(j == 31)), reads=[tW32, tPG[s_]], writes=[tPB[pk + 1]])
            k.op("act", lambda e, b=b, c=c, pk=pk: e.copy(out=KVA[:, b, c, 0:128], in_=PB[pk][:, 0:128]), reads=[tPB[pk]], writes=[tKVA[b]])
            k.op("dve", lambda e, b=b, c=c, pk=pk: e.tensor_copy(out=KVA[:, b, c, 128:256], in_=PB[pk + 1][:, 0:128]), reads=[tPB[pk + 1]], writes=[tKVA[b]])
    k.pop()
    QB = [k.sb(f"QB{i}", [128, 4, 512], BF16) for i in range(2)]; tQB = [Tok(), Tok()]
    PROD = k.sb("PROD", [128, 2048], F32); tPROD = Tok()
    STc = k.sb("STc", [128, NSEQ * 64], F32); PTc = k.sb("PTc", [128, NSEQ * 64], F32); tSTc = [Tok() for _ in range(NSEQ)]
    PCM = k.sb("PCM", [32, NSEQ, 256], F32); tPCM = Tok()
    GT16 = [k.sb(f"GT16{i}", [16, 6], F32) for i in range(2)]; GT4 = [k.sb(f"GT4{i}", [4, 24], F32) for i in range(2)]; tGTs = [Tok(), Tok()]
    OB = k.sb("OB", [16, 64], F32); tOB = Tok()
    CF = k.sb("CF", [16, 8], F32); tCF = Tok()
    KNS = [k.sb(f"KNS{i}", [4, 257], F32) for i in range(2)]; KNW = [k.sb(f"KNW{i}", [4, 257], F32) for i in range(2)]; tKN = [Tok(), Tok()]
    for i_ in range(2):
        k.op("pool", lambda e, i_=i_: e.memset(KNS[i_][:], 1.0), writes=[tKN[i_]])
        k.op("pool", lambda e, i_=i_: e.memset(KNW[i_][:], 1.0), writes=[tKN[i_]])
    STn = k.sb("STn", [4, 64], F32); PTn = k.sb("PTn", [4, 64], F32); tSTn = Tok()

    def dots(K_ap, kshape_b, Q_ap, out_ap, rd, wr):
        P, a, b_ = kshape_b
        pv = PROD[:P, :a * b_ * 64].rearrange("p (a b d) -> p a b d", a=a, b=b_)
        k.op("dve", lambda e: e.tensor_tensor(out=pv, in0=K_ap, in1=Q_ap, op=ALU.mult), reads=rd, writes=[tPROD])
        k.op("dve", lambda e: e.tensor_reduce(out=out_ap, in_=pv, axis=AX.X, op=ALU.add), reads=[tPROD], writes=wr)

    def norm_gate_store(ps_ap, nq, g, gate_ap, gate_tok, dst_ap, eng_tok):
        k.op("dve", lambda e: e.tensor_scalar_max(out=CF[:nq, 0:1], in0=ps_ap[:, 128:129], scalar1=1e-30), reads=eng_tok, writes=[tCF])
        k.op("dve", lambda e: e.reciprocal(out=CF[:nq, 0:1], in_=CF[:nq, 0:1]), reads=[tCF], writes=[tCF])
        k.op("dve", lambda e: e.tensor_tensor(out=CF[:nq, 1:2], in0=CF[:nq, 0:1], in1=gate_ap, op=ALU.mult), reads=[tCF, gate_tok], writes=[tCF])
        k.op("dve", lambda e: e.tensor_scalar(out=OB[:nq, 0:64], in0=ps_ap[:, g * 64:(g + 1) * 64], scalar1=CF[:nq, 1:2], scalar2=None, op0=ALU.mult), reads=eng_tok + [tCF], writes=[tOB])
        k.dma("sp", lambda e: e.dma_start(out=dst_ap, in_=OB[:nq, 0:64]), reads=[tOB], writes=[t_atts_d])

    def load_seq(b, s, what):
        k.dma("sp", lambda e: e.dma_start(out=QB[s][:, :, :], in_=qs_d[4 * b:4 * b + 4, :].partition_broadcast(128)), reads=[t_qs_d], writes=[tQB[s]])
        k.dma("sp", lambda e: e.dma_start(out=GT16[s][:, :], in_=gts_d[4 * b:4 * b + 4, :].rearrange("t (q m) -> (t q) m", m=6)), reads=[t_gts_d], writes=[tGTs[s]])
        k.dma("sp", lambda e: e.dma_start(out=GT4[s][:, :].rearrange("q (t m) -> q t m", m=6), in_=gts_d[4 * b:4 * b + 4, :].rearrange("t (q m) -> q t m", m=6)), reads=[t_gts_d], writes=[tGTs[s]])
        if what >= 1:
            k.dma("sp", lambda e: e.dma_start(out=KNS[s][:, 0:256], in_=kvs_d[4 * b:4 * b + 4, 0:256]), reads=[t_kvs_d], writes=[tKN[s]])
            k.dma("sp", lambda e: e.dma_start(out=KNW[s][:, 0:256], in_=kvs_d[4 * b:4 * b + 4, 256:512]), reads=[t_kvs_d], writes=[tKN[s]])

    RS = k.sb("RS", [128, 64], F32)
    for b in range(NSEQ):
        k.op("dve", lambda e, b=b: e.tensor_tensor(out=PROD[:, 0:256].rearrange("p (c f) -> p c f", c=2), in0=KVA[:, b, :, 0:128], in1=KVA[:, b, :, 0:128], op=ALU.mult), reads=[tKVA[b]], writes=[tPROD])
        k.op("dve", lambda e, b=b: e.tensor_reduce(out=RS[:, 4 * b:4 * b + 4], in_=PROD[:, 0:256].rearrange("p (a d) -> p a d", d=64), axis=AX.X, op=ALU.add), reads=[tPROD], writes=[tSM])
    k.op("act", lambda e: e.activation(out=RS[:, :], in_=RS[:, :], func=AF.Sqrt, bias=EPSC[:, :], scale=1.0 / 64), reads=[tSM, tC], writes=[tSM])
    k.op("dve", lambda e: e.reciprocal(out=RS[:, :], in_=RS[:, :]), reads=[tSM], writes=[tSM])
    for b in range(NSEQ):
        kb_ = KVA[:, b, :, 0:128].rearrange("p c (g d) -> p c g d", g=2)
        k.op("dve", lambda e, b=b, kb_=kb_: e.tensor_tensor(out=kb_, in0=kb_, in1=RS[:, 4 * b:4 * b + 4].rearrange("p (c g) -> p c g", c=2).unsqueeze(3).to_broadcast([128, 2, 2, 64]), op=ALU.mult), reads=[tKVA[b], tSM], writes=[tKVA[b]])
        k.op("pool", lambda e, b=b: e.tensor_tensor(out=KVA[:, b, :, 0:128], in0=KVA[:, b, :, 0:128], in1=GKC2[:, :].unsqueeze(1).to_broadcast([128, 2, 128]), op=ALU.mult), reads=[tKVA[b], tCS], writes=[tKVA[b]])
    for b in range(NSEQ):
        s = b % 2
        load_seq(b, s, 0)
        qv = QB[s][:, :, :].rearrange("p t (h d) -> p t h d", d=64)
        for c in range(2):
            for g in range(2):
                dots(KVA[:, b, c, g * 64:(g + 1) * 64].unsqueeze(1).unsqueeze(1).to_broadcast([128, 4, 4, 64]), (128, 4, 4), qv[:, :, 4 * g:4 * g + 4, :],
                     STc[:, b * 64 + (c * 2 + g) * 16:b * 64 + (c * 2 + g + 1) * 16].rearrange("p (t q) -> p t q", t=4), [tKVA[b], tQB[s]], [tSTc[b]])
        k.op("act", lambda e, b=b: e.activation(out=PTc[:, b * 64:(b + 1) * 64], in_=STc[:, b * 64:(b + 1) * 64], func=AF.Exp, scale=SCALE), reads=[tSTc[b]], writes=[tSTc[b]])
        for g in range(2):
            for c in range(2):
                k.op("pe", lambda e, b=b, g=g, c=c: e.matmul(PB[2][:16, 0:129], lhsT=PTc[:, b * 64 + (c * 2 + g) * 16:b * 64 + (c * 2 + g + 1) * 16], rhs=KVA[:, b, c, 128:257], start=(c == 0), stop=(c == 1)), reads=[tSTc[b], tKVA[b]], writes=[tPB[2]])
            norm_gate_store(PB[2][:16, 0:129], 16, g, GT16[s][:, g:g + 1], tGTs[s], atts_d[0, b, :, g, :, :], [tPB[2]])
        for c in range(2):
            k.op("pe", lambda e, b=b, c=c: e.transpose(PB[3][:32, c * 128:(c + 1) * 128], PTc[:, b * 64 + c * 32:b * 64 + (c + 1) * 32], C["ident_f"][:, :]), reads=[tSTc[b], tC], writes=[tPB[3]])
        k.op("act", lambda e, b=b: e.copy(out=PCM[:, b, :], in_=PB[3][:32, 0:256]), reads=[tPB[3]], writes=[tPCM])
    RSM = k.sb("RSM", [32, NSEQ], F32)
    k.op("dve", lambda e: e.tensor_reduce(out=RSM[:, :], in_=PCM[:, :, :], axis=AX.X, op=ALU.add), reads=[tPCM], writes=[tSM])
    k.op("dve", lambda e: e.reciprocal(out=RSM[:, :], in_=RSM[:, :]), reads=[tSM], writes=[tSM])
    k.op("dve", lambda e: e.tensor_tensor(out=PCM[:, :, :], in0=PCM[:, :, :], in1=RSM[:, :].unsqueeze(2).to_broadcast([32, NSEQ, 256]), op=ALU.mult), reads=[tPCM, tSM], writes=[tPCM])
    for b in range(NSEQ):
        k.op("pe", lambda e, b=b: e.matmul(PB[4][:, 0:256], lhsT=C["HSELB"][:, b * 128:(b + 1) * 128], rhs=PCM[:, b, :], start=(b == 0), stop=(b == NSEQ - 1)), reads=[tPCM, tC], writes=[tPB[4]])
    IMPs = k.sb("IMPs", [128, 128], F32); SCs = k.sb("SCs", [128, 128], F32); SC2s = k.sb("SC2s", [128, 128], F32); tSELs = Tok()
    M1s = k.sb("M1s", [128, 16], F32); RBT = k.sb("RBT", [16, 128], F32)
    PHY = k.sb("PHY", [128, 64, 2], F32); PHI = k.sb("PHI", [128, 64], I32); PHF = k.sb("PHF", [128, 64], F32); tPHY = Tok()
    IDXF = k.sb("IDXF", [128, 128], F32); IDXS = k.sb("IDXS", [128, 128], I32); tIDXS = Tok()
    for b in range(NSEQ):
        k.dma("sp", lambda e, b=b: e.dma_start(out=PHI[8 * b:8 * b + 8, :], in_=ptab[b, :].partition_broadcast(8)), writes=[tPHY])
    k.op("dve", lambda e: e.tensor_copy(out=PHF[:, :], in_=PHI[:, :]), reads=[tPHY], writes=[tPHY])
    k.op("dve", lambda e: e.tensor_scalar(out=PHY[:, :, 0], in0=PHF[:, :], scalar1=2.0, scalar2=1.0, op0=ALU.mult, op1=ALU.add), reads=[tPHY], writes=[tPHY])
    k.op("dve", lambda e: e.tensor_scalar(out=PHY[:, :, 1], in0=PHF[:, :], scalar1=2.0, scalar2=2.0, op0=ALU.mult, op1=ALU.add), reads=[tPHY], writes=[tPHY])
    pv2 = PB[4][:, 0:256].rearrange("p (n two) -> p n two", two=2)
    k.op("act", lambda e: e.copy(out=SC2s[:, :], in_=pv2[:, :, 0]), reads=[tPB[4]], writes=[tSELs])
    k.op("dve", lambda e: e.tensor_tensor(out=IMPs[:, :], in0=pv2[:, :, 1], in1=SC2s[:, :], op=ALU.add), reads=[tPB[4], tSELs], writes=[tSELs])
    k.op("dve", lambda e: e.tensor_tensor(out=SCs[:, :], in0=IMPs[:, :], in1=C["BIGS"][:, :], op=ALU.add), reads=[tSELs, tC], writes=[tSELs])
    k.op("dve", lambda e: e.max(out=M1s[:, 0:8], in_=SCs[:, :]), reads=[tSELs], writes=[tSELs])
    k.op("dve", lambda e: e.match_replace(out=SC2s[:, :], in_to_replace=M1s[:, 0:8], in_values=SCs[:, :], imm_value=-3e38), reads=[tSELs], writes=[tSELs])
    k.op("dve", lambda e: e.max(out=M1s[:, 8:16], in_=SC2s[:, :]), reads=[tSELs], writes=[tSELs])
    k.op("dve", lambda e: e.tensor_scalar(out=SC2s[:, :], in0=SCs[:, :], scalar1=M1s[:, 14:15], scalar2=None, op0=ALU.is_ge), reads=[tSELs], writes=[tSELs])
    k.op("dve", lambda e: e.tensor_tensor(out=SCs[:, :], in0=SC2s[:, :], in1=PHY[:, :, :].rearrange("p j two -> p (j two)"), op=ALU.mult), reads=[tSELs, tPHY], writes=[tSELs])
    k.op("dve", lambda e: e.max(out=M1s[:, 0:8], in_=SCs[:, :]), reads=[tSELs], writes=[tSELs])
    k.op("dve", lambda e: e.match_replace(out=SC2s[:, :], in_to_replace=M1s[:, 0:8], in_values=SCs[:, :], imm_value=0.0), reads=[tSELs], writes=[tSELs])
    k.op("dve", lambda e: e.max(out=M1s[:, 8:16], in_=SC2s[:, :]), reads=[tSELs], writes=[tSELs])
    k.op("dve", lambda e: e.tensor_scalar(out=M1s[:, :], in0=M1s[:, :], scalar1=-1.0, scalar2=0.0, op0=ALU.add, op1=ALU.max), reads=[tSELs], writes=[tSELs])
    k.op("dve", lambda e: e.tensor_scalar(out=M1s[:, :], in0=M1s[:, :], scalar1=16.0, scalar2=None, op0=ALU.mult), reads=[tSELs], writes=[tSELs])
    k.op("pe", lambda e: e.transpose(PB[3][:16, 0:128], M1s[:, :], C["ident_f"][:, :]), reads=[tSELs, tC], writes=[tPB[3]])
    k.op("act", lambda e: e.copy(out=RBT[:, :], in_=PB[3][:16, 0:128]), reads=[tPB[3]], writes=[tSELs])
    k.op("pe", lambda e: e.matmul(PB[3][:, 128:256], lhsT=C["EXP16"][:, :], rhs=RBT[:, :], start=True, stop=True), reads=[tSELs, tC], writes=[tPB[3]])
    k.op("dve", lambda e: e.tensor_scalar(out=IDXF[:, :], in0=PB[3][:, 128:256], scalar1=C["PM8"][:, 0:1], scalar2=None, op0=ALU.add), reads=[tPB[3], tC], writes=[tIDXS])
    k.op("dve", lambda e: e.tensor_copy(out=IDXS[:, :], in_=IDXF[:, :]), reads=[tIDXS], writes=[tIDXS])
    NKS = 3
    KSEL = [k.sb(f"KSEL{i}", [128, 2, 1024], F32) for i in range(NKS)]; tKSEL = [Tok() for _ in range(NKS)]
    KSV = [KSEL[i][:, :, :].rearrange("p c (i f) -> p (c i) f", i=4) for i in range(NKS)]
    csel4 = csel.rearrange("(g i) c -> g (i c)", i=4)
    STs = k.sb("STs", [128, 32], F32); PTs = k.sb("PTs", [128, 32], F32); tSTs = Tok()
    KWB = [k.sb(f"KWB{i}", [128, 4, 257], F32) for i in range(2)]; tKWB = [Tok(), Tok()]
    for i_ in range(2):
        k.op("pool", lambda e, i_=i_: e.memset(KWB[i_][:], 1.0), writes=[tKWB[i_]])
    STw = k.sb("STw", [128, 128], F32); PTw = k.sb("PTw", [128, 128], F32); tSTw = Tok()
    un = 0
    for b in range(NSEQ):
        sq = b % 2
        load_seq(b, sq, 1)
        qv = QB[sq][:, :, :].rearrange("p t (h d) -> p t h d", d=64)
        for g in range(2):
            for t in range(4):
                gt = g * 4 + t; s = un % NKS; un += 1
                for ch in range(2):
                    k.dma("pool", lambda e, b=b, gt=gt, ch=ch, s=s: e.indirect_dma_start(out=KSEL[s][:, ch, :], out_offset=None, in_=csel4,
                                                                                           in_offset=bass.IndirectOffsetOnAxis(ap=IDXS[:, b * 8 + gt:b * 8 + gt + 1], axis=0), element_offset=8 * ch * 1024), reads=[tIDXS], writes=[tKSEL[s]])
                dots(KSV[s][:, :, g * 64:(g + 1) * 64].unsqueeze(2).to_broadcast([128, 8, 4, 64]), (128, 8, 4),
                     qv[:, t, 4 * g:4 * g + 4, :].unsqueeze(1).to_broadcast([128, 8, 4, 64]), STs[:, :].rearrange("p (c q) -> p c q", c=8), [tKSEL[s], tQB[sq]], [tSTs])
                k.op("act", lambda e: e.activation(out=PTs[:, :], in_=STs[:, :], func=AF.Exp, scale=SCALE), reads=[tSTs], writes=[tSTs])
                k.op("dve", lambda e: e.tensor_scalar(out=PTs[:, :], in0=PTs[:, :], scalar1=C["M120"][:, 0:1], scalar2=None, op0=ALU.mult), reads=[tSTs, tC], writes=[tSTs])
                dots(KNS[sq][:, g * 64:(g + 1) * 64].unsqueeze(1).unsqueeze(1).to_broadcast([4, 1, 4, 64]), (4, 1, 4), qv[:4, t:t + 1, 4 * g:4 * g + 4, :],
                     STn[:, 0:4].rearrange("p (a q) -> p a q", a=1), [tKN[sq], tQB[sq]], [tSTn])
                k.op("act", lambda e: e.activation(out=PTn[:, 0:4], in_=STn[:, 0:4], func=AF.Exp, scale=SCALE), reads=[tSTn], writes=[tSTn])
                k.op("dve", lambda e, t=t: e.tensor_scalar(out=PTn[:, 0:4], in0=PTn[:, 0:4], scalar1=C["CM4"][:, t:t + 1], scalar2=None, op0=ALU.mult), reads=[tSTn, tC], writes=[tSTn])
                for ch in range(8):
                    k.op("pe", lambda e, ch=ch, s=s: e.matmul(PB[5][:4, 0:128], lhsT=PTs[:, ch * 4:(ch + 1) * 4], rhs=KSV[s][:, ch, 128:256], start=(ch == 0), stop=False, skip_group_check=True), reads=[tSTs, tKSEL[s]], writes=[tPB[5]])
                    k.op("pe", lambda e, ch=ch: e.matmul(PB[5][:4, 128:129], lhsT=PTs[:, ch * 4:(ch + 1) * 4], rhs=C["ones_f"][:, 0:1], start=False, stop=False, skip_group_check=True), reads=[tSTs, tC], writes=[tPB[5]])
                k.op("pe", lambda e, sq=sq: e.matmul(PB[5][:4, 0:129], lhsT=PTn[:, 0:4], rhs=KNS[sq][:, 128:257], start=False, stop=True, skip_group_check=True), reads=[tSTn, tKN[sq]], writes=[tPB[5]])
                norm_gate_store(PB[5][:4, 0:129], 4, g, GT4[sq][:, t * 6 + 2 + g:t * 6 + 3 + g], tGTs[sq], atts_d[1, b, t, g, :, :], [tPB[5]])
        KWt = KWB[b % 2]; tKWt = tKWB[b % 2]
        k.dma("sp", lambda e, b=b, KWt=KWt: e.dma_start(out=KWt[:, :, 0:256], in_=cwin[b].rearrange("(c p) f -> p c f", p=128)), writes=[tKWt])
        for g in range(2):
            for ch in range(4):
                dots(KWt[:, ch, g * 64:(g + 1) * 64].unsqueeze(1).unsqueeze(1).to_broadcast([128, 4, 4, 64]), (128, 4, 4), qv[:, :, 4 * g:4 * g + 4, :],
                     STw[:, (g * 4 + ch) * 16:(g * 4 + ch + 1) * 16].rearrange("p (t q) -> p t q", t=4), [tKWt, tQB[sq]], [tSTw])
            dots(KNW[sq][:, g * 64:(g + 1) * 64].unsqueeze(1).unsqueeze(1).to_broadcast([4, 4, 4, 64]), (4, 4, 4), qv[:4, :, 4 * g:4 * g + 4, :],
                 STn[:, 16 + g * 16:32 + g * 16].rearrange("p (t q) -> p t q", t=4), [tKN[sq], tQB[sq]], [tSTn])
        k.op("act", lambda e: e.activation(out=PTw[:, :], in_=STw[:, :], func=AF.Exp, scale=SCALE), reads=[tSTw], writes=[tSTw])
        k.op("dve", lambda e: e.tensor_tensor(out=PTw[:, :].rearrange("p (g c t q) -> p g c t q", g=2, c=4, t=4)[:, :, 0, :, :], in0=PTw[:, :].rearrange("p (g c t q) -> p g c t q", g=2, c=4, t=4)[:, :, 0, :, :],
                                              in1=C["MASKW"][:, :].unsqueeze(1).unsqueeze(3).to_broadcast([128, 2, 4, 4]), op=ALU.mult), reads=[tSTw, tC], writes=[tSTw])
        k.op("act", lambda e: e.activation(out=PTn[:, 16:48], in_=STn[:, 16:48], func=AF.Exp, scale=SCALE), reads=[tSTn], writes=[tSTn])
        k.op("dve", lambda e: e.tensor_tensor(out=PTn[:, 16:48].rearrange("p (g t q) -> p g t q", g=2, t=4), in0=PTn[:, 16:48].rearrange("p (g t q) -> p g t q", g=2, t=4),
                                              in1=C["CM4"][:, :].unsqueeze(1).unsqueeze(3).to_broadcast([4, 2, 4, 4]), op=ALU.mult), reads=[tSTn, tC], writes=[tSTn])
        for g in range(2):
            for ch in range(4):
                k.op("pe", lambda e, g=g, ch=ch, KWt=KWt: e.matmul(PB[2][:16, 0:129], lhsT=PTw[:, (g * 4 + ch) * 16:(g * 4 + ch + 1) * 16], rhs=KWt[:, ch, 128:257], start=(ch == 0), stop=False), reads=[tSTw, tKWt], writes=[tPB[2]])
            k.op("pe", lambda e, g=g, sq=sq: e.matmul(PB[2][:16, 0:129], lhsT=PTn[:, 16 + g * 16:32 + g * 16], rhs=KNW[sq][:, 128:257], start=False, stop=True), reads=[tSTn, tKN[sq]], writes=[tPB[2]])
            norm_gate_store(PB[2][:16, 0:129], 16, g, GT16[sq][:, 4 + g:5 + g], tGTs[sq], atts_d[2, b, :, g, :, :], [tPB[2]])
    AS = [k.sb(f"AS{i}", [TS, 512], F32) for i in range(3)]; tAS = Tok()
    for r in range(3):
        k.dma("sp", lambda e, r=r: e.dma_start(out=AS[r][:, :], in_=atts_d[r].rearrange("b t g q d -> (b t) (g q d)")), reads=[t_atts_d], writes=[tAS])
    k.op("dve", lambda e: e.tensor_tensor(out=AS[0][:, :], in0=AS[0][:, :], in1=AS[1][:, :], op=ALU.add), reads=[tAS], writes=[tAS])
    k.op("dve", lambda e: e.tensor_tensor(out=AS[0][:, :], in0=AS[0][:, :], in1=AS[2][:, :], op=ALU.add), reads=[tAS], writes=[tAS])
    k.dma("sp", lambda e: e.dma_start(out=att_d[SEQ:SEQ + TS, :], in_=AS[0][:, :]), reads=[tAS], writes=[t_att_d])
    k.pop()

    k.push()
    tG2 = Tok("gains2")
    CW = bc_load("CW", convw, 4 * 768, tok=tG2); CBt = bc_load("CBt", convb, 768, tok=tG2)
    DTB = bc_load("DTB", dtb, 8, tok=tG2); ALG = bc_load("ALG", alog, 8, tok=tG2); DSK = bc_load("DSK", dsk, 512, tok=tG2)
    GATT = bc_load("GATT", gatt, 512, tok=tG2); GSSM = bc_load("GSSM", gssm, 512, tok=tG2); BR = bc_load("BR", b_r, 20, tok=tG2)
    AN = k.sb("AN", [128, 8], F32)
    k.op("act", lambda e: e.activation(out=AN[:], in_=ALG[:], func=AF.Exp), reads=[tG2], writes=[tG2])
    k.op("dve", lambda e: e.tensor_scalar_mul(out=AN[:], in0=AN[:], scalar1=-1.0), reads=[tG2], writes=[tG2])
    ABH = k.sb("ABH", [128, 1], F32); DBH = k.sb("DBH", [128, 1], F32)
    k.dma("sp", lambda e: e.dma_start(out=ABH[:], in_=abh), writes=[tG2])
    k.dma("sp", lambda e: e.dma_start(out=DBH[:], in_=dbh), writes=[tG2])
    k.op("act", lambda e: e.activation(out=ABH[:], in_=ABH[:], func=AF.Exp), reads=[tG2], writes=[tG2])
    k.op("dve", lambda e: e.tensor_scalar_mul(out=ABH[:], in0=ABH[:], scalar1=-1.0), reads=[tG2], writes=[tG2])
    stg2 = [k.sb(f"stg2{i}", [128, D], F32) for i in range(2)]; tstg2 = [Tok(), Tok()]
    WOUT = k.sb("WOUT", [128, 8, D], BF16); tWOUT = Tok()
    for kk in range(8):
        s = kk % 2
        k.dma("sp", lambda e, kk=kk, s=s: e.dma_start(out=stg2[s][:, :], in_=w_out[kk * 128:(kk + 1) * 128, :]), writes=[tstg2[s]])
        k.op("pool", lambda e, kk=kk, s=s: e.tensor_copy(out=WOUT[:, kk, :], in_=stg2[s][:, :]), reads=[tstg2[s]], writes=[tWOUT])
    WR = k.sb("WR", [128, 8, 20], BF16); tWR = Tok()
    for kk in range(8):
        k.dma("sp", lambda e, kk=kk: e.dma_start(out=stg2[0][:, kk * 20:(kk + 1) * 20], in_=w_r[kk * 128:(kk + 1) * 128, :]), writes=[tstg2[0]])
    k.op("pool", lambda e: e.tensor_copy(out=WR[:], in_=stg2[0][:, :160].rearrange("p (k c) -> p k c", k=8)), reads=[tstg2[0]], writes=[tWR])
    G1 = k.sb("G1", [128, D], F32); A2 = k.sb("A2", [128, D], F32); B2 = k.sb("B2", [128, D], F32); tM2 = Tok()
    HT = k.sb("HT", [64, 8, 64], F32); HTB = k.sb("HTB", [64, 8, 64], BF16); tHT = Tok()
    k.op("pool", lambda e: e.memset(HT[:], 0.0), writes=[tHT])
    k.op("pool", lambda e: e.memset(HTB[:], 0.0), writes=[tHT])
    XT = k.sb("XT2", [128, D], F32); tXT = Tok()
    ATT = k.sb("ATT2", [128, 512], F32); tATT = Tok()
    ZD = k.sb("ZD", [128, 520], F32); tZD = Tok()
    XC = [k.sb(f"XC{i}", [128, 768], F32) for i in range(4)]; tXC = [Tok() for _ in range(4)]
    ACC = k.sb("ACC", [128, 768], F32); tACC = Tok()
    TMPc = k.sb("TMPc", [128, D], F32); tTMPc = Tok()
    XS = k.sb("XS", [128, 768], F32); XSB = k.sb("XSB", [128, 768], BF16); tXS = Tok()
    DT = k.sb("DT", [128, 24], F32); tDT = Tok()
    RU = k.sb("RU", [128, 8, 128], F32); tRU = Tok()
    SG = k.sb("SG", [128, 4, 128], F32); tSG = Tok()
    DEC = k.sb("DEC", [128, 8, 128], F32); tDEC = Tok()
    EX = k.sb("EX", [64, 8, 128], F32); tEX = Tok()
    BCT = k.sb("BCT", [64, 4, 128], BF16); tBCT = Tok()
    CBs = k.sb("CBs", [128, 2, 128], F32); tCBs = Tok()
    MTt = [k.sb(f"MTt{i}", [128, 128], BF16) for i in range(2)]; tMT = [Tok(), Tok()]
    CEt = [k.sb(f"CEt{i}", [64, 128], BF16) for i in range(2)]; tCE = [Tok(), Tok()]
    BWt = [k.sb(f"BWt{i}", [128, 64], BF16) for i in range(2)]; tBW = [Tok(), Tok()]
    YS = k.sb("YS", [128, 512], F32); tYS = Tok()
    MIX = k.sb("MIX", [128, D], BF16); tMIX = Tok()
    MIXT = k.sb("MIXT", [128, 8, 128], BF16); tMIXT = Tok()
    X1 = k.sb("X1", [128, D], F32); tX1 = Tok()
    H2 = k.sb("H2", [128, D], BF16); tH2 = Tok()
    H2T = k.sb("H2T", [128, 8, 128], BF16); tH2T = Tok()
    LG = k.sb("LG", [128, 20], F32); RT = k.sb("RT", [128, 64], F32); COMB = k.sb("COMB", [128, 16], F32); tRT = Tok()

    def conv_silu(T, taps):
        for w in range(4):
            if T == 128:
                k.dma("sp", lambda e, w=w: e.dma_start(out=XC[w][:T, :], in_=taps[w]), reads=[t_xbc_d, t_xbcs_d], writes=[tXC[w]])
            else:
                for b in range(NSEQ):
                    k.dma("sp", lambda e, w=w, b=b: e.dma_start(out=XC[w][4 * b:4 * b + 4, :], in_=taps[w][b, :, :]), reads=[t_xbc_d, t_xbcs_d], writes=[tXC[w]])
        k.op("dve", lambda e: e.tensor_tensor(out=ACC[:T, :], in0=XC[0][:T, :], in1=CW[:T, 0:768], op=ALU.mult), reads=[tXC[0], tG2], writes=[tACC])
        for w in range(1, 4):
            k.op("pool", lambda e, w=w: e.tensor_tensor(out=TMPc[:T, :768], in0=XC[w][:T, :], in1=CW[:T, w * 768:(w + 1) * 768], op=ALU.mult), reads=[tXC[w], tG2], writes=[tTMPc])
            k.op("dve", lambda e: e.tensor_tensor(out=ACC[:T, :], in0=ACC[:T, :], in1=TMPc[:T, :768], op=ALU.add), reads=[tACC, tTMPc], writes=[tACC])
        k.op("dve", lambda e: e.tensor_tensor(out=ACC[:T, :], in0=ACC[:T, :], in1=CBt[:T, :], op=ALU.add), reads=[tACC, tG2], writes=[tACC])
        k.op("act", lambda e: e.activation(out=XS[:T, :], in_=ACC[:T, :], func=AF.Silu), reads=[tACC], writes=[tXS])
        k.op("pool", lambda e: e.tensor_copy(out=XSB[:T, :], in_=XS[:T, :]), reads=[tXS], writes=[tXS])

    def softplus_dt(T):
        k.op("dve", lambda e: e.tensor_tensor(out=DT[:T, 0:8], in0=ZD[:T, 512:520], in1=DTB[:T, :], op=ALU.add), reads=[tZD, tG2], writes=[tDT])
        k.op("dve", lambda e: e.tensor_scalar_min(out=DT[:T, 0:8], in0=DT[:T, 0:8], scalar1=30.0), reads=[tDT], writes=[tDT])
        k.op("act", lambda e: e.activation(out=DT[:T, 0:8], in_=DT[:T, 0:8], func=AF.Exp), reads=[tDT], writes=[tDT])
        k.op("act", lambda e: e.activation(out=DT[:T, 0:8], in_=DT[:T, 0:8], func=AF.Ln, bias=1.0), reads=[tDT], writes=[tDT])

    def finish(T, row0, yout):
        k.op("act", lambda e: e.activation(out=TMPc[:T, :512], in_=ZD[:T, 0:512], func=AF.Silu), reads=[tZD], writes=[tTMPc])
        k.op("dve", lambda e: e.tensor_tensor(out=YS[:T, :], in0=YS[:T, :], in1=TMPc[:T, :512], op=ALU.mult), reads=[tYS, tTMPc], writes=[tYS])
        k.op("act", lambda e: e.activation(out=TMPc[:T, :512], in_=YS[:T, :], func=AF.Square, accum_out=SM[:T, 0:1]), reads=[tYS], writes=[tTMPc, tSM])
        rstd_of(SM[:T, 0:1], T, 1.0 / 512)
        k.op("dve", lambda e: e.scalar_tensor_tensor(out=MIX[:T, 512:1024], in0=YS[:T, :], scalar=SM[:T, 0:1], in1=GSSM[:T, :], op0=ALU.mult, op1=ALU.mult), reads=[tYS, tSM, tG2], writes=[tMIX])
        k.op("act", lambda e: e.activation(out=TMPc[:T, :512], in_=ATT[:T, :], func=AF.Square, accum_out=SM[:T, 1:2]), reads=[tATT], writes=[tTMPc, tSM])
        rstd_of(SM[:T, 1:2], T, 1.0 / 512)
        k.op("dve", lambda e: e.scalar_tensor_tensor(out=MIX[:T, 0:512], in0=ATT[:T, :], scalar=SM[:T, 1:2], in1=GATT[:T, :], op0=ALU.mult, op1=ALU.mult), reads=[tATT, tSM, tG2], writes=[tMIX])
        for kk in range(8):
            k.op("pe", lambda e, kk=kk: e.transpose(PT[0][:, kk * 128:kk * 128 + T], MIX[:T, kk * 128:(kk + 1) * 128], C["ident_bf"][:T, :T]), reads=[tMIX, tC], writes=[tPT[0]])
        k.op("act", lambda e: e.copy(out=MIXT[:, :, :T], in_=PT[0][:, :].rearrange("p (k t) -> p k t", k=8)[:, :, :T]), reads=[tPT[0]], writes=[tMIXT])
        for hf in range(2):
            for kk in range(8):
                k.op("pe", lambda e, kk=kk, hf=hf: e.matmul(PB[hf][:T, :], lhsT=MIXT[:, kk, :T], rhs=WOUT[:, kk, hf * 512:(hf + 1) * 512], start=(kk == 0), stop=(kk == 7)),
                     reads=[tMIXT, tWOUT], writes=[tPB[hf]])
            k.op("dve", lambda e, hf=hf: e.tensor_tensor(out=TMPc[:T, hf * 512:(hf + 1) * 512], in0=PB[hf][:T, :], in1=G1[:T, hf * 512:(hf + 1) * 512], op=ALU.mult), reads=[tPB[hf], tM2], writes=[tTMPc])
        k.op("dve", lambda e: e.tensor_tensor(out=X1[:T, :], in0=TMPc[:T, :], in1=XT[:T, :], op=ALU.add), reads=[tTMPc, tXT], writes=[tX1])
        k.dma("sp", lambda e: e.dma_start(out=x1_d[row0:row0 + T, :], in_=X1[:T, :]), reads=[tX1], writes=[t_x1_d])
        k.op("act", lambda e: e.activation(out=H2[:T, :], in_=X1[:T, :], func=AF.Square, accum_out=SM[:T, 2:3]), reads=[tX1], writes=[tH2, tSM])
        rstd_of(SM[:T, 2:3], T, 1.0 / D)
        k.op("dve", lambda e: e.scalar_tensor_tensor(out=TMPc[:T, :], in0=X1[:T, :], scalar=SM[:T, 2:3], in1=A2[:T, :], op0=ALU.mult, op1=ALU.mult), reads=[tX1, tSM, tM2], writes=[tTMPc])
        k.op("dve", lambda e: e.tensor_tensor(out=H2[:T, :], in0=TMPc[:T, :], in1=B2[:T, :], op=ALU.add), reads=[tTMPc, tM2], writes=[tH2])
        for kk in range(8):
            k.op("pe", lambda e, kk=kk: e.transpose(PT[0][:, kk * 128:kk * 128 + T], H2[:T, kk * 128:(kk + 1) * 128], C["ident_bf"][:T, :T]), reads=[tH2, tC], writes=[tPT[0]])
        k.op("act", lambda e: e.copy(out=H2T[:, :, :T], in_=PT[0][:, :].rearrange("p (k t) -> p k t", k=8)[:, :, :T]), reads=[tPT[0]], writes=[tH2T])
        k.dma("sp", lambda e: e.dma_start(out=h2T_d[:, :, row0:row0 + T].rearrange("k p t -> p k t"), in_=H2T[:, :, :T]), reads=[tH2T], writes=[t_h2T_d])
        for kk in range(8):
            k.op("pe", lambda e, kk=kk: e.matmul(PB[2][:T, 0:20], lhsT=H2T[:, kk, :T], rhs=WR[:, kk, :], start=(kk == 0), stop=(kk == 7)), reads=[tH2T, tWR], writes=[tPB[2]])
        k.op("dve", lambda e: e.tensor_tensor(out=LG[:T, :], in0=PB[2][:T, 0:20], in1=BR[:T, :], op=ALU.add), reads=[tPB[2], tG2], writes=[tRT])
        R = lambda a, b: RT[:T, a:b]

        def dv(fn):
            k.op("dve", fn, reads=[tRT], writes=[tRT])
        dv(lambda e: e.tensor_reduce(out=R(0, 1), in_=LG[:T, 0:4], axis=AX.X, op=ALU.max))
        dv(lambda e: e.tensor_scalar(out=R(4, 8), in0=LG[:T, 0:4], scalar1=R(0, 1), scalar2=None, op0=ALU.is_equal))
        dv(lambda e: e.tensor_scalar(out=R(8, 12), in0=LG[:T, 0:4], scalar1=R(0, 1), scalar2=None, op0=ALU.subtract))
        k.op("act", lambda e: e.activation(out=R(8, 12), in_=R(8, 12), func=AF.Exp), reads=[tRT], writes=[tRT])
        dv(lambda e: e.tensor_reduce(out=R(1, 2), in_=R(8, 12), axis=AX.X, op=ALU.add))
        dv(lambda e: e.reciprocal(out=R(1, 2), in_=R(1, 2)))
        dv(lambda e: e.tensor_tensor(out=RT[:T, 16:32].rearrange("p (g j) -> p g j", g=4), in0=LG[:T, 4:20].rearrange("p (g j) -> p g j", g=4),
                                     in1=R(4, 8).unsqueeze(2).to_broadcast([T, 4, 4]), op=ALU.mult))
        dv(lambda e: e.tensor_reduce(out=R(12, 16), in_=RT[:T, 16:32].rearrange("p (g j) -> p j g", g=4), axis=AX.X, op=ALU.add))
        dv(lambda e: e.tensor_reduce(out=R(2, 3), in_=R(12, 16), axis=AX.X, op=ALU.max))
        dv(lambda e: e.tensor_scalar(out=R(32, 36), in0=R(12, 16), scalar1=R(2, 3), scalar2=None, op0=ALU.is_equal))
        dv(lambda e: e.scalar_tensor_tensor(out=R(36, 40), in0=R(32, 36), scalar=-1e9, in1=R(12, 16), op0=ALU.mult, op1=ALU.add))
        dv(lambda e: e.tensor_reduce(out=R(3, 4), in_=R(36, 40), axis=AX.X, op=ALU.max))
        dv(lambda e: e.tensor_scalar(out=R(40, 44), in0=R(36, 40), scalar1=R(3, 4), scalar2=None, op0=ALU.is_equal))
        dv(lambda e: e.tensor_tensor(out=R(44, 45), in0=R(3, 4), in1=R(2, 3), op=ALU.subtract))
        k.op("act", lambda e: e.activation(out=R(44, 45), in_=R(44, 45), func=AF.Exp), reads=[tRT], writes=[tRT])
        dv(lambda e: e.tensor_scalar_add(out=R(45, 46), in0=R(44, 45), scalar1=1.0))
        dv(lambda e: e.reciprocal(out=R(45, 46), in_=R(45, 46)))
        dv(lambda e: e.tensor_tensor(out=R(46, 47), in0=R(45, 46), in1=R(44, 45), op=ALU.mult))
        dv(lambda e: e.tensor_tensor(out=R(45, 47), in0=R(45, 47), in1=R(1, 2).to_broadcast([T, 2]), op=ALU.mult))
        dv(lambda e: e.tensor_scalar(out=R(48, 52), in0=R(32, 36), scalar1=R(45, 46), scalar2=None, op0=ALU.mult))
        dv(lambda e: e.scalar_tensor_tensor(out=R(48, 52), in0=R(40, 44), scalar=R(46, 47), in1=R(48, 52), op0=ALU.mult, op1=ALU.add))
        dv(lambda e: e.tensor_tensor(out=COMB[:T, :].rearrange("p (g j) -> p g j", g=4), in0=R(4, 8).unsqueeze(2).to_broadcast([T, 4, 4]),
                                     in1=R(48, 52).unsqueeze(1).to_broadcast([T, 4, 4]), op=ALU.mult))
        k.dma("sp", lambda e: e.dma_start(out=comb_d[row0:row0 + T, :], in_=COMB[:T, :]), reads=[tRT], writes=[t_comb_d])

    load_mod(G1, 128, 2, tM2); load_mod(B2, 128, 3, tM2); load_mod(A2, 128, 4, tM2)
    for i in range(NT):
        r0 = i * 128
        k.dma("sp", lambda e, r0=r0: e.dma_start(out=XT[:, :], in_=xp[r0:r0 + 128, :]), writes=[tXT])
        k.dma("sp", lambda e, r0=r0: e.dma_start(out=ATT[:, :], in_=att_d[r0:r0 + 128, :]), reads=[t_att_d], writes=[tATT])
        k.dma("sp", lambda e, r0=r0: e.dma_start(out=ZD[:, :], in_=zdt_d[r0:r0 + 128, :]), reads=[t_zdt_d], writes=[tZD])
        conv_silu(128, [xbc_d[r0 + w:r0 + w + 128, :] for w in range(4)])
        softplus_dt(128)
        k.op("dve", lambda e: e.tensor_tensor(out=DT[:, 8:16], in0=DT[:, 0:8], in1=AN[:, :], op=ALU.mult), reads=[tDT, tG2], writes=[tDT])
        k.op("dve", lambda e: e.tensor_tensor(out=RU[:, :, :], in0=C["U"][:, :].unsqueeze(1).to_broadcast([128, 8, 128]),
                                              in1=DT[:, 8:16].unsqueeze(2).to_broadcast([128, 8, 128]), op=ALU.mult), reads=[tDT, tC], writes=[tRU])
        k.op("pe", lambda e: e.matmul(PB[4][:, 0:8], lhsT=C["U"][:, :], rhs=DT[:, 8:16], start=True, stop=True), reads=[tDT, tC], writes=[tPB[4]])
        k.op("dve", lambda e: e.tensor_scalar_mul(out=DT[:, 16:24], in0=PB[4][:, 0:8], scalar1=-1.0), reads=[tPB[4]], writes=[tDT])
        for hf in range(2):
            k.op("pe", lambda e, hf=hf: e.matmul(PB[2 + hf][:, :], lhsT=C["ones_f"][:, :], rhs=RU[:, 4 * hf:4 * hf + 4, :], start=True, stop=True), reads=[tRU, tC], writes=[tPB[2 + hf]])
            k.op("dve", lambda e, hf=hf: e.tensor_tensor(out=SG[:, :, :], in0=PB[2 + hf][:, :].rearrange("p (h l) -> p h l", h=4),
                                                         in1=C["NB"][:, :].unsqueeze(1).to_broadcast([128, 4, 128]), op=ALU.add), reads=[tPB[2 + hf], tC], writes=[tSG])
            for hh in range(4):
                h = 4 * hf + hh
                k.op("act", lambda e, h=h, hh=hh: e.activation(out=DEC[:, h, :], in_=SG[:, hh, :], func=AF.Exp, bias=DT[:, 16 + h:17 + h]), reads=[tSG, tDT], writes=[tDEC])
            k.op("act", lambda e, hf=hf: e.activation(out=EX[:, 4 * hf:4 * hf + 4, :], in_=PB[2 + hf][:64, :].rearrange("p (h l) -> p h l", h=4), func=AF.Exp), reads=[tPB[2 + hf]], writes=[tEX])
        for j in range(4):
            k.op("pe", lambda e, j=j: e.transpose(PT[1][:64, j * 128:(j + 1) * 128], XSB[:, 512 + j * 64:576 + j * 64], C["ident_bf"][:, :]), reads=[tXS, tC], writes=[tPT[1]])
        k.op("act", lambda e: e.copy(out=BCT[:, :, :], in_=PT[1][:64, 0:512].rearrange("p (j t) -> p j t", j=4)), reads=[tPT[1]], writes=[tBCT])
        for g in range(2):
            k.op("pe", lambda e, g=g: e.matmul(PB[5][:, g * 128:(g + 1) * 128], lhsT=BCT[:, g, :], rhs=BCT[:, 2 + g, :], start=True, stop=True), reads=[tBCT], writes=[tPB[5]])
        k.op("act", lambda e: e.copy(out=CBs[:, :, :], in_=PB[5][:, 0:256].rearrange("p (g l) -> p g l", g=2)), reads=[tPB[5]], writes=[tCBs])
        k.op("dve", lambda e: e.tensor_tensor(out=DT[:, 0:8], in0=DT[:, 0:8], in1=DT[:, 0:8], op=ALU.max), reads=[tDT], writes=[tDT])
        k.op("dve", lambda e: e.tensor_tensor(out=SM[:, 40:48], in0=DEC[:, :, 127], in1=DT[:, 0:8], op=ALU.mult), reads=[tDEC, tDT], writes=[tSM])
        for h in range(8):
            g = h // 4; s = h % 2
            k.op("dve", lambda e, h=h, g=g, s=s: e.scalar_tensor_tensor(out=MTt[s][:, :], in0=DEC[:, h, :], scalar=DT[:, h:h + 1], in1=CBs[:, g, :], op0=ALU.mult, op1=ALU.mult),
                 reads=[tDEC, tDT, tCBs], writes=[tMT[s]])
            k.op("pool", lambda e, h=h, g=g, s=s: e.tensor_tensor(out=CEt[s][:, :], in0=BCT[:, 2 + g, :], in1=EX[:, h, :], op=ALU.mult), reads=[tBCT, tEX], writes=[tCE[s]])
            k.op("pe", lambda e, h=h, s=s: e.matmul(PB[0][:, h * 64:(h + 1) * 64], lhsT=MTt[s][:, :], rhs=XSB[:, h * 64:(h + 1) * 64], start=True, stop=False), reads=[tMT[s], tXS], writes=[tPB[0]])
            k.op("pe", lambda e, h=h, s=s: e.matmul(PB[0][:, h * 64:(h + 1) * 64], lhsT=CEt[s][:, :], rhs=HTB[:, h, :], start=False, stop=True), reads=[tCE[s], tHT], writes=[tPB[0]])
            k.op("dve", lambda e, h=h, g=g, s=s: e.tensor_scalar(out=BWt[s][:, :], in0=XS[:, 512 + g * 64:576 + g * 64], scalar1=SM[:, 40 + h:41 + h], scalar2=None, op0=ALU.mult), reads=[tXS, tSM], writes=[tBW[s]])
            k.op("pe", lambda e, h=h, s=s: e.matmul(PB[1][:64, h * 64:(h + 1) * 64], lhsT=BWt[s][:, :], rhs=XSB[:, h * 64:(h + 1) * 64], start=True, stop=True), reads=[tBW[s], tXS], writes=[tPB[1]])
        k.op("dve", lambda e: e.tensor_tensor(out=HT[:, :, :], in0=HT[:, :, :], in1=EX[:, :, 127:128].to_broadcast([64, 8, 64]), op=ALU.mult), reads=[tHT, tEX], writes=[tHT])
        k.op("dve", lambda e: e.tensor_tensor(out=HT[:, :, :], in0=HT[:, :, :], in1=PB[1][:64, :].rearrange("p (h q) -> p h q", h=8), op=ALU.add), reads=[tHT, tPB[1]], writes=[tHT])
        k.op("pool", lambda e: e.tensor_copy(out=HTB[:, :, :], in_=HT[:, :, :]), reads=[tHT], writes=[tHT])
        k.op("dve", lambda e: e.tensor_tensor(out=TMPc[:, :512], in0=XS[:, 0:512], in1=DSK[:, :], op=ALU.mult), reads=[tXS, tG2], writes=[tTMPc])
        k.op("dve", lambda e: e.tensor_tensor(out=YS[:, :], in0=TMPc[:, :512], in1=PB[0][:, :], op=ALU.add), reads=[tTMPc, tPB[0]], writes=[tYS])
        finish(128, r0, None)
    for h in range(8):
        k.op("pe", lambda e, h=h: e.transpose(PB[2][:64, h * 64:(h + 1) * 64], HT[:, h, :], C["ident_f"][:64, :64]), reads=[tHT, tC], writes=[tPB[2]])
    k.op("act", lambda e: e.copy(out=TMPc[:64, :512], in_=PB[2][:64, :]), reads=[tPB[2]], writes=[tTMPc])
    k.dma("sp", lambda e: e.dma_start(out=ssmp.rearrange("h p n -> p h n"), in_=TMPc[:64, :512].rearrange("p (h n) -> p h n", h=8)), reads=[tTMPc], writes=[otok()])

    T = TS
    load_mod(G1, TS, 2, tM2); load_mod(B2, TS, 3, tM2); load_mod(A2, TS, 4, tM2)
    k.dma("sp", lambda e: e.dma_start(out=XT[:T, :], in_=xs[:, :]), writes=[tXT])
    k.dma("sp", lambda e: e.dma_start(out=ATT[:T, :], in_=att_d[SEQ:SEQ + T, :]), reads=[t_att_d], writes=[tATT])
    k.dma("sp", lambda e: e.dma_start(out=ZD[:T, :], in_=zdt_d[SEQ:SEQ + T, :]), reads=[t_zdt_d], writes=[tZD])
    conv_silu(T, [xbcs_d[:, w:w + 4, :] for w in range(4)])
    softplus_dt(T)
    k.dma("sp", lambda e: e.dma_start(out=ssc_d[:, 0:768], in_=XS[:T, :]), reads=[tXS], writes=[t_ssc_d])
    k.dma("sp", lambda e: e.dma_start(out=ssc_d[:, 768:776], in_=DT[:T, 0:8]), reads=[tDT], writes=[t_ssc_d])
    Hs = k.sb("Hs", [128, 4096], F32); tHs = Tok()
    k.dma("sp", lambda e: e.dma_start(out=Hs[:, :], in_=sssm[:, :]), writes=[tHs])
    Xbh = k.sb("Xbh", [128, 4, 64], F32); Bbh = k.sb("Bbh", [128, 4, 64], F32); Cbh = k.sb("Cbh", [128, 4, 64], F32); Dbh = k.sb("Dbh", [128, 4], F32); tBH = Tok()
    for b in range(NSEQ):
        k.dma("sp", lambda e, b=b: e.dma_start(out=Xbh[8 * b:8 * b + 8, :, :], in_=ssc_d[4 * b:4 * b + 4, 0:512].rearrange("t (h p) -> h t p", p=64)), reads=[t_ssc_d], writes=[tBH])
        k.dma("sp", lambda e, b=b: e.dma_start(out=Dbh[8 * b:8 * b + 8, :], in_=ssc_d[4 * b:4 * b + 4, 768:776].rearrange("t h -> h t")), reads=[t_ssc_d], writes=[tBH])
        for g in range(2):
            k.dma("sp", lambda e, b=b, g=g: e.dma_start(out=Bbh[8 * b + 4 * g:8 * b + 4 * g + 4, :, :], in_=ssc_d[4 * b:4 * b + 4, 512 + 64 * g:576 + 64 * g].partition_broadcast(4)), reads=[t_ssc_d], writes=[tBH])
            k.dma("sp", lambda e, b=b, g=g: e.dma_start(out=Cbh[8 * b + 4 * g:8 * b + 4 * g + 4, :, :], in_=ssc_d[4 * b:4 * b + 4, 640 + 64 * g:704 + 64 * g].partition_broadcast(4)), reads=[t_ssc_d], writes=[tBH])
    OUTER = k.sb("OUTER", [128, 4096], F32); tOUT = Tok()
    Ybh = k.sb("Ybh", [128, 4, 64], F32); tY = Tok()
    SS = k.sb("SS", [128, 8], F32); XDT = k.sb("XDT", [128, 64], F32); tSS = Tok()
    for t in range(4):
        k.op("act", lambda e, t=t: e.activation(out=SS[:, 0:1], in_=Dbh[:, t:t + 1], func=AF.Exp, scale=ABH[:, 0:1]), reads=[tBH, tG2], writes=[tSS])
        k.op("dve", lambda e, t=t: e.tensor_scalar(out=XDT[:, :], in0=Xbh[:, t, :], scalar1=Dbh[:, t:t + 1], scalar2=None, op0=ALU.mult), reads=[tBH], writes=[tSS])
        k.op("dve", lambda e, t=t: e.tensor_tensor(out=OUTER[:, :].rearrange("p (a n) -> p a n", n=64), in0=XDT[:, :].unsqueeze(2).to_broadcast([128, 64, 64]),
                                                   in1=Bbh[:, t, :].unsqueeze(1).to_broadcast([128, 64, 64]), op=ALU.mult), reads=[tSS, tBH], writes=[tOUT])
        k.op("dve", lambda e: e.scalar_tensor_tensor(out=Hs[:, :], in0=Hs[:, :], scalar=SS[:, 0:1], in1=OUTER[:, :], op0=ALU.mult, op1=ALU.add), reads=[tHs, tSS, tOUT], writes=[tHs])
        k.op("dve", lambda e, t=t: e.tensor_tensor(out=OUTER[:, :].rearrange("p (a n) -> p a n", n=64), in0=Hs[:, :].rearrange("p (a n) -> p a n", n=64),
                                                   in1=Cbh[:, t, :].unsqueeze(1).to_broadcast([128, 64, 64]), op=ALU.mult), reads=[tHs, tBH], writes=[tOUT])
        k.op("dve", lambda e, t=t: e.tensor_reduce(out=Ybh[:, t, :], in_=OUTER[:, :].rearrange("p (a n) -> p a n", n=64), axis=AX.X, op=ALU.add), reads=[tOUT], writes=[tY])
        k.op("dve", lambda e, t=t: e.scalar_tensor_tensor(out=Ybh[:, t, :], in0=Xbh[:, t, :], scalar=DBH[:, 0:1], in1=Ybh[:, t, :], op0=ALU.mult, op1=ALU.add), reads=[tBH, tY, tG2], writes=[tY])
    k.dma("sp", lambda e: e.dma_start(out=ssms[:, :], in_=Hs[:, :]), reads=[tHs], writes=[otok()])
    for b in range(NSEQ):
        k.dma("sp", lambda e, b=b: e.dma_start(out=ysd_d[b, :, :].rearrange("t (h p) -> h t p", p=64), in_=Ybh[8 * b:8 * b + 8, :, :]), reads=[tY], writes=[t_ysd_d])
    k.dma("sp", lambda e: e.dma_start(out=YS[:T, :], in_=ysd_d.rearrange("b t c -> (b t) c")), reads=[t_ysd_d], writes=[tYS])
    finish(T, SEQ, None)
    k.pop()

    k.push()
    NG = [(g * 512, 512) for g in range(4)] + [(SEQ, TS)]
    H2A = k.sb("H2A", [128, 8, NTOK], BF16); tH2A = Tok()
    for kk in range(8):
        k.dma("sp", lambda e, kk=kk: e.dma_start(out=H2A[:, kk, :], in_=h2T_d[kk, :, :]), reads=[t_h2T_d], writes=[tH2A])
    NTL = 17
    MACC = k.sb("MACC", [128, NTL, D], F32); tMACC = [Tok() for _ in range(NTL)]
    CMB = k.sb("CMB", [128, NTL, 16], F32); tCMB = Tok()
    for j in range(NTL):
        T = 128 if j < 16 else TS
        k.dma("sp", lambda e, j=j, T=T: e.dma_start(out=CMB[:T, j, :], in_=comb_d[j * 128:j * 128 + T, :]), reads=[t_comb_d], writes=[tCMB])
    k.op("pool", lambda e: e.memset(MACC[:, :, :], 0.0), writes=tMACC)
    stm = [k.sb(f"stm{i}", [128, 8, 256], F32) for i in range(2)]; tstm = [Tok(), Tok()]
    WG = [k.sb(f"WG{i}", [128, 8, 256], BF16) for i in range(2)]; WU = [k.sb(f"WU{i}", [128, 8, 256], BF16) for i in range(2)]
    WD = [k.sb(f"WD{i}", [128, 2, D], BF16) for i in range(2)]; tW = [Tok(), Tok()]
    HE = k.sb("HE", [128, 2, 512], BF16); tHE = Tok()
    SG_ = k.sb("SGm", [128, 512], F32); tSGm = Tok()
    n_exp = 16 if with_moe else 0
    sidx = 0
    for ex in range(n_exp):
        s = ex % 2
        for (W_, src) in ((WG[s], w_gate), (WU[s], w_up)):
            ss = sidx % 2; sidx += 1
            k.dma("sp", lambda e, src=src, ss=ss, ex=ex: e.dma_start(out=stm[ss][:, :, :], in_=src[ex].rearrange("(k p) f -> p k f", p=128)), writes=[tstm[ss]])
            k.op("pool", lambda e, W_=W_, ss=ss: e.tensor_copy(out=W_[:, :, :], in_=stm[ss][:, :, :]), reads=[tstm[ss]], writes=[tW[s]])
        ss = sidx % 2; sidx += 1
        k.dma("sp", lambda e, ss=ss, ex=ex: e.dma_start(out=stm[ss][:, :, :].rearrange("p (c a) f -> p c (a f)", c=2), in_=w_down[ex].rearrange("(c p) n -> p c n", p=128)), writes=[tstm[ss]])
        k.op("pool", lambda e, s=s, ss=ss: e.tensor_copy(out=WD[s][:, :, :], in_=stm[ss][:, :, :].rearrange("p (c a) f -> p c (a f)", c=2)), reads=[tstm[ss]], writes=[tW[s]])
        for (t0, nt) in NG:
            for c in range(2):
                for (W_, pb) in ((WG[s], 0), (WU[s], 1)):
                    for kk in range(8):
                        k.op("pe", lambda e, W_=W_, pb=pb, kk=kk, c=c, t0=t0, nt=nt: e.matmul(PB[pb][:, :nt], lhsT=W_[:, kk, c * 128:(c + 1) * 128], rhs=H2A[:, kk, t0:t0 + nt], start=(kk == 0), stop=(kk == 7)),
                             reads=[tW[s], tH2A], writes=[tPB[pb]])
                k.op("act", lambda e, nt=nt: e.activation(out=SG_[:, :nt], in_=PB[0][:, :nt], func=AF.Silu), reads=[tPB[0]], writes=[tSGm])
                k.op("dve", lambda e, c=c, nt=nt: e.tensor_tensor(out=HE[:, c, :nt], in0=SG_[:, :nt], in1=PB[1][:, :nt], op=ALU.mult), reads=[tSGm, tPB[1]], writes=[tHE])
            for tt in range((nt + 127) // 128):
                T = min(128, nt - tt * 128); j = (t0 + tt * 128) // 128
                for hf in range(2):
                    pb = 2 + hf
                    for c in range(2):
                        k.op("pe", lambda e, pb=pb, c=c, tt=tt, T=T, hf=hf: e.matmul(PB[pb][:T, :], lhsT=HE[:, c, tt * 128:tt * 128 + T], rhs=WD[s][:, c, hf * 512:(hf + 1) * 512], start=(c == 0), stop=(c == 1)),
                             reads=[tHE, tW[s]], writes=[tPB[pb]])
                    k.op("dve", lambda e, pb=pb, T=T, j=j, hf=hf, ex=ex: e.scalar_tensor_tensor(out=MACC[:T, j, hf * 512:(hf + 1) * 512], in0=PB[pb][:T, :], scalar=CMB[:T, j, ex:ex + 1],
                                                                                         in1=MACC[:T, j, hf * 512:(hf + 1) * 512], op0=ALU.mult, op1=ALU.add), reads=[tPB[pb], tCMB, tMACC[j]], writes=[tMACC[j]])
    G2t = k.sb("G2t", [128, D], F32); tG2t = Tok()
    X1b = [k.sb(f"X1b{i}", [128, D], F32) for i in range(2)]; tX1b = [Tok(), Tok()]
    load_mod(G2t, 128, 5, tG2t)
    for j in range(NTL):
        T = 128 if j < 16 else TS
        if j == 16:
            load_mod(G2t, TS, 5, tG2t)
        s = j % 2
        k.dma("sp", lambda e, j=j, T=T, s=s: e.dma_start(out=X1b[s][:T, :], in_=x1_d[j * 128:j * 128 + T, :]), reads=[t_x1_d], writes=[tX1b[s]])
        k.op("dve", lambda e, j=j, T=T: e.tensor_tensor(out=MACC[:T, j, :], in0=MACC[:T, j, :], in1=G2t[:T, :], op=ALU.mult), reads=[tMACC[j], tG2t], writes=[tMACC[j]])
        k.op("pool", lambda e, j=j, T=T, s=s: e.tensor_tensor(out=X1b[s][:T, :], in0=X1b[s][:T, :], in1=MACC[:T, j, :], op=ALU.add), reads=[tMACC[j], tX1b[s]], writes=[tX1b[s]])
        dst = yp[j * 128:j * 128 + T, :] if j < 16 else ys[:, :]
        k.dma("sp", lambda e, T=T, s=s, dst=dst: e.dma_start(out=dst, in_=X1b[s][:T, :]), reads=[tX1b[s]], writes=[otok()])
    k.finish(out_toks)
    k.pop()
    k.es.close()
    return k


def _run(inp, debug=False, compact=False):
    f32 = lambda a: np.ascontiguousarray(np.asarray(a, dtype=np.float32))
    consts = make_consts()
    cshapes = {n: (list(v.shape), v.dtype != np.float32) for n, v in consts.items()}
    nc = bass.Bass("TRN2", target_bir_lowering=False)
    kb = build(nc, cshapes, debug=debug, pool_rows=(1024 * 128 if compact else 1310720))

    xp = f32(inp["x_prompt"]); xs = f32(inp["x_sample"])
    w_in = f32(inp["w_in"])[0]
    perm = np.concatenate([np.arange(0, 1304), np.arange(2584, 2592), np.arange(1304, 2584)])
    w_in_p = np.ascontiguousarray(w_in[:, perm])
    w_rg = f32(inp["w_rg"])[0]; w_re = f32(inp["w_re"])[0]
    w_r = np.ascontiguousarray(np.concatenate([w_rg] + [w_re[g] for g in range(4)], axis=1))
    b_r = np.ascontiguousarray(np.concatenate([f32(inp["b_rg"])[0], f32(inp["b_re"])[0].reshape(-1)]))
    wpk = f32(inp["w_pos_k"])[0]; wpv = f32(inp["w_pos_v"])[0]
    wkblk = np.zeros((128, 4), np.float32); wvsel = np.zeros((128, 16, 64), np.float32)
    for r in range(128):
        wkblk[r, r // 32] = wpk[r % 32]
        for i in range(16):
            wvsel[r, i, 4 * i + r // 32] = wpv[r % 32]
    wk32 = np.zeros((128, 32, 128), np.float32); wv32 = np.zeros((128, 32, 128), np.float32)
    for p_ in range(128):
        ps, rg = p_ // 32, p_ % 32
        for d_ in range(8):
            for i_ in range(4):
                m_ = 16 * d_ + ps * 4 + rg // 8
                wk32[p_, d_ * 4 + i_, m_] = wpk[4 * (rg % 8) + i_]
                wv32[p_, d_ * 4 + i_, m_] = wpv[4 * (rg % 8) + i_]
    shared = {
        "g_norm1": f32(inp["g_norm1"])[0], "g_norm2": f32(inp["g_norm2"])[0],
        "w_ada": f32(inp["w_ada"])[0], "b_ada": f32(inp["b_ada"])[0], "w_in": w_in_p,
        "gq8": np.tile(f32(inp["g_q"])[0], 8), "gks2": np.tile(f32(inp["g_k_sel"])[0], 2), "gkw2": np.tile(f32(inp["g_k_win"])[0], 2),
        "gkc": f32(inp["g_k_cmp"])[0].reshape(64, 1),
        "convw": f32(inp["conv_w"])[0].reshape(-1), "convb": f32(inp["conv_b"])[0],
        "dtb": f32(inp["dt_bias"])[0], "alog": f32(inp["a_log"])[0], "abh": np.tile(f32(inp["a_log"])[0], 16).reshape(128, 1), "dbh": np.tile(f32(inp["d_skip"])[0], 16).reshape(128, 1), "dsk": np.repeat(f32(inp["d_skip"])[0], 64),
        "gatt": f32(inp["g_att_out"])[0], "gssm": f32(inp["g_ssm_out"])[0],
        "w_out": f32(inp["w_out"])[0], "w_r": w_r, "b_r": b_r,
        "w_gate": f32(inp["w_gate"])[0], "w_up": f32(inp["w_up"])[0], "w_down": f32(inp["w_down"])[0],
        "wkblk": wkblk, "wvsel": wvsel.reshape(128, 1024),
        "wk32": wk32.reshape(128, 4096), "wv32": wv32.reshape(128, 4096), "gkc2": np.tile(f32(inp["g_k_cmp"])[0], 2),
    }
    for n, v in consts.items():
        shared["c_" + n] = v
    cwin = f32(inp["cache_win"])[0].reshape(128, 512, 256)
    sssm = f32(inp["state_ssm"])[0].reshape(128 * 8, 4096)
    sconv = f32(inp["state_conv"])[0]
    ccmp = np.asarray(inp["cache_cmp"], dtype=np.float32).reshape(10240 * 128, 256)
    csel = np.asarray(inp["cache_sel"], dtype=np.float32).reshape(10240 * 128, 256)
    ptab = np.ascontiguousarray(np.asarray(inp["page_table"], dtype=np.int32))
    in_maps = []
    for c in range(NCORES):
        m = dict(shared)
        if compact:
            pg = ptab[16 * c:16 * c + 16].reshape(-1)
            m["ccmp"] = ccmp.reshape(10240, 128 * 256)[pg].reshape(-1, 256); m["csel"] = csel.reshape(10240, 128 * 256)[pg].reshape(-1, 256)
            m["ptab"] = np.arange(1024, dtype=np.int32).reshape(16, 64)
        else:
            m["ccmp"] = ccmp; m["csel"] = csel; m["ptab"] = ptab[16 * c:16 * c + 16]
        m["xp"] = xp[c]; m["xs"] = xs[16 * c:16 * c + 16].reshape(TS, D)
        m["cp"] = f32(inp["c_prompt"])[c:c + 1]; m["cs"] = f32(inp["c_sample"])[16 * c:16 * c + 16]
        m["sssm"] = sssm[128 * c:128 * c + 128]; m["sconv"] = sconv[16 * c:16 * c + 16]; m["cwin"] = cwin[16 * c:16 * c + 16]
        in_maps.append(m)
    res = run_bass_kernel_spmd(nc, in_maps, core_ids=list(range(NCORES)))
    R = res.results
    cat = lambda n: np.stack([R[c][n] for c in range(NCORES)])
    y_p = cat("yp"); y_s = cat("ys").reshape(128, 4, D)
    cmp_p = cat("cmpp").reshape(1, 8, SEQ, 2, 2, 64); cmp_s = cat("cmps").reshape(1, 128, 4, 2, 2, 64)
    sel_p = cat("selp").reshape(1, 8, SEQ, 2, 2, 64); sel_s = cat("sels").reshape(1, 128, 4, 2, 2, 64)
    win_p = cat("winp").reshape(1, 8, 512, 2, 2, 64); win_s = cat("wins").reshape(1, 128, 512, 2, 2, 64)
    ssm_p = cat("ssmp").reshape(1, 8, 8, 64, 64); ssm_s = cat("ssms").reshape(1, 128, 8, 64, 64)
    conv_p = cat("convp").reshape(1, 8, 3, 768); conv_s = cat("convs").reshape(1, 128, 3, 768)
    outs = (y_p, y_s, cmp_p, cmp_s, sel_p, sel_s, win_p, win_s, ssm_p, ssm_s, conv_p, conv_s)
    if debug:
        return outs, {n: cat(n) for n in ("att_d", "x1_d", "comb_d")}
    return outs


def kernel(**inp):
    return _run(inp, False)
```

```python
import numpy as np
import ml_dtypes
from contextlib import ExitStack
import concourse.bass as bass
import concourse.mybir as mybir
from concourse.bass_utils import run_bass_kernel_spmd

F32 = mybir.dt.float32; BF16 = mybir.dt.bfloat16; I32 = mybir.dt.int32
AF = mybir.ActivationFunctionType; ALU = mybir.AluOpType; AX = mybir.AxisListType
NCORES = 8
SEQ = 2048; D = 1024; NT = 16; TS = 64; NSEQ = 16
INW = 2592
SCALE = 0.125
EPS = 1e-6
NEGB = -30000.0


class Tok:
    __slots__ = ("name", "w", "rs")

    def __init__(self, name="t"):
        self.name = name; self.w = None; self.rs = []


class KB:
    NSLOT = 8

    def __init__(self, nc):
        self.nc = nc; self.es = ExitStack(); self.stack = [self.es]
        self.eng = {"pe": nc.tensor, "act": nc.scalar, "dve": nc.vector, "pool": nc.gpsimd, "sp": nc.sync}
        self.sem = {k: self.es.enter_context(nc.semaphore("s_" + k)) for k in self.eng}
        self.cnt = {k: 0 for k in self.eng}
        self.seen = {k: {} for k in self.eng}
        self.dsem = {}; self.dcnt = {}; self.dnext = {}
        self.nslot = {"sp": 8, "act": 2, "pool": 16}
        for q in ("sp", "act", "pool"):
            self.dsem[q] = [self.es.enter_context(nc.semaphore(f"d_{q}{i}")) for i in range(self.nslot[q])]
            self.dcnt[q] = [0] * self.nslot[q]; self.dnext[q] = 0
        self.nins = 0

    def sb(self, name, shape, dt):
        return self.stack[-1].enter_context(self.nc.sbuf_tensor(name, list(shape), dt))

    def ps(self, name, shape, dt):
        return self.es.enter_context(self.nc.psum_tensor(name, list(shape), dt))

    def _wait(self, e, ev):
        if ev is None:
            return
        key, val = ev
        if self.seen[e].get(key, 0) >= val:
            return
        self.seen[e][key] = val
        sem = self.sem[key[1]] if key[0] == "c" else self.dsem[key[1]][key[2]]
        self.eng[e].wait_ge(sem, val)

    def _deps(self, e, reads, writes):
        for t in reads:
            self._wait(e, t.w)
        for t in writes:
            self._wait(e, t.w)
            for r in t.rs:
                self._wait(e, r)

    def _commit(self, ev, reads, writes):
        for t in reads:
            t.rs = [r for r in t.rs if r[0] != ev[0]] + [ev]
        for t in writes:
            t.w = ev; t.rs = []

    def op(self, e, fn, reads=(), writes=()):
        self._deps(e, reads, writes)
        ins = fn(self.eng[e])
        self.cnt[e] += 1
        ins.then_inc(self.sem[e], 1)
        ev = (("c", e), self.cnt[e])
        self._commit(ev, reads, writes); self.nins += 1
        return ins

    def dma(self, q, fn, reads=(), writes=()):
        s = self.dnext[q]; self.dnext[q] = (s + 1) % self.nslot[q]
        key = ("d", q, s)
        if self.dcnt[q][s] > 0:
            self._wait(q, (key, self.dcnt[q][s]))
        self._deps(q, reads, writes)
        ins = fn(self.eng[q])
        self.dcnt[q][s] += 16
        ins.then_inc(self.dsem[q][s], 16)
        ev = (key, self.dcnt[q][s])
        self._commit(ev, reads, writes); self.nins += 1
        return ins

    def push(self):
        self.stack.append(ExitStack())

    def barrier(self):
        for e in self.eng:
            for e2 in self.eng:
                if e2 != e and self.cnt[e2] > 0:
                    self._wait(e, (("c", e2), self.cnt[e2]))
            for q in self.dsem:
                for s in range(self.nslot[q]):
                    if self.dcnt[q][s] > 0:
                        self._wait(e, (("d", q, s), self.dcnt[q][s]))

    def pop(self):
        self.barrier()
        self.stack.pop().close()

    def finish(self, toks):
        for t in toks:
            self._wait("sp", t.w)
            for r in t.rs:
                self._wait("sp", r)


def _bf(a):
    return np.ascontiguousarray(a.astype(np.float32)).astype(ml_dtypes.bfloat16)


def make_consts():
    c = {}
    c["ident_bf"] = _bf(np.eye(128))
    c["ident_f"] = np.eye(128, dtype=np.float32)
    tk = np.arange(128)[:, None]; tq = np.arange(128)[None, :]
    cb = np.where(tk <= tq, 0.0, NEGB)
    c["causalb"] = _bf(np.tile(cb, (1, 4)))
    ab = np.where(tk > tq, 0.0, NEGB)
    c["antib"] = _bf(np.tile(ab, (1, 4)))
    E = np.zeros((32, 16, 128), np.float32)
    for cc in range(16):
        for t in range(128):
            E[2 * cc + t // 64, cc, t] = 1.0
    c["Eall"] = _bf(E.reshape(32, 16 * 128))
    Dsel = np.zeros((4, 124), np.float32)
    for jj in range(4):
        Dsel[jj, 60 + jj] = 1.0
    c["Dsel"] = _bf(Dsel)
    p = np.arange(128)[None, :]; jj = np.arange(4)[:, None]
    cmpB = np.where(32 * jj + 31 <= p, 0.0, NEGB)
    c["cmpB"] = _bf(np.tile(cmpB, (1, 4)))
    pc = np.arange(128)[:, None]; cc = np.arange(124)[None, :]
    c["cmpM0"] = ((cc - 60) <= np.floor((pc - 31) / 32.0)).astype(np.float32)
    selA = np.zeros((128, 16, 32), np.float32); selB = np.zeros((128, 16, 32), np.float32)
    allowed = np.zeros((128, 16, 32), np.float32)
    for i in range(16):
        for pp in range(128):
            cur = 2 * i + (1 if pp >= 64 else 0)
            for n in range(32):
                if n < cur:
                    allowed[pp, i, n] = 1.0
                    if n == cur - 1:
                        selB[pp, i, n] = 2e9
                    elif n == 0:
                        selB[pp, i, n] = 1e9
                    else:
                        selA[pp, i, n] = 1.0
                else:
                    selB[pp, i, n] = -1e30
    c["selA"] = selA.reshape(128, 512); c["selB"] = selB.reshape(128, 512); c["allowed"] = allowed.reshape(128, 512)
    s = np.arange(128)[:, None]; l = np.arange(128)[None, :]
    c["U"] = (s <= l).astype(np.float32)
    c["NB"] = np.where(l >= s, 0.0, -1e30).astype(np.float32)
    c["ones_f"] = np.ones((128, 128), np.float32)
    pp = np.arange(128)
    c["PM4"] = (pp % 4).astype(np.float32).reshape(128, 1)
    c["R64"] = (pp % 64).astype(np.float32).reshape(128, 1)
    H = np.zeros((32, 8), np.float32)
    for g in range(2):
        for t in range(4):
            for q in range(4):
                H[g * 16 + t * 4 + q, g * 4 + t] = 1.0
    c["HSEL"] = H
    B = np.zeros((128, 128), np.float32); B[:, 0] = 1e9; B[:, 127] = 2e9
    HB_ = np.zeros((32, 16, 128), np.float32)
    for b_ in range(16):
        for g in range(2):
            for t in range(4):
                for q in range(4):
                    HB_[g * 16 + t * 4 + q, b_, b_ * 8 + g * 4 + t] = 1.0
    c["HSELB"] = HB_.reshape(32, 2048)
    c["BIGS"] = B
    c["I8"] = np.eye(8, dtype=np.float32)
    E16 = np.zeros((16, 128), np.float32)
    for p_ in range(128):
        E16[p_ // 8, p_] = 1.0
    c["EXP16"] = E16
    c["PM8"] = (pp % 8).astype(np.float32).reshape(128, 1)
    c["PIDX"] = pp.astype(np.float32).reshape(128, 1)
    c["PM32"] = (pp % 32).astype(np.float32).reshape(128, 1)
    c["OH4"] = (pp[:, None] // 32 == np.arange(4)[None, :]).astype(np.float32)
    c["M120"] = (pp < 120).astype(np.float32).reshape(128, 1)
    m15 = np.ones((128, 8), np.float32); m15[64:, 7] = 0.0
    c["MASK15"] = m15
    c["MASKW"] = (pp[:, None] > np.arange(4)[None, :]).astype(np.float32)
    c["CM4"] = (np.arange(4)[:, None] <= np.arange(4)[None, :]).astype(np.float32)
    return c


def build(nc, cshapes, with_moe=True, debug=False, pool_rows=1310720):
    k = KB(nc)
    k.es.enter_context(nc.allow_non_contiguous_dma(reason="small strided loads"))

    def din(name, shape, dt=F32):
        return nc.dram_tensor(name, list(shape), dt, kind="ExternalInput").ap()

    def dout(name, shape, dt=F32):
        return nc.dram_tensor(name, list(shape), dt, kind="ExternalOutput").ap()

    def dint(name, shape, dt=F32):
        kind = "ExternalOutput" if (debug and name in ("att_d", "x1_d", "comb_d")) else "Internal"
        return nc.dram_tensor(name, list(shape), dt, kind=kind).ap()

    xp = din("xp", [SEQ, D]); xs = din("xs", [TS, D]); cpr = din("cp", [1, D]); csm = din("cs", [NSEQ, D])
    sssm = din("sssm", [128, 4096]); sconv = din("sconv", [NSEQ, 3, 768]); cwin = din("cwin", [NSEQ, 512, 256])
    gn1 = din("g_norm1", [D]); gn2 = din("g_norm2", [D])
    w_ada = din("w_ada", [D, 6 * D]); b_ada = din("b_ada", [6 * D])
    w_in = din("w_in", [D, INW])
    gq8 = din("gq8", [512]); gks2 = din("gks2", [128]); gkw2 = din("gkw2", [128]); gkc = din("gkc", [64, 1])
    convw = din("convw", [4 * 768]); convb = din("convb", [768])
    dtb = din("dtb", [8]); alog = din("alog", [8]); dsk = din("dsk", [512])
    abh = din("abh", [128, 1]); dbh = din("dbh", [128, 1])
    gatt = din("gatt", [512]); gssm = din("gssm", [512])
    w_out = din("w_out", [D, D]); w_r = din("w_r", [D, 20]); b_r = din("b_r", [20])
    w_gate = din("w_gate", [16, D, 256]); w_up = din("w_up", [16, D, 256]); w_down = din("w_down", [16, 256, D])
    wkblk = din("wkblk", [128, 4]); wvsel = din("wvsel", [128, 16 * 64])
    ccmp = din("ccmp", [pool_rows, 256]); csel = din("csel", [pool_rows, 256]); ptab = din("ptab", [NSEQ, 64], I32)
    wk32 = din("wk32", [128, 4096]); wv32 = din("wv32", [128, 4096]); gkc2 = din("gkc2", [128])
    cd = {}
    for n, (shp, isbf) in cshapes.items():
        cd[n] = din("c_" + n, shp, BF16 if isbf else F32)

    yp = dout("yp", [SEQ, D]); ys = dout("ys", [TS, D])
    cmpp = dout("cmpp", [SEQ, 256]); cmps = dout("cmps", [TS, 256])
    selp = dout("selp", [SEQ, 256]); sels = dout("sels", [TS, 256])
    winp = dout("winp", [512, 256]); wins = dout("wins", [NSEQ, 512, 256])
    ssmp = dout("ssmp", [8, 64, 64]); ssms = dout("ssms", [128, 4096])
    convp = dout("convp", [3, 768]); convs = dout("convs", [NSEQ, 3, 768])

    NTOK = SEQ + TS
    mods_d = dint("mods_d", [65, 6 * D]); t_mods_d = Tok()
    xbc_d = dint("xbc_d", [SEQ + 3, 768]); t_xbc_d = Tok()
    xbcs_d = dint("xbcs_d", [NSEQ, 7, 768]); t_xbcs_d = Tok()
    zdt_d = dint("zdt_d", [NTOK, 520]); t_zdt_d = Tok()
    att_d = dint("att_d", [NTOK, 512]); t_att_d = Tok()
    x1_d = dint("x1_d", [NTOK, D]); t_x1_d = Tok()
    h2T_d = dint("h2T_d", [8, 128, NTOK], BF16); t_h2T_d = Tok()
    comb_d = dint("comb_d", [NTOK, 16]); t_comb_d = Tok()
    ssc_d = dint("ssc_d", [TS, 776]); t_ssc_d = Tok()
    ysd_d = dint("ysd_d", [NSEQ, 4, 512]); t_ysd_d = Tok()
    qs_d = dint("qs_d", [TS, 512], BF16); t_qs_d = Tok()
    kvs_d = dint("kvs_d", [TS, 512]); t_kvs_d = Tok()
    gts_d = dint("gts_d", [TS, 24]); t_gts_d = Tok()
    atts_d = dint("atts_d", [3, NSEQ, 4, 2, 4, 64]); t_atts_d = Tok()
    out_toks = []

    def otok():
        t = Tok(); out_toks.append(t); return t

    C = {}; tC = Tok("consts")
    for n, (shp, isbf) in cshapes.items():
        C[n] = k.sb("C_" + n, shp, BF16 if isbf else F32)
        k.dma("sp", lambda e, n=n: e.dma_start(out=C[n][:], in_=cd[n]), writes=[tC])
    EPSC = k.sb("EPSC", [128, 1], F32)
    k.op("dve", lambda e: e.memset(EPSC[:], EPS), writes=[tC])
    SM = k.sb("SM", [128, 64], F32); tSM = Tok()
    PB = [k.es.enter_context(nc.psum_tensor(f"pb{i}", [128, 512], F32)) for i in range(6)]
    tPB = [Tok(f"pb{i}") for i in range(6)]
    PT = [k.es.enter_context(nc.psum_tensor(f"pt{i}", [128, 1024], BF16)) for i in range(2)]
    tPT = [Tok(f"pt{i}") for i in range(2)]

    def bc_load(name, src1d, width, parts=128, tok=None):
        t = k.sb(name, [parts, width], F32)
        k.dma("sp", lambda e: e.dma_start(out=t[:], in_=src1d.partition_broadcast(parts)), writes=[tok or tC])
        return t

    def rstd_of(ap, T, scale):
        k.op("act", lambda e: e.activation(out=ap, in_=ap, func=AF.Sqrt, bias=EPSC[:T, :], scale=scale), reads=[tSM, tC], writes=[tSM])
        k.op("dve", lambda e: e.reciprocal(out=ap, in_=ap), reads=[tSM], writes=[tSM])

    k.push()
    GN1 = bc_load("GN1", gn1, D, 65); GN2 = bc_load("GN2", gn2, D, 65)
    stg = [k.sb(f"stg{i}", [128, 512], F32) for i in range(2)]; tstg = [Tok(), Tok()]
    cT = k.sb("cT", [128, 8, 65], F32); tcT = Tok()
    k.dma("sp", lambda e: e.dma_start(out=cT[:, :, 0:1], in_=cpr.rearrange("b (k p) -> p k b", p=128)), writes=[tcT])
    for kk in range(8):
        k.dma("sp", lambda e, kk=kk: e.dma_start(out=cT[:, kk, 1:17], in_=csm[:, kk * 128:(kk + 1) * 128].rearrange("b p -> p b")), writes=[tcT])
    scT = k.sb("scT", [128, 8, 17], F32)
    k.op("act", lambda e: e.activation(out=scT[:], in_=cT[:, :, 0:17], func=AF.Silu), reads=[tcT], writes=[tcT])
    L = k.sb("L", [128, 8, 65], BF16)
    k.op("dve", lambda e: e.tensor_copy(out=L[:, :, 0:1], in_=scT[:, :, 0:1]), reads=[tcT], writes=[tcT])
    k.op("dve", lambda e: e.tensor_copy(out=L[:, :, 1:65].rearrange("p k (b t) -> p k b t", t=4),
                                        in_=scT[:, :, 1:17].unsqueeze(3).to_broadcast([128, 8, NSEQ, 4])), reads=[tcT], writes=[tcT])
    wab = [k.sb(f"wab{i}", [128, 8, 512], BF16) for i in range(2)]; twab = [Tok(), Tok()]
    bab = [k.sb(f"bab{i}", [65, 512], F32) for i in range(2)]; tbab = [Tok(), Tok()]
    M65 = k.sb("M65", [65, 6 * D], F32); tM65 = Tok()
    for cg in range(12):
        s = cg % 2
        for kk in range(8):
            ss = kk % 2
            k.dma("sp", lambda e, kk=kk, ss=ss, cg=cg: e.dma_start(out=stg[ss][:, :], in_=w_ada[kk * 128:(kk + 1) * 128, cg * 512:(cg + 1) * 512]), writes=[tstg[ss]])
            k.op("pool", lambda e, kk=kk, ss=ss, s=s: e.tensor_copy(out=wab[s][:, kk, :], in_=stg[ss][:, :]), reads=[tstg[ss]], writes=[twab[s]])
        k.dma("sp", lambda e, s=s, cg=cg: e.dma_start(out=bab[s][:], in_=b_ada[cg * 512:(cg + 1) * 512].partition_broadcast(65)), writes=[tbab[s]])
        for kk in range(8):
            k.op("pe", lambda e, kk=kk, s=s: e.matmul(PB[s][:65, :], lhsT=L[:, kk, :], rhs=wab[s][:, kk, :], start=(kk == 0), stop=(kk == 7)),
                 reads=[tcT, twab[s]], writes=[tPB[s]])
        k.op("dve", lambda e, s=s, cg=cg: e.tensor_tensor(out=M65[:, cg * 512:(cg + 1) * 512], in0=PB[s][:65, :], in1=bab[s][:, :], op=ALU.add),
             reads=[tPB[s], tbab[s]], writes=[tM65])
    for (sl, G) in ((1, GN1), (4, GN2)):
        k.op("dve", lambda e, sl=sl, G=G: e.scalar_tensor_tensor(out=M65[:, sl * D:(sl + 1) * D], in0=M65[:, sl * D:(sl + 1) * D], scalar=1.0, in1=G[:, :], op0=ALU.add, op1=ALU.mult),
             reads=[tM65, tC], writes=[tM65])
    k.dma("sp", lambda e: e.dma_start(out=mods_d[:, :], in_=M65[:, :]), reads=[tM65], writes=[t_mods_d])
    k.pop()

    def load_mod(tile, T, slot, tok):
        if T == 128:
            k.dma("sp", lambda e: e.dma_start(out=tile[:, :], in_=mods_d[0, slot * D:(slot + 1) * D].partition_broadcast(128)), reads=[t_mods_d], writes=[tok])
        else:
            k.dma("sp", lambda e: e.dma_start(out=tile[:TS, :], in_=mods_d[1:65, slot * D:(slot + 1) * D]), reads=[t_mods_d], writes=[tok])

    k.push()
    tG = Tok("gains1")
    GQ = bc_load("GQ", gq8, 512, tok=tG); GKS = bc_load("GKS", gks2, 128, tok=tG); GKW = bc_load("GKW", gkw2, 128, tok=tG)
    GKC = k.sb("GKC", [64, 1], F32)
    k.dma("sp", lambda e: e.dma_start(out=GKC[:], in_=gkc), writes=[tG])
    WKB = k.sb("WKB", [128, 4], F32); WVS = k.sb("WVS", [128, 16 * 64], F32)
    k.dma("sp", lambda e: e.dma_start(out=WKB[:], in_=wkblk), writes=[tG])
    k.dma("sp", lambda e: e.dma_start(out=WVS[:], in_=wvsel), writes=[tG])
    stgw = [k.sb(f"stgw{i}", [128, INW], F32) for i in range(2)]; tstgw = [Tok(), Tok()]
    WIN = k.sb("WIN", [128, 8, INW], BF16); tWIN = Tok()
    for kk in range(8):
        s = kk % 2
        k.dma("sp", lambda e, kk=kk, s=s: e.dma_start(out=stgw[s][:], in_=w_in[kk * 128:(kk + 1) * 128, :]), writes=[tstgw[s]])
        k.op("pool", lambda e, kk=kk, s=s: e.tensor_copy(out=WIN[:, kk, :], in_=stgw[s][:]), reads=[tstgw[s]], writes=[tWIN])
    A1 = k.sb("A1", [128, D], F32); B1 = k.sb("B1", [128, D], F32); tM1 = Tok()
    KST = k.sb("KST", [64, 2, SEQ], BF16); KWT = k.sb("KWT", [64, 2, SEQ], BF16)
    VS = k.sb("VS", [128, NT, 2, 65], BF16); VW = k.sb("VW", [128, NT, 2, 65], BF16)
    KCT = k.sb("KCT", [64, 2, 64], BF16); VCA = k.sb("VCA", [64, 128], F32); VC = k.sb("VC", [64, 2, 65], BF16)
    tKS = [Tok() for _ in range(NT)]; tKW = [Tok() for _ in range(NT)]; tVS = [Tok() for _ in range(NT)]; tVW = [Tok() for _ in range(NT)]
    tKC = Tok(); tVC = Tok()
    k.op("pool", lambda e: e.memset(VS[:], 1.0), writes=tVS)
    k.op("pool", lambda e: e.memset(VW[:], 1.0), writes=tVW)
    k.op("pool", lambda e: e.memset(VC[:], 1.0), writes=[tVC])
    k.op("pool", lambda e: e.memset(KCT[:], 0.0), writes=[tKC])
    k.op("pool", lambda e: e.memset(VCA[:], 0.0), writes=[tVC])
    ZR = k.sb("ZR", [3, 768], F32); tZR = Tok()
    k.op("pool", lambda e: e.memset(ZR[:], 0.0), writes=[tZR])
    k.dma("sp", lambda e: e.dma_start(out=xbc_d[0:3, :], in_=ZR[:]), reads=[tZR], writes=[t_xbc_d])

    XT = k.sb("XT", [128, D], F32); tXT = Tok()
    TMP = k.sb("TMP", [128, D], F32); tTMP = Tok()
    HB = k.sb("HB", [128, D], BF16); tHB = Tok()
    HTt = k.sb("HTt", [128, 8, 128], BF16); tHTt = Tok()
    Ut = k.sb("Ut", [128, INW], F32); tU = Tok()
    QN = k.sb("QN", [128, 512], BF16); tQN = Tok()
    QT = k.sb("QT", [64, 8, 128], BF16); tQT = Tok()
    SELO = k.sb("SELO", [128, 256], F32); tSELO = Tok()
    WINO = k.sb("WINO", [128, 256], F32); tWINO = Tok()
    KNB = k.sb("KNB", [128, 256], BF16); tKNB = Tok()
    ATT = k.sb("ATT", [128, 512], F32); tATT = Tok()
    PTt = [k.sb(f"PTt{i}", [128, 512], BF16) for i in range(3)]; tPTt = [Tok(), Tok(), Tok()]
    GT = k.sb("GT", [128, 24], F32); tGT = Tok()
    SE = k.sb("SE", [128, 256], F32); tSE = Tok()
    S2 = k.sb("S2", [128, 64], F32); IMP = k.sb("IMP", [128, 32], F32); SC = k.sb("SC", [128, 32], F32); SC2 = k.sb("SC2", [128, 32], F32)
    M1 = k.sb("M1", [128, 8], F32); M2 = k.sb("M2", [128, 8], F32); NMB = k.sb("NMB", [128, 32], BF16); tSEL = Tok()
    NMT = k.sb("NMT", [32, 4, 128], BF16); tNMT = Tok()
    KR = k.sb("KR", [64, 16], F32); tKR = Tok()
    COEF = k.sb("COEF", [128, 8], F32); tCOEF = Tok()
    OT = k.sb("OT", [128, 256], F32); tOT = Tok()

    def headnorm(T, src, nh, gain, out32, outbf, rd, wr, col):
        w = nh * 64
        k.op("dve", lambda e: e.tensor_tensor(out=TMP[:T, :w], in0=src, in1=src, op=ALU.mult), reads=rd, writes=[tTMP])
        k.op("dve", lambda e: e.tensor_reduce(out=SM[:T, col:col + nh], in_=TMP[:T, :w].rearrange("p (h d) -> p h d", d=64), axis=AX.X, op=ALU.add), reads=[tTMP], writes=[tSM])
        rstd_of(SM[:T, col:col + nh], T, 1.0 / 64)
        k.op("dve", lambda e: e.tensor_tensor(out=TMP[:T, :w].rearrange("p (h d) -> p h d", d=64), in0=src.rearrange("p (h d) -> p h d", d=64),
                                              in1=SM[:T, col:col + nh].unsqueeze(2).to_broadcast([T, nh, 64]), op=ALU.mult), reads=rd + [tSM], writes=[tTMP])
        if out32 is not None:
            k.op("dve", lambda e: e.tensor_tensor(out=out32, in0=TMP[:T, :w], in1=gain, op=ALU.mult), reads=[tTMP, tG], writes=wr)
            k.op("act", lambda e: e.copy(out=outbf, in_=out32), reads=wr, writes=[tKNB])
        else:
            k.op("dve", lambda e: e.tensor_tensor(out=outbf, in0=TMP[:T, :w], in1=gain, op=ALU.mult), reads=[tTMP, tG], writes=wr)

    def front(T, xsrc, cmp_o, sel_o, xbc_dst, zrow0):
        k.dma("sp", lambda e: e.dma_start(out=XT[:T, :], in_=xsrc), writes=[tXT])
        k.op("act", lambda e: e.activation(out=HB[:T, :], in_=XT[:T, :], func=AF.Square, accum_out=SM[:T, 0:1]), reads=[tXT], writes=[tHB, tSM])
        rstd_of(SM[:T, 0:1], T, 1.0 / D)
        k.op("dve", lambda e: e.scalar_tensor_tensor(out=TMP[:T, :], in0=XT[:T, :], scalar=SM[:T, 0:1], in1=A1[:T, :], op0=ALU.mult, op1=ALU.mult),
             reads=[tXT, tSM, tM1], writes=[tTMP])
        k.op("dve", lambda e: e.tensor_tensor(out=HB[:T, :], in0=TMP[:T, :], in1=B1[:T, :], op=ALU.add), reads=[tTMP, tM1], writes=[tHB])
        for kk in range(8):
            k.op("pe", lambda e, kk=kk: e.transpose(PT[0][:, kk * 128:kk * 128 + T], HB[:T, kk * 128:(kk + 1) * 128], C["ident_bf"][:T, :T]), reads=[tHB, tC], writes=[tPT[0]])
        k.op("act", lambda e: e.copy(out=HTt[:, :, :T], in_=PT[0][:, :].rearrange("p (k t) -> p k t", k=8)[:, :, :T]), reads=[tPT[0]], writes=[tHTt])
        groups = [(0, 512), (512, 512), (1024, 288), (1312, 512), (1824, 512), (2336, 256)]
        for gi, (c0, w) in enumerate(groups):
            pb = gi % 2
            for kk in range(8):
                k.op("pe", lambda e, kk=kk, pb=pb, c0=c0, w=w: e.matmul(PB[pb][:T, :w], lhsT=HTt[:, kk, :T], rhs=WIN[:, kk, c0:c0 + w], start=(kk == 0), stop=(kk == 7)),
                     reads=[tHTt, tWIN], writes=[tPB[pb]])
            if gi % 2 == 0:
                k.op("act", lambda e, pb=pb, c0=c0, w=w: e.copy(out=Ut[:T, c0:c0 + w], in_=PB[pb][:T, :w]), reads=[tPB[pb]], writes=[tU])
            else:
                k.op("dve", lambda e, pb=pb, c0=c0, w=w: e.tensor_copy(out=Ut[:T, c0:c0 + w], in_=PB[pb][:T, :w]), reads=[tPB[pb]], writes=[tU])
        k.dma("sp", lambda e: e.dma_start(out=cmp_o, in_=Ut[:T, 512:768]), reads=[tU], writes=[otok()])
        if T == 128:
            k.dma("sp", lambda e: e.dma_start(out=xbc_dst, in_=Ut[:T, 1824:2592]), reads=[tU], writes=[t_xbc_d])
        else:
            for b in range(NSEQ):
                k.dma("sp", lambda e, b=b: e.dma_start(out=xbc_dst[b, :, :], in_=Ut[4 * b:4 * b + 4, 1824:2592]), reads=[tU], writes=[t_xbcs_d])
        k.dma("sp", lambda e: e.dma_start(out=zdt_d[zrow0:zrow0 + T, 0:512], in_=Ut[:T, 1312:1824]), reads=[tU], writes=[t_zdt_d])
        k.dma("sp", lambda e: e.dma_start(out=zdt_d[zrow0:zrow0 + T, 512:520], in_=Ut[:T, 1304:1312]), reads=[tU], writes=[t_zdt_d])
        headnorm(T, Ut[:T, 0:512], 8, GQ[:T, :], None, QN[:T, :], [tU], [tQN], 8)
        for h in range(8):
            k.op("pe", lambda e, h=h: e.transpose(PT[1][:64, h * 128:h * 128 + T], QN[:T, h * 64:(h + 1) * 64], C["ident_bf"][:T, :T]), reads=[tQN, tC], writes=[tPT[1]])
        k.op("act", lambda e: e.copy(out=QT[:, :, :T], in_=PT[1][:64, :].rearrange("p (h t) -> p h t", h=8)[:, :, :T]), reads=[tPT[1]], writes=[tQT])
        headnorm(T, Ut[:T, 768:896], 2, GKS[:T, :], SELO[:T, 0:128], KNB[:T, 0:128], [tU], [tSELO], 16)
        k.op("act", lambda e: e.copy(out=SELO[:T, 128:256], in_=Ut[:T, 896:1024]), reads=[tU], writes=[tSELO])
        headnorm(T, Ut[:T, 1024:1152], 2, GKW[:T, :], WINO[:T, 0:128], KNB[:T, 128:256], [tU], [tWINO], 18)
        k.op("act", lambda e: e.copy(out=WINO[:T, 128:256], in_=Ut[:T, 1152:1280]), reads=[tU], writes=[tWINO])
        k.dma("sp", lambda e: e.dma_start(out=sel_o, in_=SELO[:T, :]), reads=[tSELO], writes=[otok()])
        k.op("act", lambda e: e.activation(out=GT[:T, :], in_=Ut[:T, 1280:1304], func=AF.Sigmoid), reads=[tU], writes=[tGT])

    pvn = [0]

    pend = []; pvbank = [5]

    def flush():
        while pend:
            pend.pop(0)()

    def combine(br, g):
        flush()
        bk = pvbank[0]; pvbank[0] = 9 - bk
        PBk = PB[bk]; tPBk = tPB[bk]
        den = PBk[:, 0:260].rearrange("p (h c) -> p h c", c=65)[:, :, 64]
        k.op("dve", lambda e: e.tensor_scalar_max(out=COEF[:, 0:4], in0=den, scalar1=1e-30), reads=[tPBk], writes=[tCOEF])
        k.op("dve", lambda e: e.reciprocal(out=COEF[:, 0:4], in_=COEF[:, 0:4]), reads=[tCOEF], writes=[tCOEF])
        k.op("dve", lambda e: e.tensor_tensor(out=COEF[:, 4:8], in0=COEF[:, 0:4], in1=GT[:, br * 8 + g * 4:br * 8 + g * 4 + 4], op=ALU.mult), reads=[tCOEF, tGT], writes=[tCOEF])
        ov = PBk[:, 0:260].rearrange("p (h c) -> p h c", c=65)[:, :, 0:64]
        cf = COEF[:, 4:8].unsqueeze(2).to_broadcast([128, 4, 64])
        av = ATT[:, g * 256:(g + 1) * 256].rearrange("p (h d) -> p h d", d=64)
        if br == 0:
            k.op("dve", lambda e: e.tensor_tensor(out=av, in0=ov, in1=cf, op=ALU.mult), reads=[tPBk, tCOEF], writes=[tATT])
        else:
            k.op("dve", lambda e: e.tensor_tensor(out=OT[:, :].rearrange("p (h d) -> p h d", d=64), in0=ov, in1=cf, op=ALU.mult), reads=[tPBk, tCOEF], writes=[tOT])
            k.op("pool", lambda e: e.tensor_tensor(out=ATT[:, g * 256:(g + 1) * 256], in0=ATT[:, g * 256:(g + 1) * 256], in1=OT[:, :], op=ALU.add), reads=[tOT, tATT], writes=[tATT])

    def chunk(g, kT_ap, ktoks, bias, v_ap, vtoks, nk, first, last):
        n = pvn[0]; pvn[0] += 1
        pb = n % 2; s = n % 3
        k.op("pe", lambda e: e.matmul(PB[pb][:nk, :], lhsT=kT_ap, rhs=QT[:, 4 * g:4 * g + 4, :], start=True, stop=(bias is None)),
             reads=[tQT] + ktoks, writes=[tPB[pb]])
        if bias is not None:
            k.op("pe", lambda e: e.matmul(PB[pb][:nk, :], lhsT=bias[0], rhs=bias[1], start=False, stop=True), reads=bias[2], writes=[tPB[pb]])
        k.op("act", lambda e: e.activation(out=PTt[s][:nk, :], in_=PB[pb][:nk, :], func=AF.Exp, scale=SCALE), reads=[tPB[pb]], writes=[tPTt[s]])
        bk = pvbank[0]

        def pv():
            for h in range(4):
                k.op("pe", lambda e, h=h: e.matmul(PB[bk][:, h * 65:(h + 1) * 65], lhsT=PTt[s][:nk, h * 128:(h + 1) * 128], rhs=v_ap, start=(first and h == 0), stop=last, skip_group_check=True),
                     reads=[tPTt[s]] + vtoks, writes=[tPB[bk]])
        pend.append(pv)
        if len(pend) > 2:
            pend.pop(0)()

    load_mod(A1, 128, 1, tM1); load_mod(B1, 128, 0, tM1)
    for i in range(NT):
        r0 = i * 128
        front(128, xp[r0:r0 + 128, :], cmpp[r0:r0 + 128, :], selp[r0:r0 + 128, :], xbc_d[3 + r0:3 + r0 + 128, :], r0)
        if i >= 12:
            k.dma("sp", lambda e, i=i: e.dma_start(out=winp[(i - 12) * 128:(i - 11) * 128, :], in_=WINO[:, :]), reads=[tWINO], writes=[otok()])
        if i == NT - 1:
            k.dma("sp", lambda e: e.dma_start(out=convp[:, :], in_=xbc_d[SEQ:SEQ + 3, :]), reads=[t_xbc_d], writes=[otok()])
        for j in range(4):
            k.op("pe", lambda e, j=j: e.transpose(PT[1][:64, j * 128:(j + 1) * 128], KNB[:, j * 64:(j + 1) * 64], C["ident_bf"][:, :]), reads=[tKNB, tC], writes=[tPT[1]])
        k.op("act", lambda e: e.copy(out=KST[:, :, r0:r0 + 128], in_=PT[1][:64, 0:256].rearrange("p (g t) -> p g t", g=2)), reads=[tPT[1]], writes=[tKS[i]])
        k.op("act", lambda e: e.copy(out=KWT[:, :, r0:r0 + 128], in_=PT[1][:64, 256:512].rearrange("p (g t) -> p g t", g=2)), reads=[tPT[1]], writes=[tKW[i]])
        k.op("pool", lambda e: e.tensor_copy(out=VS[:, i, :, 0:64], in_=Ut[:, 896:1024].rearrange("p (g d) -> p g d", g=2)), reads=[tU], writes=[tVS[i]])
        k.op("pool", lambda e: e.tensor_copy(out=VW[:, i, :, 0:64], in_=Ut[:, 1152:1280].rearrange("p (g d) -> p g d", g=2)), reads=[tU], writes=[tVW[i]])
        for g in range(2):
            k.op("pe", lambda e, g=g: e.matmul(PB[2][:64, g * 4:(g + 1) * 4], lhsT=Ut[:, 512 + g * 64:576 + g * 64], rhs=WKB[:, :], start=True, stop=True), reads=[tU, tG], writes=[tPB[2]])
        k.op("act", lambda e: e.activation(out=KR[:, 0:8], in_=PB[2][:64, 0:8], func=AF.Square), reads=[tPB[2]], writes=[tKR])
        k.op("pe", lambda e: e.matmul(PB[3][:64, 0:8], lhsT=C["ones_f"][:64, :64], rhs=KR[:, 0:8], start=True, stop=True), reads=[tKR, tC], writes=[tPB[3]])
        k.op("act", lambda e: e.activation(out=KR[:, 8:16], in_=PB[3][:64, 0:8], func=AF.Sqrt, bias=EPSC[:64, :], scale=1.0 / 64), reads=[tPB[3], tC], writes=[tKR])
        k.op("dve", lambda e: e.reciprocal(out=KR[:, 8:16], in_=KR[:, 8:16]), reads=[tKR], writes=[tKR])
        k.op("dve", lambda e: e.scalar_tensor_tensor(out=KCT[:, :, 4 * i:4 * i + 4], in0=PB[2][:64, 0:8].rearrange("p (g j) -> p g j", g=2), scalar=GKC[:, 0:1],
                                                     in1=KR[:, 8:16].rearrange("p (g j) -> p g j", g=2), op0=ALU.mult, op1=ALU.mult), reads=[tPB[2], tKR, tG], writes=[tKC])
        k.op("pe", lambda e: e.matmul(PB[3][:64, 128:256], lhsT=WVS[:, i * 64:(i + 1) * 64], rhs=Ut[:, 640:768], start=True, stop=True), reads=[tU, tG], writes=[tPB[3]])
        k.op("dve", lambda e: e.tensor_tensor(out=VCA[:, :], in0=VCA[:, :], in1=PB[3][:64, 128:256], op=ALU.add), reads=[tPB[3], tVC], writes=[tVC])
        k.op("dve", lambda e: e.tensor_copy(out=VC[:, :, 0:64], in_=VCA[:, :].rearrange("p (g d) -> p g d", g=2)), reads=[tVC], writes=[tVC])
        nk = 4 * (i + 1)
        for g in range(2):
            chunk(g, KCT[:, g, 0:nk], [tKC], (C["Dsel"][:, 60 - 4 * i:60 - 4 * i + nk], C["cmpB"][:, :], [tC]), VC[:nk, g, :], [tVC], nk, True, True)
            combine(0, g)
            for h in range(4):
                k.op("pe", lambda e, h=h: e.matmul(PB[2][:, h * 64:(h + 1) * 64], lhsT=QT[:, 4 * g + h, :], rhs=KCT[:, g, :], start=True, stop=True), reads=[tQT, tKC], writes=[tPB[2]])
            k.op("act", lambda e: e.activation(out=SE[:, :], in_=PB[2][:, 0:256], func=AF.Exp, scale=SCALE), reads=[tPB[2]], writes=[tSE])
            k.op("dve", lambda e: e.tensor_tensor(out=SE[:, :].rearrange("p (h j) -> p h j", h=4), in0=SE[:, :].rearrange("p (h j) -> p h j", h=4),
                                                  in1=C["cmpM0"][:, 60 - 4 * i:124 - 4 * i].unsqueeze(1).to_broadcast([128, 4, 64]), op=ALU.mult), reads=[tSE, tC], writes=[tSE])
            k.op("dve", lambda e: e.tensor_reduce(out=SM[:, 24:28], in_=SE[:, :].rearrange("p (h j) -> p h j", h=4), axis=AX.X, op=ALU.add), reads=[tSE], writes=[tSM])
            k.op("dve", lambda e: e.tensor_scalar_max(out=SM[:, 24:28], in0=SM[:, 24:28], scalar1=1e-30), reads=[tSM], writes=[tSM])
            k.op("dve", lambda e: e.reciprocal(out=SM[:, 24:28], in_=SM[:, 24:28]), reads=[tSM], writes=[tSM])
            k.op("dve", lambda e: e.tensor_tensor(out=SE[:, :].rearrange("p (h j) -> p h j", h=4), in0=SE[:, :].rearrange("p (h j) -> p h j", h=4),
                                                  in1=SM[:, 24:28].unsqueeze(2).to_broadcast([128, 4, 64]), op=ALU.mult), reads=[tSE, tSM], writes=[tSE])
            k.op("dve", lambda e: e.tensor_reduce(out=S2[:, :], in_=SE[:, :].rearrange("p (h j) -> p j h", h=4), axis=AX.X, op=ALU.add), reads=[tSE], writes=[tSEL])
            s2v = S2[:, :].rearrange("p (n two) -> p n two", two=2)
            k.op("dve", lambda e: e.tensor_tensor(out=IMP[:, :], in0=s2v[:, :, 0], in1=s2v[:, :, 1], op=ALU.add), reads=[tSEL], writes=[tSEL])
            k.op("dve", lambda e: e.tensor_tensor(out=SC[:, :], in0=IMP[:, :], in1=C["selA"][:, i * 32:(i + 1) * 32], op=ALU.mult), reads=[tSEL, tC], writes=[tSEL])
            k.op("dve", lambda e: e.tensor_tensor(out=SC[:, :], in0=SC[:, :], in1=C["selB"][:, i * 32:(i + 1) * 32], op=ALU.add), reads=[tSEL, tC], writes=[tSEL])
            k.op("dve", lambda e: e.max(out=M1[:, :], in_=SC[:, :]), reads=[tSEL], writes=[tSEL])
            k.op("dve", lambda e: e.match_replace(out=SC2[:, :], in_to_replace=M1[:, :], in_values=SC[:, :], imm_value=-3e38), reads=[tSEL], writes=[tSEL])
            k.op("dve", lambda e: e.max(out=M2[:, :], in_=SC2[:, :]), reads=[tSEL], writes=[tSEL])
            k.op("dve", lambda e: e.tensor_scalar(out=SC2[:, :], in0=SC[:, :], scalar1=M2[:, 6:7], scalar2=None, op0=ALU.is_ge), reads=[tSEL], writes=[tSEL])
            k.op("dve", lambda e: e.tensor_tensor(out=SC2[:, :], in0=SC2[:, :], in1=C["allowed"][:, i * 32:(i + 1) * 32], op=ALU.mult), reads=[tSEL, tC], writes=[tSEL])
            k.op("dve", lambda e: e.tensor_scalar(out=NMB[:, :], in0=SC2[:, :], scalar1=-1.0, scalar2=-NEGB, op0=ALU.add, op1=ALU.mult), reads=[tSEL], writes=[tSEL])
            k.op("pe", lambda e: e.transpose(PT[1][:32, 0:128], NMB[:, :], C["ident_bf"][:, :]), reads=[tSEL, tC], writes=[tPT[1]])
            k.op("act", lambda e: e.copy(out=NMT[:, :, :], in_=PT[1][:32, 0:128].unsqueeze(1).to_broadcast([32, 4, 128])), reads=[tPT[1]], writes=[tNMT])
            for c in range(i + 1):
                if c < i:
                    bias = (C["Eall"][:, c * 128:(c + 1) * 128], NMT[:, :, :], [tC, tNMT])
                else:
                    bias = (C["ident_bf"][:, :], C["causalb"][:, :], [tC])
                chunk(g, KST[:, g, c * 128:(c + 1) * 128], [tKS[c]], bias, VS[:, c, g, :], [tVS[c]], 128, c == 0, c == i)
            combine(1, g)
            c0 = max(0, i - 4)
            for c in range(c0, i + 1):
                if c == i:
                    bias = (C["ident_bf"][:, :], C["causalb"][:, :], [tC])
                elif c == i - 4:
                    bias = (C["ident_bf"][:, :], C["antib"][:, :], [tC])
                else:
                    bias = None
                chunk(g, KWT[:, g, c * 128:(c + 1) * 128], [tKW[c]], bias, VW[:, c, g, :], [tVW[c]], 128, c == c0, c == i)
            combine(2, g)
        k.dma("sp", lambda e, r0=r0: e.dma_start(out=att_d[r0:r0 + 128, :], in_=ATT[:, :]), reads=[tATT], writes=[t_att_d])

    load_mod(A1, TS, 1, tM1); load_mod(B1, TS, 0, tM1)
    k.dma("sp", lambda e: e.dma_start(out=xbcs_d[:, 0:3, :], in_=sconv), writes=[t_xbcs_d])
    front(TS, xs[:, :], cmps[:, :], sels[:, :], xbcs_d[:, 3:7, :], SEQ)
    k.dma("sp", lambda e: e.dma_start(out=convs[:, :, :], in_=xbcs_d[:, 4:7, :]), reads=[t_xbcs_d], writes=[otok()])
    k.dma("sp", lambda e: e.dma_start(out=wins[:, 0:508, :], in_=cwin[:, 4:512, :]), writes=[otok()])
    for b in range(NSEQ):
        k.dma("sp", lambda e, b=b: e.dma_start(out=wins[b, 508:512, :], in_=WINO[4 * b:4 * b + 4, :]), reads=[tWINO], writes=[otok()])
    k.dma("sp", lambda e: e.dma_start(out=qs_d[:, :], in_=QN[:TS, :]), reads=[tQN], writes=[t_qs_d])
    k.dma("sp", lambda e: e.dma_start(out=kvs_d[:, 0:256], in_=SELO[:TS, :]), reads=[tSELO], writes=[t_kvs_d])
    k.dma("sp", lambda e: e.dma_start(out=kvs_d[:, 256:512], in_=WINO[:TS, :]), reads=[tWINO], writes=[t_kvs_d])
    GTP = k.sb("GTP", [TS, 24], F32); tGTP = Tok()
    k.op("dve", lambda e: e.tensor_copy(out=GTP[:, :].rearrange("p (q m) -> p q m", m=6), in_=GT[:TS, :].rearrange("p (m q) -> p q m", m=6)), reads=[tGT], writes=[tGTP])
    k.dma("sp", lambda e: e.dma_start(out=gts_d[:, :], in_=GTP[:, :]), reads=[tGTP], writes=[t_gts_d])
    k.pop()

    k.push()
    tCS = Tok("sconst")
    GKC2 = bc_load("GKC2", gkc2, 128, tok=tCS)
    KVA = k.sb("KVA", [128, NSEQ, 2, 257], F32); tKVA = [Tok() for _ in range(NSEQ)]
    k.op("pool", lambda e: e.memset(KVA[:], 1.0), writes=tKVA)
    k.push()
    WK32 = k.sb("WK32", [128, 32 * 128], F32); WV32 = k.sb("WV32", [128, 32 * 128], F32); tW32 = Tok()
    k.dma("sp", lambda e: e.dma_start(out=WK32[:, :], in_=wk32), writes=[tW32])
    k.dma("sp", lambda e: e.dma_start(out=WV32[:, :], in_=wv32), writes=[tW32])
    PG = [k.sb(f"PG{i}", [128, 8, 1024], F32) for i in range(2)]; tPG = [Tok(), Tok()]
    PTI = k.sb("PTI", [128, NSEQ * 64], I32); PTF = k.sb("PTF", [128, NSEQ * 64], F32); tIDXC = Tok()
    PT4 = k.sb("PT4", [128, NSEQ * 16], F32); IDX4 = k.sb("IDX4", [128, NSEQ * 16], I32)
    k.dma("sp", lambda e: e.dma_start(out=PTI[:, :], in_=ptab.rearrange("b j -> (b j)").partition_broadcast(128)), writes=[tIDXC])
    k.op("dve", lambda e: e.tensor_copy(out=PTF[:, :], in_=PTI[:, :]), reads=[tIDXC], writes=[tIDXC])
    k.op("dve", lambda e: e.tensor_tensor(out=PTF[:, :].rearrange("p (a f) -> p a f", f=4), in0=PTF[:, :].rearrange("p (a f) -> p a f", f=4),
                                          in1=C["OH4"][:, :].unsqueeze(1).to_broadcast([128, NSEQ * 16, 4]), op=ALU.mult), reads=[tIDXC, tC], writes=[tIDXC])
    k.op("dve", lambda e: e.tensor_reduce(out=PT4[:, :], in_=PTF[:, :].rearrange("p (a f) -> p a f", f=4), axis=AX.X, op=ALU.add), reads=[tIDXC], writes=[tIDXC])
    k.op("dve", lambda e: e.tensor_scalar(out=PT4[:, :], in0=PT4[:, :], scalar1=32.0, scalar2=C["PM32"][:, 0:1], op0=ALU.mult, op1=ALU.add), reads=[tIDXC, tC], writes=[tIDXC])
    k.op("dve", lambda e: e.tensor_copy(out=IDX4[:, :], in_=PT4[:, :]), reads=[tIDXC], writes=[tIDXC])
    ccmp4 = ccmp.rearrange("(g i) c -> g (i c)", i=4)
    for b in range(NSEQ):
        for c in range(2):
            s_ = (2 * b + c) % 2
            for d in range(8):
                col = b * 16 + c * 8 + d
                k.dma("pool", lambda e, d=d, col=col, s_=s_: e.indirect_dma_start(out=PG[s_][:, d, :], out_offset=None, in_=ccmp4,
                                                                                 in_offset=bass.IndirectOffsetOnAxis(ap=IDX4[:, col:col + 1], axis=0)), reads=[tIDXC], writes=[tPG[s_]])
            pk = 2 * (c % 2)
            for j in range(32):
                d, i_ = j // 4, j % 4
                k.op("pe", lambda e, j=j, d=d, i_=i_, s_=s_, pk=pk: e.matmul(PB[pk][:, 0:128], lhsT=WK32[:, j * 128:(j + 1) * 128], rhs=PG[s_][:, d, i_ * 256:i_ * 256 + 128], start=(j == 0), stop=(j == 31)), reads=[tW32, tPG[s_]], writes=[tPB[pk]])
                k.op("pe", lambda e, j=j, d=d, i_=i_, s_=s_, pk=pk: e.matmul(PB[pk + 1][:, 0:128], lhsT=WV32[:, j * 128:(j + 1) * 128], rhs=PG[s_][:, d, i_ * 256 + 128:i_ * 256 + 256], start=(j == 0), stop=(j == 31)), reads=[tW32, tPG[s_]], writes=[tPB[pk + 1]])
            k.op("act", lambda e, b=b, c=c, pk=pk: e.copy(out=KVA[:, b, c, 0:128], in_=PB[pk][:, 0:128]), reads=[tPB[pk]], writes=[tKVA[b]])
            k.op("dve", lambda e, b=b, c=c, pk=pk: e.tensor_copy(out=KVA[:, b, c, 128:256], in_=PB[pk + 1][:, 0:128]), reads=[tPB[pk + 1]], writes=[tKVA[b]])
    k.pop()
    QB = [k.sb(f"QB{i}", [128, 4, 512], BF16) for i in range(2)]; tQB = [Tok(), Tok()]
    PRODS = [k.sb(f"PROD{i}", [128, 2048], F32) for i in range(2)]; tPRODS = [Tok(), Tok()]
    PROD = PRODS[0]; tPROD = tPRODS[0]
    STc = k.sb("STc", [128, NSEQ * 64], F32); PTc = k.sb("PTc", [128, NSEQ * 64], F32); tSTc = [Tok() for _ in range(NSEQ)]
    PCM = k.sb("PCM", [32, NSEQ, 256], F32); tPCM = Tok()
    GT16 = [k.sb(f"GT16{i}", [16, 6], F32) for i in range(2)]; GT4 = [k.sb(f"GT4{i}", [4, 24], F32) for i in range(2)]; tGTs = [Tok(), Tok()]
    OBS = [k.sb(f"OB{i}", [16, 64], F32) for i in range(2)]; tOBS = [Tok(), Tok()]
    CFS = [k.sb(f"CF{i}", [16, 8], F32) for i in range(2)]; tCFS = [Tok(), Tok()]
    KNS = [k.sb(f"KNS{i}", [4, 257], F32) for i in range(2)]; KNW = [k.sb(f"KNW{i}", [4, 257], F32) for i in range(2)]; tKN = [Tok(), Tok()]
    for i_ in range(2):
        k.op("pool", lambda e, i_=i_: e.memset(KNS[i_][:], 1.0), writes=[tKN[i_]])
        k.op("pool", lambda e, i_=i_: e.memset(KNW[i_][:], 1.0), writes=[tKN[i_]])
    STn = k.sb("STn", [4, 64], F32); PTn = k.sb("PTn", [4, 64], F32); tSTn = Tok()

    def dots(K_ap, kshape_b, Q_ap, out_ap, rd, wr, pi=0):
        P, a, b_ = kshape_b
        pv = PRODS[pi][:P, :a * b_ * 64].rearrange("p (a b d) -> p a b d", a=a, b=b_)
        k.op("dve", lambda e: e.tensor_tensor(out=pv, in0=K_ap, in1=Q_ap, op=ALU.mult), reads=rd, writes=[tPRODS[pi]])
        k.op("dve", lambda e: e.tensor_reduce(out=out_ap, in_=pv, axis=AX.X, op=ALU.add), reads=[tPRODS[pi]], writes=wr)

    def norm_gate_store(ps_ap, nq, g, gate_ap, gate_tok, dst_ap, eng_tok, pi=0):
        CF = CFS[pi]; tCF = tCFS[pi]; OB = OBS[pi]; tOB = tOBS[pi]
        k.op("dve", lambda e: e.tensor_scalar_max(out=CF[:nq, 0:1], in0=ps_ap[:, 128:129], scalar1=1e-30), reads=eng_tok, writes=[tCF])
        k.op("dve", lambda e: e.reciprocal(out=CF[:nq, 0:1], in_=CF[:nq, 0:1]), reads=[tCF], writes=[tCF])
        k.op("dve", lambda e: e.tensor_tensor(out=CF[:nq, 1:2], in0=CF[:nq, 0:1], in1=gate_ap, op=ALU.mult), reads=[tCF, gate_tok], writes=[tCF])
        k.op("dve", lambda e: e.tensor_scalar(out=OB[:nq, 0:64], in0=ps_ap[:, g * 64:(g + 1) * 64], scalar1=CF[:nq, 1:2], scalar2=None, op0=ALU.mult), reads=eng_tok + [tCF], writes=[tOB])
        k.dma("sp", lambda e: e.dma_start(out=dst_ap, in_=OB[:nq, 0:64]), reads=[tOB], writes=[t_atts_d])

    def load_seq(b, s, what):
        k.dma("sp", lambda e: e.dma_start(out=QB[s][:, :, :], in_=qs_d[4 * b:4 * b + 4, :].partition_broadcast(128)), reads=[t_qs_d], writes=[tQB[s]])
        k.dma("sp", lambda e: e.dma_start(out=GT16[s][:, :], in_=gts_d[4 * b:4 * b + 4, :].rearrange("t (q m) -> (t q) m", m=6)), reads=[t_gts_d], writes=[tGTs[s]])
        k.dma("sp", lambda e: e.dma_start(out=GT4[s][:, :].rearrange("q (t m) -> q t m", m=6), in_=gts_d[4 * b:4 * b + 4, :].rearrange("t (q m) -> q t m", m=6)), reads=[t_gts_d], writes=[tGTs[s]])
        if what >= 1:
            k.dma("sp", lambda e: e.dma_start(out=KNS[s][:, 0:256], in_=kvs_d[4 * b:4 * b + 4, 0:256]), reads=[t_kvs_d], writes=[tKN[s]])
            k.dma("sp", lambda e: e.dma_start(out=KNW[s][:, 0:256], in_=kvs_d[4 * b:4 * b + 4, 256:512]), reads=[t_kvs_d], writes=[tKN[s]])

    RS = k.sb("RS", [128, 64], F32)
    for b in range(NSEQ):
        k.op("dve", lambda e, b=b: e.tensor_tensor(out=PROD[:, 0:256].rearrange("p (c f) -> p c f", c=2), in0=KVA[:, b, :, 0:128], in1=KVA[:, b, :, 0:128], op=ALU.mult), reads=[tKVA[b]], writes=[tPROD])
        k.op("dve", lambda e, b=b: e.tensor_reduce(out=RS[:, 4 * b:4 * b + 4], in_=PROD[:, 0:256].rearrange("p (a d) -> p a d", d=64), axis=AX.X, op=ALU.add), reads=[tPROD], writes=[tSM])
    k.op("act", lambda e: e.activation(out=RS[:, :], in_=RS[:, :], func=AF.Sqrt, bias=EPSC[:, :], scale=1.0 / 64), reads=[tSM, tC], writes=[tSM])
    k.op("dve", lambda e: e.reciprocal(out=RS[:, :], in_=RS[:, :]), reads=[tSM], writes=[tSM])
    for b in range(NSEQ):
        kb_ = KVA[:, b, :, 0:128].rearrange("p c (g d) -> p c g d", g=2)
        k.op("dve", lambda e, b=b, kb_=kb_: e.tensor_tensor(out=kb_, in0=kb_, in1=RS[:, 4 * b:4 * b + 4].rearrange("p (c g) -> p c g", c=2).unsqueeze(3).to_broadcast([128, 2, 2, 64]), op=ALU.mult), reads=[tKVA[b], tSM], writes=[tKVA[b]])
        k.op("pool", lambda e, b=b: e.tensor_tensor(out=KVA[:, b, :, 0:128], in0=KVA[:, b, :, 0:128], in1=GKC2[:, :].unsqueeze(1).to_broadcast([128, 2, 128]), op=ALU.mult), reads=[tKVA[b], tCS], writes=[tKVA[b]])
    for b in range(NSEQ):
        s = b % 2
        load_seq(b, s, 0)
        qv = QB[s][:, :, :].rearrange("p t (h d) -> p t h d", d=64)
        for c in range(2):
            for g in range(2):
                dots(KVA[:, b, c, g * 64:(g + 1) * 64].unsqueeze(1).unsqueeze(1).to_broadcast([128, 4, 4, 64]), (128, 4, 4), qv[:, :, 4 * g:4 * g + 4, :],
                     STc[:, b * 64 + (c * 2 + g) * 16:b * 64 + (c * 2 + g + 1) * 16].rearrange("p (t q) -> p t q", t=4), [tKVA[b], tQB[s]], [tSTc[b]])
        k.op("act", lambda e, b=b: e.activation(out=PTc[:, b * 64:(b + 1) * 64], in_=STc[:, b * 64:(b + 1) * 64], func=AF.Exp, scale=SCALE), reads=[tSTc[b]], writes=[tSTc[b]])
        for g in range(2):
            for c in range(2):
                k.op("pe", lambda e, b=b, g=g, c=c: e.matmul(PB[2][:16, 0:129], lhsT=PTc[:, b * 64 + (c * 2 + g) * 16:b * 64 + (c * 2 + g + 1) * 16], rhs=KVA[:, b, c, 128:257], start=(c == 0), stop=(c == 1)), reads=[tSTc[b], tKVA[b]], writes=[tPB[2]])
            norm_gate_store(PB[2][:16, 0:129], 16, g, GT16[s][:, g:g + 1], tGTs[s], atts_d[0, b, :, g, :, :], [tPB[2]])
        for c in range(2):
            k.op("pe", lambda e, b=b, c=c: e.transpose(PB[3][:32, c * 128:(c + 1) * 128], PTc[:, b * 64 + c * 32:b * 64 + (c + 1) * 32], C["ident_f"][:, :]), reads=[tSTc[b], tC], writes=[tPB[3]])
        k.op("act", lambda e, b=b: e.copy(out=PCM[:, b, :], in_=PB[3][:32, 0:256]), reads=[tPB[3]], writes=[tPCM])
    RSM = k.sb("RSM", [32, NSEQ], F32)
    k.op("dve", lambda e: e.tensor_reduce(out=RSM[:, :], in_=PCM[:, :, :], axis=AX.X, op=ALU.add), reads=[tPCM], writes=[tSM])
    k.op("dve", lambda e: e.reciprocal(out=RSM[:, :], in_=RSM[:, :]), reads=[tSM], writes=[tSM])
    k.op("dve", lambda e: e.tensor_tensor(out=PCM[:, :, :], in0=PCM[:, :, :], in1=RSM[:, :].unsqueeze(2).to_broadcast([32, NSEQ, 256]), op=ALU.mult), reads=[tPCM, tSM], writes=[tPCM])
    for b in range(NSEQ):
        k.op("pe", lambda e, b=b: e.matmul(PB[4][:, 0:256], lhsT=C["HSELB"][:, b * 128:(b + 1) * 128], rhs=PCM[:, b, :], start=(b == 0), stop=(b == NSEQ - 1)), reads=[tPCM, tC], writes=[tPB[4]])
    IMPs = k.sb("IMPs", [128, 128], F32); SCs = k.sb("SCs", [128, 128], F32); SC2s = k.sb("SC2s", [128, 128], F32); tSELs = Tok()
    M1s = k.sb("M1s", [128, 16], F32); RBT = k.sb("RBT", [16, 128], F32)
    PHY = k.sb("PHY", [128, 64, 2], F32); PHI = k.sb("PHI", [128, 64], I32); PHF = k.sb("PHF", [128, 64], F32); tPHY = Tok()
    IDXF = k.sb("IDXF", [128, 128], F32); IDXS = k.sb("IDXS", [128, 128], I32); tIDXS = Tok()
    for b in range(NSEQ):
        k.dma("sp", lambda e, b=b: e.dma_start(out=PHI[8 * b:8 * b + 8, :], in_=ptab[b, :].partition_broadcast(8)), writes=[tPHY])
    k.op("dve", lambda e: e.tensor_copy(out=PHF[:, :], in_=PHI[:, :]), reads=[tPHY], writes=[tPHY])
    k.op("dve", lambda e: e.tensor_scalar(out=PHY[:, :, 0], in0=PHF[:, :], scalar1=2.0, scalar2=1.0, op0=ALU.mult, op1=ALU.add), reads=[tPHY], writes=[tPHY])
    k.op("dve", lambda e: e.tensor_scalar(out=PHY[:, :, 1], in0=PHF[:, :], scalar1=2.0, scalar2=2.0, op0=ALU.mult, op1=ALU.add), reads=[tPHY], writes=[tPHY])
    pv2 = PB[4][:, 0:256].rearrange("p (n two) -> p n two", two=2)
    k.op("act", lambda e: e.copy(out=SC2s[:, :], in_=pv2[:, :, 0]), reads=[tPB[4]], writes=[tSELs])
    k.op("dve", lambda e: e.tensor_tensor(out=IMPs[:, :], in0=pv2[:, :, 1], in1=SC2s[:, :], op=ALU.add), reads=[tPB[4], tSELs], writes=[tSELs])
    k.op("dve", lambda e: e.tensor_tensor(out=SCs[:, :], in0=IMPs[:, :], in1=C["BIGS"][:, :], op=ALU.add), reads=[tSELs, tC], writes=[tSELs])
    k.op("dve", lambda e: e.max(out=M1s[:, 0:8], in_=SCs[:, :]), reads=[tSELs], writes=[tSELs])
    k.op("dve", lambda e: e.match_replace(out=SC2s[:, :], in_to_replace=M1s[:, 0:8], in_values=SCs[:, :], imm_value=-3e38), reads=[tSELs], writes=[tSELs])
    k.op("dve", lambda e: e.max(out=M1s[:, 8:16], in_=SC2s[:, :]), reads=[tSELs], writes=[tSELs])
    k.op("dve", lambda e: e.tensor_scalar(out=SC2s[:, :], in0=SCs[:, :], scalar1=M1s[:, 14:15], scalar2=None, op0=ALU.is_ge), reads=[tSELs], writes=[tSELs])
    k.op("dve", lambda e: e.tensor_tensor(out=SCs[:, :], in0=SC2s[:, :], in1=PHY[:, :, :].rearrange("p j two -> p (j two)"), op=ALU.mult), reads=[tSELs, tPHY], writes=[tSELs])
    k.op("dve", lambda e: e.max(out=M1s[:, 0:8], in_=SCs[:, :]), reads=[tSELs], writes=[tSELs])
    k.op("dve", lambda e: e.match_replace(out=SC2s[:, :], in_to_replace=M1s[:, 0:8], in_values=SCs[:, :], imm_value=0.0), reads=[tSELs], writes=[tSELs])
    k.op("dve", lambda e: e.max(out=M1s[:, 8:16], in_=SC2s[:, :]), reads=[tSELs], writes=[tSELs])
    k.op("dve", lambda e: e.tensor_scalar(out=M1s[:, :], in0=M1s[:, :], scalar1=-1.0, scalar2=0.0, op0=ALU.add, op1=ALU.max), reads=[tSELs], writes=[tSELs])
    k.op("dve", lambda e: e.tensor_scalar(out=M1s[:, :], in0=M1s[:, :], scalar1=16.0, scalar2=None, op0=ALU.mult), reads=[tSELs], writes=[tSELs])
    k.op("pe", lambda e: e.transpose(PB[3][:16, 0:128], M1s[:, :], C["ident_f"][:, :]), reads=[tSELs, tC], writes=[tPB[3]])
    k.op("act", lambda e: e.copy(out=RBT[:, :], in_=PB[3][:16, 0:128]), reads=[tPB[3]], writes=[tSELs])
    k.op("pe", lambda e: e.matmul(PB[3][:, 128:256], lhsT=C["EXP16"][:, :], rhs=RBT[:, :], start=True, stop=True), reads=[tSELs, tC], writes=[tPB[3]])
    k.op("dve", lambda e: e.tensor_scalar(out=IDXF[:, :], in0=PB[3][:, 128:256], scalar1=C["PM8"][:, 0:1], scalar2=None, op0=ALU.add), reads=[tPB[3], tC], writes=[tIDXS])
    k.op("dve", lambda e: e.tensor_copy(out=IDXS[:, :], in_=IDXF[:, :]), reads=[tIDXS], writes=[tIDXS])
    NKS = 3
    KSEL = [k.sb(f"KSEL{i}", [128, 2, 1024], F32) for i in range(NKS)]; tKSEL = [Tok() for _ in range(NKS)]
    KSV = [KSEL[i][:, :, :].rearrange("p c (i f) -> p (c i) f", i=4) for i in range(NKS)]
    csel4 = csel.rearrange("(g i) c -> g (i c)", i=4)
    STsL = [k.sb(f"STs{i}", [128, 32], F32) for i in range(2)]; PTsL = [k.sb(f"PTs{i}", [128, 32], F32) for i in range(2)]; tSTsL = [Tok(), Tok()]
    STnL = [k.sb(f"STnu{i}", [4, 4], F32) for i in range(2)]; PTnL = [k.sb(f"PTnu{i}", [4, 4], F32) for i in range(2)]; tSTnL = [Tok(), Tok()]
    KWB = [k.sb(f"KWB{i}", [128, 4, 257], F32) for i in range(2)]; tKWB = [Tok(), Tok()]
    for i_ in range(2):
        k.op("pool", lambda e, i_=i_: e.memset(KWB[i_][:], 1.0), writes=[tKWB[i_]])
    STw = k.sb("STw", [128, 128], F32); PTw = k.sb("PTw", [128, 128], F32); tSTw = Tok()
    un = 0
    for b in range(NSEQ):
        sq = b % 2
        load_seq(b, sq, 1)
        qv = QB[sq][:, :, :].rearrange("p t (h d) -> p t h d", d=64)
        for g in range(2):
            for t in range(4):
                gt = g * 4 + t; s = un % NKS; u = un % 2; un += 1
                STs = STsL[u]; PTs = PTsL[u]; tSTs = tSTsL[u]; STu = STnL[u]; PTu = PTnL[u]; tSTu = tSTnL[u]; PBu = PB[5 - u]; tPBu = tPB[5 - u]
                for ch in range(2):
                    k.dma("pool", lambda e, b=b, gt=gt, ch=ch, s=s: e.indirect_dma_start(out=KSEL[s][:, ch, :], out_offset=None, in_=csel4,
                                                                                           in_offset=bass.IndirectOffsetOnAxis(ap=IDXS[:, b * 8 + gt:b * 8 + gt + 1], axis=0), element_offset=8 * ch * 1024), reads=[tIDXS], writes=[tKSEL[s]])
                dots(KSV[s][:, :, g * 64:(g + 1) * 64].unsqueeze(2).to_broadcast([128, 8, 4, 64]), (128, 8, 4),
                     qv[:, t, 4 * g:4 * g + 4, :].unsqueeze(1).to_broadcast([128, 8, 4, 64]), STs[:, :].rearrange("p (c q) -> p c q", c=8), [tKSEL[s], tQB[sq]], [tSTs], pi=u)
                k.op("act", lambda e, STs=STs, PTs=PTs: e.activation(out=PTs[:, :], in_=STs[:, :], func=AF.Exp, scale=SCALE), reads=[tSTs], writes=[tSTs])
                k.op("dve", lambda e, PTs=PTs: e.tensor_scalar(out=PTs[:, :], in0=PTs[:, :], scalar1=C["M120"][:, 0:1], scalar2=None, op0=ALU.mult), reads=[tSTs, tC], writes=[tSTs])
                dots(KNS[sq][:, g * 64:(g + 1) * 64].unsqueeze(1).unsqueeze(1).to_broadcast([4, 1, 4, 64]), (4, 1, 4), qv[:4, t:t + 1, 4 * g:4 * g + 4, :],
                     STu[:, 0:4].rearrange("p (a q) -> p a q", a=1), [tKN[sq], tQB[sq]], [tSTu], pi=u)
                k.op("act", lambda e, STu=STu, PTu=PTu: e.activation(out=PTu[:, 0:4], in_=STu[:, 0:4], func=AF.Exp, scale=SCALE), reads=[tSTu], writes=[tSTu])
                k.op("dve", lambda e, t=t, PTu=PTu: e.tensor_scalar(out=PTu[:, 0:4], in0=PTu[:, 0:4], scalar1=C["CM4"][:, t:t + 1], scalar2=None, op0=ALU.mult), reads=[tSTu, tC], writes=[tSTu])
                for ch in range(8):
                    k.op("pe", lambda e, ch=ch, s=s, PTs=PTs, PBu=PBu: e.matmul(PBu[:4, 0:128], lhsT=PTs[:, ch * 4:(ch + 1) * 4], rhs=KSV[s][:, ch, 128:256], start=(ch == 0), stop=False, skip_group_check=True), reads=[tSTs, tKSEL[s]], writes=[tPBu])
                    k.op("pe", lambda e, ch=ch, PTs=PTs, PBu=PBu: e.matmul(PBu[:4, 128:129], lhsT=PTs[:, ch * 4:(ch + 1) * 4], rhs=C["ones_f"][:, 0:1], start=False, stop=False, skip_group_check=True), reads=[tSTs, tC], writes=[tPBu])
                k.op("pe", lambda e, sq=sq, PTu=PTu, PBu=PBu: e.matmul(PBu[:4, 0:129], lhsT=PTu[:, 0:4], rhs=KNS[sq][:, 128:257], start=False, stop=True, skip_group_check=True), reads=[tSTu, tKN[sq]], writes=[tPBu])
                norm_gate_store(PBu[:4, 0:129], 4, g, GT4[sq][:, t * 6 + 2 + g:t * 6 + 3 + g], tGTs[sq], atts_d[1, b, t, g, :, :], [tPBu], pi=u)
        KWt = KWB[b % 2]; tKWt = tKWB[b % 2]
        k.dma("sp", lambda e, b=b, KWt=KWt: e.dma_start(out=KWt[:, :, 0:256], in_=cwin[b].rearrange("(c p) f -> p c f", p=128)), writes=[tKWt])
        for g in range(2):
            for ch in range(4):
                dots(KWt[:, ch, g * 64:(g + 1) * 64].unsqueeze(1).unsqueeze(1).to_broadcast([128, 4, 4, 64]), (128, 4, 4), qv[:, :, 4 * g:4 * g + 4, :],
                     STw[:, (g * 4 + ch) * 16:(g * 4 + ch + 1) * 16].rearrange("p (t q) -> p t q", t=4), [tKWt, tQB[sq]], [tSTw])
            dots(KNW[sq][:, g * 64:(g + 1) * 64].unsqueeze(1).unsqueeze(1).to_broadcast([4, 4, 4, 64]), (4, 4, 4), qv[:4, :, 4 * g:4 * g + 4, :],
                 STn[:, 16 + g * 16:32 + g * 16].rearrange("p (t q) -> p t q", t=4), [tKN[sq], tQB[sq]], [tSTn])
        k.op("act", lambda e: e.activation(out=PTw[:, :], in_=STw[:, :], func=AF.Exp, scale=SCALE), reads=[tSTw], writes=[tSTw])
        k.op("dve", lambda e: e.tensor_tensor(out=PTw[:, :].rearrange("p (g c t q) -> p g c t q", g=2, c=4, t=4)[:, :, 0, :, :], in0=PTw[:, :].rearrange("p (g c t q) -> p g c t q", g=2, c=4, t=4)[:, :, 0, :, :],
                                              in1=C["MASKW"][:, :].unsqueeze(1).unsqueeze(3).to_broadcast([128, 2, 4, 4]), op=ALU.mult), reads=[tSTw, tC], writes=[tSTw])
        k.op("act", lambda e: e.activation(out=PTn[:, 16:48], in_=STn[:, 16:48], func=AF.Exp, scale=SCALE), reads=[tSTn], writes=[tSTn])
        k.op("dve", lambda e: e.tensor_tensor(out=PTn[:, 16:48].rearrange("p (g t q) -> p g t q", g=2, t=4), in0=PTn[:, 16:48].rearrange("p (g t q) -> p g t q", g=2, t=4),
                                              in1=C["CM4"][:, :].unsqueeze(1).unsqueeze(3).to_broadcast([4, 2, 4, 4]), op=ALU.mult), reads=[tSTn, tC], writes=[tSTn])
        for g in range(2):
            for ch in range(4):
                k.op("pe", lambda e, g=g, ch=ch, KWt=KWt: e.matmul(PB[2][:16, 0:129], lhsT=PTw[:, (g * 4 + ch) * 16:(g * 4 + ch + 1) * 16], rhs=KWt[:, ch, 128:257], start=(ch == 0), stop=False), reads=[tSTw, tKWt], writes=[tPB[2]])
            k.op("pe", lambda e, g=g, sq=sq: e.matmul(PB[2][:16, 0:129], lhsT=PTn[:, 16 + g * 16:32 + g * 16], rhs=KNW[sq][:, 128:257], start=False, stop=True), reads=[tSTn, tKN[sq]], writes=[tPB[2]])
            norm_gate_store(PB[2][:16, 0:129], 16, g, GT16[sq][:, 4 + g:5 + g], tGTs[sq], atts_d[2, b, :, g, :, :], [tPB[2]])
    AS = [k.sb(f"AS{i}", [TS, 512], F32) for i in range(3)]; tAS = Tok()
    for r in range(3):
        k.dma("sp", lambda e, r=r: e.dma_start(out=AS[r][:, :], in_=atts_d[r].rearrange("b t g q d -> (b t) (g q d)")), reads=[t_atts_d], writes=[tAS])
    k.op("dve", lambda e: e.tensor_tensor(out=AS[0][:, :], in0=AS[0][:, :], in1=AS[1][:, :], op=ALU.add), reads=[tAS], writes=[tAS])
    k.op("dve", lambda e: e.tensor_tensor(out=AS[0][:, :], in0=AS[0][:, :], in1=AS[2][:, :], op=ALU.add), reads=[tAS], writes=[tAS])
    k.dma("sp", lambda e: e.dma_start(out=att_d[SEQ:SEQ + TS, :], in_=AS[0][:, :]), reads=[tAS], writes=[t_att_d])
    k.pop()

    k.push()
    tG2 = Tok("gains2")
    CW = bc_load("CW", convw, 4 * 768, tok=tG2); CBt = bc_load("CBt", convb, 768, tok=tG2)
    DTB = bc_load("DTB", dtb, 8, tok=tG2); ALG = bc_load("ALG", alog, 8, tok=tG2); DSK = bc_load("DSK", dsk, 512, tok=tG2)
    GATT = bc_load("GATT", gatt, 512, tok=tG2); GSSM = bc_load("GSSM", gssm, 512, tok=tG2); BR = bc_load("BR", b_r, 20, tok=tG2)
    AN = k.sb("AN", [128, 8], F32)
    k.op("act", lambda e: e.activation(out=AN[:], in_=ALG[:], func=AF.Exp), reads=[tG2], writes=[tG2])
    k.op("dve", lambda e: e.tensor_scalar_mul(out=AN[:], in0=AN[:], scalar1=-1.0), reads=[tG2], writes=[tG2])
    ABH = k.sb("ABH", [128, 1], F32); DBH = k.sb("DBH", [128, 1], F32)
    k.dma("sp", lambda e: e.dma_start(out=ABH[:], in_=abh), writes=[tG2])
    k.dma("sp", lambda e: e.dma_start(out=DBH[:], in_=dbh), writes=[tG2])
    k.op("act", lambda e: e.activation(out=ABH[:], in_=ABH[:], func=AF.Exp), reads=[tG2], writes=[tG2])
    k.op("dve", lambda e: e.tensor_scalar_mul(out=ABH[:], in0=ABH[:], scalar1=-1.0), reads=[tG2], writes=[tG2])
    stg2 = [k.sb(f"stg2{i}", [128, D], F32) for i in range(2)]; tstg2 = [Tok(), Tok()]
    WOUT = k.sb("WOUT", [128, 8, D], BF16); tWOUT = Tok()
    for kk in range(8):
        s = kk % 2
        k.dma("sp", lambda e, kk=kk, s=s: e.dma_start(out=stg2[s][:, :], in_=w_out[kk * 128:(kk + 1) * 128, :]), writes=[tstg2[s]])
        k.op("pool", lambda e, kk=kk, s=s: e.tensor_copy(out=WOUT[:, kk, :], in_=stg2[s][:, :]), reads=[tstg2[s]], writes=[tWOUT])
    WR = k.sb("WR", [128, 8, 20], BF16); tWR = Tok()
    for kk in range(8):
        k.dma("sp", lambda e, kk=kk: e.dma_start(out=stg2[0][:, kk * 20:(kk + 1) * 20], in_=w_r[kk * 128:(kk + 1) * 128, :]), writes=[tstg2[0]])
    k.op("pool", lambda e: e.tensor_copy(out=WR[:], in_=stg2[0][:, :160].rearrange("p (k c) -> p k c", k=8)), reads=[tstg2[0]], writes=[tWR])
    G1 = k.sb("G1", [128, D], F32); A2 = k.sb("A2", [128, D], F32); B2 = k.sb("B2", [128, D], F32); tM2 = Tok()
    HT = k.sb("HT", [64, 8, 64], F32); HTB = k.sb("HTB", [64, 8, 64], BF16); tHT = Tok()
    k.op("pool", lambda e: e.memset(HT[:], 0.0), writes=[tHT])
    k.op("pool", lambda e: e.memset(HTB[:], 0.0), writes=[tHT])
    XT = k.sb("XT2", [128, D], F32); tXT = Tok()
    ATT = k.sb("ATT2", [128, 512], F32); tATT = Tok()
    ZD = k.sb("ZD", [128, 520], F32); tZD = Tok()
    XC = [k.sb(f"XC{i}", [128, 768], F32) for i in range(4)]; tXC = [Tok() for _ in range(4)]
    ACC = k.sb("ACC", [128, 768], F32); tACC = Tok()
    TMPc = k.sb("TMPc", [128, D], F32); tTMPc = Tok()
    XS = k.sb("XS", [128, 768], F32); XSB = k.sb("XSB", [128, 768], BF16); tXS = Tok()
    DT = k.sb("DT", [128, 24], F32); tDT = Tok()
    RU = k.sb("RU", [128, 8, 128], F32); tRU = Tok()
    SG = k.sb("SG", [128, 4, 128], F32); tSG = Tok()
    DEC = k.sb("DEC", [128, 8, 128], F32); tDEC = Tok()
    EX = k.sb("EX", [64, 8, 128], F32); tEX = Tok()
    BCT = k.sb("BCT", [64, 4, 128], BF16); tBCT = Tok()
    CBs = k.sb("CBs", [128, 2, 128], F32); tCBs = Tok()
    MTt = [k.sb(f"MTt{i}", [128, 128], BF16) for i in range(2)]; tMT = [Tok(), Tok()]
    CEt = [k.sb(f"CEt{i}", [64, 128], BF16) for i in range(2)]; tCE = [Tok(), Tok()]
    BWt = [k.sb(f"BWt{i}", [128, 64], BF16) for i in range(2)]; tBW = [Tok(), Tok()]
    YS = k.sb("YS", [128, 512], F32); tYS = Tok()
    MIX = k.sb("MIX", [128, D], BF16); tMIX = Tok()
    MIXT = k.sb("MIXT", [128, 8, 128], BF16); tMIXT = Tok()
    X1 = k.sb("X1", [128, D], F32); tX1 = Tok()
    H2 = k.sb("H2", [128, D], BF16); tH2 = Tok()
    H2T = k.sb("H2T", [128, 8, 128], BF16); tH2T = Tok()
    LG = k.sb("LG", [128, 20], F32); RT = k.sb("RT", [128, 64], F32); COMB = k.sb("COMB", [128, 16], F32); tRT = Tok()

    def conv_silu(T, taps):
        for w in range(4):
            if T == 128:
                k.dma("sp", lambda e, w=w: e.dma_start(out=XC[w][:T, :], in_=taps[w]), reads=[t_xbc_d, t_xbcs_d], writes=[tXC[w]])
            else:
                for b in range(NSEQ):
                    k.dma("sp", lambda e, w=w, b=b: e.dma_start(out=XC[w][4 * b:4 * b + 4, :], in_=taps[w][b, :, :]), reads=[t_xbc_d, t_xbcs_d], writes=[tXC[w]])
        k.op("dve", lambda e: e.tensor_tensor(out=ACC[:T, :], in0=XC[0][:T, :], in1=CW[:T, 0:768], op=ALU.mult), reads=[tXC[0], tG2], writes=[tACC])
        for w in range(1, 4):
            k.op("pool", lambda e, w=w: e.tensor_tensor(out=TMPc[:T, :768], in0=XC[w][:T, :], in1=CW[:T, w * 768:(w + 1) * 768], op=ALU.mult), reads=[tXC[w], tG2], writes=[tTMPc])
            k.op("dve", lambda e: e.tensor_tensor(out=ACC[:T, :], in0=ACC[:T, :], in1=TMPc[:T, :768], op=ALU.add), reads=[tACC, tTMPc], writes=[tACC])
        k.op("dve", lambda e: e.tensor_tensor(out=ACC[:T, :], in0=ACC[:T, :], in1=CBt[:T, :], op=ALU.add), reads=[tACC, tG2], writes=[tACC])
        k.op("act", lambda e: e.activation(out=XS[:T, :], in_=ACC[:T, :], func=AF.Silu), reads=[tACC], writes=[tXS])
        k.op("pool", lambda e: e.tensor_copy(out=XSB[:T, :], in_=XS[:T, :]), reads=[tXS], writes=[tXS])

    def softplus_dt(T):
        k.op("dve", lambda e: e.tensor_tensor(out=DT[:T, 0:8], in0=ZD[:T, 512:520], in1=DTB[:T, :], op=ALU.add), reads=[tZD, tG2], writes=[tDT])
        k.op("dve", lambda e: e.tensor_scalar_min(out=DT[:T, 0:8], in0=DT[:T, 0:8], scalar1=30.0), reads=[tDT], writes=[tDT])
        k.op("act", lambda e: e.activation(out=DT[:T, 0:8], in_=DT[:T, 0:8], func=AF.Exp), reads=[tDT], writes=[tDT])
        k.op("act", lambda e: e.activation(out=DT[:T, 0:8], in_=DT[:T, 0:8], func=AF.Ln, bias=1.0), reads=[tDT], writes=[tDT])

    def finish(T, row0, yout):
        k.op("act", lambda e: e.activation(out=TMPc[:T, :512], in_=ZD[:T, 0:512], func=AF.Silu), reads=[tZD], writes=[tTMPc])
        k.op("dve", lambda e: e.tensor_tensor(out=YS[:T, :], in0=YS[:T, :], in1=TMPc[:T, :512], op=ALU.mult), reads=[tYS, tTMPc], writes=[tYS])
        k.op("act", lambda e: e.activation(out=TMPc[:T, :512], in_=YS[:T, :], func=AF.Square, accum_out=SM[:T, 0:1]), reads=[tYS], writes=[tTMPc, tSM])
        rstd_of(SM[:T, 0:1], T, 1.0 / 512)
        k.op("dve", lambda e: e.scalar_tensor_tensor(out=MIX[:T, 512:1024], in0=YS[:T, :], scalar=SM[:T, 0:1], in1=GSSM[:T, :], op0=ALU.mult, op1=ALU.mult), reads=[tYS, tSM, tG2], writes=[tMIX])
        k.op("act", lambda e: e.activation(out=TMPc[:T, :512], in_=ATT[:T, :], func=AF.Square, accum_out=SM[:T, 1:2]), reads=[tATT], writes=[tTMPc, tSM])
        rstd_of(SM[:T, 1:2], T, 1.0 / 512)
        k.op("dve", lambda e: e.scalar_tensor_tensor(out=MIX[:T, 0:512], in0=ATT[:T, :], scalar=SM[:T, 1:2], in1=GATT[:T, :], op0=ALU.mult, op1=ALU.mult), reads=[tATT, tSM, tG2], writes=[tMIX])
        for kk in range(8):
            k.op("pe", lambda e, kk=kk: e.transpose(PT[0][:, kk * 128:kk * 128 + T], MIX[:T, kk * 128:(kk + 1) * 128], C["ident_bf"][:T, :T]), reads=[tMIX, tC], writes=[tPT[0]])
        k.op("act", lambda e: e.copy(out=MIXT[:, :, :T], in_=PT[0][:, :].rearrange("p (k t) -> p k t", k=8)[:, :, :T]), reads=[tPT[0]], writes=[tMIXT])
        for hf in range(2):
            for kk in range(8):
                k.op("pe", lambda e, kk=kk, hf=hf: e.matmul(PB[hf][:T, :], lhsT=MIXT[:, kk, :T], rhs=WOUT[:, kk, hf * 512:(hf + 1) * 512], start=(kk == 0), stop=(kk == 7)),
                     reads=[tMIXT, tWOUT], writes=[tPB[hf]])
            k.op("dve", lambda e, hf=hf: e.tensor_tensor(out=TMPc[:T, hf * 512:(hf + 1) * 512], in0=PB[hf][:T, :], in1=G1[:T, hf * 512:(hf + 1) * 512], op=ALU.mult), reads=[tPB[hf], tM2], writes=[tTMPc])
        k.op("dve", lambda e: e.tensor_tensor(out=X1[:T, :], in0=TMPc[:T, :], in1=XT[:T, :], op=ALU.add), reads=[tTMPc, tXT], writes=[tX1])
        k.dma("sp", lambda e: e.dma_start(out=x1_d[row0:row0 + T, :], in_=X1[:T, :]), reads=[tX1], writes=[t_x1_d])
        k.op("act", lambda e: e.activation(out=H2[:T, :], in_=X1[:T, :], func=AF.Square, accum_out=SM[:T, 2:3]), reads=[tX1], writes=[tH2, tSM])
        rstd_of(SM[:T, 2:3], T, 1.0 / D)
        k.op("dve", lambda e: e.scalar_tensor_tensor(out=TMPc[:T, :], in0=X1[:T, :], scalar=SM[:T, 2:3], in1=A2[:T, :], op0=ALU.mult, op1=ALU.mult), reads=[tX1, tSM, tM2], writes=[tTMPc])
        k.op("dve", lambda e: e.tensor_tensor(out=H2[:T, :], in0=TMPc[:T, :], in1=B2[:T, :], op=ALU.add), reads=[tTMPc, tM2], writes=[tH2])
        for kk in range(8):
            k.op("pe", lambda e, kk=kk: e.transpose(PT[0][:, kk * 128:kk * 128 + T], H2[:T, kk * 128:(kk + 1) * 128], C["ident_bf"][:T, :T]), reads=[tH2, tC], writes=[tPT[0]])
        k.op("act", lambda e: e.copy(out=H2T[:, :, :T], in_=PT[0][:, :].rearrange("p (k t) -> p k t", k=8)[:, :, :T]), reads=[tPT[0]], writes=[tH2T])
        k.dma("sp", lambda e: e.dma_start(out=h2T_d[:, :, row0:row0 + T].rearrange("k p t -> p k t"), in_=H2T[:, :, :T]), reads=[tH2T], writes=[t_h2T_d])
        for kk in range(8):
            k.op("pe", lambda e, kk=kk: e.matmul(PB[2][:T, 0:20], lhsT=H2T[:, kk, :T], rhs=WR[:, kk, :], start=(kk == 0), stop=(kk == 7)), reads=[tH2T, tWR], writes=[tPB[2]])
        k.op("dve", lambda e: e.tensor_tensor(out=LG[:T, :], in0=PB[2][:T, 0:20], in1=BR[:T, :], op=ALU.add), reads=[tPB[2], tG2], writes=[tRT])
        R = lambda a, b: RT[:T, a:b]

        def dv(fn):
            k.op("dve", fn, reads=[tRT], writes=[tRT])
        dv(lambda e: e.tensor_reduce(out=R(0, 1), in_=LG[:T, 0:4], axis=AX.X, op=ALU.max))
        dv(lambda e: e.tensor_scalar(out=R(4, 8), in0=LG[:T, 0:4], scalar1=R(0, 1), scalar2=None, op0=ALU.is_equal))
        dv(lambda e: e.tensor_scalar(out=R(8, 12), in0=LG[:T, 0:4], scalar1=R(0, 1), scalar2=None, op0=ALU.subtract))
        k.op("act", lambda e: e.activation(out=R(8, 12), in_=R(8, 12), func=AF.Exp), reads=[tRT], writes=[tRT])
        dv(lambda e: e.tensor_reduce(out=R(1, 2), in_=R(8, 12), axis=AX.X, op=ALU.add))
        dv(lambda e: e.reciprocal(out=R(1, 2), in_=R(1, 2)))
        dv(lambda e: e.tensor_tensor(out=RT[:T, 16:32].rearrange("p (g j) -> p g j", g=4), in0=LG[:T, 4:20].rearrange("p (g j) -> p g j", g=4),
                                     in1=R(4, 8).unsqueeze(2).to_broadcast([T, 4, 4]), op=ALU.mult))
        dv(lambda e: e.tensor_reduce(out=R(12, 16), in_=RT[:T, 16:32].rearrange("p (g j) -> p j g", g=4), axis=AX.X, op=ALU.add))
        dv(lambda e: e.tensor_reduce(out=R(2, 3), in_=R(12, 16), axis=AX.X, op=ALU.max))
        dv(lambda e: e.tensor_scalar(out=R(32, 36), in0=R(12, 16), scalar1=R(2, 3), scalar2=None, op0=ALU.is_equal))
        dv(lambda e: e.scalar_tensor_tensor(out=R(36, 40), in0=R(32, 36), scalar=-1e9, in1=R(12, 16), op0=ALU.mult, op1=ALU.add))
        dv(lambda e: e.tensor_reduce(out=R(3, 4), in_=R(36, 40), axis=AX.X, op=ALU.max))
        dv(lambda e: e.tensor_scalar(out=R(40, 44), in0=R(36, 40), scalar1=R(3, 4), scalar2=None, op0=ALU.is_equal))
        dv(lambda e: e.tensor_tensor(out=R(44, 45), in0=R(3, 4), in1=R(2, 3), op=ALU.subtract))
        k.op("act", lambda e: e.activation(out=R(44, 45), in_=R(44, 45), func=AF.Exp), reads=[tRT], writes=[tRT])
        dv(lambda e: e.tensor_scalar_add(out=R(45, 46), in0=R(44, 45), scalar1=1.0))
        dv(lambda e: e.reciprocal(out=R(45, 46), in_=R(45, 46)))
        dv(lambda e: e.tensor_tensor(out=R(46, 47), in0=R(45, 46), in1=R(44, 45), op=ALU.mult))
        dv(lambda e: e.tensor_tensor(out=R(45, 47), in0=R(45, 47), in1=R(1, 2).to_broadcast([T, 2]), op=ALU.mult))
        dv(lambda e: e.tensor_scalar(out=R(48, 52), in0=R(32, 36), scalar1=R(45, 46), scalar2=None, op0=ALU.mult))
        dv(lambda e: e.scalar_tensor_tensor(out=R(48, 52), in0=R(40, 44), scalar=R(46, 47), in1=R(48, 52), op0=ALU.mult, op1=ALU.add))
        dv(lambda e: e.tensor_tensor(out=COMB[:T, :].rearrange("p (g j) -> p g j", g=4), in0=R(4, 8).unsqueeze(2).to_broadcast([T, 4, 4]),
                                     in1=R(48, 52).unsqueeze(1).to_broadcast([T, 4, 4]), op=ALU.mult))
        k.dma("sp", lambda e: e.dma_start(out=comb_d[row0:row0 + T, :], in_=COMB[:T, :]), reads=[tRT], writes=[t_comb_d])

    load_mod(G1, 128, 2, tM2); load_mod(B2, 128, 3, tM2); load_mod(A2, 128, 4, tM2)
    for i in range(NT):
        r0 = i * 128
        k.dma("sp", lambda e, r0=r0: e.dma_start(out=XT[:, :], in_=xp[r0:r0 + 128, :]), writes=[tXT])
        k.dma("sp", lambda e, r0=r0: e.dma_start(out=ATT[:, :], in_=att_d[r0:r0 + 128, :]), reads=[t_att_d], writes=[tATT])
        k.dma("sp", lambda e, r0=r0: e.dma_start(out=ZD[:, :], in_=zdt_d[r0:r0 + 128, :]), reads=[t_zdt_d], writes=[tZD])
        conv_silu(128, [xbc_d[r0 + w:r0 + w + 128, :] for w in range(4)])
        softplus_dt(128)
        k.op("dve", lambda e: e.tensor_tensor(out=DT[:, 8:16], in0=DT[:, 0:8], in1=AN[:, :], op=ALU.mult), reads=[tDT, tG2], writes=[tDT])
        k.op("dve", lambda e: e.tensor_tensor(out=RU[:, :, :], in0=C["U"][:, :].unsqueeze(1).to_broadcast([128, 8, 128]),
                                              in1=DT[:, 8:16].unsqueeze(2).to_broadcast([128, 8, 128]), op=ALU.mult), reads=[tDT, tC], writes=[tRU])
        k.op("pe", lambda e: e.matmul(PB[4][:, 0:8], lhsT=C["U"][:, :], rhs=DT[:, 8:16], start=True, stop=True), reads=[tDT, tC], writes=[tPB[4]])
        k.op("dve", lambda e: e.tensor_scalar_mul(out=DT[:, 16:24], in0=PB[4][:, 0:8], scalar1=-1.0), reads=[tPB[4]], writes=[tDT])
        for hf in range(2):
            k.op("pe", lambda e, hf=hf: e.matmul(PB[2 + hf][:, :], lhsT=C["ones_f"][:, :], rhs=RU[:, 4 * hf:4 * hf + 4, :], start=True, stop=True), reads=[tRU, tC], writes=[tPB[2 + hf]])
            k.op("dve", lambda e, hf=hf: e.tensor_tensor(out=SG[:, :, :], in0=PB[2 + hf][:, :].rearrange("p (h l) -> p h l", h=4),
                                                         in1=C["NB"][:, :].unsqueeze(1).to_broadcast([128, 4, 128]), op=ALU.add), reads=[tPB[2 + hf], tC], writes=[tSG])
            for hh in range(4):
                h = 4 * hf + hh
                k.op("act", lambda e, h=h, hh=hh: e.activation(out=DEC[:, h, :], in_=SG[:, hh, :], func=AF.Exp, bias=DT[:, 16 + h:17 + h]), reads=[tSG, tDT], writes=[tDEC])
            k.op("act", lambda e, hf=hf: e.activation(out=EX[:, 4 * hf:4 * hf + 4, :], in_=PB[2 + hf][:64, :].rearrange("p (h l) -> p h l", h=4), func=AF.Exp), reads=[tPB[2 + hf]], writes=[tEX])
        for j in range(4):
            k.op("pe", lambda e, j=j: e.transpose(PT[1][:64, j * 128:(j + 1) * 128], XSB[:, 512 + j * 64:576 + j * 64], C["ident_bf"][:, :]), reads=[tXS, tC], writes=[tPT[1]])
        k.op("act", lambda e: e.copy(out=BCT[:, :, :], in_=PT[1][:64, 0:512].rearrange("p (j t) -> p j t", j=4)), reads=[tPT[1]], writes=[tBCT])
        for g in range(2):
            k.op("pe", lambda e, g=g: e.matmul(PB[5][:, g * 128:(g + 1) * 128], lhsT=BCT[:, g, :], rhs=BCT[:, 2 + g, :], start=True, stop=True), reads=[tBCT], writes=[tPB[5]])
        k.op("act", lambda e: e.copy(out=CBs[:, :, :], in_=PB[5][:, 0:256].rearrange("p (g l) -> p g l", g=2)), reads=[tPB[5]], writes=[tCBs])
        k.op("dve", lambda e: e.tensor_tensor(out=DT[:, 0:8], in0=DT[:, 0:8], in1=DT[:, 0:8], op=ALU.max), reads=[tDT], writes=[tDT])
        k.op("dve", lambda e: e.tensor_tensor(out=SM[:, 40:48], in0=DEC[:, :, 127], in1=DT[:, 0:8], op=ALU.mult), reads=[tDEC, tDT], writes=[tSM])
        for h in range(8):
            g = h // 4; s = h % 2
            k.op("dve", lambda e, h=h, g=g, s=s: e.scalar_tensor_tensor(out=MTt[s][:, :], in0=DEC[:, h, :], scalar=DT[:, h:h + 1], in1=CBs[:, g, :], op0=ALU.mult, op1=ALU.mult),
                 reads=[tDEC, tDT, tCBs], writes=[tMT[s]])
            k.op("pool", lambda e, h=h, g=g, s=s: e.tensor_tensor(out=CEt[s][:, :], in0=BCT[:, 2 + g, :], in1=EX[:, h, :], op=ALU.mult), reads=[tBCT, tEX], writes=[tCE[s]])
            k.op("pe", lambda e, h=h, s=s: e.matmul(PB[0][:, h * 64:(h + 1) * 64], lhsT=MTt[s][:, :], rhs=XSB[:, h * 64:(h + 1) * 64], start=True, stop=False), reads=[tMT[s], tXS], writes=[tPB[0]])
            k.op("pe", lambda e, h=h, s=s: e.matmul(PB[0][:, h * 64:(h + 1) * 64], lhsT=CEt[s][:, :], rhs=HTB[:, h, :], start=False, stop=True), reads=[tCE[s], tHT], writes=[tPB[0]])
            k.op("dve", lambda e, h=h, g=g, s=s: e.tensor_scalar(out=BWt[s][:, :], in0=XS[:, 512 + g * 64:576 + g * 64], scalar1=SM[:, 40 + h:41 + h], scalar2=None, op0=ALU.mult), reads=[tXS, tSM], writes=[tBW[s]])
            k.op("pe", lambda e, h=h, s=s: e.matmul(PB[1][:64, h * 64:(h + 1) * 64], lhsT=BWt[s][:, :], rhs=XSB[:, h * 64:(h + 1) * 64], start=True, stop=True), reads=[tBW[s], tXS], writes=[tPB[1]])
        k.op("dve", lambda e: e.tensor_tensor(out=HT[:, :, :], in0=HT[:, :, :], in1=EX[:, :, 127:128].to_broadcast([64, 8, 64]), op=ALU.mult), reads=[tHT, tEX], writes=[tHT])
        k.op("dve", lambda e: e.tensor_tensor(out=HT[:, :, :], in0=HT[:, :, :], in1=PB[1][:64, :].rearrange("p (h q) -> p h q", h=8), op=ALU.add), reads=[tHT, tPB[1]], writes=[tHT])
        k.op("pool", lambda e: e.tensor_copy(out=HTB[:, :, :], in_=HT[:, :, :]), reads=[tHT], writes=[tHT])
        k.op("dve", lambda e: e.tensor_tensor(out=TMPc[:, :512], in0=XS[:, 0:512], in1=DSK[:, :], op=ALU.mult), reads=[tXS, tG2], writes=[tTMPc])
        k.op("dve", lambda e: e.tensor_tensor(out=YS[:, :], in0=TMPc[:, :512], in1=PB[0][:, :], op=ALU.add), reads=[tTMPc, tPB[0]], writes=[tYS])
        finish(128, r0, None)
    for h in range(8):
        k.op("pe", lambda e, h=h: e.transpose(PB[2][:64, h * 64:(h + 1) * 64], HT[:, h, :], C["ident_f"][:64, :64]), reads=[tHT, tC], writes=[tPB[2]])
    k.op("act", lambda e: e.copy(out=TMPc[:64, :512], in_=PB[2][:64, :]), reads=[tPB[2]], writes=[tTMPc])
    k.dma("sp", lambda e: e.dma_start(out=ssmp.rearrange("h p n -> p h n"), in_=TMPc[:64, :512].rearrange("p (h n) -> p h n", h=8)), reads=[tTMPc], writes=[otok()])

    T = TS
    load_mod(G1, TS, 2, tM2); load_mod(B2, TS, 3, tM2); load_mod(A2, TS, 4, tM2)
    k.dma("sp", lambda e: e.dma_start(out=XT[:T, :], in_=xs[:, :]), writes=[tXT])
    k.dma("sp", lambda e: e.dma_start(out=ATT[:T, :], in_=att_d[SEQ:SEQ + T, :]), reads=[t_att_d], writes=[tATT])
    k.dma("sp", lambda e: e.dma_start(out=ZD[:T, :], in_=zdt_d[SEQ:SEQ + T, :]), reads=[t_zdt_d], writes=[tZD])
    conv_silu(T, [xbcs_d[:, w:w + 4, :] for w in range(4)])
    softplus_dt(T)
    k.dma("sp", lambda e: e.dma_start(out=ssc_d[:, 0:768], in_=XS[:T, :]), reads=[tXS], writes=[t_ssc_d])
    k.dma("sp", lambda e: e.dma_start(out=ssc_d[:, 768:776], in_=DT[:T, 0:8]), reads=[tDT], writes=[t_ssc_d])
    Hs = k.sb("Hs", [128, 4096], F32); tHs = Tok()
    k.dma("sp", lambda e: e.dma_start(out=Hs[:, :], in_=sssm[:, :]), writes=[tHs])
    Xbh = k.sb("Xbh", [128, 4, 64], F32); Bbh = k.sb("Bbh", [128, 4, 64], F32); Cbh = k.sb("Cbh", [128, 4, 64], F32); Dbh = k.sb("Dbh", [128, 4], F32); tBH = Tok()
    for b in range(NSEQ):
        k.dma("sp", lambda e, b=b: e.dma_start(out=Xbh[8 * b:8 * b + 8, :, :], in_=ssc_d[4 * b:4 * b + 4, 0:512].rearrange("t (h p) -> h t p", p=64)), reads=[t_ssc_d], writes=[tBH])
        k.dma("sp", lambda e, b=b: e.dma_start(out=Dbh[8 * b:8 * b + 8, :], in_=ssc_d[4 * b:4 * b + 4, 768:776].rearrange("t h -> h t")), reads=[t_ssc_d], writes=[tBH])
        for g in range(2):
            k.dma("sp", lambda e, b=b, g=g: e.dma_start(out=Bbh[8 * b + 4 * g:8 * b + 4 * g + 4, :, :], in_=ssc_d[4 * b:4 * b + 4, 512 + 64 * g:576 + 64 * g].partition_broadcast(4)), reads=[t_ssc_d], writes=[tBH])
            k.dma("sp", lambda e, b=b, g=g: e.dma_start(out=Cbh[8 * b + 4 * g:8 * b + 4 * g + 4, :, :], in_=ssc_d[4 * b:4 * b + 4, 640 + 64 * g:704 + 64 * g].partition_broadcast(4)), reads=[t_ssc_d], writes=[tBH])
    OUTER = k.sb("OUTER", [128, 4096], F32); tOUT = Tok()
    Ybh = k.sb("Ybh", [128, 4, 64], F32); tY = Tok()
    SS = k.sb("SS", [128, 8], F32); XDT = k.sb("XDT", [128, 64], F32); tSS = Tok()
    for t in range(4):
        k.op("act", lambda e, t=t: e.activation(out=SS[:, 0:1], in_=Dbh[:, t:t + 1], func=AF.Exp, scale=ABH[:, 0:1]), reads=[tBH, tG2], writes=[tSS])
        k.op("dve", lambda e, t=t: e.tensor_scalar(out=XDT[:, :], in0=Xbh[:, t, :], scalar1=Dbh[:, t:t + 1], scalar2=None, op0=ALU.mult), reads=[tBH], writes=[tSS])
        k.op("dve", lambda e, t=t: e.tensor_tensor(out=OUTER[:, :].rearrange("p (a n) -> p a n", n=64), in0=XDT[:, :].unsqueeze(2).to_broadcast([128, 64, 64]),
                                                   in1=Bbh[:, t, :].unsqueeze(1).to_broadcast([128, 64, 64]), op=ALU.mult), reads=[tSS, tBH], writes=[tOUT])
        k.op("dve", lambda e: e.scalar_tensor_tensor(out=Hs[:, :], in0=Hs[:, :], scalar=SS[:, 0:1], in1=OUTER[:, :], op0=ALU.mult, op1=ALU.add), reads=[tHs, tSS, tOUT], writes=[tHs])
        k.op("dve", lambda e, t=t: e.tensor_tensor(out=OUTER[:, :].rearrange("p (a n) -> p a n", n=64), in0=Hs[:, :].rearrange("p (a n) -> p a n", n=64),
                                                   in1=Cbh[:, t, :].unsqueeze(1).to_broadcast([128, 64, 64]), op=ALU.mult), reads=[tHs, tBH], writes=[tOUT])
        k.op("dve", lambda e, t=t: e.tensor_reduce(out=Ybh[:, t, :], in_=OUTER[:, :].rearrange("p (a n) -> p a n", n=64), axis=AX.X, op=ALU.add), reads=[tOUT], writes=[tY])
        k.op("dve", lambda e, t=t: e.scalar_tensor_tensor(out=Ybh[:, t, :], in0=Xbh[:, t, :], scalar=DBH[:, 0:1], in1=Ybh[:, t, :], op0=ALU.mult, op1=ALU.add), reads=[tBH, tY, tG2], writes=[tY])
    k.dma("sp", lambda e: e.dma_start(out=ssms[:, :], in_=Hs[:, :]), reads=[tHs], writes=[otok()])
    for b in range(NSEQ):
        k.dma("sp", lambda e, b=b: e.dma_start(out=ysd_d[b, :, :].rearrange("t (h p) -> h t p", p=64), in_=Ybh[8 * b:8 * b + 8, :, :]), reads=[tY], writes=[t_ysd_d])
    k.dma("sp", lambda e: e.dma_start(out=YS[:T, :], in_=ysd_d.rearrange("b t c -> (b t) c")), reads=[t_ysd_d], writes=[tYS])
    finish(T, SEQ, None)
    k.pop()

    k.push()
    NG = [(g * 512, 512) for g in range(4)] + [(SEQ, TS)]
    H2A = k.sb("H2A", [128, 8, NTOK], BF16); tH2A = Tok()
    for kk in range(8):
        k.dma("sp", lambda e, kk=kk: e.dma_start(out=H2A[:, kk, :], in_=h2T_d[kk, :, :]), reads=[t_h2T_d], writes=[tH2A])
    NTL = 17
    MACC = k.sb("MACC", [128, NTL, D], F32); tMACC = [Tok() for _ in range(NTL)]
    CMB = k.sb("CMB", [128, NTL, 16], F32); tCMB = Tok()
    for j in range(NTL):
        T = 128 if j < 16 else TS
        k.dma("sp", lambda e, j=j, T=T: e.dma_start(out=CMB[:T, j, :], in_=comb_d[j * 128:j * 128 + T, :]), reads=[t_comb_d], writes=[tCMB])
    k.op("pool", lambda e: e.memset(MACC[:, :, :], 0.0), writes=tMACC)
    stm = [k.sb(f"stm{i}", [128, 8, 256], F32) for i in range(2)]; tstm = [Tok(), Tok()]
    WG = [k.sb(f"WG{i}", [128, 8, 256], BF16) for i in range(2)]; WU = [k.sb(f"WU{i}", [128, 8, 256], BF16) for i in range(2)]
    WD = [k.sb(f"WD{i}", [128, 2, D], BF16) for i in range(2)]; tW = [Tok(), Tok()]
    HE = k.sb("HE", [128, 2, 512], BF16); tHE = Tok()
    SG_ = k.sb("SGm", [128, 512], F32); tSGm = Tok()
    n_exp = 16 if with_moe else 0
    sidx = 0
    for ex in range(n_exp):
        s = ex % 2
        for (W_, src) in ((WG[s], w_gate), (WU[s], w_up)):
            ss = sidx % 2; sidx += 1
            k.dma("sp", lambda e, src=src, ss=ss, ex=ex: e.dma_start(out=stm[ss][:, :, :], in_=src[ex].rearrange("(k p) f -> p k f", p=128)), writes=[tstm[ss]])
            k.op("pool", lambda e, W_=W_, ss=ss: e.tensor_copy(out=W_[:, :, :], in_=stm[ss][:, :, :]), reads=[tstm[ss]], writes=[tW[s]])
        ss = sidx % 2; sidx += 1
        k.dma("sp", lambda e, ss=ss, ex=ex: e.dma_start(out=stm[ss][:, :, :].rearrange("p (c a) f -> p c (a f)", c=2), in_=w_down[ex].rearrange("(c p) n -> p c n", p=128)), writes=[tstm[ss]])
        k.op("pool", lambda e, s=s, ss=ss: e.tensor_copy(out=WD[s][:, :, :], in_=stm[ss][:, :, :].rearrange("p (c a) f -> p c (a f)", c=2)), reads=[tstm[ss]], writes=[tW[s]])
        for (t0, nt) in NG:
            for c in range(2):
                for (W_, pb) in ((WG[s], 0), (WU[s], 1)):
                    for kk in range(8):
                        k.op("pe", lambda e, W_=W_, pb=pb, kk=kk, c=c, t0=t0, nt=nt: e.matmul(PB[pb][:, :nt], lhsT=W_[:, kk, c * 128:(c + 1) * 128], rhs=H2A[:, kk, t0:t0 + nt], start=(kk == 0), stop=(kk == 7)),
                             reads=[tW[s], tH2A], writes=[tPB[pb]])
                k.op("act", lambda e, nt=nt: e.activation(out=SG_[:, :nt], in_=PB[0][:, :nt], func=AF.Silu), reads=[tPB[0]], writes=[tSGm])
                k.op("dve", lambda e, c=c, nt=nt: e.tensor_tensor(out=HE[:, c, :nt], in0=SG_[:, :nt], in1=PB[1][:, :nt], op=ALU.mult), reads=[tSGm, tPB[1]], writes=[tHE])
            for tt in range((nt + 127) // 128):
                T = min(128, nt - tt * 128); j = (t0 + tt * 128) // 128
                for hf in range(2):
                    pb = 2 + hf
                    for c in range(2):
                        k.op("pe", lambda e, pb=pb, c=c, tt=tt, T=T, hf=hf: e.matmul(PB[pb][:T, :], lhsT=HE[:, c, tt * 128:tt * 128 + T], rhs=WD[s][:, c, hf * 512:(hf + 1) * 512], start=(c == 0), stop=(c == 1)),
                             reads=[tHE, tW[s]], writes=[tPB[pb]])
                    k.op("dve", lambda e, pb=pb, T=T, j=j, hf=hf, ex=ex: e.scalar_tensor_tensor(out=MACC[:T, j, hf * 512:(hf + 1) * 512], in0=PB[pb][:T, :], scalar=CMB[:T, j, ex:ex + 1],
                                                                                         in1=MACC[:T, j, hf * 512:(hf + 1) * 512], op0=ALU.mult, op1=ALU.add), reads=[tPB[pb], tCMB, tMACC[j]], writes=[tMACC[j]])
    G2t = k.sb("G2t", [128, D], F32); tG2t = Tok()
    X1b = [k.sb(f"X1b{i}", [128, D], F32) for i in range(2)]; tX1b = [Tok(), Tok()]
    load_mod(G2t, 128, 5, tG2t)
    for j in range(NTL):
        T = 128 if j < 16 else TS
        if j == 16:
            load_mod(G2t, TS, 5, tG2t)
        s = j % 2
        k.dma("sp", lambda e, j=j, T=T, s=s: e.dma_start(out=X1b[s][:T, :], in_=x1_d[j * 128:j * 128 + T, :]), reads=[t_x1_d], writes=[tX1b[s]])
        k.op("dve", lambda e, j=j, T=T: e.tensor_tensor(out=MACC[:T, j, :], in0=MACC[:T, j, :], in1=G2t[:T, :], op=ALU.mult), reads=[tMACC[j], tG2t], writes=[tMACC[j]])
        k.op("pool", lambda e, j=j, T=T, s=s: e.tensor_tensor(out=X1b[s][:T, :], in0=X1b[s][:T, :], in1=MACC[:T, j, :], op=ALU.add), reads=[tMACC[j], tX1b[s]], writes=[tX1b[s]])
        dst = yp[j * 128:j * 128 + T, :] if j < 16 else ys[:, :]
        k.dma("sp", lambda e, T=T, s=s, dst=dst: e.dma_start(out=dst, in_=X1b[s][:T, :]), reads=[tX1b[s]], writes=[otok()])
    k.finish(out_toks)
    k.pop()
    k.es.close()
    return k


def _run(inp, debug=False, compact=False):
    f32 = lambda a: np.ascontiguousarray(np.asarray(a, dtype=np.float32))
    consts = make_consts()
    cshapes = {n: (list(v.shape), v.dtype != np.float32) for n, v in consts.items()}
    nc = bass.Bass("TRN2", target_bir_lowering=False)
    kb = build(nc, cshapes, debug=debug, pool_rows=(1024 * 128 if compact else 1310720))

    xp = f32(inp["x_prompt"]); xs = f32(inp["x_sample"])
    w_in = f32(inp["w_in"])[0]
    perm = np.concatenate([np.arange(0, 1304), np.arange(2584, 2592), np.arange(1304, 2584)])
    w_in_p = np.ascontiguousarray(w_in[:, perm])
    w_rg = f32(inp["w_rg"])[0]; w_re = f32(inp["w_re"])[0]
    w_r = np.ascontiguousarray(np.concatenate([w_rg] + [w_re[g] for g in range(4)], axis=1))
    b_r = np.ascontiguousarray(np.concatenate([f32(inp["b_rg"])[0], f32(inp["b_re"])[0].reshape(-1)]))
    wpk = f32(inp["w_pos_k"])[0]; wpv = f32(inp["w_pos_v"])[0]
    wkblk = np.zeros((128, 4), np.float32); wvsel = np.zeros((128, 16, 64), np.float32)
    for r in range(128):
        wkblk[r, r // 32] = wpk[r % 32]
        for i in range(16):
            wvsel[r, i, 4 * i + r // 32] = wpv[r % 32]
    wk32 = np.zeros((128, 32, 128), np.float32); wv32 = np.zeros((128, 32, 128), np.float32)
    for p_ in range(128):
        ps, rg = p_ // 32, p_ % 32
        for d_ in range(8):
            for i_ in range(4):
                m_ = 16 * d_ + ps * 4 + rg // 8
                wk32[p_, d_ * 4 + i_, m_] = wpk[4 * (rg % 8) + i_]
                wv32[p_, d_ * 4 + i_, m_] = wpv[4 * (rg % 8) + i_]
    shared = {
        "g_norm1": f32(inp["g_norm1"])[0], "g_norm2": f32(inp["g_norm2"])[0],
        "w_ada": f32(inp["w_ada"])[0], "b_ada": f32(inp["b_ada"])[0], "w_in": w_in_p,
        "gq8": np.tile(f32(inp["g_q"])[0], 8), "gks2": np.tile(f32(inp["g_k_sel"])[0], 2), "gkw2": np.tile(f32(inp["g_k_win"])[0], 2),
        "gkc": f32(inp["g_k_cmp"])[0].reshape(64, 1),
        "convw": f32(inp["conv_w"])[0].reshape(-1), "convb": f32(inp["conv_b"])[0],
        "dtb": f32(inp["dt_bias"])[0], "alog": f32(inp["a_log"])[0], "abh": np.tile(f32(inp["a_log"])[0], 16).reshape(128, 1), "dbh": np.tile(f32(inp["d_skip"])[0], 16).reshape(128, 1), "dsk": np.repeat(f32(inp["d_skip"])[0], 64),
        "gatt": f32(inp["g_att_out"])[0], "gssm": f32(inp["g_ssm_out"])[0],
        "w_out": f32(inp["w_out"])[0], "w_r": w_r, "b_r": b_r,
        "w_gate": f32(inp["w_gate"])[0], "w_up": f32(inp["w_up"])[0], "w_down": f32(inp["w_down"])[0],
        "wkblk": wkblk, "wvsel": wvsel.reshape(128, 1024),
        "wk32": wk32.reshape(128, 4096), "wv32": wv32.reshape(128, 4096), "gkc2": np.tile(f32(inp["g_k_cmp"])[0], 2),
    }
    for n, v in consts.items():
        shared["c_" + n] = v
    cwin = f32(inp["cache_win"])[0].reshape(128, 512, 256)
    sssm = f32(inp["state_ssm"])[0].reshape(128 * 8, 4096)
    sconv = f32(inp["state_conv"])[0]
    ccmp = np.asarray(inp["cache_cmp"], dtype=np.float32).reshape(10240 * 128, 256)
    csel = np.asarray(inp["cache_sel"], dtype=np.float32).reshape(10240 * 128, 256)
    ptab = np.ascontiguousarray(np.asarray(inp["page_table"], dtype=np.int32))
    in_maps = []
    for c in range(NCORES):
        m = dict(shared)
        if compact:
            pg = ptab[16 * c:16 * c + 16].reshape(-1)
            m["ccmp"] = ccmp.reshape(10240, 128 * 256)[pg].reshape(-1, 256); m["csel"] = csel.reshape(10240, 128 * 256)[pg].reshape(-1, 256)
            m["ptab"] = np.arange(1024, dtype=np.int32).reshape(16, 64)
        else:
            m["ccmp"] = ccmp; m["csel"] = csel; m["ptab"] = ptab[16 * c:16 * c + 16]
        m["xp"] = xp[c]; m["xs"] = xs[16 * c:16 * c + 16].reshape(TS, D)
        m["cp"] = f32(inp["c_prompt"])[c:c + 1]; m["cs"] = f32(inp["c_sample"])[16 * c:16 * c + 16]
        m["sssm"] = sssm[128 * c:128 * c + 128]; m["sconv"] = sconv[16 * c:16 * c + 16]; m["cwin"] = cwin[16 * c:16 * c + 16]
        in_maps.append(m)
    res = run_bass_kernel_spmd(nc, in_maps, core_ids=list(range(NCORES)))
    R = res.results
    cat = lambda n: np.stack([R[c][n] for c in range(NCORES)])
    y_p = cat("yp"); y_s = cat("ys").reshape(128, 4, D)
    cmp_p = cat("cmpp").reshape(1, 8, SEQ, 2, 2, 64); cmp_s = cat("cmps").reshape(1, 128, 4, 2, 2, 64)
    sel_p = cat("selp").reshape(1, 8, SEQ, 2, 2, 64); sel_s = cat("sels").reshape(1, 128, 4, 2, 2, 64)
    win_p = cat("winp").reshape(1, 8, 512, 2, 2, 64); win_s = cat("wins").reshape(1, 128, 512, 2, 2, 64)
    ssm_p = cat("ssmp").reshape(1, 8, 8, 64, 64); ssm_s = cat("ssms").reshape(1, 128, 8, 64, 64)
    conv_p = cat("convp").reshape(1, 8, 3, 768); conv_s = cat("convs").reshape(1, 128, 3, 768)
    outs = (y_p, y_s, cmp_p, cmp_s, sel_p, sel_s, win_p, win_s, ssm_p, ssm_s, conv_p, conv_s)
    if debug:
        return outs, {n: cat(n) for n in ("att_d", "x1_d", "comb_d")}
    return outs


def kernel(**inp):
    return _run(inp, False)
```

```python
import numpy as np
import ml_dtypes
from contextlib import ExitStack
import concourse.bass as bass
import concourse.mybir as mybir
from concourse.bass_utils import run_bass_kernel_spmd

F32 = mybir.dt.float32; BF16 = mybir.dt.bfloat16; I32 = mybir.dt.int32
AF = mybir.ActivationFunctionType; ALU = mybir.AluOpType; AX = mybir.AxisListType
NCORES = 8
SEQ = 2048; D = 1024; NT = 16; TS = 64; NSEQ = 16
INW = 2592
SCALE = 0.125
EPS = 1e-6
NEGB = -30000.0


class Tok:
    __slots__ = ("name", "w", "rs")

    def __init__(self, name="t"):
        self.name = name; self.w = None; self.rs = []


class KB:
    NSLOT = 8

    def __init__(self, nc):
        self.nc = nc; self.es = ExitStack(); self.stack = [self.es]
        self.eng = {"pe": nc.tensor, "act": nc.scalar, "dve": nc.vector, "pool": nc.gpsimd, "sp": nc.sync}
        self.sem = {k: self.es.enter_context(nc.semaphore("s_" + k)) for k in self.eng}
        self.cnt = {k: 0 for k in self.eng}
        self.seen = {k: {} for k in self.eng}
        self.dsem = {}; self.dcnt = {}; self.dnext = {}
        self.nslot = {"sp": 8, "act": 2, "pool": 16}
        for q in ("sp", "act", "pool"):
            self.dsem[q] = [self.es.enter_context(nc.semaphore(f"d_{q}{i}")) for i in range(self.nslot[q])]
            self.dcnt[q] = [0] * self.nslot[q]; self.dnext[q] = 0
        self.nins = 0

    def sb(self, name, shape, dt):
        return self.stack[-1].enter_context(self.nc.sbuf_tensor(name, list(shape), dt))

    def ps(self, name, shape, dt):
        return self.es.enter_context(self.nc.psum_tensor(name, list(shape), dt))

    def _wait(self, e, ev):
        if ev is None:
            return
        key, val = ev
        if self.seen[e].get(key, 0) >= val:
            return
        self.seen[e][key] = val
        sem = self.sem[key[1]] if key[0] == "c" else self.dsem[key[1]][key[2]]
        self.eng[e].wait_ge(sem, val)

    def _deps(self, e, reads, writes):
        for t in reads:
            self._wait(e, t.w)
        for t in writes:
            self._wait(e, t.w)
            for r in t.rs:
                self._wait(e, r)

    def _commit(self, ev, reads, writes):
        for t in reads:
            t.rs = [r for r in t.rs if r[0] != ev[0]] + [ev]
        for t in writes:
            t.w = ev; t.rs = []

    def op(self, e, fn, reads=(), writes=()):
        self._deps(e, reads, writes)
        ins = fn(self.eng[e])
        self.cnt[e] += 1
        ins.then_inc(self.sem[e], 1)
        ev = (("c", e), self.cnt[e])
        self._commit(ev, reads, writes); self.nins += 1
        return ins

    def dma(self, q, fn, reads=(), writes=()):
        s = self.dnext[q]; self.dnext[q] = (s + 1) % self.nslot[q]
        key = ("d", q, s)
        if self.dcnt[q][s] > 0:
            self._wait(q, (key, self.dcnt[q][s]))
        self._deps(q, reads, writes)
        ins = fn(self.eng[q])
        self.dcnt[q][s] += 16
        ins.then_inc(self.dsem[q][s], 16)
        ev = (key, self.dcnt[q][s])
        self._commit(ev, reads, writes); self.nins += 1
        return ins

    def push(self):
        self.stack.append(ExitStack())

    def barrier(self):
        for e in self.eng:
            for e2 in self.eng:
                if e2 != e and self.cnt[e2] > 0:
                    self._wait(e, (("c", e2), self.cnt[e2]))
            for q in self.dsem:
                for s in range(self.nslot[q]):
                    if self.dcnt[q][s] > 0:
                        self._wait(e, (("d", q, s), self.dcnt[q][s]))

    def pop(self):
        self.barrier()
        self.stack.pop().close()

    def finish(self, toks):
        for t in toks:
            self._wait("sp", t.w)
            for r in t.rs:
                self._wait("sp", r)


def _bf(a):
    return np.ascontiguousarray(a.astype(np.float32)).astype(ml_dtypes.bfloat16)


def make_consts():
    c = {}
    c["ident_bf"] = _bf(np.eye(128))
    c["ident_f"] = np.eye(128, dtype=np.float32)
    tk = np.arange(128)[:, None]; tq = np.arange(128)[None, :]
    cb = np.where(tk <= tq, 0.0, NEGB)
    c["causalb"] = _bf(np.tile(cb, (1, 4)))
    ab = np.where(tk > tq, 0.0, NEGB)
    c["antib"] = _bf(np.tile(ab, (1, 4)))
    E = np.zeros((32, 16, 128), np.float32)
    for cc in range(16):
        for t in range(128):
            E[2 * cc + t // 64, cc, t] = 1.0
    c["Eall"] = _bf(E.reshape(32, 16 * 128))
    Dsel = np.zeros((4, 124), np.float32)
    for jj in range(4):
        Dsel[jj, 60 + jj] = 1.0
    c["Dsel"] = _bf(Dsel)
    p = np.arange(128)[None, :]; jj = np.arange(4)[:, None]
    cmpB = np.where(32 * jj + 31 <= p, 0.0, NEGB)
    c["cmpB"] = _bf(np.tile(cmpB, (1, 4)))
    pc = np.arange(128)[:, None]; cc = np.arange(124)[None, :]
    c["cmpM0"] = ((cc - 60) <= np.floor((pc - 31) / 32.0)).astype(np.float32)
    selA = np.zeros((128, 16, 32), np.float32); selB = np.zeros((128, 16, 32), np.float32)
    allowed = np.zeros((128, 16, 32), np.float32)
    for i in range(16):
        for pp in range(128):
            cur = 2 * i + (1 if pp >= 64 else 0)
            for n in range(32):
                if n < cur:
                    allowed[pp, i, n] = 1.0
                    if n == cur - 1:
                        selB[pp, i, n] = 2e9
                    elif n == 0:
                        selB[pp, i, n] = 1e9
                    else:
                        selA[pp, i, n] = 1.0
                else:
                    selB[pp, i, n] = -1e30
    c["selA"] = selA.reshape(128, 512); c["selB"] = selB.reshape(128, 512); c["allowed"] = allowed.reshape(128, 512)
    s = np.arange(128)[:, None]; l = np.arange(128)[None, :]
    c["U"] = (s <= l).astype(np.float32)
    c["NB"] = np.where(l >= s, 0.0, -1e30).astype(np.float32)
    c["ones_f"] = np.ones((128, 128), np.float32)
    pp = np.arange(128)
    c["PM4"] = (pp % 4).astype(np.float32).reshape(128, 1)
    c["R64"] = (pp % 64).astype(np.float32).reshape(128, 1)
    H = np.zeros((32, 8), np.float32)
    for g in range(2):
        for t in range(4):
            for q in range(4):
                H[g * 16 + t * 4 + q, g * 4 + t] = 1.0
    c["HSEL"] = H
    B = np.zeros((128, 128), np.float32); B[:, 0] = 1e9; B[:, 127] = 2e9
    HB_ = np.zeros((32, 16, 128), np.float32)
    for b_ in range(16):
        for g in range(2):
            for t in range(4):
                for q in range(4):
                    HB_[g * 16 + t * 4 + q, b_, b_ * 8 + g * 4 + t] = 1.0
    c["HSELB"] = HB_.reshape(32, 2048)
    c["BIGS"] = B
    c["I8"] = np.eye(8, dtype=np.float32)
    E16 = np.zeros((16, 128), np.float32)
    for p_ in range(128):
        E16[p_ // 8, p_] = 1.0
    c["EXP16"] = E16
    c["PM8"] = (pp % 8).astype(np.float32).reshape(128, 1)
    c["PIDX"] = pp.astype(np.float32).reshape(128, 1)
    c["PM32"] = (pp % 32).astype(np.float32).reshape(128, 1)
    c["OH4"] = (pp[:, None] // 32 == np.arange(4)[None, :]).astype(np.float32)
    c["M120"] = (pp < 120).astype(np.float32).reshape(128, 1)
    m15 = np.ones((128, 8), np.float32); m15[64:, 7] = 0.0
    c["MASK15"] = m15
    c["MASKW"] = (pp[:, None] > np.arange(4)[None, :]).astype(np.float32)
    c["CM4"] = (np.arange(4)[:, None] <= np.arange(4)[None, :]).astype(np.float32)
    return c


def build(nc, cshapes, with_moe=True, debug=False, pool_rows=1310720):
    k = KB(nc)
    k.es.enter_context(nc.allow_non_contiguous_dma(reason="small strided loads"))

    def din(name, shape, dt=F32):
        return nc.dram_tensor(name, list(shape), dt, kind="ExternalInput").ap()

    def dout(name, shape, dt=F32):
        return nc.dram_tensor(name, list(shape), dt, kind="ExternalOutput").ap()

    def dint(name, shape, dt=F32):
        kind = "ExternalOutput" if (debug and name in ("att_d", "x1_d", "comb_d")) else "Internal"
        return nc.dram_tensor(name, list(shape), dt, kind=kind).ap()

    xp = din("xp", [SEQ, D]); xs = din("xs", [TS, D]); cpr = din("cp", [1, D]); csm = din("cs", [NSEQ, D])
    sssm = din("sssm", [128, 4096]); sconv = din("sconv", [NSEQ, 3, 768]); cwin = din("cwin", [NSEQ, 512, 256])
    gn1 = din("g_norm1", [D]); gn2 = din("g_norm2", [D])
    w_ada = din("w_ada", [D, 6 * D]); b_ada = din("b_ada", [6 * D])
    w_in = din("w_in", [D, INW])
    gq8 = din("gq8", [512]); gks2 = din("gks2", [128]); gkw2 = din("gkw2", [128]); gkc = din("gkc", [64, 1])
    convw = din("convw", [4 * 768]); convb = din("convb", [768])
    dtb = din("dtb", [8]); alog = din("alog", [8]); dsk = din("dsk", [512])
    abh = din("abh", [128, 1]); dbh = din("dbh", [128, 1])
    gatt = din("gatt", [512]); gssm = din("gssm", [512])
    w_out = din("w_out", [D, D]); w_r = din("w_r", [D, 20]); b_r = din("b_r", [20])
    w_gate = din("w_gate", [16, D, 256]); w_up = din("w_up", [16, D, 256]); w_down = din("w_down", [16, 256, D])
    wkblk = din("wkblk", [128, 4]); wvsel = din("wvsel", [128, 16 * 64])
    ccmp = din("ccmp", [pool_rows, 256]); csel = din("csel", [pool_rows, 256]); ptab = din("ptab", [NSEQ, 64], I32)
    wk32 = din("wk32", [128, 4096]); wv32 = din("wv32", [128, 4096]); gkc2 = din("gkc2", [128])
    cd = {}
    for n, (shp, isbf) in cshapes.items():
        cd[n] = din("c_" + n, shp, BF16 if isbf else F32)

    yp = dout("yp", [SEQ, D]); ys = dout("ys", [TS, D])
    cmpp = dout("cmpp", [SEQ, 256]); cmps = dout("cmps", [TS, 256])
    selp = dout("selp", [SEQ, 256]); sels = dout("sels", [TS, 256])
    winp = dout("winp", [512, 256]); wins = dout("wins", [NSEQ, 512, 256])
    ssmp = dout("ssmp", [8, 64, 64]); ssms = dout("ssms", [128, 4096])
    convp = dout("convp", [3, 768]); convs = dout("convs", [NSEQ, 3, 768])

    NTOK = SEQ + TS
    mods_d = dint("mods_d", [65, 6 * D]); t_mods_d = Tok()
    xbc_d = dint("xbc_d", [SEQ + 3, 768]); t_xbc_d = Tok()
    xbcs_d = dint("xbcs_d", [NSEQ, 7, 768]); t_xbcs_d = Tok()
    zdt_d = dint("zdt_d", [NTOK, 520]); t_zdt_d = Tok()
    att_d = dint("att_d", [NTOK, 512]); t_att_d = Tok()
    x1_d = dint("x1_d", [NTOK, D]); t_x1_d = Tok()
    h2T_d = dint("h2T_d", [8, 128, NTOK], BF16); t_h2T_d = Tok()
    comb_d = dint("comb_d", [NTOK, 16]); t_comb_d = Tok()
    ssc_d = dint("ssc_d", [TS, 776]); t_ssc_d = Tok()
    ysd_d = dint("ysd_d", [NSEQ, 4, 512]); t_ysd_d = Tok()
    qs_d = dint("qs_d", [TS, 512], BF16); t_qs_d = Tok()
    kvs_d = dint("kvs_d", [TS, 512]); t_kvs_d = Tok()
    gts_d = dint("gts_d", [TS, 24]); t_gts_d = Tok()
    atts_d = dint("atts_d", [3, NSEQ, 4, 2, 4, 64]); t_atts_d = Tok()
    out_toks = []

    def otok():
        t = Tok(); out_toks.append(t); return t

    C = {}; tC = Tok("consts")
    for n, (shp, isbf) in cshapes.items():
        C[n] = k.sb("C_" + n, shp, BF16 if isbf else F32)
        k.dma("sp", lambda e, n=n: e.dma_start(out=C[n][:], in_=cd[n]), writes=[tC])
    EPSC = k.sb("EPSC", [128, 1], F32)
    k.op("dve", lambda e: e.memset(EPSC[:], EPS), writes=[tC])
    SM = k.sb("SM", [128, 64], F32); tSM = Tok()
    PB = [k.es.enter_context(nc.psum_tensor(f"pb{i}", [128, 512], F32)) for i in range(6)]
    tPB = [Tok(f"pb{i}") for i in range(6)]
    PT = [k.es.enter_context(nc.psum_tensor(f"pt{i}", [128, 1024], BF16)) for i in range(2)]
    tPT = [Tok(f"pt{i}") for i in range(2)]

    def bc_load(name, src1d, width, parts=128, tok=None):
        t = k.sb(name, [parts, width], F32)
        k.dma("sp", lambda e: e.dma_start(out=t[:], in_=src1d.partition_broadcast(parts)), writes=[tok or tC])
        return t

    def rstd_of(ap, T, scale):
        k.op("act", lambda e: e.activation(out=ap, in_=ap, func=AF.Sqrt, bias=EPSC[:T, :], scale=scale), reads=[tSM, tC], writes=[tSM])
        k.op("dve", lambda e: e.reciprocal(out=ap, in_=ap), reads=[tSM], writes=[tSM])

    k.push()
    GN1 = bc_load("GN1", gn1, D, 65); GN2 = bc_load("GN2", gn2, D, 65)
    stg = [k.sb(f"stg{i}", [128, 512], F32) for i in range(2)]; tstg = [Tok(), Tok()]
    cT = k.sb("cT", [128, 8, 65], F32); tcT = Tok()
    k.dma("sp", lambda e: e.dma_start(out=cT[:, :, 0:1], in_=cpr.rearrange("b (k p) -> p k b", p=128)), writes=[tcT])
    for kk in range(8):
        k.dma("sp", lambda e, kk=kk: e.dma_start(out=cT[:, kk, 1:17], in_=csm[:, kk * 128:(kk + 1) * 128].rearrange("b p -> p b")), writes=[tcT])
    scT = k.sb("scT", [128, 8, 17], F32)
    k.op("act", lambda e: e.activation(out=scT[:], in_=cT[:, :, 0:17], func=AF.Silu), reads=[tcT], writes=[tcT])
    L = k.sb("L", [128, 8, 65], BF16)
    k.op("dve", lambda e: e.tensor_copy(out=L[:, :, 0:1], in_=scT[:, :, 0:1]), reads=[tcT], writes=[tcT])
    k.op("dve", lambda e: e.tensor_copy(out=L[:, :, 1:65].rearrange("p k (b t) -> p k b t", t=4),
                                        in_=scT[:, :, 1:17].unsqueeze(3).to_broadcast([128, 8, NSEQ, 4])), reads=[tcT], writes=[tcT])
    wab = [k.sb(f"wab{i}", [128, 8, 512], BF16) for i in range(2)]; twab = [Tok(), Tok()]
    bab = [k.sb(f"bab{i}", [65, 512], F32) for i in range(2)]; tbab = [Tok(), Tok()]
    M65 = k.sb("M65", [65, 6 * D], F32); tM65 = Tok()
    for cg in range(12):
        s = cg % 2
        for kk in range(8):
            ss = kk % 2
            k.dma("sp", lambda e, kk=kk, ss=ss, cg=cg: e.dma_start(out=stg[ss][:, :], in_=w_ada[kk * 128:(kk + 1) * 128, cg * 512:(cg + 1) * 512]), writes=[tstg[ss]])
            k.op("pool", lambda e, kk=kk, ss=ss, s=s: e.tensor_copy(out=wab[s][:, kk, :], in_=stg[ss][:, :]), reads=[tstg[ss]], writes=[twab[s]])
        k.dma("sp", lambda e, s=s, cg=cg: e.dma_start(out=bab[s][:], in_=b_ada[cg * 512:(cg + 1) * 512].partition_broadcast(65)), writes=[tbab[s]])
        for kk in range(8):
            k.op("pe", lambda e, kk=kk, s=s: e.matmul(PB[s][:65, :], lhsT=L[:, kk, :], rhs=wab[s][:, kk, :], start=(kk == 0), stop=(kk == 7)),
                 reads=[tcT, twab[s]], writes=[tPB[s]])
        k.op("dve", lambda e, s=s, cg=cg: e.tensor_tensor(out=M65[:, cg * 512:(cg + 1) * 512], in0=PB[s][:65, :], in1=bab[s][:, :], op=ALU.add),
             reads=[tPB[s], tbab[s]], writes=[tM65])
    for (sl, G) in ((1, GN1), (4, GN2)):
        k.op("dve", lambda e, sl=sl, G=G: e.scalar_tensor_tensor(out=M65[:, sl * D:(sl + 1) * D], in0=M65[:, sl * D:(sl + 1) * D], scalar=1.0, in1=G[:, :], op0=ALU.add, op1=ALU.mult),
             reads=[tM65, tC], writes=[tM65])
    k.dma("sp", lambda e: e.dma_start(out=mods_d[:, :], in_=M65[:, :]), reads=[tM65], writes=[t_mods_d])
    k.pop()

    def load_mod(tile, T, slot, tok):
        if T == 128:
            k.dma("sp", lambda e: e.dma_start(out=tile[:, :], in_=mods_d[0, slot * D:(slot + 1) * D].partition_broadcast(128)), reads=[t_mods_d], writes=[tok])
        else:
            k.dma("sp", lambda e: e.dma_start(out=tile[:TS, :], in_=mods_d[1:65, slot * D:(slot + 1) * D]), reads=[t_mods_d], writes=[tok])

    k.push()
    tG = Tok("gains1")
    GQ = bc_load("GQ", gq8, 512, tok=tG); GKS = bc_load("GKS", gks2, 128, tok=tG); GKW = bc_load("GKW", gkw2, 128, tok=tG)
    GKC = k.sb("GKC", [64, 1], F32)
    k.dma("sp", lambda e: e.dma_start(out=GKC[:], in_=gkc), writes=[tG])
    WKB = k.sb("WKB", [128, 4], F32); WVS = k.sb("WVS", [128, 16 * 64], F32)
    k.dma("sp", lambda e: e.dma_start(out=WKB[:], in_=wkblk), writes=[tG])
    k.dma("sp", lambda e: e.dma_start(out=WVS[:], in_=wvsel), writes=[tG])
    stgw = [k.sb(f"stgw{i}", [128, INW], F32) for i in range(2)]; tstgw = [Tok(), Tok()]
    WIN = k.sb("WIN", [128, 8, INW], BF16); tWIN = Tok()
    for kk in range(8):
        s = kk % 2
        k.dma("sp", lambda e, kk=kk, s=s: e.dma_start(out=stgw[s][:], in_=w_in[kk * 128:(kk + 1) * 128, :]), writes=[tstgw[s]])
        k.op("pool", lambda e, kk=kk, s=s: e.tensor_copy(out=WIN[:, kk, :], in_=stgw[s][:]), reads=[tstgw[s]], writes=[tWIN])
    A1 = k.sb("A1", [128, D], F32); B1 = k.sb("B1", [128, D], F32); tM1 = Tok()
    KST = k.sb("KST", [64, 2, SEQ], BF16); KWT = k.sb("KWT", [64, 2, SEQ], BF16)
    VS = k.sb("VS", [128, NT, 2, 65], BF16); VW = k.sb("VW", [128, NT, 2, 65], BF16)
    KCT = k.sb("KCT", [64, 2, 64], BF16); VCA = k.sb("VCA", [64, 128], F32); VC = k.sb("VC", [64, 2, 65], BF16)
    tKS = [Tok() for _ in range(NT)]; tKW = [Tok() for _ in range(NT)]; tVS = [Tok() for _ in range(NT)]; tVW = [Tok() for _ in range(NT)]
    tKC = Tok(); tVC = Tok()
    k.op("pool", lambda e: e.memset(VS[:], 1.0), writes=tVS)
    k.op("pool", lambda e: e.memset(VW[:], 1.0), writes=tVW)
    k.op("pool", lambda e: e.memset(VC[:], 1.0), writes=[tVC])
    k.op("pool", lambda e: e.memset(KCT[:], 0.0), writes=[tKC])
    k.op("pool", lambda e: e.memset(VCA[:], 0.0), writes=[tVC])
    ZR = k.sb("ZR", [3, 768], F32); tZR = Tok()
    k.op("pool", lambda e: e.memset(ZR[:], 0.0), writes=[tZR])
    k.dma("sp", lambda e: e.dma_start(out=xbc_d[0:3, :], in_=ZR[:]), reads=[tZR], writes=[t_xbc_d])

    XT = k.sb("XT", [128, D], F32); tXT = Tok()
    TMP = k.sb("TMP", [128, D], F32); tTMP = Tok()
    HB = k.sb("HB", [128, D], BF16); tHB = Tok()
    HTt = k.sb("HTt", [128, 8, 128], BF16); tHTt = Tok()
    Ut = k.sb("Ut", [128, INW], F32); tU = Tok()
    QN = k.sb("QN", [128, 512], BF16); tQN = Tok()
    QT = k.sb("QT", [64, 8, 128], BF16); tQT = Tok()
    SELO = k.sb("SELO", [128, 256], F32); tSELO = Tok()
    WINO = k.sb("WINO", [128, 256], F32); tWINO = Tok()
    KNB = k.sb("KNB", [128, 256], BF16); tKNB = Tok()
    ATT = k.sb("ATT", [128, 512], F32); tATT = Tok()
    PTt = [k.sb(f"PTt{i}", [128, 512], BF16) for i in range(3)]; tPTt = [Tok(), Tok(), Tok()]
    GT = k.sb("GT", [128, 24], F32); tGT = Tok()
    SE = k.sb("SE", [128, 256], F32); tSE = Tok()
    S2 = k.sb("S2", [128, 64], F32); IMP = k.sb("IMP", [128, 32], F32); SC = k.sb("SC", [128, 32], F32); SC2 = k.sb("SC2", [128, 32], F32)
    M1 = k.sb("M1", [128, 8], F32); M2 = k.sb("M2", [128, 8], F32); NMB = k.sb("NMB", [128, 32], BF16); tSEL = Tok()
    NMT = k.sb("NMT", [32, 4, 128], BF16); tNMT = Tok()
    KR = k.sb("KR", [64, 16], F32); tKR = Tok()
    COEF = k.sb("COEF", [128, 8], F32); tCOEF = Tok()
    OT = k.sb("OT", [128, 256], F32); tOT = Tok()

    def headnorm(T, src, nh, gain, out32, outbf, rd, wr, col):
        w = nh * 64
        k.op("dve", lambda e: e.tensor_tensor(out=TMP[:T, :w], in0=src, in1=src, op=ALU.mult), reads=rd, writes=[tTMP])
        k.op("dve", lambda e: e.tensor_reduce(out=SM[:T, col:col + nh], in_=TMP[:T, :w].rearrange("p (h d) -> p h d", d=64), axis=AX.X, op=ALU.add), reads=[tTMP], writes=[tSM])
        rstd_of(SM[:T, col:col + nh], T, 1.0 / 64)
        k.op("dve", lambda e: e.tensor_tensor(out=TMP[:T, :w].rearrange("p (h d) -> p h d", d=64), in0=src.rearrange("p (h d) -> p h d", d=64),
                                              in1=SM[:T, col:col + nh].unsqueeze(2).to_broadcast([T, nh, 64]), op=ALU.mult), reads=rd + [tSM], writes=[tTMP])
        if out32 is not None:
            k.op("dve", lambda e: e.tensor_tensor(out=out32, in0=TMP[:T, :w], in1=gain, op=ALU.mult), reads=[tTMP, tG], writes=wr)
            k.op("act", lambda e: e.copy(out=outbf, in_=out32), reads=wr, writes=[tKNB])
        else:
            k.op("dve", lambda e: e.tensor_tensor(out=outbf, in0=TMP[:T, :w], in1=gain, op=ALU.mult), reads=[tTMP, tG], writes=wr)

    def front(T, xsrc, cmp_o, sel_o, xbc_dst, zrow0):
        k.dma("sp", lambda e: e.dma_start(out=XT[:T, :], in_=xsrc), writes=[tXT])
        k.op("act", lambda e: e.activation(out=HB[:T, :], in_=XT[:T, :], func=AF.Square, accum_out=SM[:T, 0:1]), reads=[tXT], writes=[tHB, tSM])
        rstd_of(SM[:T, 0:1], T, 1.0 / D)
        k.op("dve", lambda e: e.scalar_tensor_tensor(out=TMP[:T, :], in0=XT[:T, :], scalar=SM[:T, 0:1], in1=A1[:T, :], op0=ALU.mult, op1=ALU.mult),
             reads=[tXT, tSM, tM1], writes=[tTMP])
        k.op("dve", lambda e: e.tensor_tensor(out=HB[:T, :], in0=TMP[:T, :], in1=B1[:T, :], op=ALU.add), reads=[tTMP, tM1], writes=[tHB])
        for kk in range(8):
            k.op("pe", lambda e, kk=kk: e.transpose(PT[0][:, kk * 128:kk * 128 + T], HB[:T, kk * 128:(kk + 1) * 128], C["ident_bf"][:T, :T]), reads=[tHB, tC], writes=[tPT[0]])
        k.op("act", lambda e: e.copy(out=HTt[:, :, :T], in_=PT[0][:, :].rearrange("p (k t) -> p k t", k=8)[:, :, :T]), reads=[tPT[0]], writes=[tHTt])
        groups = [(0, 512), (512, 512), (1024, 288), (1312, 512), (1824, 512), (2336, 256)]
        for gi, (c0, w) in enumerate(groups):
            pb = gi % 2
            for kk in range(8):
                k.op("pe", lambda e, kk=kk, pb=pb, c0=c0, w=w: e.matmul(PB[pb][:T, :w], lhsT=HTt[:, kk, :T], rhs=WIN[:, kk, c0:c0 + w], start=(kk == 0), stop=(kk == 7)),
                     reads=[tHTt, tWIN], writes=[tPB[pb]])
            if gi % 2 == 0:
                k.op("act", lambda e, pb=pb, c0=c0, w=w: e.copy(out=Ut[:T, c0:c0 + w], in_=PB[pb][:T, :w]), reads=[tPB[pb]], writes=[tU])
            else:
                k.op("dve", lambda e, pb=pb, c0=c0, w=w: e.tensor_copy(out=Ut[:T, c0:c0 + w], in_=PB[pb][:T, :w]), reads=[tPB[pb]], writes=[tU])
        k.dma("sp", lambda e: e.dma_start(out=cmp_o, in_=Ut[:T, 512:768]), reads=[tU], writes=[otok()])
        if T == 128:
            k.dma("sp", lambda e: e.dma_start(out=xbc_dst, in_=Ut[:T, 1824:2592]), reads=[tU], writes=[t_xbc_d])
        else:
            for b in range(NSEQ):
                k.dma("sp", lambda e, b=b: e.dma_start(out=xbc_dst[b, :, :], in_=Ut[4 * b:4 * b + 4, 1824:2592]), reads=[tU], writes=[t_xbcs_d])
        k.dma("sp", lambda e: e.dma_start(out=zdt_d[zrow0:zrow0 + T, 0:512], in_=Ut[:T, 1312:1824]), reads=[tU], writes=[t_zdt_d])
        k.dma("sp", lambda e: e.dma_start(out=zdt_d[zrow0:zrow0 + T, 512:520], in_=Ut[:T, 1304:1312]), reads=[tU], writes=[t_zdt_d])
        headnorm(T, Ut[:T, 0:512], 8, GQ[:T, :], None, QN[:T, :], [tU], [tQN], 8)
        for h in range(8):
            k.op("pe", lambda e, h=h: e.transpose(PT[1][:64, h * 128:h * 128 + T], QN[:T, h * 64:(h + 1) * 64], C["ident_bf"][:T, :T]), reads=[tQN, tC], writes=[tPT[1]])
        k.op("act", lambda e: e.copy(out=QT[:, :, :T], in_=PT[1][:64, :].rearrange("p (h t) -> p h t", h=8)[:, :, :T]), reads=[tPT[1]], writes=[tQT])
        headnorm(T, Ut[:T, 768:896], 2, GKS[:T, :], SELO[:T, 0:128], KNB[:T, 0:128], [tU], [tSELO], 16)
        k.op("act", lambda e: e.copy(out=SELO[:T, 128:256], in_=Ut[:T, 896:1024]), reads=[tU], writes=[tSELO])
        headnorm(T, Ut[:T, 1024:1152], 2, GKW[:T, :], WINO[:T, 0:128], KNB[:T, 128:256], [tU], [tWINO], 18)
        k.op("act", lambda e: e.copy(out=WINO[:T, 128:256], in_=Ut[:T, 1152:1280]), reads=[tU], writes=[tWINO])
        k.dma("sp", lambda e: e.dma_start(out=sel_o, in_=SELO[:T, :]), reads=[tSELO], writes=[otok()])
        k.op("act", lambda e: e.activation(out=GT[:T, :], in_=Ut[:T, 1280:1304], func=AF.Sigmoid), reads=[tU], writes=[tGT])

    pvn = [0]

    pend = []; pvbank = [5]

    def flush():
        while pend:
            pend.pop(0)()

    def combine(br, g):
        flush()
        bk = pvbank[0]; pvbank[0] = 9 - bk
        PBk = PB[bk]; tPBk = tPB[bk]
        den = PBk[:, 0:260].rearrange("p (h c) -> p h c", c=65)[:, :, 64]
        k.op("dve", lambda e: e.tensor_scalar_max(out=COEF[:, 0:4], in0=den, scalar1=1e-30), reads=[tPBk], writes=[tCOEF])
        k.op("dve", lambda e: e.reciprocal(out=COEF[:, 0:4], in_=COEF[:, 0:4]), reads=[tCOEF], writes=[tCOEF])
        k.op("dve", lambda e: e.tensor_tensor(out=COEF[:, 4:8], in0=COEF[:, 0:4], in1=GT[:, br * 8 + g * 4:br * 8 + g * 4 + 4], op=ALU.mult), reads=[tCOEF, tGT], writes=[tCOEF])
        ov = PBk[:, 0:260].rearrange("p (h c) -> p h c", c=65)[:, :, 0:64]
        cf = COEF[:, 4:8].unsqueeze(2).to_broadcast([128, 4, 64])
        av = ATT[:, g * 256:(g + 1) * 256].rearrange("p (h d) -> p h d", d=64)
        if br == 0:
            k.op("dve", lambda e: e.tensor_tensor(out=av, in0=ov, in1=cf, op=ALU.mult), reads=[tPBk, tCOEF], writes=[tATT])
        else:
            k.op("dve", lambda e: e.tensor_tensor(out=OT[:, :].rearrange("p (h d) -> p h d", d=64), in0=ov, in1=cf, op=ALU.mult), reads=[tPBk, tCOEF], writes=[tOT])
            k.op("pool", lambda e: e.tensor_tensor(out=ATT[:, g * 256:(g + 1) * 256], in0=ATT[:, g * 256:(g + 1) * 256], in1=OT[:, :], op=ALU.add), reads=[tOT, tATT], writes=[tATT])

    def chunk(g, kT_ap, ktoks, bias, v_ap, vtoks, nk, first, last):
        n = pvn[0]; pvn[0] += 1
        pb = n % 2; s = n % 3
        k.op("pe", lambda e: e.matmul(PB[pb][:nk, :], lhsT=kT_ap, rhs=QT[:, 4 * g:4 * g + 4, :], start=True, stop=(bias is None)),
             reads=[tQT] + ktoks, writes=[tPB[pb]])
        if bias is not None:
            k.op("pe", lambda e: e.matmul(PB[pb][:nk, :], lhsT=bias[0], rhs=bias[1], start=False, stop=True), reads=bias[2], writes=[tPB[pb]])
        k.op("act", lambda e: e.activation(out=PTt[s][:nk, :], in_=PB[pb][:nk, :], func=AF.Exp, scale=SCALE), reads=[tPB[pb]], writes=[tPTt[s]])
        bk = pvbank[0]

        def pv():
            for h in range(4):
                k.op("pe", lambda e, h=h: e.matmul(PB[bk][:, h * 65:(h + 1) * 65], lhsT=PTt[s][:nk, h * 128:(h + 1) * 128], rhs=v_ap, start=(first and h == 0), stop=last, skip_group_check=True),
                     reads=[tPTt[s]] + vtoks, writes=[tPB[bk]])
        pend.append(pv)
        if len(pend) > 2:
            pend.pop(0)()

    load_mod(A1, 128, 1, tM1); load_mod(B1, 128, 0, tM1)
    for i in range(NT):
        r0 = i * 128
        front(128, xp[r0:r0 + 128, :], cmpp[r0:r0 + 128, :], selp[r0:r0 + 128, :], xbc_d[3 + r0:3 + r0 + 128, :], r0)
        if i >= 12:
            k.dma("sp", lambda e, i=i: e.dma_start(out=winp[(i - 12) * 128:(i - 11) * 128, :], in_=WINO[:, :]), reads=[tWINO], writes=[otok()])
        if i == NT - 1:
            k.dma("sp", lambda e: e.dma_start(out=convp[:, :], in_=xbc_d[SEQ:SEQ + 3, :]), reads=[t_xbc_d], writes=[otok()])
        for j in range(4):
            k.op("pe", lambda e, j=j: e.transpose(PT[1][:64, j * 128:(j + 1) * 128], KNB[:, j * 64:(j + 1) * 64], C["ident_bf"][:, :]), reads=[tKNB, tC], writes=[tPT[1]])
        k.op("act", lambda e: e.copy(out=KST[:, :, r0:r0 + 128], in_=PT[1][:64, 0:256].rearrange("p (g t) -> p g t", g=2)), reads=[tPT[1]], writes=[tKS[i]])
        k.op("act", lambda e: e.copy(out=KWT[:, :, r0:r0 + 128], in_=PT[1][:64, 256:512].rearrange("p (g t) -> p g t", g=2)), reads=[tPT[1]], writes=[tKW[i]])
        k.op("pool", lambda e: e.tensor_copy(out=VS[:, i, :, 0:64], in_=Ut[:, 896:1024].rearrange("p (g d) -> p g d", g=2)), reads=[tU], writes=[tVS[i]])
        k.op("pool", lambda e: e.tensor_copy(out=VW[:, i, :, 0:64], in_=Ut[:, 1152:1280].rearrange("p (g d) -> p g d", g=2)), reads=[tU], writes=[tVW[i]])
        for g in range(2):
            k.op("pe", lambda e, g=g: e.matmul(PB[2][:64, g * 4:(g + 1) * 4], lhsT=Ut[:, 512 + g * 64:576 + g * 64], rhs=WKB[:, :], start=True, stop=True), reads=[tU, tG], writes=[tPB[2]])
        k.op("act", lambda e: e.activation(out=KR[:, 0:8], in_=PB[2][:64, 0:8], func=AF.Square), reads=[tPB[2]], writes=[tKR])
        k.op("pe", lambda e: e.matmul(PB[3][:64, 0:8], lhsT=C["ones_f"][:64, :64], rhs=KR[:, 0:8], start=True, stop=True), reads=[tKR, tC], writes=[tPB[3]])
        k.op("act", lambda e: e.activation(out=KR[:, 8:16], in_=PB[3][:64, 0:8], func=AF.Sqrt, bias=EPSC[:64, :], scale=1.0 / 64), reads=[tPB[3], tC], writes=[tKR])
        k.op("dve", lambda e: e.reciprocal(out=KR[:, 8:16], in_=KR[:, 8:16]), reads=[tKR], writes=[tKR])
        k.op("dve", lambda e: e.scalar_tensor_tensor(out=KCT[:, :, 4 * i:4 * i + 4], in0=PB[2][:64, 0:8].rearrange("p (g j) -> p g j", g=2), scalar=GKC[:, 0:1],
                                                     in1=KR[:, 8:16].rearrange("p (g j) -> p g j", g=2), op0=ALU.mult, op1=ALU.mult), reads=[tPB[2], tKR, tG], writes=[tKC])
        k.op("pe", lambda e: e.matmul(PB[3][:64, 128:256], lhsT=WVS[:, i * 64:(i + 1) * 64], rhs=Ut[:, 640:768], start=True, stop=True), reads=[tU, tG], writes=[tPB[3]])
        k.op("dve", lambda e: e.tensor_tensor(out=VCA[:, :], in0=VCA[:, :], in1=PB[3][:64, 128:256], op=ALU.add), reads=[tPB[3], tVC], writes=[tVC])
        k.op("dve", lambda e: e.tensor_copy(out=VC[:, :, 0:64], in_=VCA[:, :].rearrange("p (g d) -> p g d", g=2)), reads=[tVC], writes=[tVC])
        nk = 4 * (i + 1)
        for g in range(2):
            chunk(g, KCT[:, g, 0:nk], [tKC], (C["Dsel"][:, 60 - 4 * i:60 - 4 * i + nk], C["cmpB"][:, :], [tC]), VC[:nk, g, :], [tVC], nk, True, True)
            combine(0, g)
            for h in range(4):
                k.op("pe", lambda e, h=h: e.matmul(PB[2][:, h * 64:(h + 1) * 64], lhsT=QT[:, 4 * g + h, :], rhs=KCT[:, g, :], start=True, stop=True), reads=[tQT, tKC], writes=[tPB[2]])
            k.op("act", lambda e: e.activation(out=SE[:, :], in_=PB[2][:, 0:256], func=AF.Exp, scale=SCALE), reads=[tPB[2]], writes=[tSE])
            k.op("dve", lambda e: e.tensor_tensor(out=SE[:, :].rearrange("p (h j) -> p h j", h=4), in0=SE[:, :].rearrange("p (h j) -> p h j", h=4),
                                                  in1=C["cmpM0"][:, 60 - 4 * i:124 - 4 * i].unsqueeze(1).to_broadcast([128, 4, 64]), op=ALU.mult), reads=[tSE, tC], writes=[tSE])
            k.op("dve", lambda e: e.tensor_reduce(out=SM[:, 24:28], in_=SE[:, :].rearrange("p (h j) -> p h j", h=4), axis=AX.X, op=ALU.add), reads=[tSE], writes=[tSM])
            k.op("dve", lambda e: e.tensor_scalar_max(out=SM[:, 24:28], in0=SM[:, 24:28], scalar1=1e-30), reads=[tSM], writes=[tSM])
            k.op("dve", lambda e: e.reciprocal(out=SM[:, 24:28], in_=SM[:, 24:28]), reads=[tSM], writes=[tSM])
            k.op("dve", lambda e: e.tensor_tensor(out=SE[:, :].rearrange("p (h j) -> p h j", h=4), in0=SE[:, :].rearrange("p (h j) -> p h j", h=4),
                                                  in1=SM[:, 24:28].unsqueeze(2).to_broadcast([128, 4, 64]), op=ALU.mult), reads=[tSE, tSM], writes=[tSE])
            k.op("dve", lambda e: e.tensor_reduce(out=S2[:, :], in_=SE[:, :].rearrange("p (h j) -> p j h", h=4), axis=AX.X, op=ALU.add), reads=[tSE], writes=[tSEL])
            s2v = S2[:, :].rearrange("p (n two) -> p n two", two=2)
            k.op("dve", lambda e: e.tensor_tensor(out=IMP[:, :], in0=s2v[:, :, 0], in1=s2v[:, :, 1], op=ALU.add), reads=[tSEL], writes=[tSEL])
            k.op("dve", lambda e: e.tensor_tensor(out=SC[:, :], in0=IMP[:, :], in1=C["selA"][:, i * 32:(i + 1) * 32], op=ALU.mult), reads=[tSEL, tC], writes=[tSEL])
            k.op("dve", lambda e: e.tensor_tensor(out=SC[:, :], in0=SC[:, :], in1=C["selB"][:, i * 32:(i + 1) * 32], op=ALU.add), reads=[tSEL, tC], writes=[tSEL])
            k.op("dve", lambda e: e.max(out=M1[:, :], in_=SC[:, :]), reads=[tSEL], writes=[tSEL])
            k.op("dve", lambda e: e.match_replace(out=SC2[:, :], in_to_replace=M1[:, :], in_values=SC[:, :], imm_value=-3e38), reads=[tSEL], writes=[tSEL])
            k.op("dve", lambda e: e.max(out=M2[:, :], in_=SC2[:, :]), reads=[tSEL], writes=[tSEL])
            k.op("dve", lambda e: e.tensor_scalar(out=SC2[:, :], in0=SC[:, :], scalar1=M2[:, 6:7], scalar2=None, op0=ALU.is_ge), reads=[tSEL], writes=[tSEL])
            k.op("dve", lambda e: e.tensor_tensor(out=SC2[:, :], in0=SC2[:, :], in1=C["allowed"][:, i * 32:(i + 1) * 32], op=ALU.mult), reads=[tSEL, tC], writes=[tSEL])
            k.op("dve", lambda e: e.tensor_scalar(out=NMB[:, :], in0=SC2[:, :], scalar1=-1.0, scalar2=-NEGB, op0=ALU.add, op1=ALU.mult), reads=[tSEL], writes=[tSEL])
            k.op("pe", lambda e: e.transpose(PT[1][:32, 0:128], NMB[:, :], C["ident_bf"][:, :]), reads=[tSEL, tC], writes=[tPT[1]])
            k.op("act", lambda e: e.copy(out=NMT[:, :, :], in_=PT[1][:32, 0:128].unsqueeze(1).to_broadcast([32, 4, 128])), reads=[tPT[1]], writes=[tNMT])
            for c in range(i + 1):
                if c < i:
                    bias = (C["Eall"][:, c * 128:(c + 1) * 128], NMT[:, :, :], [tC, tNMT])
                else:
                    bias = (C["ident_bf"][:, :], C["causalb"][:, :], [tC])
                chunk(g, KST[:, g, c * 128:(c + 1) * 128], [tKS[c]], bias, VS[:, c, g, :], [tVS[c]], 128, c == 0, c == i)
            combine(1, g)
            c0 = max(0, i - 4)
            for c in range(c0, i + 1):
                if c == i:
                    bias = (C["ident_bf"][:, :], C["causalb"][:, :], [tC])
                elif c == i - 4:
                    bias = (C["ident_bf"][:, :], C["antib"][:, :], [tC])
                else:
                    bias = None
                chunk(g, KWT[:, g, c * 128:(c + 1) * 128], [tKW[c]], bias, VW[:, c, g, :], [tVW[c]], 128, c == c0, c == i)
            combine(2, g)
        k.dma("sp", lambda e, r0=r0: e.dma_start(out=att_d[r0:r0 + 128, :], in_=ATT[:, :]), reads=[tATT], writes=[t_att_d])

    load_mod(A1, TS, 1, tM1); load_mod(B1, TS, 0, tM1)
    k.dma("sp", lambda e: e.dma_start(out=xbcs_d[:, 0:3, :], in_=sconv), writes=[t_xbcs_d])
    front(TS, xs[:, :], cmps[:, :], sels[:, :], xbcs_d[:, 3:7, :], SEQ)
    k.dma("sp", lambda e: e.dma_start(out=convs[:, :, :], in_=xbcs_d[:, 4:7, :]), reads=[t_xbcs_d], writes=[otok()])
    k.dma("sp", lambda e: e.dma_start(out=wins[:, 0:508, :], in_=cwin[:, 4:512, :]), writes=[otok()])
    for b in range(NSEQ):
        k.dma("sp", lambda e, b=b: e.dma_start(out=wins[b, 508:512, :], in_=WINO[4 * b:4 * b + 4, :]), reads=[tWINO], writes=[otok()])
    k.dma("sp", lambda e: e.dma_start(out=qs_d[:, :], in_=QN[:TS, :]), reads=[tQN], writes=[t_qs_d])
    k.dma("sp", lambda e: e.dma_start(out=kvs_d[:, 0:256], in_=SELO[:TS, :]), reads=[tSELO], writes=[t_kvs_d])
    k.dma("sp", lambda e: e.dma_start(out=kvs_d[:, 256:512], in_=WINO[:TS, :]), reads=[tWINO], writes=[t_kvs_d])
    GTP = k.sb("GTP", [TS, 24], F32); tGTP = Tok()
    k.op("dve", lambda e: e.tensor_copy(out=GTP[:, :].rearrange("p (q m) -> p q m", m=6), in_=GT[:TS, :].rearrange("p (m q) -> p q m", m=6)), reads=[tGT], writes=[tGTP])
    k.dma("sp", lambda e: e.dma_start(out=gts_d[:, :], in_=GTP[:, :]), reads=[tGTP], writes=[t_gts_d])
    k.pop()

    k.push()
    tCS = Tok("sconst")
    GKC2 = bc_load("GKC2", gkc2, 128, tok=tCS)
    KVA = k.sb("KVA", [128, NSEQ, 2, 257], F32); tKVA = [Tok() for _ in range(NSEQ)]
    k.op("pool", lambda e: e.memset(KVA[:], 1.0), writes=tKVA)
    k.push()
    WK32 = k.sb("WK32", [128, 32 * 128], F32); WV32 = k.sb("WV32", [128, 32 * 128], F32); tW32 = Tok()
    k.dma("sp", lambda e: e.dma_start(out=WK32[:, :], in_=wk32), writes=[tW32])
    k.dma("sp", lambda e: e.dma_start(out=WV32[:, :], in_=wv32), writes=[tW32])
    PG = [k.sb(f"PG{i}", [128, 8, 1024], F32) for i in range(2)]; tPG = [Tok(), Tok()]
    PTI = k.sb("PTI", [128, NSEQ * 64], I32); PTF = k.sb("PTF", [128, NSEQ * 64], F32); tIDXC = Tok()
    PT4 = k.sb("PT4", [128, NSEQ * 16], F32); IDX4 = k.sb("IDX4", [128, NSEQ * 16], I32)
    k.dma("sp", lambda e: e.dma_start(out=PTI[:, :], in_=ptab.rearrange("b j -> (b j)").partition_broadcast(128)), writes=[tIDXC])
    k.op("dve", lambda e: e.tensor_copy(out=PTF[:, :], in_=PTI[:, :]), reads=[tIDXC], writes=[tIDXC])
    k.op("dve", lambda e: e.tensor_tensor(out=PTF[:, :].rearrange("p (a f) -> p a f", f=4), in0=PTF[:, :].rearrange("p (a f) -> p a f", f=4),
                                          in1=C["OH4"][:, :].unsqueeze(1).to_broadcast([128, NSEQ * 16, 4]), op=ALU.mult), reads=[tIDXC, tC], writes=[tIDXC])
    k.op("dve", lambda e: e.tensor_reduce(out=PT4[:, :], in_=PTF[:, :].rearrange("p (a f) -> p a f", f=4), axis=AX.X, op=ALU.add), reads=[tIDXC], writes=[tIDXC])
    k.op("dve", lambda e: e.tensor_scalar(out=PT4[:, :], in0=PT4[:, :], scalar1=32.0, scalar2=C["PM32"][:, 0:1], op0=ALU.mult, op1=ALU.add), reads=[tIDXC, tC], writes=[tIDXC])
    k.op("dve", lambda e: e.tensor_copy(out=IDX4[:, :], in_=PT4[:, :]), reads=[tIDXC], writes=[tIDXC])
    ccmp4 = ccmp.rearrange("(g i) c -> g (i c)", i=4)
    for b in range(NSEQ):
        for c in range(2):
            s_ = (2 * b + c) % 2
            for d in range(8):
                col = b * 16 + c * 8 + d
                k.dma("pool", lambda e, d=d, col=col, s_=s_: e.indirect_dma_start(out=PG[s_][:, d, :], out_offset=None, in_=ccmp4,
                                                                                 in_offset=bass.IndirectOffsetOnAxis(ap=IDX4[:, col:col + 1], axis=0)), reads=[tIDXC], writes=[tPG[s_]])
            pk = 2 * (c % 2)
            for j in range(32):
                d, i_ = j // 4, j % 4
                k.op("pe", lambda e, j=j, d=d, i_=i_, s_=s_, pk=pk: e.matmul(PB[pk][:, 0:128], lhsT=WK32[:, j * 128:(j + 1) * 128], rhs=PG[s_][:, d, i_ * 256:i_ * 256 + 128], start=(j == 0), stop=(j == 31)), reads=[tW32, tPG[s_]], writes=[tPB[pk]])
                k.op("pe", lambda e, j=j, d=d, i_=i_, s_=s_, pk=pk: e.matmul(PB[pk + 1][:, 0:128], lhsT=WV32[:, j * 128:(j + 1) * 128], rhs=PG[s_][:, d, i_ * 256 + 128:i_ * 256 + 256], start=(j == 0), stop=(j == 31)), reads=[tW32, tPG[s_]], writes=[tPB[pk + 1]])
            k.op("act", lambda e, b=b, c=c, pk=pk: e.copy(out=KVA[:, b, c, 0:128], in_=PB[pk][:, 0:128]), reads=[tPB[pk]], writes=[tKVA[b]])
            k.op("dve", lambda e, b=b, c=c, pk=pk: e.tensor_copy(out=KVA[:, b, c, 128:256], in_=PB[pk + 1][:, 0:128]), reads=[tPB[pk + 1]], writes=[tKVA[b]])
    k.pop()
    QB = [k.sb(f"QB{i}", [128, 4, 512], BF16) for i in range(2)]; tQB = [Tok(), Tok()]
    PRODS = [k.sb(f"PROD{i}", [128, 2048], F32) for i in range(2)]; tPRODS = [Tok(), Tok()]
    PROD = PRODS[0]; tPROD = tPRODS[0]
    STc = k.sb("STc", [128, NSEQ * 64], F32); PTc = k.sb("PTc", [128, NSEQ * 64], F32); tSTc = [Tok() for _ in range(NSEQ)]
    PCM = k.sb("PCM", [32, NSEQ, 256], F32); tPCM = Tok()
    GT16 = [k.sb(f"GT16{i}", [16, 6], F32) for i in range(2)]; GT4 = [k.sb(f"GT4{i}", [4, 24], F32) for i in range(2)]; tGTs = [Tok(), Tok()]
    OBS = [k.sb(f"OB{i}", [16, 64], F32) for i in range(2)]; tOBS = [Tok(), Tok()]
    CFS = [k.sb(f"CF{i}", [16, 8], F32) for i in range(2)]; tCFS = [Tok(), Tok()]
    KNS = [k.sb(f"KNS{i}", [4, 257], F32) for i in range(2)]; KNW = [k.sb(f"KNW{i}", [4, 257], F32) for i in range(2)]; tKN = [Tok(), Tok()]
    for i_ in range(2):
        k.op("pool", lambda e, i_=i_: e.memset(KNS[i_][:], 1.0), writes=[tKN[i_]])
        k.op("pool", lambda e, i_=i_: e.memset(KNW[i_][:], 1.0), writes=[tKN[i_]])
    STn = k.sb("STn", [4, 64], F32); PTn = k.sb("PTn", [4, 64], F32); tSTn = Tok()

    def dots(K_ap, kshape_b, Q_ap, out_ap, rd, wr, pi=0):
        P, a, b_ = kshape_b
        pv = PRODS[pi][:P, :a * b_ * 64].rearrange("p (a b d) -> p a b d", a=a, b=b_)
        k.op("dve", lambda e: e.tensor_tensor(out=pv, in0=K_ap, in1=Q_ap, op=ALU.mult), reads=rd, writes=[tPRODS[pi]])
        k.op("dve", lambda e: e.tensor_reduce(out=out_ap, in_=pv, axis=AX.X, op=ALU.add), reads=[tPRODS[pi]], writes=wr)

    def norm_gate_store(ps_ap, nq, g, gate_ap, gate_tok, dst_ap, eng_tok, pi=0):
        CF = CFS[pi]; tCF = tCFS[pi]; OB = OBS[pi]; tOB = tOBS[pi]
        k.op("dve", lambda e: e.tensor_scalar_max(out=CF[:nq, 0:1], in0=ps_ap[:, 128:129], scalar1=1e-30), reads=eng_tok, writes=[tCF])
        k.op("dve", lambda e: e.reciprocal(out=CF[:nq, 0:1], in_=CF[:nq, 0:1]), reads=[tCF], writes=[tCF])
        k.op("dve", lambda e: e.tensor_tensor(out=CF[:nq, 1:2], in0=CF[:nq, 0:1], in1=gate_ap, op=ALU.mult), reads=[tCF, gate_tok], writes=[tCF])
        k.op("dve", lambda e: e.tensor_scalar(out=OB[:nq, 0:64], in0=ps_ap[:, g * 64:(g + 1) * 64], scalar1=CF[:nq, 1:2], scalar2=None, op0=ALU.mult), reads=eng_tok + [tCF], writes=[tOB])
        k.dma("sp", lambda e: e.dma_start(out=dst_ap, in_=OB[:nq, 0:64]), reads=[tOB], writes=[t_atts_d])

    def load_seq(b, s, what):
        k.dma("sp", lambda e: e.dma_start(out=QB[s][:, :, :], in_=qs_d[4 * b:4 * b + 4, :].partition_broadcast(128)), reads=[t_qs_d], writes=[tQB[s]])
        k.dma("sp", lambda e: e.dma_start(out=GT16[s][:, :], in_=gts_d[4 * b:4 * b + 4, :].rearrange("t (q m) -> (t q) m", m=6)), reads=[t_gts_d], writes=[tGTs[s]])
        k.dma("sp", lambda e: e.dma_start(out=GT4[s][:, :].rearrange("q (t m) -> q t m", m=6), in_=gts_d[4 * b:4 * b + 4, :].rearrange("t (q m) -> q t m", m=6)), reads=[t_gts_d], writes=[tGTs[s]])
        if what >= 1:
            k.dma("sp", lambda e: e.dma_start(out=KNS[s][:, 0:256], in_=kvs_d[4 * b:4 * b + 4, 0:256]), reads=[t_kvs_d], writes=[tKN[s]])
            k.dma("sp", lambda e: e.dma_start(out=KNW[s][:, 0:256], in_=kvs_d[4 * b:4 * b + 4, 256:512]), reads=[t_kvs_d], writes=[tKN[s]])

    RS = k.sb("RS", [128, 64], F32)
    for b in range(NSEQ):
        k.op("dve", lambda e, b=b: e.tensor_tensor(out=PROD[:, 0:256].rearrange("p (c f) -> p c f", c=2), in0=KVA[:, b, :, 0:128], in1=KVA[:, b, :, 0:128], op=ALU.mult), reads=[tKVA[b]], writes=[tPROD])
        k.op("dve", lambda e, b=b: e.tensor_reduce(out=RS[:, 4 * b:4 * b + 4], in_=PROD[:, 0:256].rearrange("p (a d) -> p a d", d=64), axis=AX.X, op=ALU.add), reads=[tPROD], writes=[tSM])
    k.op("act", lambda e: e.activation(out=RS[:, :], in_=RS[:, :], func=AF.Sqrt, bias=EPSC[:, :], scale=1.0 / 64), reads=[tSM, tC], writes=[tSM])
    k.op("dve", lambda e: e.reciprocal(out=RS[:, :], in_=RS[:, :]), reads=[tSM], writes=[tSM])
    for b in range(NSEQ):
        kb_ = KVA[:, b, :, 0:128].rearrange("p c (g d) -> p c g d", g=2)
        k.op("dve", lambda e, b=b, kb_=kb_: e.tensor_tensor(out=kb_, in0=kb_, in1=RS[:, 4 * b:4 * b + 4].rearrange("p (c g) -> p c g", c=2).unsqueeze(3).to_broadcast([128, 2, 2, 64]), op=ALU.mult), reads=[tKVA[b], tSM], writes=[tKVA[b]])
        k.op("pool", lambda e, b=b: e.tensor_tensor(out=KVA[:, b, :, 0:128], in0=KVA[:, b, :, 0:128], in1=GKC2[:, :].unsqueeze(1).to_broadcast([128, 2, 128]), op=ALU.mult), reads=[tKVA[b], tCS], writes=[tKVA[b]])
    for b in range(NSEQ):
        s = b % 2
        load_seq(b, s, 0)
        qv = QB[s][:, :, :].rearrange("p t (h d) -> p t h d", d=64)
        for c in range(2):
            for g in range(2):
                dots(KVA[:, b, c, g * 64:(g + 1) * 64].unsqueeze(1).unsqueeze(1).to_broadcast([128, 4, 4, 64]), (128, 4, 4), qv[:, :, 4 * g:4 * g + 4, :],
                     STc[:, b * 64 + (c * 2 + g) * 16:b * 64 + (c * 2 + g + 1) * 16].rearrange("p (t q) -> p t q", t=4), [tKVA[b], tQB[s]], [tSTc[b]])
        k.op("act", lambda e, b=b: e.activation(out=PTc[:, b * 64:(b + 1) * 64], in_=STc[:, b * 64:(b + 1) * 64], func=AF.Exp, scale=SCALE), reads=[tSTc[b]], writes=[tSTc[b]])
        for g in range(2):
            for c in range(2):
                k.op("pe", lambda e, b=b, g=g, c=c: e.matmul(PB[2][:16, 0:129], lhsT=PTc[:, b * 64 + (c * 2 + g) * 16:b * 64 + (c * 2 + g + 1) * 16], rhs=KVA[:, b, c, 128:257], start=(c == 0), stop=(c == 1)), reads=[tSTc[b], tKVA[b]], writes=[tPB[2]])
            norm_gate_store(PB[2][:16, 0:129], 16, g, GT16[s][:, g:g + 1], tGTs[s], atts_d[0, b, :, g, :, :], [tPB[2]])
        for c in range(2):
            k.op("pe", lambda e, b=b, c=c: e.transpose(PB[3][:32, c * 128:(c + 1) * 128], PTc[:, b * 64 + c * 32:b * 64 + (c + 1) * 32], C["ident_f"][:, :]), reads=[tSTc[b], tC], writes=[tPB[3]])
        k.op("act", lambda e, b=b: e.copy(out=PCM[:, b, :], in_=PB[3][:32, 0:256]), reads=[tPB[3]], writes=[tPCM])
    RSM = k.sb("RSM", [32, NSEQ], F32)
    k.op("dve", lambda e: e.tensor_reduce(out=RSM[:, :], in_=PCM[:, :, :], axis=AX.X, op=ALU.add), reads=[tPCM], writes=[tSM])
    k.op("dve", lambda e: e.reciprocal(out=RSM[:, :], in_=RSM[:, :]), reads=[tSM], writes=[tSM])
    k.op("dve", lambda e: e.tensor_tensor(out=PCM[:, :, :], in0=PCM[:, :, :], in1=RSM[:, :].unsqueeze(2).to_broadcast([32, NSEQ, 256]), op=ALU.mult), reads=[tPCM, tSM], writes=[tPCM])
    for b in range(NSEQ):
        k.op("pe", lambda e, b=b: e.matmul(PB[4][:, 0:256], lhsT=C["HSELB"][:, b * 128:(b + 1) * 128], rhs=PCM[:, b, :], start=(b == 0), stop=(b == NSEQ - 1)), reads=[tPCM, tC], writes=[tPB[4]])
    IMPs = k.sb("IMPs", [128, 128], F32); SCs = k.sb("SCs", [128, 128], F32); SC2s = k.sb("SC2s", [128, 128], F32); tSELs = Tok()
    M1s = k.sb("M1s", [128, 16], F32); RBT = k.sb("RBT", [16, 128], F32)
    PHY = k.sb("PHY", [128, 64, 2], F32); PHI = k.sb("PHI", [128, 64], I32); PHF = k.sb("PHF", [128, 64], F32); tPHY = Tok()
    IDXF = k.sb("IDXF", [128, 128], F32); IDXS = k.sb("IDXS", [128, 128], I32); tIDXS = Tok()
    for b in range(NSEQ):
        k.dma("sp", lambda e, b=b: e.dma_start(out=PHI[8 * b:8 * b + 8, :], in_=ptab[b, :].partition_broadcast(8)), writes=[tPHY])
    k.op("dve", lambda e: e.tensor_copy(out=PHF[:, :], in_=PHI[:, :]), reads=[tPHY], writes=[tPHY])
    k.op("dve", lambda e: e.tensor_scalar(out=PHY[:, :, 0], in0=PHF[:, :], scalar1=2.0, scalar2=1.0, op0=ALU.mult, op1=ALU.add), reads=[tPHY], writes=[tPHY])
    k.op("dve", lambda e: e.tensor_scalar(out=PHY[:, :, 1], in0=PHF[:, :], scalar1=2.0, scalar2=2.0, op0=ALU.mult, op1=ALU.add), reads=[tPHY], writes=[tPHY])
    pv2 = PB[4][:, 0:256].rearrange("p (n two) -> p n two", two=2)
    k.op("act", lambda e: e.copy(out=SC2s[:, :], in_=pv2[:, :, 0]), reads=[tPB[4]], writes=[tSELs])
    k.op("dve", lambda e: e.tensor_tensor(out=IMPs[:, :], in0=pv2[:, :, 1], in1=SC2s[:, :], op=ALU.add), reads=[tPB[4], tSELs], writes=[tSELs])
    k.op("dve", lambda e: e.tensor_tensor(out=SCs[:, :], in0=IMPs[:, :], in1=C["BIGS"][:, :], op=ALU.add), reads=[tSELs, tC], writes=[tSELs])
    k.op("dve", lambda e: e.max(out=M1s[:, 0:8], in_=SCs[:, :]), reads=[tSELs], writes=[tSELs])
    k.op("dve", lambda e: e.match_replace(out=SC2s[:, :], in_to_replace=M1s[:, 0:8], in_values=SCs[:, :], imm_value=-3e38), reads=[tSELs], writes=[tSELs])
    k.op("dve", lambda e: e.max(out=M1s[:, 8:16], in_=SC2s[:, :]), reads=[tSELs], writes=[tSELs])
    k.op("dve", lambda e: e.tensor_scalar(out=SC2s[:, :], in0=SCs[:, :], scalar1=M1s[:, 14:15], scalar2=None, op0=ALU.is_ge), reads=[tSELs], writes=[tSELs])
    k.op("dve", lambda e: e.tensor_tensor(out=SCs[:, :], in0=SC2s[:, :], in1=PHY[:, :, :].rearrange("p j two -> p (j two)"), op=ALU.mult), reads=[tSELs, tPHY], writes=[tSELs])
    k.op("dve", lambda e: e.max(out=M1s[:, 0:8], in_=SCs[:, :]), reads=[tSELs], writes=[tSELs])
    k.op("dve", lambda e: e.match_replace(out=SC2s[:, :], in_to_replace=M1s[:, 0:8], in_values=SCs[:, :], imm_value=0.0), reads=[tSELs], writes=[tSELs])
    k.op("dve", lambda e: e.max(out=M1s[:, 8:16], in_=SC2s[:, :]), reads=[tSELs], writes=[tSELs])
    k.op("dve", lambda e: e.tensor_scalar(out=M1s[:, :], in0=M1s[:, :], scalar1=-1.0, scalar2=0.0, op0=ALU.add, op1=ALU.max), reads=[tSELs], writes=[tSELs])
    k.op("dve", lambda e: e.tensor_scalar(out=M1s[:, :], in0=M1s[:, :], scalar1=16.0, scalar2=None, op0=ALU.mult), reads=[tSELs], writes=[tSELs])
    k.op("pe", lambda e: e.transpose(PB[3][:16, 0:128], M1s[:, :], C["ident_f"][:, :]), reads=[tSELs, tC], writes=[tPB[3]])
    k.op("act", lambda e: e.copy(out=RBT[:, :], in_=PB[3][:16, 0:128]), reads=[tPB[3]], writes=[tSELs])
    k.op("pe", lambda e: e.matmul(PB[3][:, 128:256], lhsT=C["EXP16"][:, :], rhs=RBT[:, :], start=True, stop=True), reads=[tSELs, tC], writes=[tPB[3]])
    k.op("dve", lambda e: e.tensor_scalar(out=IDXF[:, :], in0=PB[3][:, 128:256], scalar1=C["PM8"][:, 0:1], scalar2=None, op0=ALU.add), reads=[tPB[3], tC], writes=[tIDXS])
    k.op("dve", lambda e: e.tensor_copy(out=IDXS[:, :], in_=IDXF[:, :]), reads=[tIDXS], writes=[tIDXS])
    NKS = 3
    KSEL = [k.sb(f"KSEL{i}", [128, 2, 1024], F32) for i in range(NKS)]; tKSEL = [Tok() for _ in range(NKS)]
    KSV = [KSEL[i][:, :, :].rearrange("p c (i f) -> p (c i) f", i=4) for i in range(NKS)]
    csel4 = csel.rearrange("(g i) c -> g (i c)", i=4)
    STsL = [k.sb(f"STs{i}", [128, 32], F32) for i in range(2)]; PTsL = [k.sb(f"PTs{i}", [128, 32], F32) for i in range(2)]; tSTsL = [Tok(), Tok()]
    STnL = [k.sb(f"STnu{i}", [4, 4], F32) for i in range(2)]; PTnL = [k.sb(f"PTnu{i}", [4, 4], F32) for i in range(2)]; tSTnL = [Tok(), Tok()]
    KWB = [k.sb(f"KWB{i}", [128, 4, 257], F32) for i in range(2)]; tKWB = [Tok(), Tok()]
    for i_ in range(2):
        k.op("pool", lambda e, i_=i_: e.memset(KWB[i_][:], 1.0), writes=[tKWB[i_]])
    STw = k.sb("STw", [128, 128], F32); PTw = k.sb("PTw", [128, 128], F32); tSTw = Tok()
    un = 0
    for b in range(NSEQ):
        sq = b % 2
        load_seq(b, sq, 1)
        qv = QB[sq][:, :, :].rearrange("p t (h d) -> p t h d", d=64)
        for g in range(2):
            for t in range(4):
                gt = g * 4 + t; s = un % NKS; u = un % 2; un += 1
                STs = STsL[u]; PTs = PTsL[u]; tSTs = tSTsL[u]; STu = STnL[u]; PTu = PTnL[u]; tSTu = tSTnL[u]; PBu = PB[5 - u]; tPBu = tPB[5 - u]
                for ch in range(2):
                    k.dma("pool", lambda e, b=b, gt=gt, ch=ch, s=s: e.indirect_dma_start(out=KSEL[s][:, ch, :], out_offset=None, in_=csel4,
                                                                                           in_offset=bass.IndirectOffsetOnAxis(ap=IDXS[:, b * 8 + gt:b * 8 + gt + 1], axis=0), element_offset=8 * ch * 1024), reads=[tIDXS], writes=[tKSEL[s]])
                dots(KSV[s][:, :, g * 64:(g + 1) * 64].unsqueeze(2).to_broadcast([128, 8, 4, 64]), (128, 8, 4),
                     qv[:, t, 4 * g:4 * g + 4, :].unsqueeze(1).to_broadcast([128, 8, 4, 64]), STs[:, :].rearrange("p (c q) -> p c q", c=8), [tKSEL[s], tQB[sq]], [tSTs], pi=u)
                k.op("act", lambda e, STs=STs, PTs=PTs: e.activation(out=PTs[:, :], in_=STs[:, :], func=AF.Exp, scale=SCALE), reads=[tSTs], writes=[tSTs])
                k.op("dve", lambda e, PTs=PTs: e.tensor_scalar(out=PTs[:, :], in0=PTs[:, :], scalar1=C["M120"][:, 0:1], scalar2=None, op0=ALU.mult), reads=[tSTs, tC], writes=[tSTs])
                dots(KNS[sq][:, g * 64:(g + 1) * 64].unsqueeze(1).unsqueeze(1).to_broadcast([4, 1, 4, 64]), (4, 1, 4), qv[:4, t:t + 1, 4 * g:4 * g + 4, :],
                     STu[:, 0:4].rearrange("p (a q) -> p a q", a=1), [tKN[sq], tQB[sq]], [tSTu], pi=u)
                k.op("act", lambda e, STu=STu, PTu=PTu: e.activation(out=PTu[:, 0:4], in_=STu[:, 0:4], func=AF.Exp, scale=SCALE), reads=[tSTu], writes=[tSTu])
                k.op("dve", lambda e, t=t, PTu=PTu: e.tensor_scalar(out=PTu[:, 0:4], in0=PTu[:, 0:4], scalar1=C["CM4"][:, t:t + 1], scalar2=None, op0=ALU.mult), reads=[tSTu, tC], writes=[tSTu])
                for ch in range(8):
                    k.op("pe", lambda e, ch=ch, s=s, PTs=PTs, PBu=PBu: e.matmul(PBu[:4, 0:128], lhsT=PTs[:, ch * 4:(ch + 1) * 4], rhs=KSV[s][:, ch, 128:256], start=(ch == 0), stop=False, skip_group_check=True), reads=[tSTs, tKSEL[s]], writes=[tPBu])
                    k.op("pe", lambda e, ch=ch, PTs=PTs, PBu=PBu: e.matmul(PBu[:4, 128:129], lhsT=PTs[:, ch * 4:(ch + 1) * 4], rhs=C["ones_f"][:, 0:1], start=False, stop=False, skip_group_check=True), reads=[tSTs, tC], writes=[tPBu])
                k.op("pe", lambda e, sq=sq, PTu=PTu, PBu=PBu: e.matmul(PBu[:4, 0:129], lhsT=PTu[:, 0:4], rhs=KNS[sq][:, 128:257], start=False, stop=True, skip_group_check=True), reads=[tSTu, tKN[sq]], writes=[tPBu])
                norm_gate_store(PBu[:4, 0:129], 4, g, GT4[sq][:, t * 6 + 2 + g:t * 6 + 3 + g], tGTs[sq], atts_d[1, b, t, g, :, :], [tPBu], pi=u)
        KWt = KWB[b % 2]; tKWt = tKWB[b % 2]
        k.dma("sp", lambda e, b=b, KWt=KWt: e.dma_start(out=KWt[:, :, 0:256], in_=cwin[b].rearrange("(c p) f -> p c f", p=128)), writes=[tKWt])
        for g in range(2):
            for ch in range(4):
                dots(KWt[:, ch, g * 64:(g + 1) * 64].unsqueeze(1).unsqueeze(1).to_broadcast([128, 4, 4, 64]), (128, 4, 4), qv[:, :, 4 * g:4 * g + 4, :],
                     STw[:, (g * 4 + ch) * 16:(g * 4 + ch + 1) * 16].rearrange("p (t q) -> p t q", t=4), [tKWt, tQB[sq]], [tSTw])
            dots(KNW[sq][:, g * 64:(g + 1) * 64].unsqueeze(1).unsqueeze(1).to_broadcast([4, 4, 4, 64]), (4, 4, 4), qv[:4, :, 4 * g:4 * g + 4, :],
                 STn[:, 16 + g * 16:32 + g * 16].rearrange("p (t q) -> p t q", t=4), [tKN[sq], tQB[sq]], [tSTn])
        k.op("act", lambda e: e.activation(out=PTw[:, :], in_=STw[:, :], func=AF.Exp, scale=SCALE), reads=[tSTw], writes=[tSTw])
        k.op("dve", lambda e: e.tensor_tensor(out=PTw[:, :].rearrange("p (g c t q) -> p g c t q", g=2, c=4, t=4)[:, :, 0, :, :], in0=PTw[:, :].rearrange("p (g c t q) -> p g c t q", g=2, c=4, t=4)[:, :, 0, :, :],
                                              in1=C["MASKW"][:, :].unsqueeze(1).unsqueeze(3).to_broadcast([128, 2, 4, 4]), op=ALU.mult), reads=[tSTw, tC], writes=[tSTw])
        k.op("act", lambda e: e.activation(out=PTn[:, 16:48], in_=STn[:, 16:48], func=AF.Exp, scale=SCALE), reads=[tSTn], writes=[tSTn])
        k.op("dve", lambda e: e.tensor_tensor(out=PTn[:, 16:48].rearrange("p (g t q) -> p g t q", g=2, t=4), in0=PTn[:, 16:48].rearrange("p (g t q) -> p g t q", g=2, t=4),
                                              in1=C["CM4"][:, :].unsqueeze(1).unsqueeze(3).to_broadcast([4, 2, 4, 4]), op=ALU.mult), reads=[tSTn, tC], writes=[tSTn])
        for g in range(2):
            for ch in range(4):
                k.op("pe", lambda e, g=g, ch=ch, KWt=KWt: e.matmul(PB[2][:16, 0:129], lhsT=PTw[:, (g * 4 + ch) * 16:(g * 4 + ch + 1) * 16], rhs=KWt[:, ch, 128:257], start=(ch == 0), stop=False), reads=[tSTw, tKWt], writes=[tPB[2]])
            k.op("pe", lambda e, g=g, sq=sq: e.matmul(PB[2][:16, 0:129], lhsT=PTn[:, 16 + g * 16:32 + g * 16], rhs=KNW[sq][:, 128:257], start=False, stop=True), reads=[tSTn, tKN[sq]], writes=[tPB[2]])
            norm_gate_store(PB[2][:16, 0:129], 16, g, GT16[sq][:, 4 + g:5 + g], tGTs[sq], atts_d[2, b, :, g, :, :], [tPB[2]])
    AS = [k.sb(f"AS{i}", [TS, 512], F32) for i in range(3)]; tAS = Tok()
    for r in range(3):
        k.dma("sp", lambda e, r=r: e.dma_start(out=AS[r][:, :], in_=atts_d[r].rearrange("b t g q d -> (b t) (g q d)")), reads=[t_atts_d], writes=[tAS])
    k.op("dve", lambda e: e.tensor_tensor(out=AS[0][:, :], in0=AS[0][:, :], in1=AS[1][:, :], op=ALU.add), reads=[tAS], writes=[tAS])
    k.op("dve", lambda e: e.tensor_tensor(out=AS[0][:, :], in0=AS[0][:, :], in1=AS[2][:, :], op=ALU.add), reads=[tAS], writes=[tAS])
    k.dma("sp", lambda e: e.dma_start(out=att_d[SEQ:SEQ + TS, :], in_=AS[0][:, :]), reads=[tAS], writes=[t_att_d])
    k.pop()

    k.push()
    tG2 = Tok("gains2")
    CW = bc_load("CW", convw, 4 * 768, tok=tG2); CBt = bc_load("CBt", convb, 768, tok=tG2)
    DTB = bc_load("DTB", dtb, 8, tok=tG2); ALG = bc_load("ALG", alog, 8, tok=tG2); DSK = bc_load("DSK", dsk, 512, tok=tG2)
    GATT = bc_load("GATT", gatt, 512, tok=tG2); GSSM = bc_load("GSSM", gssm, 512, tok=tG2); BR = bc_load("BR", b_r, 20, tok=tG2)
    AN = k.sb("AN", [128, 8], F32)
    k.op("act", lambda e: e.activation(out=AN[:], in_=ALG[:], func=AF.Exp), reads=[tG2], writes=[tG2])
    k.op("dve", lambda e: e.tensor_scalar_mul(out=AN[:], in0=AN[:], scalar1=-1.0), reads=[tG2], writes=[tG2])
    ABH = k.sb("ABH", [128, 1], F32); DBH = k.sb("DBH", [128, 1], F32)
    k.dma("sp", lambda e: e.dma_start(out=ABH[:], in_=abh), writes=[tG2])
    k.dma("sp", lambda e: e.dma_start(out=DBH[:], in_=dbh), writes=[tG2])
    k.op("act", lambda e: e.activation(out=ABH[:], in_=ABH[:], func=AF.Exp), reads=[tG2], writes=[tG2])
    k.op("dve", lambda e: e.tensor_scalar_mul(out=ABH[:], in0=ABH[:], scalar1=-1.0), reads=[tG2], writes=[tG2])
    stg2 = [k.sb(f"stg2{i}", [128, D], F32) for i in range(2)]; tstg2 = [Tok(), Tok()]
    WOUT = k.sb("WOUT", [128, 8, D], BF16); tWOUT = Tok()
    for kk in range(8):
        s = kk % 2
        k.dma("sp", lambda e, kk=kk, s=s: e.dma_start(out=stg2[s][:, :], in_=w_out[kk * 128:(kk + 1) * 128, :]), writes=[tstg2[s]])
        k.op("pool", lambda e, kk=kk, s=s: e.tensor_copy(out=WOUT[:, kk, :], in_=stg2[s][:, :]), reads=[tstg2[s]], writes=[tWOUT])
    WR = k.sb("WR", [128, 8, 20], BF16); tWR = Tok()
    for kk in range(8):
        k.dma("sp", lambda e, kk=kk: e.dma_start(out=stg2[0][:, kk * 20:(kk + 1) * 20], in_=w_r[kk * 128:(kk + 1) * 128, :]), writes=[tstg2[0]])
    k.op("pool", lambda e: e.tensor_copy(out=WR[:], in_=stg2[0][:, :160].rearrange("p (k c) -> p k c", k=8)), reads=[tstg2[0]], writes=[tWR])
    G1 = k.sb("G1", [128, D], F32); A2 = k.sb("A2", [128, D], F32); B2 = k.sb("B2", [128, D], F32); tM2 = Tok()
    HT = k.sb("HT", [64, 8, 64], F32); HTB = k.sb("HTB", [64, 8, 64], BF16); tHT = Tok()
    k.op("pool", lambda e: e.memset(HT[:], 0.0), writes=[tHT])
    k.op("pool", lambda e: e.memset(HTB[:], 0.0), writes=[tHT])
    XT = k.sb("XT2", [128, D], F32); tXT = Tok()
    ATT = k.sb("ATT2", [128, 512], F32); tATT = Tok()
    ZD = k.sb("ZD", [128, 520], F32); tZD = Tok()
    XC = [k.sb(f"XC{i}", [128, 768], F32) for i in range(4)]; tXC = [Tok() for _ in range(4)]
    ACC = k.sb("ACC", [128, 768], F32); tACC = Tok()
    TMPc = k.sb("TMPc", [128, D], F32); tTMPc = Tok()
    XS = k.sb("XS", [128, 768], F32); XSB = k.sb("XSB", [128, 768], BF16); tXS = Tok()
    DT = k.sb("DT", [128, 24], F32); tDT = Tok()
    RU = k.sb("RU", [128, 8, 128], F32); tRU = Tok()
    SG = k.sb("SG", [128, 4, 128], F32); tSG = Tok()
    DEC = k.sb("DEC", [128, 8, 128], F32); tDEC = Tok()
    EX = k.sb("EX", [64, 8, 128], F32); tEX = Tok()
    BCT = k.sb("BCT", [64, 4, 128], BF16); tBCT = Tok()
    CBs = k.sb("CBs", [128, 2, 128], F32); tCBs = Tok()
    MTt = [k.sb(f"MTt{i}", [128, 128], BF16) for i in range(2)]; tMT = [Tok(), Tok()]
    CEt = [k.sb(f"CEt{i}", [64, 128], BF16) for i in range(2)]; tCE = [Tok(), Tok()]
    BWt = [k.sb(f"BWt{i}", [128, 64], BF16) for i in range(2)]; tBW = [Tok(), Tok()]
    YS = k.sb("YS", [128, 512], F32); tYS = Tok()
    MIX = k.sb("MIX", [128, D], BF16); tMIX = Tok()
    MIXT = k.sb("MIXT", [128, 8, 128], BF16); tMIXT = Tok()
    X1 = k.sb("X1", [128, D], F32); tX1 = Tok()
    H2 = k.sb("H2", [128, D], BF16); tH2 = Tok()
    H2T = k.sb("H2T", [128, 8, 128], BF16); tH2T = Tok()
    LG = k.sb("LG", [128, 20], F32); RT = k.sb("RT", [128, 64], F32); COMB = k.sb("COMB", [128, 16], F32); tRT = Tok()

    def conv_silu(T, taps):
        for w in range(4):
            if T == 128:
                k.dma("sp", lambda e, w=w: e.dma_start(out=XC[w][:T, :], in_=taps[w]), reads=[t_xbc_d, t_xbcs_d], writes=[tXC[w]])
            else:
                for b in range(NSEQ):
                    k.dma("sp", lambda e, w=w, b=b: e.dma_start(out=XC[w][4 * b:4 * b + 4, :], in_=taps[w][b, :, :]), reads=[t_xbc_d, t_xbcs_d], writes=[tXC[w]])
        k.op("dve", lambda e: e.tensor_tensor(out=ACC[:T, :], in0=XC[0][:T, :], in1=CW[:T, 0:768], op=ALU.mult), reads=[tXC[0], tG2], writes=[tACC])
        for w in range(1, 4):
            k.op("pool", lambda e, w=w: e.tensor_tensor(out=TMPc[:T, :768], in0=XC[w][:T, :], in1=CW[:T, w * 768:(w + 1) * 768], op=ALU.mult), reads=[tXC[w], tG2], writes=[tTMPc])
            k.op("dve", lambda e: e.tensor_tensor(out=ACC[:T, :], in0=ACC[:T, :], in1=TMPc[:T, :768], op=ALU.add), reads=[tACC, tTMPc], writes=[tACC])
        k.op("dve", lambda e: e.tensor_tensor(out=ACC[:T, :], in0=ACC[:T, :], in1=CBt[:T, :], op=ALU.add), reads=[tACC, tG2], writes=[tACC])
        k.op("act", lambda e: e.activation(out=XS[:T, :], in_=ACC[:T, :], func=AF.Silu), reads=[tACC], writes=[tXS])
        k.op("pool", lambda e: e.tensor_copy(out=XSB[:T, :], in_=XS[:T, :]), reads=[tXS], writes=[tXS])

    def softplus_dt(T):
        k.op("dve", lambda e: e.tensor_tensor(out=DT[:T, 0:8], in0=ZD[:T, 512:520], in1=DTB[:T, :], op=ALU.add), reads=[tZD, tG2], writes=[tDT])
        k.op("dve", lambda e: e.tensor_scalar_min(out=DT[:T, 0:8], in0=DT[:T, 0:8], scalar1=30.0), reads=[tDT], writes=[tDT])
        k.op("act", lambda e: e.activation(out=DT[:T, 0:8], in_=DT[:T, 0:8], func=AF.Exp), reads=[tDT], writes=[tDT])
        k.op("act", lambda e: e.activation(out=DT[:T, 0:8], in_=DT[:T, 0:8], func=AF.Ln, bias=1.0), reads=[tDT], writes=[tDT])

    def finish(T, row0, yout):
        k.op("act", lambda e: e.activation(out=TMPc[:T, :512], in_=ZD[:T, 0:512], func=AF.Silu), reads=[tZD], writes=[tTMPc])
        k.op("dve", lambda e: e.tensor_tensor(out=YS[:T, :], in0=YS[:T, :], in1=TMPc[:T, :512], op=ALU.mult), reads=[tYS, tTMPc], writes=[tYS])
        k.op("act", lambda e: e.activation(out=TMPc[:T, :512], in_=YS[:T, :], func=AF.Square, accum_out=SM[:T, 0:1]), reads=[tYS], writes=[tTMPc, tSM])
        rstd_of(SM[:T, 0:1], T, 1.0 / 512)
        k.op("dve", lambda e: e.scalar_tensor_tensor(out=MIX[:T, 512:1024], in0=YS[:T, :], scalar=SM[:T, 0:1], in1=GSSM[:T, :], op0=ALU.mult, op1=ALU.mult), reads=[tYS, tSM, tG2], writes=[tMIX])
        k.op("act", lambda e: e.activation(out=TMPc[:T, :512], in_=ATT[:T, :], func=AF.Square, accum_out=SM[:T, 1:2]), reads=[tATT], writes=[tTMPc, tSM])
        rstd_of(SM[:T, 1:2], T, 1.0 / 512)
        k.op("dve", lambda e: e.scalar_tensor_tensor(out=MIX[:T, 0:512], in0=ATT[:T, :], scalar=SM[:T, 1:2], in1=GATT[:T, :], op0=ALU.mult, op1=ALU.mult), reads=[tATT, tSM, tG2], writes=[tMIX])
        for kk in range(8):
            k.op("pe", lambda e, kk=kk: e.transpose(PT[0][:, kk * 128:kk * 128 + T], MIX[:T, kk * 128:(kk + 1) * 128], C["ident_bf"][:T, :T]), reads=[tMIX, tC], writes=[tPT[0]])
        k.op("act", lambda e: e.copy(out=MIXT[:, :, :T], in_=PT[0][:, :].rearrange("p (k t) -> p k t", k=8)[:, :, :T]), reads=[tPT[0]], writes=[tMIXT])
        for hf in range(2):
            for kk in range(8):
                k.op("pe", lambda e, kk=kk, hf=hf: e.matmul(PB[hf][:T, :], lhsT=MIXT[:, kk, :T], rhs=WOUT[:, kk, hf * 512:(hf + 1) * 512], start=(kk == 0), stop=(kk == 7)),
                     reads=[tMIXT, tWOUT], writes=[tPB[hf]])
            k.op("dve", lambda e, hf=hf: e.tensor_tensor(out=TMPc[:T, hf * 512:(hf + 1) * 512], in0=PB[hf][:T, :], in1=G1[:T, hf * 512:(hf + 1) * 512], op=ALU.mult), reads=[tPB[hf], tM2], writes=[tTMPc])
        k.op("dve", lambda e: e.tensor_tensor(out=X1[:T, :], in0=TMPc[:T, :], in1=XT[:T, :], op=ALU.add), reads=[tTMPc, tXT], writes=[tX1])
        k.dma("sp", lambda e: e.dma_start(out=x1_d[row0:row0 + T, :], in_=X1[:T, :]), reads=[tX1], writes=[t_x1_d])
        k.op("act", lambda e: e.activation(out=H2[:T, :], in_=X1[:T, :], func=AF.Square, accum_out=SM[:T, 2:3]), reads=[tX1], writes=[tH2, tSM])
        rstd_of(SM[:T, 2:3], T, 1.0 / D)
        k.op("dve", lambda e: e.scalar_tensor_tensor(out=TMPc[:T, :], in0=X1[:T, :], scalar=SM[:T, 2:3], in1=A2[:T, :], op0=ALU.mult, op1=ALU.mult), reads=[tX1, tSM, tM2], writes=[tTMPc])
        k.op("dve", lambda e: e.tensor_tensor(out=H2[:T, :], in0=TMPc[:T, :], in1=B2[:T, :], op=ALU.add), reads=[tTMPc, tM2], writes=[tH2])
        for kk in range(8):
            k.op("pe", lambda e, kk=kk: e.transpose(PT[0][:, kk * 128:kk * 128 + T], H2[:T, kk * 128:(kk + 1) * 128], C["ident_bf"][:T, :T]), reads=[tH2, tC], writes=[tPT[0]])
        k.op("act", lambda e: e.copy(out=H2T[:, :, :T], in_=PT[0][:, :].rearrange("p (k t) -> p k t", k=8)[:, :, :T]), reads=[tPT[0]], writes=[tH2T])
        k.dma("sp", lambda e: e.dma_start(out=h2T_d[:, :, row0:row0 + T].rearrange("k p t -> p k t"), in_=H2T[:, :, :T]), reads=[tH2T], writes=[t_h2T_d])
        for kk in range(8):
            k.op("pe", lambda e, kk=kk: e.matmul(PB[2][:T, 0:20], lhsT=H2T[:, kk, :T], rhs=WR[:, kk, :], start=(kk == 0), stop=(kk == 7)), reads=[tH2T, tWR], writes=[tPB[2]])
        k.op("dve", lambda e: e.tensor_tensor(out=LG[:T, :], in0=PB[2][:T, 0:20], in1=BR[:T, :], op=ALU.add), reads=[tPB[2], tG2], writes=[tRT])
        R = lambda a, b: RT[:T, a:b]

        def dv(fn):
            k.op("dve", fn, reads=[tRT], writes=[tRT])
        dv(lambda e: e.tensor_reduce(out=R(0, 1), in_=LG[:T, 0:4], axis=AX.X, op=ALU.max))
        dv(lambda e: e.tensor_scalar(out=R(4, 8), in0=LG[:T, 0:4], scalar1=R(0, 1), scalar2=None, op0=ALU.is_equal))
        dv(lambda e: e.tensor_scalar(out=R(8, 12), in0=LG[:T, 0:4], scalar1=R(0, 1), scalar2=None, op0=ALU.subtract))
        k.op("act", lambda e: e.activation(out=R(8, 12), in_=R(8, 12), func=AF.Exp), reads=[tRT], writes=[tRT])
        dv(lambda e: e.tensor_reduce(out=R(1, 2), in_=R(8, 12), axis=AX.X, op=ALU.add))
        dv(lambda e: e.reciprocal(out=R(1, 2), in_=R(1, 2)))
        dv(lambda e: e.tensor_tensor(out=RT[:T, 16:32].rearrange("p (g j) -> p g j", g=4), in0=LG[:T, 4:20].rearrange("p (g j) -> p g j", g=4),
                                     in1=R(4, 8).unsqueeze(2).to_broadcast([T, 4, 4]), op=ALU.mult))
        dv(lambda e: e.tensor_reduce(out=R(12, 16), in_=RT[:T, 16:32].rearrange("p (g j) -> p j g", g=4), axis=AX.X, op=ALU.add))
        dv(lambda e: e.tensor_reduce(out=R(2, 3), in_=R(12, 16), axis=AX.X, op=ALU.max))
        dv(lambda e: e.tensor_scalar(out=R(32, 36), in0=R(12, 16), scalar1=R(2, 3), scalar2=None, op0=ALU.is_equal))
        dv(lambda e: e.scalar_tensor_tensor(out=R(36, 40), in0=R(32, 36), scalar=-1e9, in1=R(12, 16), op0=ALU.mult, op1=ALU.add))
        dv(lambda e: e.tensor_reduce(out=R(3, 4), in_=R(36, 40), axis=AX.X, op=ALU.max))
        dv(lambda e: e.tensor_scalar(out=R(40, 44), in0=R(36, 40), scalar1=R(3, 4), scalar2=None, op0=ALU.is_equal))
        dv(lambda e: e.tensor_tensor(out=R(44, 45), in0=R(3, 4), in1=R(2, 3), op=ALU.subtract))
        k.op("act", lambda e: e.activation(out=R(44, 45), in_=R(44, 45), func=AF.Exp), reads=[tRT], writes=[tRT])
        dv(lambda e: e.tensor_scalar_add(out=R(45, 46), in0=R(44, 45), scalar1=1.0))
        dv(lambda e: e.reciprocal(out=R(45, 46), in_=R(45, 46)))
        dv(lambda e: e.tensor_tensor(out=R(46, 47), in0=R(45, 46), in1=R(44, 45), op=ALU.mult))
        dv(lambda e: e.tensor_tensor(out=R(45, 47), in0=R(45, 47), in1=R(1, 2).to_broadcast([T, 2]), op=ALU.mult))
        dv(lambda e: e.tensor_scalar(out=R(48, 52), in0=R(32, 36), scalar1=R(45, 46), scalar2=None, op0=ALU.mult))
        dv(lambda e: e.scalar_tensor_tensor(out=R(48, 52), in0=R(40, 44), scalar=R(46, 47), in1=R(48, 52), op0=ALU.mult, op1=ALU.add))
        dv(lambda e: e.tensor_tensor(out=COMB[:T, :].rearrange("p (g j) -> p g j", g=4), in0=R(4, 8).unsqueeze(2).to_broadcast([T, 4, 4]),
                                     in1=R(48, 52).unsqueeze(1).to_broadcast([T, 4, 4]), op=ALU.mult))
        k.dma("sp", lambda e: e.dma_start(out=comb_d[row0:row0 + T, :], in_=COMB[:T, :]), reads=[tRT], writes=[t_comb_d])

    load_mod(G1, 128, 2, tM2); load_mod(B2, 128, 3, tM2); load_mod(A2, 128, 4, tM2)
    for i in range(NT):
        r0 = i * 128
        k.dma("sp", lambda e, r0=r0: e.dma_start(out=XT[:, :], in_=xp[r0:r0 + 128, :]), writes=[tXT])
        k.dma("sp", lambda e, r0=r0: e.dma_start(out=ATT[:, :], in_=att_d[r0:r0 + 128, :]), reads=[t_att_d], writes=[tATT])
        k.dma("sp", lambda e, r0=r0: e.dma_start(out=ZD[:, :], in_=zdt_d[r0:r0 + 128, :]), reads=[t_zdt_d], writes=[tZD])
        conv_silu(128, [xbc_d[r0 + w:r0 + w + 128, :] for w in range(4)])
        softplus_dt(128)
        k.op("dve", lambda e: e.tensor_tensor(out=DT[:, 8:16], in0=DT[:, 0:8], in1=AN[:, :], op=ALU.mult), reads=[tDT, tG2], writes=[tDT])
        k.op("dve", lambda e: e.tensor_tensor(out=RU[:, :, :], in0=C["U"][:, :].unsqueeze(1).to_broadcast([128, 8, 128]),
                                              in1=DT[:, 8:16].unsqueeze(2).to_broadcast([128, 8, 128]), op=ALU.mult), reads=[tDT, tC], writes=[tRU])
        k.op("pe", lambda e: e.matmul(PB[4][:, 0:8], lhsT=C["U"][:, :], rhs=DT[:, 8:16], start=True, stop=True), reads=[tDT, tC], writes=[tPB[4]])
        k.op("dve", lambda e: e.tensor_scalar_mul(out=DT[:, 16:24], in0=PB[4][:, 0:8], scalar1=-1.0), reads=[tPB[4]], writes=[tDT])
        for hf in range(2):
            k.op("pe", lambda e, hf=hf: e.matmul(PB[2 + hf][:, :], lhsT=C["ones_f"][:, :], rhs=RU[:, 4 * hf:4 * hf + 4, :], start=True, stop=True), reads=[tRU, tC], writes=[tPB[2 + hf]])
            k.op("dve", lambda e, hf=hf: e.tensor_tensor(out=SG[:, :, :], in0=PB[2 + hf][:, :].rearrange("p (h l) -> p h l", h=4),
                                                         in1=C["NB"][:, :].unsqueeze(1).to_broadcast([128, 4, 128]), op=ALU.add), reads=[tPB[2 + hf], tC], writes=[tSG])
            for hh in range(4):
                h = 4 * hf + hh
                k.op("act", lambda e, h=h, hh=hh: e.activation(out=DEC[:, h, :], in_=SG[:, hh, :], func=AF.Exp, bias=DT[:, 16 + h:17 + h]), reads=[tSG, tDT], writes=[tDEC])
            k.op("act", lambda e, hf=hf: e.activation(out=EX[:, 4 * hf:4 * hf + 4, :], in_=PB[2 + hf][:64, :].rearrange("p (h l) -> p h l", h=4), func=AF.Exp), reads=[tPB[2 + hf]], writes=[tEX])
        for j in range(4):
            k.op("pe", lambda e, j=j: e.transpose(PT[1][:64, j * 128:(j + 1) * 128], XSB[:, 512 + j * 64:576 + j * 64], C["ident_bf"][:, :]), reads=[tXS, tC], writes=[tPT[1]])
        k.op("act", lambda e: e.copy(out=BCT[:, :, :], in_=PT[1][:64, 0:512].rearrange("p (j t) -> p j t", j=4)), reads=[tPT[1]], writes=[tBCT])
        for g in range(2):
            k.op("pe", lambda e, g=g: e.matmul(PB[5][:, g * 128:(g + 1) * 128], lhsT=BCT[:, g, :], rhs=BCT[:, 2 + g, :], start=True, stop=True), reads=[tBCT], writes=[tPB[5]])
        k.op("act", lambda e: e.copy(out=CBs[:, :, :], in_=PB[5][:, 0:256].rearrange("p (g l) -> p g l", g=2)), reads=[tPB[5]], writes=[tCBs])
        k.op("dve", lambda e: e.tensor_tensor(out=DT[:, 0:8], in0=DT[:, 0:8], in1=DT[:, 0:8], op=ALU.max), reads=[tDT], writes=[tDT])
        k.op("dve", lambda e: e.tensor_tensor(out=SM[:, 40:48], in0=DEC[:, :, 127], in1=DT[:, 0:8], op=ALU.mult), reads=[tDEC, tDT], writes=[tSM])
        for h in range(8):
            g = h // 4; s = h % 2
            k.op("dve", lambda e, h=h, g=g, s=s: e.scalar_tensor_tensor(out=MTt[s][:, :], in0=DEC[:, h, :], scalar=DT[:, h:h + 1], in1=CBs[:, g, :], op0=ALU.mult, op1=ALU.mult),
                 reads=[tDEC, tDT, tCBs], writes=[tMT[s]])
            k.op("pool", lambda e, h=h, g=g, s=s: e.tensor_tensor(out=CEt[s][:, :], in0=BCT[:, 2 + g, :], in1=EX[:, h, :], op=ALU.mult), reads=[tBCT, tEX], writes=[tCE[s]])
            k.op("pe", lambda e, h=h, s=s: e.matmul(PB[0][:, h * 64:(h + 1) * 64], lhsT=MTt[s][:, :], rhs=XSB[:, h * 64:(h + 1) * 64], start=True, stop=False), reads=[tMT[s], tXS], writes=[tPB[0]])
            k.op("pe", lambda e, h=h, s=s: e.matmul(PB[0][:, h * 64:(h + 1) * 64], lhsT=CEt[s][:, :], rhs=HTB[:, h, :], start=False, stop=True), reads=[tCE[s], tHT], writes=[tPB[0]])
            k.op("dve", lambda e, h=h, g=g, s=s: e.tensor_scalar(out=BWt[s][:, :], in0=XS[:, 512 + g * 64:576 + g * 64], scalar1=SM[:, 40 + h:41 + h], scalar2=None, op0=ALU.mult), reads=[tXS, tSM], writes=[tBW[s]])
            k.op("pe", lambda e, h=h, s=s: e.matmul(PB[1][:64, h * 64:(h + 1) * 64], lhsT=BWt[s][:, :], rhs=XSB[:, h * 64:(h + 1) * 64], start=True, stop=True), reads=[tBW[s], tXS], writes=[tPB[1]])
        k.op("dve", lambda e: e.tensor_tensor(out=HT[:, :, :], in0=HT[:, :, :], in1=EX[:, :, 127:128].to_broadcast([64, 8, 64]), op=ALU.mult), reads=[tHT, tEX], writes=[tHT])
        k.op("dve", lambda e: e.tensor_tensor(out=HT[:, :, :], in0=HT[:, :, :], in1=PB[1][:64, :].rearrange("p (h q) -> p h q", h=8), op=ALU.add), reads=[tHT, tPB[1]], writes=[tHT])
        k.op("pool", lambda e: e.tensor_copy(out=HTB[:, :, :], in_=HT[:, :, :]), reads=[tHT], writes=[tHT])
        k.op("dve", lambda e: e.tensor_tensor(out=TMPc[:, :512], in0=XS[:, 0:512], in1=DSK[:, :], op=ALU.mult), reads=[tXS, tG2], writes=[tTMPc])
        k.op("dve", lambda e: e.tensor_tensor(out=YS[:, :], in0=TMPc[:, :512], in1=PB[0][:, :], op=ALU.add), reads=[tTMPc, tPB[0]], writes=[tYS])
        finish(128, r0, None)
    for h in range(8):
        k.op("pe", lambda e, h=h: e.transpose(PB[2][:64, h * 64:(h + 1) * 64], HT[:, h, :], C["ident_f"][:64, :64]), reads=[tHT, tC], writes=[tPB[2]])
    k.op("act", lambda e: e.copy(out=TMPc[:64, :512], in_=PB[2][:64, :]), reads=[tPB[2]], writes=[tTMPc])
    k.dma("sp", lambda e: e.dma_start(out=ssmp.rearrange("h p n -> p h n"), in_=TMPc[:64, :512].rearrange("p (h n) -> p h n", h=8)), reads=[tTMPc], writes=[otok()])

    T = TS
    load_mod(G1, TS, 2, tM2); load_mod(B2, TS, 3, tM2); load_mod(A2, TS, 4, tM2)
    k.dma("sp", lambda e: e.dma_start(out=XT[:T, :], in_=xs[:, :]), writes=[tXT])
    k.dma("sp", lambda e: e.dma_start(out=ATT[:T, :], in_=att_d[SEQ:SEQ + T, :]), reads=[t_att_d], writes=[tATT])
    k.dma("sp", lambda e: e.dma_start(out=ZD[:T, :], in_=zdt_d[SEQ:SEQ + T, :]), reads=[t_zdt_d], writes=[tZD])
    conv_silu(T, [xbcs_d[:, w:w + 4, :] for w in range(4)])
    softplus_dt(T)
    k.dma("sp", lambda e: e.dma_start(out=ssc_d[:, 0:768], in_=XS[:T, :]), reads=[tXS], writes=[t_ssc_d])
    k.dma("sp", lambda e: e.dma_start(out=ssc_d[:, 768:776], in_=DT[:T, 0:8]), reads=[tDT], writes=[t_ssc_d])
    Hs = k.sb("Hs", [128, 4096], F32); tHs = Tok()
    k.dma("sp", lambda e: e.dma_start(out=Hs[:, :], in_=sssm[:, :]), writes=[tHs])
    Xbh = k.sb("Xbh", [128, 4, 64], F32); Bbh = k.sb("Bbh", [128, 4, 64], F32); Cbh = k.sb("Cbh", [128, 4, 64], F32); Dbh = k.sb("Dbh", [128, 4], F32); tBH = Tok()
    for b in range(NSEQ):
        k.dma("sp", lambda e, b=b: e.dma_start(out=Xbh[8 * b:8 * b + 8, :, :], in_=ssc_d[4 * b:4 * b + 4, 0:512].rearrange("t (h p) -> h t p", p=64)), reads=[t_ssc_d], writes=[tBH])
        k.dma("sp", lambda e, b=b: e.dma_start(out=Dbh[8 * b:8 * b + 8, :], in_=ssc_d[4 * b:4 * b + 4, 768:776].rearrange("t h -> h t")), reads=[t_ssc_d], writes=[tBH])
        for g in range(2):
            k.dma("sp", lambda e, b=b, g=g: e.dma_start(out=Bbh[8 * b + 4 * g:8 * b + 4 * g + 4, :, :], in_=ssc_d[4 * b:4 * b + 4, 512 + 64 * g:576 + 64 * g].partition_broadcast(4)), reads=[t_ssc_d], writes=[tBH])
            k.dma("sp", lambda e, b=b, g=g: e.dma_start(out=Cbh[8 * b + 4 * g:8 * b + 4 * g + 4, :, :], in_=ssc_d[4 * b:4 * b + 4, 640 + 64 * g:704 + 64 * g].partition_broadcast(4)), reads=[t_ssc_d], writes=[tBH])
    OUTER = k.sb("OUTER", [128, 4096], F32); tOUT = Tok()
    Ybh = k.sb("Ybh", [128, 4, 64], F32); tY = Tok()
    SS = k.sb("SS", [128, 8], F32); XDT = k.sb("XDT", [128, 64], F32); tSS = Tok()
    for t in range(4):
        k.op("act", lambda e, t=t: e.activation(out=SS[:, 0:1], in_=Dbh[:, t:t + 1], func=AF.Exp, scale=ABH[:, 0:1]), reads=[tBH, tG2], writes=[tSS])
        k.op("dve", lambda e, t=t: e.tensor_scalar(out=XDT[:, :], in0=Xbh[:, t, :], scalar1=Dbh[:, t:t + 1], scalar2=None, op0=ALU.mult), reads=[tBH], writes=[tSS])
        k.op("dve", lambda e, t=t: e.tensor_tensor(out=OUTER[:, :].rearrange("p (a n) -> p a n", n=64), in0=XDT[:, :].unsqueeze(2).to_broadcast([128, 64, 64]),
                                                   in1=Bbh[:, t, :].unsqueeze(1).to_broadcast([128, 64, 64]), op=ALU.mult), reads=[tSS, tBH], writes=[tOUT])
        k.op("dve", lambda e: e.scalar_tensor_tensor(out=Hs[:, :], in0=Hs[:, :], scalar=SS[:, 0:1], in1=OUTER[:, :], op0=ALU.mult, op1=ALU.add), reads=[tHs, tSS, tOUT], writes=[tHs])
        k.op("dve", lambda e, t=t: e.tensor_tensor(out=OUTER[:, :].rearrange("p (a n) -> p a n", n=64), in0=Hs[:, :].rearrange("p (a n) -> p a n", n=64),
                                                   in1=Cbh[:, t, :].unsqueeze(1).to_broadcast([128, 64, 64]), op=ALU.mult), reads=[tHs, tBH], writes=[tOUT])
        k.op("dve", lambda e, t=t: e.tensor_reduce(out=Ybh[:, t, :], in_=OUTER[:, :].rearrange("p (a n) -> p a n", n=64), axis=AX.X, op=ALU.add), reads=[tOUT], writes=[tY])
        k.op("dve", lambda e, t=t: e.scalar_tensor_tensor(out=Ybh[:, t, :], in0=Xbh[:, t, :], scalar=DBH[:, 0:1], in1=Ybh[:, t, :], op0=ALU.mult, op1=ALU.add), reads=[tBH, tY, tG2], writes=[tY])
    k.dma("sp", lambda e: e.dma_start(out=ssms[:, :], in_=Hs[:, :]), reads=[tHs], writes=[otok()])
    for b in range(NSEQ):
        k.dma("sp", lambda e, b=b: e.dma_start(out=ysd_d[b, :, :].rearrange("t (h p) -> h t p", p=64), in_=Ybh[8 * b:8 * b + 8, :, :]), reads=[tY], writes=[t_ysd_d])
    k.dma("sp", lambda e: e.dma_start(out=YS[:T, :], in_=ysd_d.rearrange("b t c -> (b t) c")), reads=[t_ysd_d], writes=[tYS])
    finish(T, SEQ, None)
    k.pop()

    k.push()
    NG = [(g * 512, 512) for g in range(4)] + [(SEQ, TS)]
    H2A = k.sb("H2A", [128, 8, NTOK], BF16); tH2A = Tok()
    for kk in range(8):
        k.dma("sp", lambda e, kk=kk: e.dma_start(out=H2A[:, kk, :], in_=h2T_d[kk, :, :]), reads=[t_h2T_d], writes=[tH2A])
    NTL = 17
    MACC = k.sb("MACC", [128, NTL, D], F32); tMACC = [Tok() for _ in range(NTL)]
    CMB = k.sb("CMB", [128, NTL, 16], F32); tCMB = Tok()
    for j in range(NTL):
        T = 128 if j < 16 else TS
        k.dma("sp", lambda e, j=j, T=T: e.dma_start(out=CMB[:T, j, :], in_=comb_d[j * 128:j * 128 + T, :]), reads=[t_comb_d], writes=[tCMB])
    k.op("pool", lambda e: e.memset(MACC[:, :, :], 0.0), writes=tMACC)
    stm = [k.sb(f"stm{i}", [128, 8, 256], F32) for i in range(2)]; tstm = [Tok(), Tok()]
    WG = [k.sb(f"WG{i}", [128, 8, 256], BF16) for i in range(2)]; WU = [k.sb(f"WU{i}", [128, 8, 256], BF16) for i in range(2)]
    WD = [k.sb(f"WD{i}", [128, 2, D], BF16) for i in range(2)]; tW = [Tok(), Tok()]
    HE = k.sb("HE", [128, 2, 512], BF16); tHE = Tok()
    SG_ = k.sb("SGm", [128, 512], F32); tSGm = Tok()
    n_exp = 16 if with_moe else 0
    sidx = 0
    for ex in range(n_exp):
        s = ex % 2
        for (W_, src) in ((WG[s], w_gate), (WU[s], w_up)):
            ss = sidx % 2; sidx += 1
            k.dma("sp", lambda e, src=src, ss=ss, ex=ex: e.dma_start(out=stm[ss][:, :, :], in_=src[ex].rearrange("(k p) f -> p k f", p=128)), writes=[tstm[ss]])
            k.op("pool", lambda e, W_=W_, ss=ss: e.tensor_copy(out=W_[:, :, :], in_=stm[ss][:, :, :]), reads=[tstm[ss]], writes=[tW[s]])
        ss = sidx % 2; sidx += 1
        k.dma("sp", lambda e, ss=ss, ex=ex: e.dma_start(out=stm[ss][:, :, :].rearrange("p (c a) f -> p c (a f)", c=2), in_=w_down[ex].rearrange("(c p) n -> p c n", p=128)), writes=[tstm[ss]])
        k.op("pool", lambda e, s=s, ss=ss: e.tensor_copy(out=WD[s][:, :, :], in_=stm[ss][:, :, :].rearrange("p (c a) f -> p c (a f)", c=2)), reads=[tstm[ss]], writes=[tW[s]])
        for (t0, nt) in NG:
            for c in range(2):
                for (W_, pb) in ((WG[s], 2 * c), (WU[s], 2 * c + 1)):
                    for kk in range(8):
                        k.op("pe", lambda e, W_=W_, pb=pb, kk=kk, c=c, t0=t0, nt=nt: e.matmul(PB[pb][:, :nt], lhsT=W_[:, kk, c * 128:(c + 1) * 128], rhs=H2A[:, kk, t0:t0 + nt], start=(kk == 0), stop=(kk == 7)),
                             reads=[tW[s], tH2A], writes=[tPB[pb]])
                k.op("act", lambda e, nt=nt, c=c: e.activation(out=SG_[:, :nt], in_=PB[2 * c][:, :nt], func=AF.Silu), reads=[tPB[2 * c]], writes=[tSGm])
                k.op("dve", lambda e, c=c, nt=nt: e.tensor_tensor(out=HE[:, c, :nt], in0=SG_[:, :nt], in1=PB[2 * c + 1][:, :nt], op=ALU.mult), reads=[tSGm, tPB[2 * c + 1]], writes=[tHE])
            for tt in range((nt + 127) // 128):
                T = min(128, nt - tt * 128); j = (t0 + tt * 128) // 128
                for hf in range(2):
                    pb = 4 + hf
                    for c in range(2):
                        k.op("pe", lambda e, pb=pb, c=c, tt=tt, T=T, hf=hf: e.matmul(PB[pb][:T, :], lhsT=HE[:, c, tt * 128:tt * 128 + T], rhs=WD[s][:, c, hf * 512:(hf + 1) * 512], start=(c == 0), stop=(c == 1)),
                             reads=[tHE, tW[s]], writes=[tPB[pb]])
                    k.op("dve", lambda e, pb=pb, T=T, j=j, hf=hf, ex=ex: e.scalar_tensor_tensor(out=MACC[:T, j, hf * 512:(hf + 1) * 512], in0=PB[pb][:T, :], scalar=CMB[:T, j, ex:ex + 1],
                                                                                         in1=MACC[:T, j, hf * 512:(hf + 1) * 512], op0=ALU.mult, op1=ALU.add), reads=[tPB[pb], tCMB, tMACC[j]], writes=[tMACC[j]])
    G2t = k.sb("G2t", [128, D], F32); tG2t = Tok()
    X1b = [k.sb(f"X1b{i}", [128, D], F32) for i in range(2)]; tX1b = [Tok(), Tok()]
    load_mod(G2t, 128, 5, tG2t)
    for j in range(NTL):
        T = 128 if j < 16 else TS
        if j == 16:
            load_mod(G2t, TS, 5, tG2t)
        s = j % 2
        k.dma("sp", lambda e, j=j, T=T, s=s: e.dma_start(out=X1b[s][:T, :], in_=x1_d[j * 128:j * 128 + T, :]), reads=[t_x1_d], writes=[tX1b[s]])
        k.op("dve", lambda e, j=j, T=T: e.tensor_tensor(out=MACC[:T, j, :], in0=MACC[:T, j, :], in1=G2t[:T, :], op=ALU.mult), reads=[tMACC[j], tG2t], writes=[tMACC[j]])
        k.op("pool", lambda e, j=j, T=T, s=s: e.tensor_tensor(out=X1b[s][:T, :], in0=X1b[s][:T, :], in1=MACC[:T, j, :], op=ALU.add), reads=[tMACC[j], tX1b[s]], writes=[tX1b[s]])
        dst = yp[j * 128:j * 128 + T, :] if j < 16 else ys[:, :]
        k.dma("sp", lambda e, T=T, s=s, dst=dst: e.dma_start(out=dst, in_=X1b[s][:T, :]), reads=[tX1b[s]], writes=[otok()])
    k.finish(out_toks)
    k.pop()
    k.es.close()
    return k


def _run(inp, debug=False, compact=False):
    f32 = lambda a: np.ascontiguousarray(np.asarray(a, dtype=np.float32))
    consts = make_consts()
    cshapes = {n: (list(v.shape), v.dtype != np.float32) for n, v in consts.items()}
    nc = bass.Bass("TRN2", target_bir_lowering=False)
    kb = build(nc, cshapes, debug=debug, pool_rows=(1024 * 128 if compact else 1310720))

    xp = f32(inp["x_prompt"]); xs = f32(inp["x_sample"])
    w_in = f32(inp["w_in"])[0]
    perm = np.concatenate([np.arange(0, 1304), np.arange(2584, 2592), np.arange(1304, 2584)])
    w_in_p = np.ascontiguousarray(w_in[:, perm])
    w_rg = f32(inp["w_rg"])[0]; w_re = f32(inp["w_re"])[0]
    w_r = np.ascontiguousarray(np.concatenate([w_rg] + [w_re[g] for g in range(4)], axis=1))
    b_r = np.ascontiguousarray(np.concatenate([f32(inp["b_rg"])[0], f32(inp["b_re"])[0].reshape(-1)]))
    wpk = f32(inp["w_pos_k"])[0]; wpv = f32(inp["w_pos_v"])[0]
    wkblk = np.zeros((128, 4), np.float32); wvsel = np.zeros((128, 16, 64), np.float32)
    for r in range(128):
        wkblk[r, r // 32] = wpk[r % 32]
        for i in range(16):
            wvsel[r, i, 4 * i + r // 32] = wpv[r % 32]
    wk32 = np.zeros((128, 32, 128), np.float32); wv32 = np.zeros((128, 32, 128), np.float32)
    for p_ in range(128):
        ps, rg = p_ // 32, p_ % 32
        for d_ in range(8):
            for i_ in range(4):
                m_ = 16 * d_ + ps * 4 + rg // 8
                wk32[p_, d_ * 4 + i_, m_] = wpk[4 * (rg % 8) + i_]
                wv32[p_, d_ * 4 + i_, m_] = wpv[4 * (rg % 8) + i_]
    shared = {
        "g_norm1": f32(inp["g_norm1"])[0], "g_norm2": f32(inp["g_norm2"])[0],
        "w_ada": f32(inp["w_ada"])[0], "b_ada": f32(inp["b_ada"])[0], "w_in": w_in_p,
        "gq8": np.tile(f32(inp["g_q"])[0], 8), "gks2": np.tile(f32(inp["g_k_sel"])[0], 2), "gkw2": np.tile(f32(inp["g_k_win"])[0], 2),
        "gkc": f32(inp["g_k_cmp"])[0].reshape(64, 1),
        "convw": f32(inp["conv_w"])[0].reshape(-1), "convb": f32(inp["conv_b"])[0],
        "dtb": f32(inp["dt_bias"])[0], "alog": f32(inp["a_log"])[0], "abh": np.tile(f32(inp["a_log"])[0], 16).reshape(128, 1), "dbh": np.tile(f32(inp["d_skip"])[0], 16).reshape(128, 1), "dsk": np.repeat(f32(inp["d_skip"])[0], 64),
        "gatt": f32(inp["g_att_out"])[0], "gssm": f32(inp["g_ssm_out"])[0],
        "w_out": f32(inp["w_out"])[0], "w_r": w_r, "b_r": b_r,
        "w_gate": f32(inp["w_gate"])[0], "w_up": f32(inp["w_up"])[0], "w_down": f32(inp["w_down"])[0],
        "wkblk": wkblk, "wvsel": wvsel.reshape(128, 1024),
        "wk32": wk32.reshape(128, 4096), "wv32": wv32.reshape(128, 4096), "gkc2": np.tile(f32(inp["g_k_cmp"])[0], 2),
    }
    for n, v in consts.items():
        shared["c_" + n] = v
    cwin = f32(inp["cache_win"])[0].reshape(128, 512, 256)
    sssm = f32(inp["state_ssm"])[0].reshape(128 * 8, 4096)
    sconv = f32(inp["state_conv"])[0]
    ccmp = np.asarray(inp["cache_cmp"], dtype=np.float32).reshape(10240 * 128, 256)
    csel = np.asarray(inp["cache_sel"], dtype=np.float32).reshape(10240 * 128, 256)
    ptab = np.ascontiguousarray(np.asarray(inp["page_table"], dtype=np.int32))
    in_maps = []
    for c in range(NCORES):
        m = dict(shared)
        if compact:
            pg = ptab[16 * c:16 * c + 16].reshape(-1)
            m["ccmp"] = ccmp.reshape(10240, 128 * 256)[pg].reshape(-1, 256); m["csel"] = csel.reshape(10240, 128 * 256)[pg].reshape(-1, 256)
            m["ptab"] = np.arange(1024, dtype=np.int32).reshape(16, 64)
        else:
            m["ccmp"] = ccmp; m["csel"] = csel; m["ptab"] = ptab[16 * c:16 * c + 16]
        m["xp"] = xp[c]; m["xs"] = xs[16 * c:16 * c + 16].reshape(TS, D)
        m["cp"] = f32(inp["c_prompt"])[c:c + 1]; m["cs"] = f32(inp["c_sample"])[16 * c:16 * c + 16]
        m["sssm"] = sssm[128 * c:128 * c + 128]; m["sconv"] = sconv[16 * c:16 * c + 16]; m["cwin"] = cwin[16 * c:16 * c + 16]
        in_maps.append(m)
    res = run_bass_kernel_spmd(nc, in_maps, core_ids=list(range(NCORES)))
    R = res.results
    cat = lambda n: np.stack([R[c][n] for c in range(NCORES)])
    y_p = cat("yp"); y_s = cat("ys").reshape(128, 4, D)
    cmp_p = cat("cmpp").reshape(1, 8, SEQ, 2, 2, 64); cmp_s = cat("cmps").reshape(1, 128, 4, 2, 2, 64)
    sel_p = cat("selp").reshape(1, 8, SEQ, 2, 2, 64); sel_s = cat("sels").reshape(1, 128, 4, 2, 2, 64)
    win_p = cat("winp").reshape(1, 8, 512, 2, 2, 64); win_s = cat("wins").reshape(1, 128, 512, 2, 2, 64)
    ssm_p = cat("ssmp").reshape(1, 8, 8, 64, 64); ssm_s = cat("ssms").reshape(1, 128, 8, 64, 64)
    conv_p = cat("convp").reshape(1, 8, 3, 768); conv_s = cat("convs").reshape(1, 128, 3, 768)
    outs = (y_p, y_s, cmp_p, cmp_s, sel_p, sel_s, win_p, win_s, ssm_p, ssm_s, conv_p, conv_s)
    if debug:
        return outs, {n: cat(n) for n in ("att_d", "x1_d", "comb_d")}
    return outs


def kernel(**inp):
    return _run(inp, False)
```
